# Optimizing a Trainium2 kernel written in Bass

```python
import math
import jax, jax.numpy as jnp
from jax import lax
import numpy as np

D_MODEL = 2048
BATCH = 4
SEQ = 4096
DEPTH = 1

GRID_W = 64
CTX_LEN = 256
HA_HEADS = 8
HA_DK = 128
HA_DV = 128
GB_HEADS = 8
GB_DK = 128
GB_DV = 128
CONV_K = 5
CHUNK = 64
N_EXPERTS = 16
CAPACITY_FACTOR = 2
D_EXPERT = 1024
NORM_EPS = 1e-6

HA_QK = HA_HEADS * HA_DK
HA_V = HA_HEADS * HA_DV
GB_QK = GB_HEADS * GB_DK
GB_V = GB_HEADS * GB_DV
GB_QKV = 2 * GB_QK + GB_V
IN_SPLITS = (HA_QK, HA_QK, HA_QK, HA_V, HA_V, GB_QKV, GB_V, 2 * GB_HEADS, 2 * GB_HEADS, 2 * D_MODEL)
N_IN = sum(IN_SPLITS)
SPLIT_POINTS = tuple(sum(IN_SPLITS[:k]) for k in range(1, len(IN_SPLITS)))

kernel_name = 'hybrid_hgrn2_gdn_ec_moe_dit_layer'


def rms_norm(x, gain):
    xf = x.astype(jnp.float32)
    y = xf * lax.rsqrt(jnp.mean(xf * xf, axis=-1, keepdims=True) + NORM_EPS)
    return (y * gain.astype(jnp.float32)).astype(x.dtype)


def head_rms_norm(o, gain, n_heads):
    b, t, w = o.shape
    oh = o.reshape(b, t, n_heads, w // n_heads)
    oh = oh * lax.rsqrt(jnp.mean(oh * oh, axis=-1, keepdims=True) + NORM_EPS)
    return oh.reshape(b, t, w) * gain.astype(jnp.float32)


def l2_normalize(a):
    return a * lax.rsqrt(jnp.sum(a * a, axis=-1, keepdims=True) + NORM_EPS)


def to_heads(a, n_heads):
    b, t, w = a.shape
    return a.reshape(b, t, n_heads, w // n_heads).transpose(0, 2, 1, 3)


def from_heads(a):
    b, h, t, d = a.shape
    return a.transpose(0, 2, 1, 3).reshape(b, t, h * d)


def to_col_major(a, rows):
    b, t, w = a.shape
    return a.reshape(b, rows, GRID_W, w).transpose(0, 2, 1, 3).reshape(b, t, w)


def from_col_major(a, rows):
    b, t, w = a.shape
    return a.reshape(b, GRID_W, rows, w).transpose(0, 2, 1, 3).reshape(b, t, w)


def centred_depthwise_conv(a, w):
    return lax.conv_general_dilated(
        a, w.astype(a.dtype)[:, None, :], window_strides=(1,),
        padding=[(CONV_K // 2, CONV_K // 2)],
        dimension_numbers=('NWC', 'WIO', 'NWC'), feature_group_count=a.shape[-1])


def modulate(h, shift, scale):
    return h * (1 + scale) + shift


def _tril(inclusive):
    i = jnp.arange(CHUNK)
    return (i[:, None] >= i[None, :]) if inclusive else (i[:, None] > i[None, :])


def hgrn2_chunk(state, xs):
    q, k, v, g = xs
    G = jnp.cumsum(g, axis=2)
    mask = _tril(True)[:, :, None]
    diff = G[:, :, :, None, :] - G[:, :, None, :, :]
    decay = jnp.where(mask, jnp.exp(jnp.where(mask, diff, 0.0)), 0.0)
    scores = jnp.einsum('bhik,bhjk,bhijk->bhij', q, k, decay)
    o = (jnp.einsum('bhik,bhkv->bhiv', q * jnp.exp(G), state)
         + jnp.einsum('bhij,bhjv->bhiv', scores, v))
    G_last = G[:, :, -1, :]
    state = (jnp.exp(G_last)[..., None] * state
             + jnp.einsum('bhjk,bhjv->bhkv', k * jnp.exp(G_last[:, :, None, :] - G), v))
    return state, o


def gdn_chunk(state, xs):
    q, k, v, g, beta = xs
    G = jnp.cumsum(g, axis=-1)
    incl = _tril(True)
    strict = _tril(False)
    diff = G[..., :, None] - G[..., None, :]
    gamma = jnp.where(incl, jnp.exp(jnp.where(incl, diff, 0.0)), 0.0)
    kk = jnp.einsum('bhik,bhjk->bhij', k, k)
    m = jnp.eye(CHUNK, dtype=jnp.float32) + jnp.where(strict, beta[..., :, None] * kk * gamma, 0.0)
    rhs = jnp.concatenate([v * beta[..., None], k * (beta * jnp.exp(G))[..., None]], axis=-1)
    sol = lax.linalg.triangular_solve(m, rhs, left_side=True, lower=True, unit_diagonal=True)
    u, w = sol[..., :GB_DV], sol[..., GB_DV:]
    v_new = u - jnp.einsum('bhik,bhkv->bhiv', w, state)
    qk = jnp.einsum('bhik,bhjk->bhij', q, k) * gamma
    o = (jnp.einsum('bhik,bhkv->bhiv', q * jnp.exp(G)[..., None], state)
         + jnp.einsum('bhij,bhjv->bhiv', qk, v_new))
    G_last = G[..., -1]
    state = (jnp.exp(G_last)[..., None, None] * state
             + jnp.einsum('bhjk,bhjv->bhkv', k * jnp.exp(G_last[..., None] - G)[..., None], v_new))
    return state, o


def _scan_chunks(chunk_fn, state, seqs):
    b, h, t = seqs[0].shape[:3]
    n = t // CHUNK
    xs = tuple(jnp.moveaxis(a.reshape((b, h, n, CHUNK) + a.shape[3:]), 2, 0) for a in seqs)
    state, o = lax.scan(chunk_fn, state, xs)
    return state, jnp.moveaxis(o, 0, 2).reshape(b, h, t, o.shape[-1])


def _context_prefixed_scan(chunk_fn, state0, ctx_seqs, lat_seqs, reverse):
    if reverse:
        ctx_seqs = tuple(jnp.flip(a, axis=2) for a in ctx_seqs)
        lat_seqs = tuple(jnp.flip(a, axis=2) for a in lat_seqs)
    s_ctx, o_ctx = _scan_chunks(chunk_fn, state0, ctx_seqs)
    _, o_lat = _scan_chunks(chunk_fn, s_ctx, lat_seqs)
    if reverse:
        o_ctx = jnp.flip(o_ctx, axis=2)
        o_lat = jnp.flip(o_lat, axis=2)
    return o_ctx, o_lat


def hgrn2_branch(ctx_parts, lat_parts, lb_fwd, lb_bwd):
    def prep(q, f_logit, i, lower):
        f_logit = f_logit.astype(jnp.float32)
        lower = lower.astype(jnp.float32)
        f = lower + (1.0 - lower) * jax.nn.sigmoid(f_logit)
        k = (1.0 - lower) * jax.nn.sigmoid(-f_logit)
        return (to_heads(jax.nn.silu(q.astype(jnp.float32)), HA_HEADS), to_heads(k, HA_HEADS),
                to_heads(i.astype(jnp.float32), HA_HEADS), to_heads(jnp.log(f), HA_HEADS))
    qc, fcf, fcb, ic = ctx_parts
    ql, flf, flb, il = lat_parts
    s0 = jnp.zeros((qc.shape[0], HA_HEADS, HA_DK, HA_DV), jnp.float32)
    oc_f, ol_f = _context_prefixed_scan(hgrn2_chunk, s0, prep(qc, fcf, ic, lb_fwd), prep(ql, flf, il, lb_fwd), False)
    oc_b, ol_b = _context_prefixed_scan(hgrn2_chunk, s0, prep(qc, fcb, ic, lb_bwd), prep(ql, flb, il, lb_bwd), True)
    return from_heads(oc_f + oc_b), from_heads(ol_f + ol_b)


def gdn_branch(ctx_parts, lat_parts, conv_w, a_log, dt_bias):
    def prep(qkv, a_logit, b_logit):
        y = jax.nn.silu(centred_depthwise_conv(qkv, conv_w).astype(jnp.float32))
        q, k, v = jnp.split(y, [GB_QK, 2 * GB_QK], axis=-1)
        q = l2_normalize(to_heads(q, GB_HEADS)) * (GB_DK ** -0.5)
        k = l2_normalize(to_heads(k, GB_HEADS))
        v = to_heads(v, GB_HEADS)
        a_f, a_b = jnp.split(a_logit.astype(jnp.float32), 2, axis=-1)
        b_f, b_b = jnp.split(b_logit.astype(jnp.float32), 2, axis=-1)
        def gates(a, bl, d):
            g = -jnp.exp(a_log[d].astype(jnp.float32)) * jax.nn.softplus(a + dt_bias[d].astype(jnp.float32))
            return g.transpose(0, 2, 1), jax.nn.sigmoid(bl).transpose(0, 2, 1)
        g_f, beta_f = gates(a_f, b_f, 0)
        g_b, beta_b = gates(a_b, b_b, 1)
        return (q, k, v, g_f, beta_f), (q, k, v, g_b, beta_b)
    c_fwd, c_bwd = prep(*ctx_parts)
    l_fwd, l_bwd = prep(*lat_parts)
    s0 = jnp.zeros((ctx_parts[0].shape[0], GB_HEADS, GB_DK, GB_DV), jnp.float32)
    oc_f, ol_f = _context_prefixed_scan(gdn_chunk, s0, c_fwd, l_fwd, False)
    oc_b, ol_b = _context_prefixed_scan(gdn_chunk, s0, c_bwd, l_bwd, True)
    return from_heads(oc_f + oc_b), from_heads(ol_f + ol_b)


def token_mixer(h_lat, h_ctx, rows, need_ctx, w_in, conv_w, a_log, dt_bias, lb_fwd, lb_bwd,
                ha_gain, gb_gain, w_ba, w_bb, w_out):
    pl = jnp.split(h_lat @ w_in, SPLIT_POINTS, axis=-1)
    pc = jnp.split(h_ctx @ w_in, SPLIT_POINTS, axis=-1)
    oA_c, oA_l = hgrn2_branch((pc[0], pc[1], pc[2], pc[3]), (pl[0], pl[1], pl[2], pl[3]), lb_fwd, lb_bwd)
    lat_b = tuple(to_col_major(p, rows) for p in (pl[5], pl[7], pl[8]))
    oB_c, oB_l = gdn_branch((pc[5], pc[7], pc[8]), lat_b, conv_w, a_log, dt_bias)
    oB_l = from_col_major(oB_l, rows)

    def merge(oA, oB, parts, dtype):
        yA = head_rms_norm(oA * jax.nn.sigmoid(parts[4].astype(jnp.float32)), ha_gain, HA_HEADS).astype(dtype)
        yB = (head_rms_norm(oB, gb_gain, GB_HEADS) * jax.nn.silu(parts[6].astype(jnp.float32))).astype(dtype)
        gate_a, gate_b = jnp.split(jax.nn.sigmoid(parts[9]), 2, axis=-1)
        return (gate_a * (yA @ w_ba) + gate_b * (yB @ w_bb)) @ w_out

    y_lat = merge(oA_l, oB_l, pl, h_lat.dtype)
    y_ctx = merge(oA_c, oB_c, pc, h_ctx.dtype) if need_ctx else None
    return y_lat, y_ctx


def expert_choice_ffn(h, router_w, w_gate, w_up, w_down):
    b, n, d = h.shape
    cap = max(1, (CAPACITY_FACTOR * n) // N_EXPERTS)
    aff = jax.nn.softmax(jnp.einsum('bnd,de->bne', h, router_w).astype(jnp.float32), axis=-1)
    gate, idx = lax.top_k(jnp.swapaxes(aff, 1, 2), cap)
    xs = jax.vmap(lambda hb, ib: hb[ib])(h, idx)
    a = jnp.einsum('becd,edf->becf', xs, w_gate)
    u = jnp.einsum('becd,edf->becf', xs, w_up)
    y = jnp.einsum('becf,efd->becd', jax.nn.silu(a) * u, w_down) * gate[..., None].astype(h.dtype)
    return jax.vmap(lambda yb, ib: jnp.zeros((n, d), yb.dtype).at[ib.reshape(-1)].add(yb.reshape(-1, d)))(y, idx)


def setup_inputs(seed: int = 0) -> dict:
    key = jax.random.key(seed)
    ks = jax.random.split(key, 24)
    f32 = jnp.float32

    def nrm(k, shape, scale):
        return jax.random.normal(k, shape, f32) * scale

    dt = jnp.exp(jax.random.uniform(ks[12], (DEPTH, 2, GB_HEADS), f32, minval=math.log(1e-3), maxval=math.log(1e-1)))
    return {
        'x': nrm(ks[0], (BATCH, SEQ, D_MODEL), 1.0),
        'c': nrm(ks[1], (BATCH, D_MODEL), 1.0),
        'ctx': nrm(ks[2], (BATCH, CTX_LEN, D_MODEL), 1.0),
        'c_ctx': nrm(ks[3], (D_MODEL,), 1.0),
        'ada_w': nrm(ks[4], (DEPTH, D_MODEL, 6 * D_MODEL), 0.5 * D_MODEL ** -0.5),
        'ada_b': nrm(ks[5], (DEPTH, 6 * D_MODEL), 0.01),
        'norm_mix': 1.0 + nrm(ks[6], (DEPTH, D_MODEL), 0.02),
        'norm_ffn': 1.0 + nrm(ks[7], (DEPTH, D_MODEL), 0.02),
        'w_in': nrm(ks[8], (DEPTH, D_MODEL, N_IN), D_MODEL ** -0.5),
        'gdn_conv': nrm(ks[9], (DEPTH, CONV_K, GB_QKV), CONV_K ** -0.5),
        'gdn_a_log': jnp.log(jax.random.uniform(ks[10], (DEPTH, 2, GB_HEADS), f32, minval=1.0, maxval=16.0)),
        'gdn_dt_bias': dt + jnp.log(-jnp.expm1(-dt)),
        'hgrn_lb': 1.0 + nrm(ks[11], (2, DEPTH + 1, HA_QK), 0.1),
        'hgrn_norm': 1.0 + nrm(ks[13], (DEPTH, HA_V), 0.02),
        'gdn_norm': 1.0 + nrm(ks[14], (DEPTH, GB_V), 0.02),
        'w_branch_a': nrm(ks[15], (DEPTH, HA_V, D_MODEL), HA_V ** -0.5),
        'w_branch_b': nrm(ks[16], (DEPTH, GB_V, D_MODEL), GB_V ** -0.5),
        'w_out': nrm(ks[17], (DEPTH, D_MODEL, D_MODEL), D_MODEL ** -0.5),
        'router_w': nrm(ks[18], (DEPTH, D_MODEL, N_EXPERTS), D_MODEL ** -0.5),
        'w_gate': nrm(ks[19], (DEPTH, N_EXPERTS, D_MODEL, D_EXPERT), D_MODEL ** -0.5),
        'w_up': nrm(ks[20], (DEPTH, N_EXPERTS, D_MODEL, D_EXPERT), D_MODEL ** -0.5),
        'w_down': nrm(ks[21], (DEPTH, N_EXPERTS, D_EXPERT, D_MODEL), D_EXPERT ** -0.5),
        'final_norm': 1.0 + nrm(ks[22], (D_MODEL,), 0.02),
    }


def reference(x, c, ctx, c_ctx, ada_w, ada_b, norm_mix, norm_ffn, w_in, gdn_conv, gdn_a_log, gdn_dt_bias,
              hgrn_lb, hgrn_norm, gdn_norm, w_branch_a, w_branch_b, w_out, router_w, w_gate, w_up, w_down,
              final_norm):
    rows = x.shape[1] // GRID_W
    lower = jnp.cumsum(jax.nn.softmax(hgrn_lb.astype(jnp.float32), axis=1), axis=1)
    x_lat, x_ctx = x, ctx
    for l in range(DEPTH):
        need_ctx = l < DEPTH - 1
        ml = [m[:, None, :] for m in jnp.split(jax.nn.silu(c) @ ada_w[l] + ada_b[l], 6, axis=-1)]
        mc = jnp.split(jax.nn.silu(c_ctx) @ ada_w[l] + ada_b[l], 6, axis=-1)
        h_lat = modulate(rms_norm(x_lat, norm_mix[l]), ml[0], ml[1])
        h_ctx = modulate(rms_norm(x_ctx, norm_mix[l]), mc[0], mc[1])
        y_lat, y_ctx = token_mixer(h_lat, h_ctx, rows, need_ctx, w_in[l], gdn_conv[l], gdn_a_log[l], gdn_dt_bias[l],
                                   lower[0, l], lower[1, l], hgrn_norm[l], gdn_norm[l],
                                   w_branch_a[l], w_branch_b[l], w_out[l])
        x_lat = x_lat + ml[2] * y_lat
        h2 = modulate(rms_norm(x_lat, norm_ffn[l]), ml[3], ml[4])
        x_lat = x_lat + ml[5] * expert_choice_ffn(h2, router_w[l], w_gate[l], w_up[l], w_down[l])
        if need_ctx:
            x_ctx = x_ctx + mc[2] * y_ctx
            h2c = modulate(rms_norm(x_ctx, norm_ffn[l]), mc[3], mc[4])
            x_ctx = x_ctx + mc[5] * expert_choice_ffn(h2c, router_w[l], w_gate[l], w_up[l], w_down[l])
    return rms_norm(x_lat, final_norm)
```

```python
import contextlib
import numpy as np
import concourse.bass as bass
import concourse.mybir as mybir
from concourse.bass_utils import run_bass_kernel_spmd

F32 = mybir.dt.float32
BF16 = mybir.dt.bfloat16
I32 = mybir.dt.int32
U32 = mybir.dt.uint32
AF = mybir.ActivationFunctionType
ALU = mybir.AluOpType

ENGS = ("pe", "act", "dve", "pool", "sp")
NEG = -30000.0


class Buf:
    __slots__ = ("name", "ws", "rs", "t", "multi", "dgroup", "excl")

    def __init__(self, name, t=None, multi=False, dgroup=None, excl=False):
        self.excl = excl
        self.name = name
        self.ws = {}
        self.rs = {}
        self.t = t
        self.multi = multi
        self.dgroup = dgroup or name

    def __getitem__(self, idx):
        return self.t[idx]


class Sched:
    def __init__(self, nc):
        self.nc = nc
        self.ops = {e: [] for e in ENGS}
        self.val = {}
        self.seen = {e: {} for e in ENGS}
        self.uid = 0
        self.dmap = {}
        self.dfree = {"sw": [], "hw": []}
        self.nd = 0

    def dkey(self, dgroup, kind):
        k = (dgroup, kind)
        if k not in self.dmap:
            if self.dfree[kind]:
                self.dmap[k] = self.dfree[kind].pop()
            else:
                self.dmap[k] = f"ds{self.nd}_{kind}"
                self.nd += 1
        return self.dmap[k]

    def release(self, dgroups):
        for k in list(self.dmap):
            if k[0] in dgroups:
                self.dfree[k[1]].append(self.dmap.pop(k))

    def dram(self, name, shape, dt, kind="Internal"):
        return Buf(name, self.nc.dram_tensor(name, list(shape), dt, kind=kind).ap(), multi=True)

    def emit(self, eng, fn, reads=(), writes=(), dma=None):
        deps = {}

        def add(k, v):
            if deps.get(k, 0) < v:
                deps[k] = v
        for b in reads:
            for k, v in b.ws.items():
                add(k, v)
            if b.excl:
                for k, v in b.rs.items():
                    if k != "c_" + eng:
                        add(k, v)
        for b in writes:
            if b.multi:
                continue
            for k, v in b.ws.items():
                add(k, v)
            for k, v in b.rs.items():
                add(k, v)
        seen = self.seen[eng]
        waits = []
        for k, v in deps.items():
            if seen.get(k, 0) < v:
                seen[k] = v
                waits.append((k, v))
        sk = self.dkey(dma.dgroup, "sw" if eng == "pool" else "hw") if dma is not None else ("c_" + eng)
        inc = 16 if dma is not None else 1
        self.val[sk] = self.val.get(sk, 0) + inc
        tv = self.val[sk]
        for b in reads:
            if b.rs.get(sk, 0) < tv:
                b.rs[sk] = tv
        for b in writes:
            if b.multi:
                b.ws[sk] = tv
            else:
                b.ws = {sk: tv}
                b.rs = {}
        self.ops[eng].append((waits, fn, sk, inc))

    def dma(self, eng, out, in_, reads, writes, sem=None, **kw):
        if sem is None:
            sem = [b for b in list(writes) + list(reads) if not b.multi][0]
        self.emit(eng, lambda e: e.dma_start(out=out, in_=in_, **kw), reads, writes, dma=sem)

    def emit_cc(self, fn, reads, writes):
        deps = {}
        for b in reads:
            for k, v in b.ws.items():
                if deps.get(k, 0) < v:
                    deps[k] = v
        seen = self.seen["pool"]
        waits = []
        for k, v in deps.items():
            if seen.get(k, 0) < v:
                seen[k] = v
                waits.append((k, v))
        sk = "cc_sem"
        self.val[sk] = self.val.get(sk, 0) + 1
        for b in writes:
            b.ws[sk] = self.val[sk]
        self.ops["pool"].append((waits, fn, sk, 1))

    def barrier(self):
        cur = dict(self.val)
        for eng in ENGS:
            seen = self.seen[eng]
            waits = []
            for k, v in cur.items():
                if seen.get(k, 0) < v:
                    seen[k] = v
                    waits.append((k, v))
            if waits:
                self.ops[eng].append((waits, None, None, 0))

    def finish(self):
        nc = self.nc
        sems = {k: nc.alloc_semaphore(k) for k in self.val}
        engobj = {"pe": "tensor", "act": "scalar", "dve": "vector", "pool": "gpsimd", "sp": "sync"}
        final = list(self.val.items())
        with nc.Block() as block:
            for eng in ENGS:
                ops = self.ops[eng]
                is_final = eng == "sp"

                def body(e, ops=ops, is_final=is_final):
                    for waits, fn, sk, inc in ops:
                        for k, v in waits:
                            e.wait_ge(sems[k], v)
                        if fn is not None:
                            if sk == "cc_sem":
                                fn(e).then_inc(sems[sk])
                            else:
                                fn(e).then_inc(sems[sk], inc)
                    if is_final:
                        for k, v in final:
                            e.wait_ge(sems[k], v)
                getattr(block, engobj[eng])(body)
        return nc


class Arena:
    def __init__(self, S, tag):
        self.S = S
        self.tag = tag
        self.st = contextlib.ExitStack()
        self.groups = set()

    def sb(self, name, shape, dt, multi=False, dgroup=None):
        nm = f"{self.tag}_{name}"
        t = self.st.enter_context(self.S.nc.sbuf_tensor(nm, list(shape), dt))
        b = Buf(nm, t, multi=multi, dgroup=dgroup and f"{self.tag}_{dgroup}")
        self.groups.add(b.dgroup)
        return b

    def ps(self, name, shape, dt=F32):
        nm = f"{self.tag}_{name}"
        t = self.st.enter_context(self.S.nc.psum_tensor(nm, list(shape), dt))
        return Buf(nm, t, excl=True)

    def close(self):
        self.S.barrier()
        self.S.release(self.groups)
        self.st.close()


class Cfg:
    def __init__(self, D=2048, H=8, T=4096, GW=64, NCTX=256, E=16, DE=1024, CAP=512, eps=1e-6):
        self.D, self.H, self.T, self.GW, self.NCTX = D, H, T, GW, NCTX
        self.E, self.DE, self.CAP, self.eps = E, DE, CAP, eps
        self.R = T // GW
        self.HV = H * 128
        self.KD = D // 128
        self.TT = T + NCTX
        HV = self.HV
        o = 0
        self.c_hq = o; o += HV
        self.c_hff = o; o += HV
        self.c_hfb = o; o += HV
        self.c_hi = o; o += HV
        self.c_hog = o; o += HV
        self.c_gqkv = o; o += 3 * HV
        self.c_gz = o; o += HV
        self.c_ga = o; o += 2 * H
        self.c_gb = o; o += 2 * H
        self.c_mg = o; o += 2 * D
        self.NIN = o


FULL = Cfg()


def token_blocks(cfg, lo, hi, nb=512):
    out = []
    for a, b in ((0, cfg.NCTX), (cfg.NCTX, cfg.TT)):
        a, b = max(a, lo), min(b, hi)
        t = a
        while t < b:
            n = min(nb, b - t)
            out.append((t, n))
            t += n
    return out


def make_consts(S, A, cfg):
    C = {}
    nc = S.nc
    identb = A.sb("identb", [128, 128], BF16)
    identf = A.sb("identf", [128, 128], F32)
    for ident in (identb, identf):
        S.emit("pool", lambda e, ident=ident: e.memset(ident[:], 0.0), writes=[ident])
        S.emit("pool", lambda e, ident=ident: e.affine_select(
            out=ident[:], in_=ident[:], pattern=[[-1, 128]], compare_op=ALU.not_equal, fill=1.0,
            base=0, channel_multiplier=1), reads=[ident], writes=[ident])
    C["identb"], C["identf"] = identb, identf
    onesb = A.sb("onesb", [128, 128], BF16)
    S.emit("pool", lambda e: e.memset(onesb[:], 1.0), writes=[onesb])
    C["onesb"] = onesb
    onesf = A.sb("onesf", [128, 128], F32)
    S.emit("pool", lambda e: e.memset(onesf[:], 1.0), writes=[onesf])
    C["onesf"] = onesf
    epsc = A.sb("epsc", [128, 1], F32)
    S.emit("pool", lambda e: e.memset(epsc[:], cfg.eps), writes=[epsc])
    C["epsc"] = epsc
    onec = A.sb("onec", [128, 1], F32)
    S.emit("pool", lambda e: e.memset(onec[:], 1.0), writes=[onec])
    C["onec"] = onec
    return C


def phase_ada(S, cfg, G, I):
    D, KD = cfg.D, cfg.KD
    A = Arena(S, "ada")
    NCB = 6 * KD
    SW = 512
    cT = A.sb("cT", [128, KD, 2], F32)
    s2 = A.sb("s2", [128, KD, 2], F32)
    bT = A.sb("bT", [128, NCB], F32)
    S.dma("sp", cT[:, :, 0], I["c"][0].rearrange("(k p) -> p k", p=128), [I["c"]], [cT], allow_slow_non_contiguous=True)
    S.dma("sp", cT[:, :, 1], I["c_ctx"][:].rearrange("(k p) -> p k", p=128), [I["c_ctx"]], [cT], allow_slow_non_contiguous=True)
    S.dma("sp", bT[:], I["ada_b"][0].rearrange("(k p) -> p k", p=128), [I["ada_b"]], [bT], allow_slow_non_contiguous=True)
    S.emit("act", lambda e: e.activation(out=s2[:], in_=cT[:], func=AF.Silu), [cT], [s2])
    wsl = [A.sb(f"w{i}", [128, KD, SW], F32) for i in range(2)]
    pa = A.ps("pa", [128, NCB, 2], F32)
    mod = G["mod"]
    wv = I["ada_w"][0].rearrange("(k p) n -> p k n", p=128)
    nslab = 6 * D // SW
    for s in range(nslab):
        w = wsl[s % 2]
        S.dma("sp" if s % 2 == 0 else "pool", w[:], wv[:, :, s * SW:(s + 1) * SW], [I["ada_w"]], [w])
        for j in range(SW // 128):
            cb = s * (SW // 128) + j

            def mm(e, w=w, j=j, cb=cb):
                r = None
                for k in range(KD):
                    r = e.matmul(pa[:, cb, :], lhsT=w[:, k, j * 128:(j + 1) * 128], rhs=s2[:, k, :],
                                 start=(k == 0), stop=(k == KD - 1))
                return r
            S.emit("pe", mm, [w, s2], [pa])
    S.emit("dve", lambda e: e.tensor_tensor(out=mod[:], in0=pa[:], in1=bT[:].to_broadcast([128, NCB, 2]) if False else
                                            bT[:].rearrange("p (n o) -> p n o", o=1).to_broadcast([128, NCB, 2]),
                                            op=ALU.add), [pa, bT], [mod])
    A.close()


def phase_coef(S, cfg, G, I):
    KD, D = cfg.KD, cfg.D
    A = Arena(S, "coef")
    mod = G["mod"]
    nm = A.sb("nm", [128, KD], F32)
    nf = A.sb("nf", [128, KD], F32)
    S.dma("sp", nm[:], I["norm_mix"][0].rearrange("(k p) -> p k", p=128), [I["norm_mix"]], [nm], allow_slow_non_contiguous=True)
    S.dma("sp", nf[:], I["norm_ffn"][0].rearrange("(k p) -> p k", p=128), [I["norm_ffn"]], [nf], allow_slow_non_contiguous=True)
    co = G["coef"]
    def mk(dst, gain, sc_j, s):
        S.emit("dve", lambda e: e.scalar_tensor_tensor(out=co[:, dst, :], in0=mod[:, sc_j * KD:(sc_j + 1) * KD, s], scalar=1.0,
                                                       in1=gain[:], op0=ALU.add, op1=ALU.mult), [mod, gain, co], [co])
    mk(0, nm, 1, 0)
    S.emit("dve", lambda e: e.tensor_copy(out=co[:, 1, :], in_=mod[:, 0:KD, 0]), [mod, co], [co])
    mk(2, nm, 1, 1)
    S.emit("dve", lambda e: e.tensor_copy(out=co[:, 3, :], in_=mod[:, 0:KD, 1]), [mod, co], [co])
    mk(4, nf, 4, 0)
    S.emit("dve", lambda e: e.tensor_copy(out=co[:, 5, :], in_=mod[:, 3 * KD:4 * KD, 0]), [mod, co], [co])
    gt = A.sb("gt", [128, 2, KD], F32)
    S.emit("dve", lambda e: e.tensor_copy(out=gt[:, 0, :], in_=mod[:, 2 * KD:3 * KD, 0]), [mod], [gt])
    S.emit("dve", lambda e: e.tensor_copy(out=gt[:, 1, :], in_=mod[:, 5 * KD:6 * KD, 0]), [mod, gt], [gt])
    for j in range(2):
        S.dma("sp", G["grow"][j].rearrange("(k p) -> p k", p=128), gt[:, j, :], [gt], [G["grow"]], allow_slow_non_contiguous=True)
    A.close()


def norm_tiles(S, A, cfg, C, tiles, hT, coefbuf, f32T=None):
    D, KD = cfg.D, cfg.KD
    xt = [A.sb(f"nx{i}", [128, D], F32) for i in range(2)]
    xn = [A.sb(f"nxn{i}", [128, D], BF16 if f32T is None else F32) for i in range(2)]
    junk = A.sb("njunk", [128, D], BF16)
    st = [A.sb(f"nst{i}", [128, 8], F32) for i in range(2)]
    tdt = BF16 if f32T is None else F32
    per = 8 if f32T is None else 4
    pts = [A.ps(f"npt{i}", [128, per, 128], tdt) for i in range(2)]
    ident = C["identb"] if f32T is None else C["identf"]
    co = coefbuf
    ng = KD // per if KD >= per else 1
    per = min(per, KD)
    gi = 0
    for ti, (pieces, srcbufs, col0, ci) in enumerate(tiles):
        x = xt[ti % 2]
        for k, (ap, p0, npp) in enumerate(pieces):
            S.dma("sp" if ti % 2 == 0 else "pool", x[p0:p0 + npp, :], ap, srcbufs, [x])
        s = st[ti % 2]
        import os
        NV = int(os.environ.get("K_NV", "9"))
        if NV < 1:
            continue
        S.emit("act", lambda e, x=x, s=s: e.activation(out=junk[:], in_=x[:], func=AF.Square, accum_out=s[:, 0:1]),
               [x], [junk, s])
        S.emit("dve", lambda e, s=s: e.tensor_scalar(out=s[:, 1:2], in0=s[:, 0:1], scalar1=1.0 / D, scalar2=cfg.eps,
                                                     op0=ALU.mult, op1=ALU.add), [s], [s])
        S.emit("act", lambda e, s=s: e.activation(out=s[:, 2:3], in_=s[:, 1:2], func=AF.Sqrt), [s], [s])
        S.emit("dve", lambda e, s=s: e.reciprocal(out=s[:, 3:4], in_=s[:, 2:3]), [s], [s])
        if NV < 2:
            continue
        n = xn[ti % 2]
        S.emit("dve", lambda e, x=x, s=s, n=n: e.tensor_scalar(out=n[:], in0=x[:], scalar1=s[:, 3:4], scalar2=None,
                                                               op0=ALU.mult), [x, s], [n])
        if NV < 3:
            continue
        for g in range(ng):
            pt = pts[gi % 2]
            gi += 1

            def tr(e, n=n, pt=pt, g=g):
                r = None
                for j in range(per):
                    c = g * per + j
                    r = e.transpose(out=pt[:, j, :], in_=n[:, c * 128:(c + 1) * 128], identity=ident[:])
                return r
            S.emit("pe", tr, [n, ident], [pt])
            if NV < 4:
                continue
            for j in range(per):
                c = g * per + j
                eng = "act" if gi % 2 == 0 else "dve"
                outs = [(hT, hT[:, c, col0:col0 + 128])]
                if f32T is not None:
                    outs.append((f32T, f32T[:, c, col0:col0 + 128]))
                for oi, (ob, oap) in enumerate(outs):
                    eng2 = eng if oi == 0 else ("dve" if eng == "act" else "act")
                    if eng2 == "act":
                        S.emit("act", lambda e, pt=pt, j=j, c=c, oap=oap, ci=ci: e.activation(
                            out=oap, in_=pt[:, j, :], func=AF.Identity, scale=co[:, ci, c:c + 1], bias=co[:, ci + 1, c:c + 1]),
                            [pt, co], [ob])
                    else:
                        S.emit("dve", lambda e, pt=pt, j=j, c=c, oap=oap, ci=ci: e.tensor_scalar(
                            out=oap, in0=pt[:, j, :], scalar1=co[:, ci, c:c + 1], scalar2=co[:, ci + 1, c:c + 1],
                            op0=ALU.mult, op1=ALU.add), [pt, co], [ob])


def lat_tile_pieces(cfg, xin, ti, order):
    if order == "r":
        return [(xin[0, ti * 128:(ti + 1) * 128, :], 0, 128)]
    R, GW = cfg.R, cfg.GW
    xv = xin[0].rearrange("(r c) d -> c r d", c=GW)
    out = []
    if R >= 128:
        per = R // 128
        cc, sub = divmod(ti, per)
        out.append((xv[cc, sub * 128:(sub + 1) * 128, :], 0, 128))
    else:
        ncol = 128 // R
        for k in range(ncol):
            out.append((xv[ti * ncol + k, :, :], k * R, R))
    return out


def act_tiles(cfg, I, order):
    tiles = []
    for t in range(cfg.NCTX // 128):
        tiles.append(([(I["ctx"][0, t * 128:(t + 1) * 128, :], 0, 128)], [I["ctx"]], t * 128, 2))
    for t in range(cfg.T // 128):
        tiles.append((lat_tile_pieces(cfg, I["x"], t, order), [I["x"]], cfg.NCTX + t * 128, 0))
    return tiles


def project(S, A, cfg, hT, w_in, groups, tok_lo=0):
    KD, TT = cfg.KD, cfg.TT
    SW = 256
    wsl = [A.sb(f"pw{i}", [128, KD, SW], BF16) for i in range(2)]
    pps = [A.ps(f"pp{i}", [128, 512], F32) for i in range(4)]
    stg = {}
    wv = w_in[0].rearrange("(k p) n -> p k n", p=128)
    si = 0
    pi = 0
    gi = 0
    for g in groups:
        col0, ncols, lay, func, dst, dt = g["col0"], g["ncols"], g["layout"], g["func"], g["dst"], g["dt"]
        tlo = g.get("tok_lo", 0)
        key = (lay, dt)
        if key not in stg:
            stg[key] = [A.sb(f"ps{lay}{len(stg)}_{i}", [128, 512], dt) for i in range(3)]
        stgs = stg[key]
        if lay == "F":
            blocks = token_blocks(cfg, tlo, TT)
            for s0 in range(0, ncols, SW):
                sw = min(SW, ncols - s0)
                w = wsl[si % 2]
                S.dma("pool", w[:, :, 0:sw], wv[:, :, col0 + s0:col0 + s0 + sw], [w_in], [w])
                si += 1
                for j in range(0, sw, 128):
                    for (t0, nb) in blocks:
                        pp = pps[pi % 4]
                        pi += 1

                        def mm(e, w=w, j=j, t0=t0, nb=nb, pp=pp):
                            r = None
                            for k in range(KD):
                                r = e.matmul(pp[:, 0:nb], lhsT=w[:, k, j:j + 128], rhs=hT[:, k, t0:t0 + nb],
                                             start=(k == 0), stop=(k == KD - 1))
                            return r
                        S.emit("pe", mm, [w, hT], [pp])
                        sg = stgs[gi % 3]
                        gi += 1
                        if func is None:
                            S.emit("dve", lambda e, sg=sg, pp=pp, nb=nb: e.tensor_copy(out=sg[:, 0:nb], in_=pp[:, 0:nb]), [pp], [sg])
                        else:
                            S.emit("act", lambda e, sg=sg, pp=pp, nb=nb, func=func: e.activation(out=sg[:, 0:nb], in_=pp[:, 0:nb], func=func),
                                   [pp], [sg])
                        r0 = s0 + j
                        S.dma("sp", dst[r0:r0 + 128, t0 - tlo:t0 - tlo + nb], sg[:, 0:nb], [sg], [dst])
        else:
            assert ncols <= 512 or ncols % 256 == 0
            for s0 in range(0, ncols, SW):
                sw = min(SW, ncols - s0)
                w = wsl[si % 2]
                S.dma("pool", w[:, :, 0:sw], wv[:, :, col0 + s0:col0 + s0 + sw], [w_in], [w])
                si += 1
                for t0 in range(tlo, TT, 128):
                    pp = pps[pi % 4]
                    pi += 1

                    def mm(e, w=w, sw=sw, t0=t0, pp=pp):
                        r = None
                        for k in range(KD):
                            r = e.matmul(pp[:, 0:sw], lhsT=hT[:, k, t0:t0 + 128], rhs=w[:, k, 0:sw],
                                         start=(k == 0), stop=(k == KD - 1))
                        return r
                    S.emit("pe", mm, [w, hT], [pp])
                    sg = stgs[gi % 3]
                    gi += 1
                    S.emit("dve", lambda e, sg=sg, pp=pp, sw=sw: e.tensor_copy(out=sg[:, 0:sw], in_=pp[:, 0:sw]), [pp], [sg])
                    S.dma("sp", dst[t0 - tlo:t0 - tlo + 128, s0:s0 + sw], sg[:, 0:sw], [sg], [dst])


def phase_proj(S, cfg, G, I, C, order):
    A = Arena(S, "pr" + order)
    KD, TT, HV, NCTX, H, D = cfg.KD, cfg.TT, cfg.HV, cfg.NCTX, cfg.H, cfg.D
    hT = A.sb("hT", [128, KD, TT], BF16, multi=True)
    norm_tiles(S, A, cfg, C, act_tiles(cfg, I, order), hT, G["coef"])
    if order == "r":
        groups = [
            dict(col0=cfg.c_hq, ncols=HV, layout="F", func=AF.Silu, dst=G["hqT"], dt=BF16, tok_lo=NCTX),
            dict(col0=cfg.c_hff, ncols=HV, layout="F", func=None, dst=G["hffT"], dt=BF16),
            dict(col0=cfg.c_hfb, ncols=HV, layout="F", func=None, dst=G["hfbT"], dt=BF16),
            dict(col0=cfg.c_hi, ncols=HV, layout="T", func=None, dst=G["hi"], dt=BF16),
            dict(col0=cfg.c_hog, ncols=HV, layout="F", func=AF.Sigmoid, dst=G["hogT"], dt=BF16, tok_lo=NCTX),
            dict(col0=cfg.c_gz, ncols=HV, layout="F", func=AF.Silu, dst=G["gzT"], dt=BF16, tok_lo=NCTX),
            dict(col0=cfg.c_mg, ncols=2 * D, layout="F", func=AF.Sigmoid, dst=G["mgT"], dt=BF16, tok_lo=NCTX),
        ]
    else:
        groups = [
            dict(col0=cfg.c_gqkv, ncols=3 * HV, layout="F", func=None, dst=G["gqkvT"], dt=BF16),
            dict(col0=cfg.c_ga, ncols=4 * H, layout="T", func=None, dst=G["gab"], dt=F32),
        ]
    import os
    sel = os.environ.get("K_GROUPS")
    if sel is not None:
        groups = [groups[int(i)] for i in sel.split(",") if i != ""]
    project(S, A, cfg, hT, I["w_in"], groups)
    A.close()


def phase_hgrn(S, cfg, G, I, C):
    A = Arena(S, "hg")
    H, T, TT, NCTX, HV = cfg.H, cfg.T, cfg.TT, cfg.NCTX, cfg.HV
    CH = 64
    NCH = TT // CH
    NCC = NCTX // CH
    order = [list(range(NCH)), list(range(NCC - 1, -1, -1)) + list(range(NCH - 1, NCC - 1, -1))]
    SEG = 1024
    segs = []
    for a, b in ((0, NCTX), (NCTX, TT)):
        t = a
        while t < b:
            n = min(SEG, b - t)
            segs.append((t, n))
            t += n
    lbT = A.sb("lbT", [128, 2, 2, H], F32)
    for d in range(2):
        for sl in range(2):
            S.dma("sp", lbT[:, d, sl, :], I["hgrn_lb"][d, sl].rearrange("(h p) -> p h", p=128), [I["hgrn_lb"]], [lbT],
                  allow_slow_non_contiguous=True)
    low = A.sb("low", [128, 2, H], F32)
    oml = A.sb("oml", [128, 2, H], F32)
    noml = A.sb("noml", [128, 2, H], F32)
    S.emit("dve", lambda e: e.tensor_tensor(out=low[:], in0=lbT[:, :, 0, :], in1=lbT[:, :, 1, :], op=ALU.subtract), [lbT], [low])
    S.emit("act", lambda e: e.activation(out=low[:], in_=low[:], func=AF.Sigmoid), [low], [low])
    S.emit("dve", lambda e: e.tensor_scalar(out=oml[:], in0=low[:], scalar1=-1.0, scalar2=1.0, op0=ALU.mult, op1=ALU.add), [low], [oml])
    S.emit("dve", lambda e: e.tensor_scalar(out=noml[:], in0=low[:], scalar1=1.0, scalar2=-1.0, op0=ALU.mult, op1=ALU.add), [low], [noml])
    gain = A.sb("gain", [128, H], F32)
    S.dma("sp", gain[:], I["hgrn_norm"][0].rearrange("(h p) -> p h", p=128), [I["hgrn_norm"]], [gain], allow_slow_non_contiguous=True)
    msk01 = A.sb("msk01", [128, SEG], F32)
    S.emit("pool", lambda e: e.memset(msk01[:], 1.0), writes=[msk01])
    S.emit("pool", lambda e: e.memset(msk01[:].rearrange("p (n c) -> p n c", c=CH)[:, :, 0:1], 0.0), [msk01], [msk01])
    cm = []
    for d in range(2):
        m = A.sb(f"cm{d}", [CH, CH], F32)
        S.emit("pool", lambda e, m=m: e.memset(m[:], 1.0), writes=[m])
        sg = 1 if d == 0 else -1
        S.emit("pool", lambda e, m=m, sg=sg: e.affine_select(out=m[:], in_=m[:], pattern=[[sg, CH]], compare_op=ALU.is_ge, fill=0.0,
                                                             base=0, channel_multiplier=-sg), [m], [m])
        cm.append(m)
    identb, onesb = C["identb"], C["onesb"]
    qT2 = [A.sb(f"qT{i}", [128, T], BF16) for i in range(2)]
    fl2 = [[A.sb(f"fl{d}{i}", [128, TT], BF16) for d in range(2)] for i in range(2)]
    vv2 = [A.sb(f"vv{i}", [CH, NCH, 128], BF16) for i in range(2)]
    og2 = [A.sb("og0", [128, T], BF16)] * 2
    qt = [A.sb(f"qt{d}", [128, T], BF16) for d in range(2)]
    kt = [A.sb(f"kt{d}", [128, TT], BF16) for d in range(2)]
    kh = [A.sb(f"kh{d}", [128, TT], BF16) for d in range(2)]
    egt = [A.sb(f"egt{d}", [128, NCH], F32) for d in range(2)]
    tsets = [[A.sb(f"t{n}{i}", [128, SEG], F32) for n in "ABCD"] for i in range(2)]
    OA = A.sb("OA", [128, T], F32)
    Sf = [[A.sb(f"S{d}{i}", [128, 128], F32) for i in range(2)] for d in range(2)]
    Sb = [[A.sb(f"Sb{d}{i}", [128, 128], BF16) for i in range(3)] for d in range(2)]
    sTs = [[A.sb(f"sTs{d}{i}", [CH, CH], BF16) for i in range(3)] for d in range(2)]
    khs = [[A.sb(f"khs{d}{i}", [CH, 128], BF16) for i in range(3)] for d in range(2)]
    sqb = A.sb("sqb", [128, 512], BF16)
    rt = A.sb("rt", [128, 512], F32)
    ys = [A.sb("ys0", [128, 512], BF16)] * 2
    pst = [A.ps(f"pst{d}", [CH, 512], F32) for d in range(2)]
    ptr = [A.ps(f"ptr{d}", [CH, 1024], BF16) for d in range(2)]
    po = [A.ps(f"po{d}", [128, 512], F32) for d in range(2)]
    pd = [A.ps(f"pd{d}", [128, 512], F32) for d in range(2)]

    def load_head(h):
        r0 = h * 128
        qT, fl, vv, og = qT2[h % 2], fl2[h % 2], vv2[h % 2], og2[h % 2]
        S.dma("sp", qT[:], G["hqT"][r0:r0 + 128, :], [G["hqT"]], [qT])
        S.dma("sp", fl[0][:], G["hffT"][r0:r0 + 128, :], [G["hffT"]], [fl[0]])
        S.dma("sp", fl[1][:], G["hfbT"][r0:r0 + 128, :], [G["hfbT"]], [fl[1]])
        S.dma("sp", vv[:], G["hi"][:, r0:r0 + 128].rearrange("(n c) v -> c n v", c=CH), [G["hi"]], [vv])

    tsi_box = [0]

    def do_head(h, qT, fl, vv, og):
        r0 = h * 128
        S.dma("sp", og[:], G["hogT"][r0:r0 + 128, :], [G["hogT"]], [og])
        for d in range(2):
            lo_, om_, nom_ = low[:, d, h:h + 1], oml[:, d, h:h + 1], noml[:, d, h:h + 1]
            for (a, n) in segs:
                tA, tB, tC, tD = tsets[tsi_box[0] % 2]
                tsi_box[0] += 1
                nch = n // CH
                c0 = a // CH
                v3 = lambda t, n=n: t[:, 0:n].rearrange("p (n c) -> p n c", c=CH)
                S.emit("act", lambda e, tA=tA, tB=tB, tC=tC, tD=tD, a=a, n=n, d=d: e.activation(out=tA[:, 0:n], in_=fl[d][:, a:a + n], func=AF.Sigmoid), [fl[d]], [tA])
                S.emit("act", lambda e, tA=tA, tB=tB, tC=tC, tD=tD, n=n, lo_=lo_, om_=om_: e.activation(out=tB[:, 0:n], in_=tA[:, 0:n], func=AF.Ln, scale=om_, bias=lo_),
                       [tA, low, oml], [tB])
                S.emit("dve", lambda e, tA=tA, tB=tB, tC=tC, tD=tD, n=n, om_=om_, nom_=nom_: e.tensor_scalar(out=tC[:, 0:n], in0=tA[:, 0:n], scalar1=nom_, scalar2=om_,
                                                                                op0=ALU.mult, op1=ALU.add), [tA, oml, noml], [tC])
                S.emit("dve", lambda e, tA=tA, tB=tB, tC=tC, tD=tD, n=n: e.tensor_tensor_scan(out=tA[:, 0:n], data0=msk01[:, 0:n], data1=tB[:, 0:n], initial=0.0,
                                                                  op0=ALU.mult, op1=ALU.add), [msk01, tB, tA], [tA])
                tot = lambda nch=nch, v3=v3, tA=tA: v3(tA)[:, :, CH - 1:CH]
                if d == 0:
                    Gd = tA
                else:
                    S.emit("dve", lambda e, tA=tA, tB=tB, tC=tC, tD=tD, n=n: e.tensor_tensor(out=tD[:, 0:n], in0=tB[:, 0:n], in1=tA[:, 0:n], op=ALU.subtract), [tA, tB], [tD])
                    S.emit("dve", lambda e, tA=tA, tB=tB, tC=tC, tD=tD, v3=v3, tot=tot, nch=nch: e.tensor_tensor(out=v3(tB), in0=v3(tD), in1=tot().to_broadcast([128, nch, CH]),
                                                                                     op=ALU.add), [tD, tA], [tB])
                    Gd = tB
                S.emit("act", lambda e, tA=tA, tB=tB, tC=tC, tD=tD, c0=c0, nch=nch, d=d, tot=tot: e.activation(out=egt[d][:, c0:c0 + nch].rearrange("p (n o) -> p n o", o=1),
                                                                                  in_=tot(), func=AF.Exp), [tA], [egt[d]])
                if a >= NCTX:
                    S.emit("act", lambda e, tA=tA, tB=tB, tC=tC, tD=tD, n=n, Gd=Gd: e.activation(out=tD[:, 0:n], in_=Gd[:, 0:n], func=AF.Exp), [Gd], [tD])
                    S.emit("dve", lambda e, tA=tA, tB=tB, tC=tC, tD=tD, n=n, a=a, d=d: e.tensor_tensor(out=qt[d][:, a - NCTX:a - NCTX + n], in0=qT[:, a - NCTX:a - NCTX + n],
                                                                           in1=tD[:, 0:n], op=ALU.mult), [qT, tD], [qt[d]])
                S.emit("act", lambda e, tA=tA, tB=tB, tC=tC, tD=tD, n=n, Gd=Gd: e.activation(out=tD[:, 0:n], in_=Gd[:, 0:n], func=AF.Exp, scale=-1.0), [Gd], [tD])
                S.emit("pool", lambda e, tA=tA, tB=tB, tC=tC, tD=tD, n=n, a=a, d=d: e.tensor_tensor(out=kt[d][:, a:a + n], in0=tC[:, 0:n], in1=tD[:, 0:n], op=ALU.mult),
                       [tC, tD], [kt[d]])
                S.emit("dve", lambda e, tA=tA, tB=tB, tC=tC, tD=tD, v3=v3, tot=tot, nch=nch, Gd=Gd: e.tensor_tensor(out=v3(tD), in0=tot().to_broadcast([128, nch, CH]),
                                                                                       in1=v3(Gd), op=ALU.subtract), [tA, Gd], [tD])
                S.emit("act", lambda e, tA=tA, tB=tB, tC=tC, tD=tD, n=n: e.activation(out=tD[:, 0:n], in_=tD[:, 0:n], func=AF.Exp), [tD], [tD])
                S.emit("pool", lambda e, tA=tA, tB=tB, tC=tC, tD=tD, n=n, a=a, d=d: e.tensor_tensor(out=kh[d][:, a:a + n], in0=tC[:, 0:n], in1=tD[:, 0:n], op=ALU.mult),
                       [tC, tD], [kh[d]])
        for d in range(2):
            S.emit("pool", lambda e, d=d: e.memset(Sf[d][0][:], 0.0), writes=[Sf[d][0]])
            S.emit("pool", lambda e, d=d: e.memset(Sb[d][0][:], 0.0), writes=[Sb[d][0]])
        OAc = {}

        def genA(d, n):
            c = order[d][n]
            t0 = c * CH
            sl = n % 3
            if c >= NCC:
                q0 = t0 - NCTX
                S.emit("pe", lambda e: e.matmul(pst[d][:, 0:CH], lhsT=kt[d][:, t0:t0 + CH], rhs=qt[d][:, q0:q0 + CH],
                                                start=True, stop=True), [kt[d], qt[d]], [pst[d]])
                yield
                S.emit("dve", lambda e: e.tensor_tensor(out=sTs[d][sl][:], in0=pst[d][:, 0:CH], in1=cm[d][:], op=ALU.mult),
                       [pst[d], cm[d]], [sTs[d][sl]])
                yield
            S.emit("pe", lambda e: e.transpose(out=ptr[d][:, 0:128], in_=kh[d][:, t0:t0 + CH], identity=identb[:]),
                   [kh[d], identb], [ptr[d]])
            yield
            S.emit("act", lambda e: e.copy(out=khs[d][sl][:], in_=ptr[d][:, 0:128]), [ptr[d]], [khs[d][sl]])
            yield

        def genB(d, n):
            c = order[d][n]
            t0 = c * CH
            sl = n % 3
            Sf_o, Sf_n = Sf[d][n % 2], Sf[d][(n + 1) % 2]
            Sb_o, Sb_n = Sb[d][n % 3], Sb[d][(n + 1) % 3]
            S.emit("pe", lambda e: e.matmul(pd[d][:, 0:128], lhsT=khs[d][sl][:], rhs=vv[:, c, :], start=True, stop=True),
                   [khs[d][sl], vv], [pd[d]])
            yield
            S.emit("dve", lambda e: e.scalar_tensor_tensor(out=Sf_n[:], in0=Sf_o[:], scalar=egt[d][:, c:c + 1], in1=pd[d][:, 0:128],
                                                           op0=ALU.mult, op1=ALU.add), [Sf_o, egt[d], pd[d]], [Sf_n])
            yield
            S.emit("act", lambda e: e.copy(out=Sb_n[:], in_=Sf_n[:]), [Sf_n], [Sb_n])
            yield
            if c >= NCC:
                q0 = t0 - NCTX

                def mo(e):
                    e.matmul(po[d][:, 0:CH], lhsT=Sb_o[:], rhs=qt[d][:, q0:q0 + CH], start=True, stop=False)
                    return e.matmul(po[d][:, 0:CH], lhsT=vv[:, c, :], rhs=sTs[d][sl][:], start=False, stop=True)
                S.emit("pe", mo, [Sb_o, qt[d], vv, sTs[d][sl]], [po[d]])
                yield
                if c not in OAc:
                    OAc[c] = Buf(f"OAc{c}")
                    OAc[c].ws = dict(OA.ws)
                    OAc[c].rs = dict(OA.rs)
                    S.emit("act", lambda e: e.copy(out=OA[:, q0:q0 + CH], in_=po[d][:, 0:CH]), [po[d]], [OAc[c]])
                else:
                    S.emit("dve", lambda e: e.tensor_tensor(out=OA[:, q0:q0 + CH], in0=po[d][:, 0:CH], in1=OA[:, q0:q0 + CH],
                                                            op=ALU.add), [po[d], OAc[c]], [OAc[c]])
                yield

        def run_streams(streams):
            while streams:
                for g in list(streams):
                    try:
                        next(g)
                    except StopIteration:
                        streams.remove(g)

        run_streams([genA(0, 0), genA(1, 0)])
        for n in range(NCH):
            sts = []
            if n + 1 < NCH:
                sts += [genA(0, n + 1), genA(1, n + 1)]
            sts += [genB(0, n), genB(1, n)]
            run_streams(sts)
        for bk in OAc.values():
            for k_, v_ in bk.ws.items():
                if OA.ws.get(k_, 0) < v_:
                    OA.ws[k_] = v_
        S.emit("dve", lambda e: e.tensor_tensor(out=OA[:], in0=OA[:], in1=og[:], op=ALU.mult), [OA, og], [OA])
        for bi, t0 in enumerate(range(0, T, 512)):
            nb = min(512, T - t0)
            S.emit("act", lambda e, t0=t0, nb=nb: e.activation(out=sqb[:, 0:nb], in_=OA[:, t0:t0 + nb], func=AF.Square), [OA], [sqb])
            S.emit("pe", lambda e, nb=nb: e.matmul(po[0][:, 0:nb], lhsT=onesb[:], rhs=sqb[:, 0:nb], start=True, stop=True), [onesb, sqb], [po[0]])
            S.emit("act", lambda e, nb=nb: e.activation(out=rt[:, 0:nb], in_=po[0][:, 0:nb], func=AF.Ln, scale=1.0 / 128, bias=C["epsc"][:, 0:1]),
                   [po[0], C["epsc"]], [rt])
            S.emit("act", lambda e, nb=nb: e.activation(out=rt[:, 0:nb], in_=rt[:, 0:nb], func=AF.Exp, scale=-0.5), [rt], [rt])
            y = ys[bi % 2]
            S.emit("dve", lambda e, t0=t0, nb=nb, y=y, h=h: e.scalar_tensor_tensor(out=y[:, 0:nb], in0=OA[:, t0:t0 + nb], scalar=gain[:, h:h + 1],
                                                                                   in1=rt[:, 0:nb], op0=ALU.mult, op1=ALU.mult), [OA, gain, rt], [y])
            S.dma("sp", G["yAT"][r0:r0 + 128, t0:t0 + nb], y[:, 0:nb], [y], [G["yAT"]])

    load_head(0)
    for h in range(H):
        if h + 1 < H:
            load_head(h + 1)
        do_head(h, qT2[h % 2], fl2[h % 2], vv2[h % 2], og2[h % 2])
    A.close()


def phase_gconv(S, cfg, G, I, C):
    A = Arena(S, "gc")
    H, T, TT, NCTX, HV = cfg.H, cfg.T, cfg.TT, cfg.NCTX, cfg.HV
    NB = 3 * HV // 128
    cw = A.sb("cw", [128, 5, NB], F32)
    for j in range(5):
        S.dma("sp", cw[:, j, :], I["gdn_conv"][0, j].rearrange("(n p) -> p n", p=128), [I["gdn_conv"]], [cw], allow_slow_non_contiguous=True)
    xp = [A.sb(f"xp{i}", [128, TT + 8], BF16) for i in range(2)]
    for x in xp:
        S.emit("pool", lambda e, x=x: e.memset(x[:], 0.0), writes=[x])
    y = A.sb("y", [128, TT], F32)
    sq = A.sb("sq", [128, 512], BF16)
    rt = A.sb("rt", [128, 512], F32)
    ob = [A.sb(f"ob{i}", [128, TT], BF16) for i in range(2)]
    pn = [A.ps(f"pn{i}", [128, 512], F32) for i in range(2)]
    segs = [(0, NCTX, 2), (NCTX, T, NCTX + 6)]
    dsts = [G["gq"], G["gk"], G["gv"]]
    pi = 0
    for fb in range(NB):
        kind, hb = divmod(fb, H)
        x = xp[fb % 2]
        r0 = fb * 128
        for (a, n, off) in segs:
            if kind == 0 and a == 0:
                continue
            S.dma("sp" if fb % 2 == 0 else "pool", x[:, off:off + n], G["gqkvT"][r0:r0 + 128, a:a + n], [G["gqkvT"]], [x])
            S.emit("dve", lambda e, x=x, a=a, n=n, off=off, fb=fb: e.tensor_scalar(out=y[:, a:a + n], in0=x[:, off - 2:off - 2 + n], scalar1=cw[:, 0, fb:fb + 1],
                                                                                  scalar2=None, op0=ALU.mult), [x, cw], [y])
            for j in range(1, 5):
                S.emit("dve", lambda e, x=x, a=a, n=n, off=off, fb=fb, j=j: e.scalar_tensor_tensor(
                    out=y[:, a:a + n], in0=x[:, off - 2 + j:off - 2 + j + n], scalar=cw[:, j, fb:fb + 1], in1=y[:, a:a + n],
                    op0=ALU.mult, op1=ALU.add), [x, cw, y], [y])
        lo = 0 if kind != 0 else NCTX
        o = ob[fb % 2]
        S.emit("act", lambda e, lo=lo: e.activation(out=y[:, lo:TT], in_=y[:, lo:TT], func=AF.Silu), [y], [y])
        if kind == 2:
            S.emit("dve", lambda e, o=o: e.tensor_copy(out=o[:], in_=y[:]), [y], [o])
        else:
            sc = (128.0 ** -0.5) if kind == 0 else 1.0
            for (t0, nb) in token_blocks(cfg, lo, TT):
                p = pn[pi % 2]
                pi += 1
                S.emit("dve", lambda e, t0=t0, nb=nb: e.tensor_tensor(out=sq[:, 0:nb], in0=y[:, t0:t0 + nb], in1=y[:, t0:t0 + nb], op=ALU.mult), [y], [sq])
                S.emit("pe", lambda e, p=p, nb=nb: e.matmul(p[:, 0:nb], lhsT=C["onesb"][:], rhs=sq[:, 0:nb], start=True, stop=True), [C["onesb"], sq], [p])
                S.emit("act", lambda e, p=p, nb=nb: e.activation(out=rt[:, 0:nb], in_=p[:, 0:nb], func=AF.Ln, bias=C["epsc"][:, 0:1]), [p, C["epsc"]], [rt])
                S.emit("act", lambda e, nb=nb: e.activation(out=rt[:, 0:nb], in_=rt[:, 0:nb], func=AF.Exp, scale=-0.5), [rt], [rt])
                S.emit("dve", lambda e, o=o, t0=t0, nb=nb, sc=sc: e.scalar_tensor_tensor(out=o[:, t0:t0 + nb], in0=y[:, t0:t0 + nb], scalar=sc, in1=rt[:, 0:nb],
                                                                                        op0=ALU.mult, op1=ALU.mult), [y, rt], [o])
        S.dma("sp", dsts[kind][hb * 128:(hb + 1) * 128, lo:TT], o[:, lo:TT], [o], [dsts[kind]])
    A.close()


def phase_gdn(S, cfg, G, I, C):
    H, T, TT, NCTX, HV, R, GW = cfg.H, cfg.T, cfg.TT, cfg.NCTX, cfg.HV, cfg.R, cfg.GW
    NCHK = TT // 128
    NCC = NCTX // 128
    order = [list(range(NCHK)), list(range(NCC - 1, -1, -1)) + list(range(NCHK - 1, NCC - 1, -1))]
    HG = min(4, H)
    identb, identf, onesb, onesf = C["identb"], C["identf"], C["onesb"], C["onesf"]
    A = Arena(S, "gd")
    def indicator(name, sg, strict):
        m = A.sb(name, [128, 128], F32)
        S.emit("pool", lambda e: e.memset(m[:], 1.0), writes=[m])
        S.emit("pool", lambda e: e.affine_select(out=m[:], in_=m[:], pattern=[[sg, 128]], compare_op=ALU.is_ge, fill=0.0,
                                                 base=-strict, channel_multiplier=-sg), [m], [m])
        return m
    VI = [indicator("VIf", 1, 0), indicator("VIb", -1, 0)]
    VS = [indicator("VSf", 1, 1), indicator("VSb", -1, 1)]
    NMI, NMS = [], []
    for d in range(2):
        for (lst, src, nm) in ((NMI, VI[d], f"NMI{d}"), (NMS, VS[d], f"NMS{d}")):
            m = A.sb(nm, [128, 128], BF16)
            S.emit("dve", lambda e, m=m, src=src: e.tensor_scalar(out=m[:], in0=src[:], scalar1=-1.0, scalar2=-NEG, op0=ALU.add, op1=ALU.mult), [src], [m])
            lst.append(m)
    dmask = A.sb("dmask", [128, 128], BF16)
    S.emit("pool", lambda e: e.memset(dmask[:], 1.0), writes=[dmask])
    S.emit("pool", lambda e: e.affine_select(out=dmask[:].rearrange("p (b c) -> p b c", c=32), in_=dmask[:].rearrange("p (b c) -> p b c", c=32),
                                             pattern=[[-32, 4], [0, 32]], compare_op=ALU.is_ge, fill=0.0, base=0, channel_multiplier=1), [dmask], [dmask])
    S.emit("pool", lambda e: e.affine_select(out=dmask[:].rearrange("p (b c) -> p b c", c=32), in_=dmask[:].rearrange("p (b c) -> p b c", c=32),
                                             pattern=[[32, 4], [0, 32]], compare_op=ALU.is_ge, fill=0.0, base=31, channel_multiplier=-1), [dmask], [dmask])
    omask = A.sb("omask", [128, 128], BF16)
    S.emit("dve", lambda e: e.tensor_scalar(out=omask[:], in0=dmask[:], scalar1=-1.0, scalar2=1.0, op0=ALU.mult, op1=ALU.add), [dmask], [omask])
    LC = [VI[0], VI[1]]
    LR = [VS[1], VS[0]]
    W2 = 2 * H
    sh = [128, NCHK, W2]
    bet = A.sb("bet", sh, F32)
    gsp = A.sb("gsp", [128, NCHK, 2, W2], BF16)
    lsp = A.sb("lsp", [128, NCHK, 2, W2], BF16)
    egr = A.sb("egr", sh, F32)
    egt = A.sb("egt", sh, F32)
    nbe = A.sb("nbe", sh, F32)
    A0 = Arena(S, "gd0")
    gg = A0.sb("gg", sh, F32)
    lnb = A0.sb("lnb", sh, F32)
    gres = A0.sb("gres", sh, F32)
    gab = A0.sb("gab", [128, NCHK, 4 * H], F32)
    S.dma("sp", gab[:], G["gab"][:, :].rearrange("(n p) c -> p n c", p=128), [G["gab"]], [gab])
    cst = A0.sb("cst", [128, 2, 2 * H], F32)
    S.dma("sp", cst[:, 0, :], I["gdn_dt_bias"][0].rearrange("d h -> (d h)").partition_broadcast(128), [I["gdn_dt_bias"]], [cst])
    S.dma("sp", cst[:, 1, :], I["gdn_a_log"][0].rearrange("d h -> (d h)").partition_broadcast(128), [I["gdn_a_log"]], [cst])
    nea = A0.sb("nea", [128, 2 * H], F32)
    S.emit("act", lambda e: e.activation(out=nea[:], in_=cst[:, 1, :], func=AF.Exp), [cst], [nea])
    S.emit("dve", lambda e: e.tensor_scalar(out=nea[:], in0=nea[:], scalar1=-1.0, scalar2=None, op0=ALU.mult), [nea], [nea])
    gx = A0.sb("gx", sh, F32); gt1 = A0.sb("gt1", sh, F32); gt2 = A0.sb("gt2", sh, F32)
    bc = lambda t: t.rearrange("p (o c) -> p o c", o=1).to_broadcast(sh)
    S.emit("dve", lambda e: e.tensor_tensor(out=gx[:], in0=gab[:, :, 0:W2], in1=bc(cst[:, 0, :]), op=ALU.add), [gab, cst], [gx])
    S.emit("dve", lambda e: e.tensor_scalar(out=gt1[:], in0=gx[:], scalar1=-1.0, scalar2=None, op0=ALU.mult), [gx], [gt1])
    S.emit("dve", lambda e: e.tensor_tensor(out=gt1[:], in0=gt1[:], in1=gx[:], op=ALU.max), [gt1, gx], [gt1])
    S.emit("act", lambda e: e.activation(out=gt1[:], in_=gt1[:], func=AF.Exp, scale=-1.0), [gt1], [gt1])
    S.emit("act", lambda e: e.activation(out=gt1[:], in_=gt1[:], func=AF.Ln, bias=C["onec"][:, 0:1]), [gt1, C["onec"]], [gt1])
    S.emit("dve", lambda e: e.tensor_scalar(out=gt2[:], in0=gx[:], scalar1=0.0, scalar2=None, op0=ALU.max), [gx], [gt2])
    S.emit("dve", lambda e: e.tensor_tensor(out=gt2[:], in0=gt2[:], in1=gt1[:], op=ALU.add), [gt2, gt1], [gt2])
    S.emit("dve", lambda e: e.tensor_tensor(out=gg[:], in0=gt2[:], in1=bc(nea[:]), op=ALU.mult), [gt2, nea], [gg])
    S.emit("act", lambda e: e.activation(out=bet[:], in_=gab[:, :, W2:2 * W2], func=AF.Sigmoid), [gab], [bet])
    S.emit("act", lambda e: e.activation(out=lnb[:], in_=bet[:], func=AF.Ln), [bet], [lnb])
    for src, dst in ((gg, gsp), (lnb, lsp)):
        S.emit("dve", lambda e, src=src, dst=dst: e.tensor_copy(out=dst[:, :, 0, :], in_=src[:]), [src], [dst])
        S.emit("dve", lambda e, src=src, dst=dst: e.tensor_tensor(out=gres[:], in0=src[:], in1=dst[:, :, 0, :], op=ALU.subtract), [src, dst], [gres])
        S.emit("dve", lambda e, dst=dst: e.tensor_copy(out=dst[:, :, 1, :], in_=gres[:]), [gres, dst], [dst])
    pA = A.ps("pA", [128, 512], F32)
    pg = pA
    for n in range(NCHK):
        def mm(e, n=n):
            e.matmul(pg[:, 0:H], lhsT=LC[0][:], rhs=gg[:, n, 0:H], start=True, stop=True)
            e.matmul(pg[:, H:W2], lhsT=LC[1][:], rhs=gg[:, n, H:W2], start=True, stop=True)
            e.matmul(pg[:, W2:W2 + H], lhsT=LR[0][:], rhs=gg[:, n, 0:H], start=True, stop=True)
            e.matmul(pg[:, W2 + H:2 * W2], lhsT=LR[1][:], rhs=gg[:, n, H:W2], start=True, stop=True)
            return e.matmul(pg[:, 2 * W2:3 * W2], lhsT=onesf[:], rhs=gg[:, n, :], start=True, stop=True)
        S.emit("pe", mm, [LC[0], LC[1], LR[0], LR[1], onesf, gg], [pg])
        S.emit("act", lambda e, n=n: e.activation(out=egr[:, n, :], in_=pg[:, W2:2 * W2], func=AF.Exp), [pg, egr], [egr])
        S.emit("act", lambda e, n=n: e.activation(out=egt[:, n, :], in_=pg[:, 2 * W2:3 * W2], func=AF.Exp), [pg, egt], [egt])
        S.emit("act", lambda e, n=n: e.activation(out=nbe[:, n, :], in_=pg[:, 0:W2], func=AF.Exp), [pg, nbe], [nbe])
    S.emit("dve", lambda e: e.scalar_tensor_tensor(out=nbe[:], in0=nbe[:], scalar=-1.0, in1=bet[:], op0=ALU.mult, op1=ALU.mult), [nbe, bet], [nbe])
    if "dbg_g" in G:
        for i, t in enumerate((gg, gg, egr, egt, nbe, bet)):
            S.dma("sp", G["dbg_g"][i], t[:], [t], [G["dbg_g"]])
    A0.close()
    LCb = []
    for d in range(2):
        m = A.sb(f"LCb{d}", [128, 128], BF16)
        S.emit("dve", lambda e, m=m, d=d: e.tensor_copy(out=m[:], in_=LC[d][:]), [LC[d]], [m])
        LCb.append(m)
    gain = A.sb("gain", [128, H], F32)
    S.dma("sp", gain[:], I["gdn_norm"][0].rearrange("(h p) -> p h", p=128), [I["gdn_norm"]], [gain], allow_slow_non_contiguous=True)

    NU = 2 * HG
    ld = {nm: [[A.sb(f"ld{nm}{d}{i}", [128, HG, 128], BF16) for i in range(3 if nm == "k" else 2)] for d in range(2)] for nm in "kqv"}
    U = []
    for u in range(NU):
        ub = {}
        for nm in ("TT", "PT", "qg", "kd", "vb"):
            ub[nm] = [A.sb(f"u{u}{nm}{i}", [128, 128], BF16) for i in range(2 if nm == "TT" else 3)]
        ub["X"] = [[A.sb(f"u{u}X{j}{i}", [128, 4, 128], BF16) for i in range(2)] for j in range(2)]
        ub["Z0"] = [A.sb(f"u{u}Z0{j}", [128, 128], BF16) for j in range(2)]
        ub["OT"] = [A.sb(f"u{u}OT{j}", [128, 128], BF16) for j in range(2)]
        ub["gm"] = [A.sb(f"u{u}gm{j}", [128, 2, 128], BF16) for j in range(2)]
        ub["eg"] = [A.sb(f"u{u}eg{j}", [128, 128], BF16) for j in range(2)]
        ub["DD"] = A.sb(f"u{u}DD", [128, 2, 128], BF16)
        ub["NN"] = A.sb(f"u{u}NN", [128, 2, 128], BF16)
        ub["N2"] = A.sb(f"u{u}N2", [128, 2, 128], BF16)
        ub["PP"] = A.sb(f"u{u}PP", [128, 2, 128], BF16)
        ub["S"] = A.sb(f"u{u}S", [128, 128], F32)
        ub["Sb"] = A.sb(f"u{u}Sb", [128, 128], BF16)
        ub["r"] = A.sb(f"u{u}r", [128, 128], BF16)
        ub["vn"] = A.sb(f"u{u}vn", [128, 128], BF16)
        U.append(ub)
    ngu = [A.sb(f"ngu{i}", [128, 1], F32) for i in range(2)]
    OB = A.sb("OB", [128, HG, T], F32)
    zq = A.sb("zq", [128, 512], BF16); sqb = A.sb("sqb", [128, 512], BF16); rt = A.sb("rt", [128, 512], F32)
    ys = [sqb, sqb]
    djunk = rt
    pR = A.ps("pR", [128, 512], F32)
    pT = A.ps("pT", [128, 1024], BF16)
    pI = [A.ps(f"pI{i}", [128, 512], F32) for i in range(4)]
    pS = A.ps("pS", [128, 512], F32)
    ncol = 128 // R if R < 128 else 1
    v4 = lambda p: p[:, 0:512].rearrange("p (a b) -> p a b", b=128)

    def run_streams(streams):
        streams = [g for g in streams if g is not None]
        while streams:
            for g in list(streams):
                try:
                    next(g)
                except StopIteration:
                    streams.remove(g)

    for hg in range(H // HG):
        for ub in U:
            S.emit("pool", lambda e, ub=ub: e.memset(ub["S"][:], 0.0), writes=[ub["S"]])
            S.emit("pool", lambda e, ub=ub: e.memset(ub["Sb"][:], 0.0), writes=[ub["Sb"]])
        OBc = {}

        def units_of(s):
            out = []
            for d in range(2):
                c = order[d][s]
                for hl in range(HG):
                    out.append((d, hl, c, c >= NCC))
            return out

        def gen_P(s):
            s3, s2 = s % 3, s % 2
            for d in range(2):
                c = order[d][s]
                t0 = c * 128
                lat = c >= NCC
                for nm, src in (("k", G["gk"]), ("q", G["gq"]), ("v", G["gv"])):
                    if nm == "q" and not lat:
                        continue
                    dst = ld[nm][d][s3 if nm == "k" else s2]
                    S.dma("sp" if d == 0 else "pool", dst[:],
                          src[hg * HG * 128:(hg + 1) * HG * 128, t0:t0 + 128].rearrange("(h p) t -> p h t", p=128), [src], [dst])
            yield
            ii = 0
            for (d, hl, c, lat) in units_of(s):
                u = d * HG + hl
                ub = U[u]
                h = hg * HG + hl
                dh = d * H + h
                kT = ld["k"][d][s3]; qTb = ld["q"][d][s2]; vT = ld["v"][d][s2]
                X0 = ub["X"][s2][0]
                X1 = ub["X"][s2][1]
                Z0 = ub["Z0"][s2]
                OT = ub["OT"][s2]
                def mA(e, kT=kT, qTb=qTb, hl=hl, lat=lat):
                    r = e.matmul(pA[:, 0:128], lhsT=kT[:, hl, :], rhs=kT[:, hl, :], start=True, stop=True)
                    if lat:
                        r = e.matmul(pA[:, 128:256], lhsT=kT[:, hl, :], rhs=qTb[:, hl, :], start=True, stop=True)
                    return r
                S.emit("pe", mA, [kT, qTb], [pA])
                yield
                gm = ub["gm"][s2]
                eg = ub["eg"][s2]
                S.emit("dve", lambda e, Z0=Z0, gm=gm: e.scalar_tensor_tensor(out=Z0[:], in0=pA[:, 0:128], scalar=-1.0, in1=gm[:, 1, :],
                                                                             op0=ALU.mult, op1=ALU.mult), [pA, gm], [Z0])
                yield
                def mT(e, Z0=Z0, kT=kT, vT=vT, hl=hl):
                    e.transpose(out=pT[:, 0:128], in_=Z0[:], identity=identb[:])
                    e.transpose(out=pT[:, 128:256], in_=kT[:, hl, :], identity=identb[:])
                    return e.transpose(out=pT[:, 256:384], in_=vT[:, hl, :], identity=identb[:])
                S.emit("pe", mT, [Z0, kT, vT, identb], [pT])
                if lat:
                    S.emit("dve", lambda e, ub=ub, s3=s3, gm=gm: e.tensor_tensor(out=ub["PT"][s3][:], in0=pA[:, 128:256], in1=gm[:, 0, :], op=ALU.mult),
                           [pA, gm], [ub["PT"][s3]])
                S.emit("pool", lambda e, X0=X0, Z0=Z0: e.tensor_tensor(out=X0[:, 0, :], in0=Z0[:], in1=dmask[:], op=ALU.mult), [Z0, dmask, X0], [X0])
                yield
                if lat:
                    S.emit("dve", lambda e, ub=ub, s3=s3, eg=eg, qTb=qTb, hl=hl: e.tensor_tensor(out=ub["qg"][s3][:], in0=qTb[:, hl, :], in1=eg[:], op=ALU.mult),
                           [qTb, eg], [ub["qg"][s3]])
                S.emit("pool", lambda e, X1=X1, X0=X0: e.tensor_tensor(out=X1[:, 1, :], in0=X0[:, 0, :], in1=identb[:], op=ALU.add), [X0, identb, X1], [X1])
                yield
                S.emit("dve", lambda e, X0=X0: e.tensor_tensor(out=X0[:, 2, :], in0=pT[:, 0:128], in1=dmask[:], op=ALU.mult), [pT, dmask, X0], [X0])
                S.emit("dve", lambda e, OT=OT: e.tensor_tensor(out=OT[:], in0=pT[:, 0:128], in1=omask[:], op=ALU.mult), [pT, omask], [OT])
                S.emit("act", lambda e, ub=ub, s3=s3, c=c, dh=dh: e.activation(out=ub["kd"][s3][:], in_=pT[:, 128:256], func=AF.Copy, scale=egr[:, c, dh:dh + 1]),
                       [pT, egr], [ub["kd"][s3]])
                S.emit("act", lambda e, ub=ub, s3=s3, c=c, dh=dh: e.activation(out=ub["vb"][s3][:], in_=pT[:, 256:384], func=AF.Copy, scale=bet[:, c, dh:dh + 1]),
                       [pT, bet], [ub["vb"][s3]])
                yield
                S.emit("pool", lambda e, X1=X1, X0=X0: e.tensor_tensor(out=X1[:, 3, :], in0=X0[:, 2, :], in1=identb[:], op=ALU.add), [X0, identb, X1], [X1])
                yield

        def gen_G(s):
            s2 = s % 2
            ii = 0
            for (d, hl, c, lat) in units_of(s):
                u = d * HG + hl
                ub = U[u]
                h = hg * HG + hl
                dh = d * H + h
                gm = ub["gm"][s2]
                eg = ub["eg"][s2]
                ng = ngu[ii % 2]
                ii += 1
                def mR(e, c=c, dh=dh, d=d, lat=lat):
                    ghi = gsp[:, c, 0, dh:dh + 1].to_broadcast([128, 128])
                    glo = gsp[:, c, 1, dh:dh + 1].to_broadcast([128, 128])
                    e.matmul(pR[:, 256:384], lhsT=ghi, rhs=LCb[d][:], start=True, stop=False)
                    e.matmul(pR[:, 256:384], lhsT=glo, rhs=LCb[d][:], start=False, stop=True)
                    if lat:
                        e.matmul(pR[:, 0:128], lhsT=ghi, rhs=LCb[d][:], start=True, stop=False)
                        e.matmul(pR[:, 0:128], lhsT=glo, rhs=LCb[d][:], start=False, stop=False)
                        e.matmul(pR[:, 0:128], lhsT=identb[:], rhs=NMI[d][:], start=False, stop=True)
                    e.matmul(pR[:, 128:256], lhsT=ghi, rhs=LCb[d][:], start=True, stop=False)
                    e.matmul(pR[:, 128:256], lhsT=glo, rhs=LCb[d][:], start=False, stop=False)
                    e.matmul(pR[:, 128:256], lhsT=lsp[:, c, 0, dh:dh + 1].to_broadcast([128, 128]), rhs=identb[:], start=False, stop=False)
                    e.matmul(pR[:, 128:256], lhsT=lsp[:, c, 1, dh:dh + 1].to_broadcast([128, 128]), rhs=identb[:], start=False, stop=False)
                    return e.matmul(pR[:, 128:256], lhsT=identb[:], rhs=NMS[d][:], start=False, stop=True)
                S.emit("pe", mR, [gsp, lsp, LCb[d], identb, NMI[d], NMS[d]], [pR])
                yield
                S.emit("dve", lambda e: e.tensor_tensor(out=djunk[:, 0:128], in0=pR[:, 256:384], in1=identf[:], op=ALU.mult), [pR, identf], [djunk])
                yield
                S.emit("dve", lambda e, ng=ng: e.tensor_reduce(out=ng[:, 0:1], in_=djunk[:, 0:128], axis=mybir.AxisListType.X, op=ALU.add, negate=True), [djunk], [ng])
                yield
                lo = 0 if lat else 1
                S.emit("act", lambda e, gm=gm, ng=ng, lo=lo: e.activation(out=gm[:, lo:2, :], in_=pR[:, lo * 128:256].rearrange("p (a b) -> p a b", b=128),
                                                                          func=AF.Exp, bias=ng[:, 0:1]), [pR, ng], [gm])
                if lat:
                    S.emit("act", lambda e, eg=eg: e.activation(out=eg[:], in_=pR[:, 256:384], func=AF.Exp), [pR], [eg])
                yield

        def gen_I(s, bank):
            s3, s2 = s % 3, s % 2
            p = pI[bank]
            for (d, hl, c, lat) in units_of(s):
                u = d * HG + hl
                if u % 4 != bank:
                    continue
                ub = U[u]
                X = ub["X"][s2]
                DD, NN, N2, PP, OT = ub["DD"], ub["NN"], ub["N2"], ub["PP"], ub["OT"][s2]
                for st in range(9):
                    if st == 0:
                        Xc, Xn = X[0], X[1]
                        def m0(e, Xc=Xc):
                            e.matmul(p[:, 0:128], lhsT=Xc[:, 2, :], rhs=Xc[:, 0, :], start=True, stop=True)
                            return e.matmul(p[:, 256:384], lhsT=Xc[:, 0, :], rhs=Xc[:, 2, :], start=True, stop=True)
                        S.emit("pe", m0, [Xc], [p])
                        yield
                        S.emit("act", lambda e, Xn=Xn: e.copy(out=Xn[:, 0:4:2, :], in_=v4(p)[:, 0:4:2, :]), [p, Xn], [Xn])
                    elif st < 4:
                        Xc, Xn = X[st % 2], X[(st + 1) % 2]
                        def m1(e, Xc=Xc):
                            e.matmul(p[:, 0:256], lhsT=Xc[:, 2, :], rhs=Xc[:, 0:2, :], start=True, stop=True)
                            return e.matmul(p[:, 256:512], lhsT=Xc[:, 0, :], rhs=Xc[:, 2:4, :], start=True, stop=True)
                        S.emit("pe", m1, [Xc], [p])
                        yield
                        S.emit("act", lambda e, Xn=Xn: e.copy(out=Xn[:, 0:4:2, :], in_=v4(p)[:, 0:4:2, :]), [p, Xn], [Xn])
                        yield
                        S.emit("dve", lambda e, Xn=Xn, Xc=Xc: e.tensor_tensor(out=Xn[:, 1:4:2, :], in0=v4(p)[:, 1:4:2, :], in1=Xc[:, 1:4:2, :], op=ALU.add),
                               [p, Xc, Xn], [Xn])
                    elif st == 4:
                        Xc = X[0]
                        def m4(e, Xc=Xc):
                            e.matmul(p[:, 128:256], lhsT=Xc[:, 2, :], rhs=Xc[:, 1, :], start=True, stop=True)
                            return e.matmul(p[:, 384:512], lhsT=Xc[:, 0, :], rhs=Xc[:, 3, :], start=True, stop=True)
                        S.emit("pe", m4, [Xc], [p])
                        yield
                        S.emit("dve", lambda e, DD=DD, Xc=Xc: e.tensor_tensor(out=DD[:], in0=v4(p)[:, 1:4:2, :], in1=Xc[:, 1:4:2, :], op=ALU.add), [p, Xc], [DD])
                    elif st == 5:
                        def m5(e, DD=DD, OT=OT):
                            e.matmul(p[:, 0:128], lhsT=OT[:], rhs=DD[:, 0, :], start=True, stop=True)
                            return e.matmul(p[:, 128:256], lhsT=DD[:, 0, :], rhs=OT[:], start=True, stop=True)
                        S.emit("pe", m5, [DD, OT], [p])
                        yield
                        S.emit("act", lambda e, NN=NN: e.copy(out=NN[:], in_=v4(p)[:, 0:2, :]), [p], [NN])
                        yield
                        S.emit("dve", lambda e, PP=PP: e.tensor_tensor(out=PP[:, 0, :], in0=p[:, 0:128], in1=identb[:], op=ALU.add), [p, identb, PP], [PP])
                    elif st == 6:
                        def m6(e, NN=NN):
                            e.matmul(p[:, 0:128], lhsT=NN[:, 1, :], rhs=NN[:, 0, :], start=True, stop=True)
                            return e.matmul(p[:, 128:256], lhsT=NN[:, 0, :], rhs=NN[:, 1, :], start=True, stop=True)
                        S.emit("pe", m6, [NN], [p])
                        yield
                        S.emit("act", lambda e, N2=N2: e.copy(out=N2[:], in_=v4(p)[:, 0:2, :]), [p], [N2])
                    elif st == 7:
                        S.emit("pe", lambda e, N2=N2, PP=PP: e.matmul(p[:, 0:128], lhsT=N2[:, 1, :], rhs=PP[:, 0, :], start=True, stop=True), [N2, PP], [p])
                        yield
                        S.emit("dve", lambda e, PP=PP: e.tensor_tensor(out=PP[:, 1, :], in0=p[:, 0:128], in1=PP[:, 0, :], op=ALU.add), [p, PP], [PP])
                    else:
                        S.emit("pe", lambda e, DD=DD, PP=PP: e.matmul(p[:, 0:128], lhsT=DD[:, 1, :], rhs=PP[:, 1, :], start=True, stop=True), [DD, PP], [p])
                        yield
                        S.emit("act", lambda e, ub=ub, s3=s3: e.copy(out=ub["TT"][s2][:], in_=p[:, 0:128]), [p], [ub["TT"][s2]])
                    yield

        def gen_S(s):
            s3 = s % 3
            for (d, hl, c, lat) in units_of(s):
                u = d * HG + hl
                ub = U[u]
                h = hg * HG + hl
                dh = d * H + h
                kT = ld["k"][d][s3]
                S.emit("pe", lambda e, kT=kT, hl=hl, ub=ub: e.matmul(pS[:, 0:128], lhsT=kT[:, hl, :], rhs=ub["Sb"][:], start=True, stop=True), [kT, ub["Sb"]], [pS])
                yield
                S.emit("dve", lambda e, ub=ub, s3=s3, c=c, dh=dh: e.scalar_tensor_tensor(out=ub["r"][:], in0=pS[:, 0:128], scalar=nbe[:, c, dh:dh + 1], in1=ub["vb"][s3][:],
                                                                                        op0=ALU.mult, op1=ALU.add), [pS, nbe, ub["vb"][s3]], [ub["r"]])
                yield
                S.emit("pe", lambda e, ub=ub, s=s: e.matmul(pS[:, 128:256], lhsT=ub["TT"][s % 2][:], rhs=ub["r"][:], start=True, stop=True), [ub["TT"][s % 2], ub["r"]], [pS])
                yield
                S.emit("act", lambda e, ub=ub: e.copy(out=ub["vn"][:], in_=pS[:, 128:256]), [pS], [ub["vn"]])
                yield
                def mo(e, ub=ub, s3=s3, lat=lat):
                    if lat:
                        e.matmul(pS[:, 384:512], lhsT=ub["Sb"][:], rhs=ub["qg"][s3][:], start=True, stop=False)
                        e.matmul(pS[:, 384:512], lhsT=ub["vn"][:], rhs=ub["PT"][s3][:], start=False, stop=True)
                    return e.matmul(pS[:, 256:384], lhsT=ub["kd"][s3][:], rhs=ub["vn"][:], start=True, stop=True)
                S.emit("pe", mo, [ub["Sb"], ub["qg"][s3], ub["vn"], ub["PT"][s3], ub["kd"][s3]], [pS])
                yield
                if lat:
                    cl = c - NCC
                    if R >= 128:
                        per = R // 128
                        cc0, sub = divmod(cl, per)
                        oap = OB[:, hl, :].rearrange("p (r c) -> p c r", c=GW)[:, cc0, sub * 128:(sub + 1) * 128]
                        iap = pS[:, 384:512]
                    else:
                        oap = OB[:, hl, :].rearrange("p (r c) -> p c r", c=GW)[:, cl * ncol:(cl + 1) * ncol, :]
                        iap = pS[:, 384:512].rearrange("p (a b) -> p a b", b=R)
                    key = (hl, cl)
                    if key not in OBc:
                        OBc[key] = Buf(f"OBc{hl}_{cl}")
                        OBc[key].ws = dict(OB.ws)
                        OBc[key].rs = dict(OB.rs)
                        S.emit("act", lambda e, oap=oap, iap=iap: e.copy(out=oap, in_=iap), [pS], [OBc[key]])
                    else:
                        S.emit("dve", lambda e, oap=oap, iap=iap: e.tensor_tensor(out=oap, in0=iap, in1=oap, op=ALU.add), [pS, OBc[key]], [OBc[key]])
                S.emit("dve", lambda e, ub=ub, c=c, dh=dh: e.scalar_tensor_tensor(out=ub["S"][:], in0=ub["S"][:], scalar=egt[:, c, dh:dh + 1], in1=pS[:, 256:384],
                                                                                 op0=ALU.mult, op1=ALU.add), [ub["S"], egt, pS], [ub["S"]])
                yield
                S.emit("act", lambda e, ub=ub: e.copy(out=ub["Sb"][:], in_=ub["S"][:]), [ub["S"]], [ub["Sb"]])
                yield

        for it in range(-3, NCHK):
            sts = []
            if 0 <= it + 3 < NCHK:
                sts.append(gen_G(it + 3))
            if 0 <= it + 2 < NCHK:
                sts.append(gen_P(it + 2))
            if 0 <= it + 1 < NCHK:
                sts += [gen_I(it + 1, b) for b in range(4)]
            if it >= 0:
                sts.append(gen_S(it))
            run_streams(sts)
            if "dbg_S" in G and it == NCC - 1 and hg == 0:
                for u in range(NU):
                    S.dma("sp", G["dbg_S"][u], U[u]["S"][:], [U[u]["S"]], [G["dbg_S"]])
        for key, bk in OBc.items():
            for k_, v_ in bk.ws.items():
                if OB.ws.get(k_, 0) < v_:
                    OB.ws[k_] = v_
        for hl in range(HG):
            h = hg * HG + hl
            if "dbg_ob" in G:
                S.dma("sp", G["dbg_ob"][h * 128:(h + 1) * 128, :], OB[:, hl, :], [OB], [G["dbg_ob"]])
            for bi, t0 in enumerate(range(0, T, 512)):
                nb = min(512, T - t0)
                S.dma("sp", zq[:, 0:nb], G["gzT"][h * 128:(h + 1) * 128, t0:t0 + nb], [G["gzT"]], [zq])
                S.emit("act", lambda e, hl=hl, t0=t0, nb=nb: e.activation(out=sqb[:, 0:nb], in_=OB[:, hl, t0:t0 + nb], func=AF.Square), [OB], [sqb])
                S.emit("pe", lambda e, nb=nb: e.matmul(pA[:, 0:nb], lhsT=onesb[:], rhs=sqb[:, 0:nb], start=True, stop=True), [onesb, sqb], [pA])
                S.emit("act", lambda e, nb=nb: e.activation(out=rt[:, 0:nb], in_=pA[:, 0:nb], func=AF.Ln, scale=1.0 / 128, bias=C["epsc"][:, 0:1]), [pA, C["epsc"]], [rt])
                S.emit("act", lambda e, nb=nb: e.activation(out=rt[:, 0:nb], in_=rt[:, 0:nb], func=AF.Exp, scale=-0.5), [rt], [rt])
                S.emit("dve", lambda e, nb=nb: e.tensor_tensor(out=rt[:, 0:nb], in0=rt[:, 0:nb], in1=zq[:, 0:nb], op=ALU.mult), [rt, zq], [rt])
                y = ys[bi % 2]
                S.emit("dve", lambda e, hl=hl, h=h, t0=t0, nb=nb, y=y: e.scalar_tensor_tensor(out=y[:, 0:nb], in0=OB[:, hl, t0:t0 + nb], scalar=gain[:, h:h + 1], in1=rt[:, 0:nb],
                                                                                             op0=ALU.mult, op1=ALU.mult), [OB, gain, rt], [y])
                S.dma("sp", G["yBT"][h * 128:(h + 1) * 128, t0:t0 + nb], y[:, 0:nb], [y], [G["yBT"]])
    A.close()


def phase_merge(S, cfg, G, I, C, cfgL):
    A = Arena(S, "mg")
    D, T, HV, KD = cfg.D, cfg.T, cfg.HV, cfg.KD
    KH = HV // 128
    HL = cfgL.H
    NRK = cfg.H // HL
    wa = A.sb("wa", [128, KH, D], BF16)
    wb = A.sb("wb", [128, KH, D], BF16)
    for k in range(KH):
        S.dma("pool", wa[:, k, :], I["w_branch_a"][0, k * 128:(k + 1) * 128, :], [I["w_branch_a"]], [wa])
        S.dma("pool", wb[:, k, :], I["w_branch_b"][0, k * 128:(k + 1) * 128, :], [I["w_branch_b"]], [wb])
    ya = [A.sb(f"ya{i}", [128, KH, 512], BF16) for i in range(2)]
    yb = [A.sb(f"yb{i}", [128, KH, 512], BF16) for i in range(2)]
    ga = [A.sb(f"ga{i}", [128, 512], BF16) for i in range(2)]
    gb = [A.sb(f"gb{i}", [128, 512], BF16) for i in range(2)]
    t1 = [A.sb(f"t1{i}", [128, 512], F32) for i in range(2)]
    t2 = [A.sb(f"t2{i}", [128, 512], F32) for i in range(2)]
    mo = [A.sb(f"mo{i}", [128, 512], BF16) for i in range(2)]
    pa = [A.ps(f"pa{i}", [128, 512], F32) for i in range(2)]
    pb = [A.ps(f"pb{i}", [128, 512], F32) for i in range(2)]
    it = 0
    for bi, t0 in enumerate(range(0, T, 512)):
        nb = min(512, T - t0)
        a_, b_ = ya[bi % 2], yb[bi % 2]
        CR = min(256, 2 * HL * 128)
        for r in range(NRK):
            for l in range(HL):
                for (dst, rho) in ((a_, l * 128), (b_, HL * 128 + l * 128)):
                    ck, off = divmod(rho, CR)
                    S.dma("sp", dst[:, r * HL + l, 0:nb], G["yall"][ck, r * CR + off:r * CR + off + 128, t0:t0 + nb], [G["yall"]], [dst])
        for dc in range(KD):
            i2 = it % 2
            it += 1
            S.dma("sp", ga[i2][:, 0:nb], G["mgT"][dc * 128:(dc + 1) * 128, t0:t0 + nb], [G["mgT"]], [ga[i2]])
            S.dma("sp", gb[i2][:, 0:nb], G["mgT"][D + dc * 128:D + (dc + 1) * 128, t0:t0 + nb], [G["mgT"]], [gb[i2]])
            def mm(e, w, y, p, dc=dc, nb=nb):
                r = None
                for k in range(KH):
                    r = e.matmul(p[:, 0:nb], lhsT=w[:, k, dc * 128:(dc + 1) * 128], rhs=y[:, k, 0:nb], start=(k == 0), stop=(k == KH - 1))
                return r
            S.emit("pe", lambda e, i2=i2, a_=a_, mm=mm: mm(e, wa, a_, pa[i2]), [wa, a_], [pa[i2]])
            S.emit("pe", lambda e, i2=i2, b_=b_, mm=mm: mm(e, wb, b_, pb[i2]), [wb, b_], [pb[i2]])
            S.emit("dve", lambda e, i2=i2, nb=nb: e.tensor_tensor(out=t1[i2][:, 0:nb], in0=pa[i2][:, 0:nb], in1=ga[i2][:, 0:nb], op=ALU.mult), [pa[i2], ga[i2]], [t1[i2]])
            S.emit("dve", lambda e, i2=i2, nb=nb: e.tensor_tensor(out=t2[i2][:, 0:nb], in0=pb[i2][:, 0:nb], in1=gb[i2][:, 0:nb], op=ALU.mult), [pb[i2], gb[i2]], [t2[i2]])
            S.emit("pool", lambda e, i2=i2, nb=nb: e.tensor_tensor(out=mo[i2][:, 0:nb], in0=t1[i2][:, 0:nb], in1=t2[i2][:, 0:nb], op=ALU.add), [t1[i2], t2[i2]], [mo[i2]])
            S.dma("sp", G["mT"][dc * 128:(dc + 1) * 128, t0:t0 + nb], mo[i2][:, 0:nb], [mo[i2]], [G["mT"]])
    A.close()


def load_row_bcast(S, A, name, src_ap, srcbuf, n):
    t = A.sb(name, [128, n], F32)
    S.dma("sp", t[:], src_ap.partition_broadcast(128), [srcbuf], [t])
    return t


def phase_outproj(S, cfg, G, I, C):
    A = Arena(S, "op")
    D, T, KD = cfg.D, cfg.T, cfg.KD
    wo = A.sb("wo", [128, KD, D], BF16)
    for k in range(KD):
        S.dma("pool", wo[:, k, :], I["w_out"][0, k * 128:(k + 1) * 128, :], [I["w_out"]], [wo])
    g1 = load_row_bcast(S, A, "g1", G["grow"][0], G["grow"], D)
    mt = [A.sb(f"mt{i}", [128, KD, 128], BF16) for i in range(2)]
    xt = [A.sb(f"xt{i}", [128, D], F32) for i in range(2)]
    tm = [A.sb(f"tm{i}", [128, 512], F32) for i in range(2)]
    pp = [A.ps(f"pp{i}", [128, 512], F32) for i in range(4)]
    pi = 0
    for ti in range(T // 128):
        t0 = ti * 128
        m_, x_ = mt[ti % 2], xt[ti % 2]
        S.dma("sp", m_[:], G["mT"][:, t0:t0 + 128].rearrange("(k p) t -> p k t", p=128), [G["mT"]], [m_])
        S.dma("pool", x_[:], I["x"][0, t0:t0 + 128, :], [I["x"]], [x_])
        for oc in range(0, D, 512):
            ow = min(512, D - oc)
            p = pp[pi % 4]
            t_ = tm[pi % 2]
            pi += 1
            def mm(e, m_=m_, p=p, oc=oc, ow=ow):
                r = None
                for k in range(KD):
                    r = e.matmul(p[:, 0:ow], lhsT=m_[:, k, :], rhs=wo[:, k, oc:oc + ow], start=(k == 0), stop=(k == KD - 1))
                return r
            S.emit("pe", mm, [m_, wo], [p])
            S.emit("dve", lambda e, p=p, t_=t_, oc=oc, ow=ow: e.tensor_tensor(out=t_[:, 0:ow], in0=p[:, 0:ow], in1=g1[:, oc:oc + ow], op=ALU.mult), [p, g1], [t_])
            S.emit("pool", lambda e, x_=x_, t_=t_, oc=oc, ow=ow: e.tensor_tensor(out=x_[:, oc:oc + ow], in0=x_[:, oc:oc + ow], in1=t_[:, 0:ow], op=ALU.add), [x_, t_], [x_])
        S.dma("sp", G["x2"][t0:t0 + 128, :], x_[:], [x_], [G["x2"]])
    A.close()


def phase_route(S, cfg, G, I, C, R_):
    A = Arena(S, "rt")
    D, T, KD, E, CAP = cfg.D, cfg.T, cfg.KD, cfg.E, cfg.CAP
    NSG = CAP // 128
    identf = C["identf"]
    co = G["coef"]
    for j, ci in enumerate((4, 5)):
        S.dma("sp", G["crow"][j].rearrange("(k p) -> p k", p=128), co[:, ci, :], [co], [G["crow"]], allow_slow_non_contiguous=True)
    a2 = load_row_bcast(S, A, "a2", G["crow"][0], G["crow"], D)
    b2 = load_row_bcast(S, A, "b2", G["crow"][1], G["crow"], D)
    rw = A.sb("rw", [128, KD, E], F32)
    S.dma("sp", rw[:], I["router_w"][0].rearrange("(k p) e -> p k e", p=128), [I["router_w"]], [rw])
    affE = [A.sb(f"affE{i}", [E, T], F32) for i in range(2)]
    xt = [A.sb(f"xt{i}", [128, D], F32) for i in range(2)]
    hf = [A.sb(f"hf{i}", [128, D], F32) for i in range(2)]
    hb = [A.sb(f"hb{i}", [128, D], BF16) for i in range(2)]
    hT = [A.sb(f"hT{i}", [128, KD, 128], F32) for i in range(2)]
    junk = A.sb("junk", [128, D], BF16)
    st = [A.sb(f"st{i}", [128, 8], F32) for i in range(2)]
    sm = [A.sb(f"sm{i}", [128, E + 8], F32) for i in range(2)]
    pt = [A.ps(f"pt{i}", [128, 4, 128], F32) for i in range(2)]
    pl = A.ps("pl", [128, 512], F32)
    pq = A.ps("pq", [128, 512], F32)
    gi = 0
    for ti in range(T // 128):
        t0 = ti * 128
        x = xt[ti % 2]; s = st[ti % 2]; h = hf[ti % 2]; hbb = hb[ti % 2]; hTt = hT[ti % 2]; m = sm[ti % 2]
        S.dma("sp" if ti % 2 == 0 else "pool", x[:], G["x2"][t0:t0 + 128, :], [G["x2"]], [x])
        S.emit("act", lambda e, x=x, s=s: e.activation(out=junk[:], in_=x[:], func=AF.Square, accum_out=s[:, 0:1]), [x], [junk, s])
        S.emit("dve", lambda e, s=s: e.tensor_scalar(out=s[:, 1:2], in0=s[:, 0:1], scalar1=1.0 / D, scalar2=cfg.eps, op0=ALU.mult, op1=ALU.add), [s], [s])
        S.emit("act", lambda e, s=s: e.activation(out=s[:, 2:3], in_=s[:, 1:2], func=AF.Sqrt), [s], [s])
        S.emit("dve", lambda e, s=s: e.reciprocal(out=s[:, 3:4], in_=s[:, 2:3]), [s], [s])
        S.emit("dve", lambda e, x=x, s=s, h=h: e.scalar_tensor_tensor(out=h[:], in0=x[:], scalar=s[:, 3:4], in1=a2[:], op0=ALU.mult, op1=ALU.mult), [x, s, a2], [h])
        S.emit("pool", lambda e, h=h: e.tensor_tensor(out=h[:], in0=h[:], in1=b2[:], op=ALU.add), [h, b2], [h])
        S.emit("act", lambda e, h=h, hbb=hbb: e.copy(out=hbb[:], in_=h[:]), [h], [hbb])
        S.dma("sp", G["h2"][t0:t0 + 128, :], hbb[:], [hbb], [G["h2"]])
        for g in range(0, KD, 4):
            p = pt[gi % 2]
            gi += 1
            ng = min(4, KD - g)
            def tr(e, h=h, p=p, g=g, ng=ng):
                r = None
                for j in range(ng):
                    r = e.transpose(out=p[:, j, :], in_=h[:, (g + j) * 128:(g + j + 1) * 128], identity=identf[:])
                return r
            S.emit("pe", tr, [h, identf], [p])
            if (gi % 2) == 0:
                S.emit("act", lambda e, p=p, hTt=hTt, g=g, ng=ng: e.copy(out=hTt[:, g:g + ng, :], in_=p[:, 0:ng, :]), [p, hTt], [hTt])
            else:
                S.emit("dve", lambda e, p=p, hTt=hTt, g=g, ng=ng: e.tensor_copy(out=hTt[:, g:g + ng, :], in_=p[:, 0:ng, :]), [p, hTt], [hTt])
        def ml(e, hTt=hTt):
            r = None
            for k in range(KD):
                r = e.matmul(pl[:, 0:E], lhsT=hTt[:, k, :], rhs=rw[:, k, :], start=(k == 0), stop=(k == KD - 1))
            return r
        S.emit("pe", ml, [hTt, rw], [pl])
        S.emit("dve", lambda e, m=m: e.reduce_max(out=m[:, E:E + 1], in_=pl[:, 0:E], axis=mybir.AxisListType.X), [pl], [m])
        S.emit("dve", lambda e, m=m: e.tensor_scalar(out=m[:, E + 1:E + 2], in0=m[:, E:E + 1], scalar1=-1.0, scalar2=None, op0=ALU.mult), [m], [m])
        S.emit("act", lambda e, m=m: e.activation(out=m[:, 0:E], in_=pl[:, 0:E], func=AF.Exp, bias=m[:, E + 1:E + 2], accum_out=m[:, E + 2:E + 3]), [pl, m], [m])
        S.emit("dve", lambda e, m=m: e.reciprocal(out=m[:, E + 3:E + 4], in_=m[:, E + 2:E + 3]), [m], [m])
        S.emit("dve", lambda e, m=m: e.tensor_scalar(out=m[:, 0:E], in0=m[:, 0:E], scalar1=m[:, E + 3:E + 4], scalar2=None, op0=ALU.mult), [m], [m])
        S.emit("pe", lambda e, m=m: e.transpose(out=pq[0:E, 0:128], in_=m[:, 0:E], identity=identf[:]), [m, identf], [pq])
        S.emit("act", lambda e, t0=t0: e.copy(out=affE[0][:, t0:t0 + 128], in_=pq[0:E, 0:128]), [pq, affE[0]], [affE[0]])
    vals = A.sb("vals", [E, CAP], F32)
    idxu = A.sb("idxu", [E, CAP], U32)
    idxf = A.sb("idxf", [E, CAP], F32)
    cur = 0
    for it in range(CAP // 8):
        a = affE[cur]; b = affE[1 - cur]
        S.emit("dve", lambda e, a=a, it=it: e.max(out=vals[:, it * 8:(it + 1) * 8], in_=a[:]), [a, vals], [vals])
        S.emit("dve", lambda e, a=a, it=it: e.max_index(out=idxu[:, it * 8:(it + 1) * 8], in_max=vals[:, it * 8:(it + 1) * 8], in_values=a[:]), [a, vals, idxu], [idxu])
        if it + 1 < CAP // 8:
            S.emit("dve", lambda e, a=a, b=b, it=it: e.match_replace(out=b[:], in_to_replace=vals[:, it * 8:(it + 1) * 8], in_values=a[:], imm_value=-1.0), [a, vals], [b])
            cur = 1 - cur
    S.emit("dve", lambda e: e.tensor_copy(out=idxf[:], in_=idxu[:]), [idxu], [idxf])
    idxT, gateT = R_["idxT"], R_["gateT"]
    for sg in range(NSG):
        S.emit("pe", lambda e, sg=sg: e.transpose(out=pq[:, 0:E], in_=idxf[:, sg * 128:(sg + 1) * 128], identity=identf[0:E, 0:E]), [idxf, identf], [pq])
        S.emit("dve", lambda e, sg=sg: e.tensor_copy(out=idxT[:, sg, :], in_=pq[:, 0:E]), [pq, idxT], [idxT])
        S.emit("pe", lambda e, sg=sg: e.transpose(out=pq[:, 0:E], in_=vals[:, sg * 128:(sg + 1) * 128], identity=identf[0:E, 0:E]), [vals, identf], [pq])
        S.emit("act", lambda e, sg=sg: e.copy(out=gateT[:, sg, :], in_=pq[:, 0:E]), [pq, gateT], [gateT])
    A.close()


def phase_experts(S, cfg, G, I, C, R_, EL):
    A = Arena(S, "ex")
    D, T, KD, E, CAP, DE = cfg.D, cfg.T, cfg.KD, EL, cfg.CAP, cfg.DE
    NSG = CAP // 128
    FH = min(getattr(cfg, "FH", 512), DE)
    NFH = DE // FH
    FC = FH // 128
    identb = C["identb"]
    idxT, gateT = R_["idxT"], R_["gateT"]
    g2 = load_row_bcast(S, A, "g2", G["grow"][1], G["grow"], D)
    wg = [A.sb(f"wg{i}", [128, KD, FH], BF16) for i in range(2)]
    wu = [A.sb(f"wu{i}", [128, KD, FH], BF16) for i in range(2)]
    wd = [A.sb(f"wd{i}", [128, FC, D], BF16) for i in range(2)]
    xs = A.sb("xs", [128, NSG, D], BF16)
    xsT = A.sb("xsT", [128, KD, CAP], BF16)
    sa = A.sb("sa", [128, 512], F32)
    sa2 = A.sb("sa2", [128, 512], F32)
    hd = A.sb("hd", [128, FC, CAP], BF16)
    ysb = A.sb("ysb", [128, NSG, D], F32)
    ptr = [A.ps(f"ptr{i}", [128, 8, 128], BF16) for i in range(2)]
    pg = [A.ps(f"pg{i}", [128, 512], F32) for i in range(2)]
    pu = [A.ps(f"pu{i}", [128, 512], F32) for i in range(2)]
    py = [A.ps(f"py{i}", [128, 512], F32) for i in range(2)]
    gi = 0
    yi = 0
    S.emit("pool", lambda e: e.memset(ysb[:], 0.0), writes=[ysb])
    for t0 in range(0, T, 128 * NSG):
        S.dma("sp", G["macc"][t0:t0 + 128 * NSG, :].rearrange("(n p) d -> p n d", p=128), ysb[:], [ysb], [G["macc"]])
    G["x2s"].ws = dict(G["macc"].ws)
    halves = [(ex, fh) for ex in range(E) for fh in range(NFH)]

    def load_weights(k):
        ex, fh = halves[k]
        w_g, w_u, w_d = wg[k % 2], wu[k % 2], wd[k % 2]
        f0 = fh * FH
        S.dma("pool", w_g[:], I["w_gate"][0, ex, :, f0:f0 + FH].rearrange("(k p) f -> p k f", p=128), [I["w_gate"]], [w_g])
        S.dma("pool", w_u[:], I["w_up"][0, ex, :, f0:f0 + FH].rearrange("(k p) f -> p k f", p=128), [I["w_up"]], [w_u])
        S.dma("pool", w_d[:], I["w_down"][0, ex, f0:f0 + FH, :].rearrange("(k p) d -> p k d", p=128), [I["w_down"]], [w_d])

    def gather(ex):
        for sg in range(NSG):
            S.emit("pool", lambda e, sg=sg, ex=ex: e.indirect_dma_start(out=xs[:, sg, :], out_offset=None, in_=G["h2"][:, :],
                                                                        in_offset=bass.IndirectOffsetOnAxis(ap=idxT[:, sg, ex:ex + 1], axis=0)),
                   [idxT, G["h2"]], [xs], dma=xs)

    load_weights(0)
    gather(0)
    for k, (ex, fh) in enumerate(halves):
        w_g, w_u, w_d = wg[k % 2], wu[k % 2], wd[k % 2]
        if k + 1 < len(halves):
            load_weights(k + 1)
        if fh == 0:
            ti = 0
            for sg in range(NSG):
                for g in range(0, KD, 8):
                    p = ptr[ti % 2]
                    ng = min(8, KD - g)
                    def tr(e, p=p, sg=sg, g=g, ng=ng):
                        r = None
                        for j in range(ng):
                            r = e.transpose(out=p[:, j, :], in_=xs[:, sg, (g + j) * 128:(g + j + 1) * 128], identity=identb[:])
                        return r
                    S.emit("pe", tr, [xs, identb], [p])
                    if ti % 2 == 0:
                        S.emit("act", lambda e, p=p, sg=sg, g=g, ng=ng: e.copy(out=xsT[:, g:g + ng, sg * 128:(sg + 1) * 128], in_=p[:, 0:ng, :]), [p, xsT], [xsT])
                    else:
                        S.emit("dve", lambda e, p=p, sg=sg, g=g, ng=ng: e.tensor_copy(out=xsT[:, g:g + ng, sg * 128:(sg + 1) * 128], in_=p[:, 0:ng, :]), [p, xsT], [xsT])
                    ti += 1
            if ex + 1 < E:
                gather(ex + 1)
        for fc in range(FC):
            p_g, p_u = pg[gi % 2], pu[gi % 2]
            gi += 1
            for (w_, p_) in ((w_g, p_g), (w_u, p_u)):
                def mm(e, w_=w_, p_=p_, fc=fc):
                    r = None
                    for kk in range(KD):
                        r = e.matmul(p_[:, 0:CAP], lhsT=w_[:, kk, fc * 128:(fc + 1) * 128], rhs=xsT[:, kk, :], start=(kk == 0), stop=(kk == KD - 1))
                    return r
                S.emit("pe", mm, [w_, xsT], [p_])
            S.emit("act", lambda e, p_g=p_g: e.activation(out=sa[:, 0:CAP], in_=p_g[:, 0:CAP], func=AF.Silu), [p_g], [sa])
            S.emit("dve", lambda e, p_u=p_u, fc=fc: e.tensor_tensor(out=hd[:, fc, :], in0=p_u[:, 0:CAP], in1=sa[:, 0:CAP], op=ALU.mult), [p_u, sa, hd], [hd])
        for sg in range(NSG):
            for oc in range(0, D, 512):
                ow = min(512, D - oc)
                p_y = py[yi % 2]
                yi += 1
                def my(e, p_y=p_y, sg=sg, oc=oc, ow=ow, w_d=w_d):
                    r = None
                    for fc in range(FC):
                        r = e.matmul(p_y[:, 0:ow], lhsT=hd[:, fc, sg * 128:(sg + 1) * 128], rhs=w_d[:, fc, oc:oc + ow], start=(fc == 0), stop=(fc == FC - 1))
                    return r
                S.emit("pe", my, [hd, w_d], [p_y])
                if fh == 0:
                    S.emit("dve", lambda e, p_y=p_y, sg=sg, oc=oc, ow=ow, ex=ex: e.scalar_tensor_tensor(
                        out=ysb[:, sg, oc:oc + ow], in0=p_y[:, 0:ow], scalar=gateT[:, sg, ex:ex + 1], in1=g2[:, oc:oc + ow], op0=ALU.mult, op1=ALU.mult),
                        [p_y, gateT, g2, ysb], [ysb])
                else:
                    S.emit("act", lambda e, p_y=p_y, sg=sg, ow=ow, ex=ex: e.activation(out=sa2[:, 0:ow], in_=p_y[:, 0:ow], func=AF.Copy, scale=gateT[:, sg, ex:ex + 1]),
                           [p_y, gateT], [sa2])
                    S.emit("dve", lambda e, oc=oc, ow=ow: e.tensor_tensor(out=sa2[:, 0:ow], in0=sa2[:, 0:ow], in1=g2[:, oc:oc + ow], op=ALU.mult), [sa2, g2], [sa2])
                    S.emit("dve", lambda e, sg=sg, oc=oc, ow=ow: e.tensor_tensor(out=ysb[:, sg, oc:oc + ow], in0=ysb[:, sg, oc:oc + ow], in1=sa2[:, 0:ow], op=ALU.add),
                           [ysb, sa2], [ysb])
        if fh == NFH - 1:
            for sg in range(NSG):
                S.emit("pool", lambda e, sg=sg, ex=ex: e.indirect_dma_start(out=G["macc"][:, :], out_offset=bass.IndirectOffsetOnAxis(ap=idxT[:, sg, ex:ex + 1], axis=0),
                                                                            in_=ysb[:, sg, :], in_offset=None, compute_op=ALU.add),
                       [idxT, ysb, G["x2s"]], [G["x2s"]], dma=ysb)
    A.close()


def phase_moe_exchange(S, cfg, G, nsplit, grp):
    A = Arena(S, "mx")
    D, T = cfg.D, cfg.T
    xt = [A.sb(f"xt{i}", [128, D], F32) for i in range(2)]
    xb = [A.sb(f"xb{i}", [128, D], BF16) for i in range(2)]
    for ti in range(T // 128):
        t0 = ti * 128
        x, b = xt[ti % 2], xb[ti % 2]
        S.dma("sp", x[:], G["macc"][t0:t0 + 128, :], [G["macc"], G["x2s"]], [x])
        if ti % 2 == 0:
            S.emit("dve", lambda e, x=x, b=b: e.tensor_copy(out=b[:], in_=x[:]), [x], [b])
        else:
            S.emit("act", lambda e, x=x, b=b: e.copy(out=b[:], in_=x[:]), [x], [b])
        S.dma("sp", G["maccb"][t0:t0 + 128, :], b[:], [b], [G["maccb"]])
    MR = min(512, T)
    for ck in range(T // MR):
        S.emit_cc(lambda e, ck=ck: e.collective_compute("AllGather", ALU.bypass, replica_groups=grp, ins=[G["maccb"][ck * MR:(ck + 1) * MR, :]],
                                                        outs=[G["mall"][ck]]), [G["maccb"]], [G["mall"]])
    A.close()


def phase_final(S, cfg, G, I, C, out, nsplit=1):
    A = Arena(S, "fn")
    D, T = cfg.D, cfg.T
    MR = min(512, T)
    mb_ = [[A.sb(f"mb{r}{i}", [128, D], BF16) for i in range(2)] for r in range(nsplit)]
    fn = load_row_bcast(S, A, "fnw", I["final_norm"][:], I["final_norm"], D)
    xt = [A.sb(f"xt{i}", [128, D], F32) for i in range(2)]
    st = [A.sb(f"st{i}", [128, 8], F32) for i in range(2)]
    junk = A.sb("junk", [128, D], BF16)
    for ti in range(T // 128):
        t0 = ti * 128
        x = xt[ti % 2]; s = st[ti % 2]
        S.dma("sp" if ti % 2 == 0 else "pool", x[:], G["x2"][t0:t0 + 128, :], [G["x2"]], [x])
        ck, off = divmod(t0, MR)
        for r in range(nsplit):
            m_ = mb_[r][ti % 2]
            S.dma("sp", m_[:], G["mall"][ck, r * MR + off:r * MR + off + 128, :], [G["mall"]], [m_])
            if r % 2 == 0:
                S.emit("dve", lambda e, x=x, m_=m_: e.tensor_tensor(out=x[:], in0=x[:], in1=m_[:], op=ALU.add), [x, m_], [x])
            else:
                S.emit("pool", lambda e, x=x, m_=m_: e.tensor_tensor(out=x[:], in0=x[:], in1=m_[:], op=ALU.add), [x, m_], [x])
        S.emit("act", lambda e, x=x, s=s: e.activation(out=junk[:], in_=x[:], func=AF.Square, accum_out=s[:, 0:1]), [x], [junk, s])
        S.emit("dve", lambda e, s=s: e.tensor_scalar(out=s[:, 1:2], in0=s[:, 0:1], scalar1=1.0 / D, scalar2=cfg.eps, op0=ALU.mult, op1=ALU.add), [s], [s])
        S.emit("act", lambda e, s=s: e.activation(out=s[:, 2:3], in_=s[:, 1:2], func=AF.Sqrt), [s], [s])
        S.emit("dve", lambda e, s=s: e.reciprocal(out=s[:, 3:4], in_=s[:, 2:3]), [s], [s])
        S.emit("dve", lambda e, x=x, s=s: e.scalar_tensor_tensor(out=x[:], in0=x[:], scalar=s[:, 3:4], in1=fn[:], op0=ALU.mult, op1=ALU.mult), [x, s, fn], [x])
        S.dma("sp", out[t0:t0 + 128, :], x[:], [x], [out])
    A.close()


def declare_io(S, cfg, debug=(), cfgL=None):
    cfgL = cfgL or cfg
    D, T, NCTX, E, DE, TT = cfg.D, cfg.T, cfg.NCTX, cfg.E, cfg.DE, cfg.TT
    HVF = cfg.HV
    HV, H = cfgL.HV, cfgL.H
    I = {}

    def inp(name, shape):
        I[name] = S.dram(name, shape, F32, kind="ExternalInput")
    inp("x", [1, T, D]); inp("c", [1, D]); inp("ctx", [1, NCTX, D]); inp("c_ctx", [D])
    inp("ada_w", [1, D, 6 * D]); inp("ada_b", [1, 6 * D]); inp("norm_mix", [1, D]); inp("norm_ffn", [1, D])
    inp("w_in", [1, D, cfgL.NIN]); inp("gdn_conv", [1, 5, 3 * HV]); inp("gdn_a_log", [1, 2, H]); inp("gdn_dt_bias", [1, 2, H])
    inp("hgrn_lb", [2, 2, HV]); inp("hgrn_norm", [1, HV]); inp("gdn_norm", [1, HV])
    inp("w_branch_a", [1, HVF, D]); inp("w_branch_b", [1, HVF, D]); inp("w_out", [1, D, D]); inp("router_w", [1, D, E])
    EL = E // (cfg.H // cfgL.H)
    inp("w_gate", [1, EL, D, DE]); inp("w_up", [1, EL, D, DE]); inp("w_down", [1, EL, DE, D]); inp("final_norm", [D])
    G = {}

    def scr(name, shape, dt):
        G[name] = S.dram(name, shape, dt, kind="ExternalOutput" if name in debug else "Internal")
    scr("grow", [2, D], F32)
    scr("hqT", [HV, T], BF16); scr("hffT", [HV, TT], BF16); scr("hfbT", [HV, TT], BF16); scr("hi", [TT, HV], BF16)
    scr("hogT", [HV, T], BF16); scr("gzT", [HV, T], BF16); scr("mgT", [2 * D, T], BF16)
    scr("gqkvT", [3 * HV, TT], BF16); scr("gab", [TT, 4 * H], F32)
    scr("ycat", [2 * HV, T], BF16)
    G["yAT"] = Buf("yAT", G["ycat"].t[0:HV, :], multi=True)
    G["yBT"] = Buf("yBT", G["ycat"].t[HV:2 * HV, :], multi=True)
    NR = cfg.H // cfgL.H
    CR = min(256, 2 * HV)
    scr("yall", [2 * HV // CR, NR * CR, T], BF16)
    scr("gq", [HV, TT], BF16); scr("gk", [HV, TT], BF16); scr("gv", [HV, TT], BF16)
    scr("mT", [D, T], BF16); scr("x2", [T, D], F32); scr("h2", [T, D], BF16); scr("crow", [2, D], F32)
    G["x2s"] = Buf("x2s")
    scr("macc", [T, D], F32)
    scr("maccb", [T, D], BF16)
    MR = min(512, T)
    scr("mall", [T // MR, (cfg.H // cfgL.H) * MR, D], BF16)
    scr("dbg_mod", [128, 6 * cfg.KD * 2], F32)
    if "dbg_S" in debug:
        scr("dbg_S", [2 * min(4, H), 128, 128], F32)
    if "dbg_ob" in debug:
        scr("dbg_ob", [HV, T], F32)
    if "dbg_g" in debug:
        scr("dbg_g", [6, 128, TT // 128, 2 * H], F32)
    scr("dbg_coef", [128, 6 * cfg.KD], F32)
    out = S.dram("out", [T, D], F32, kind="ExternalOutput")
    return I, G, out


def local_cfg(cfg, nsplit):
    return Cfg(D=cfg.D, H=cfg.H // nsplit, T=cfg.T, GW=cfg.GW, NCTX=cfg.NCTX, E=cfg.E, DE=cfg.DE, CAP=cfg.CAP, eps=cfg.eps)


def build_program(cfg=FULL, debug=(), phases=None, nsplit=2, groups=None):
    nc = bass.Bass("TRN2", target_bir_lowering=False)
    S = Sched(nc)
    cfgL = local_cfg(cfg, nsplit) if nsplit > 1 else cfg
    I, G, out = declare_io(S, cfg, debug, cfgL)
    P = Arena(S, "glob")
    C = make_consts(S, P, cfg)
    G["mod"] = P.sb("mod", [128, 6 * cfg.KD, 2], F32)
    G["coef"] = P.sb("coef", [128, 6, cfg.KD], F32)
    ph = phases or ("ada", "coef", "prr", "prc", "hgrn", "gconv", "gdn", "merge", "outproj", "route", "experts", "final")
    if "ada" in ph:
        phase_ada(S, cfg, G, I)
    if "coef" in ph:
        phase_coef(S, cfg, G, I)
    if "dbg_mod" in debug:
        S.dma("sp", G["dbg_mod"][:], G["mod"][:].rearrange("p n s -> p (n s)"), [G["mod"]], [G["dbg_mod"]])
        S.dma("sp", G["dbg_coef"][:], G["coef"][:].rearrange("p n s -> p (n s)"), [G["coef"]], [G["dbg_coef"]])
    if "prr" in ph:
        phase_proj(S, cfgL, G, I, C, "r")
    if "prc" in ph:
        phase_proj(S, cfgL, G, I, C, "c")
    if "hgrn" in ph:
        phase_hgrn(S, cfgL, G, I, C)
    if "gconv" in ph:
        phase_gconv(S, cfgL, G, I, C)
    if "gdn" in ph:
        phase_gdn(S, cfgL, G, I, C)
    if nsplit > 1:
        grp = groups or [[b + 4 * r for r in range(nsplit)] for b in range(4)]
        CR = min(256, 2 * cfgL.HV)
        for ck in range(2 * cfgL.HV // CR):
            S.emit_cc(lambda e, ck=ck: e.collective_compute("AllGather", ALU.bypass, replica_groups=grp, ins=[G["ycat"][ck * CR:(ck + 1) * CR, :]],
                                                            outs=[G["yall"][ck]]),
                      [G["yAT"], G["yBT"]], [G["yall"]])
    if "merge" in ph:
        phase_merge(S, cfg, G, I, C, cfgL)
    if "outproj" in ph:
        phase_outproj(S, cfg, G, I, C)
    R_ = {"idxT": P.sb("idxT", [128, cfg.CAP // 128, cfg.E], I32), "gateT": P.sb("gateT", [128, cfg.CAP // 128, cfg.E], F32)}
    if "route" in ph:
        phase_route(S, cfg, G, I, C, R_)
    grp = groups or [[b + 4 * r for r in range(max(nsplit, 1))] for b in range(4)]
    if "experts" in ph:
        phase_experts(S, cfg, G, I, C, R_, cfg.E // nsplit)
        phase_moe_exchange(S, cfg, G, nsplit, grp)
    if "final" in ph:
        phase_final(S, cfg, G, I, C, out, nsplit)
    P.close()
    S.finish()
    build_program.last_sched = S
    return nc


_PER_BATCH = ("x", "c", "ctx")
_NC_CACHE = {}


def _head_cols(v, hf, nsplit, H, axis=-1):
    v = np.asarray(v)
    w = (H // nsplit) * 128
    sl = [slice(None)] * v.ndim
    sl[axis] = slice(hf * w, (hf + 1) * w)
    return v[tuple(sl)]


def shard_inputs(inputs, cfg, b, hf, nsplit):
    H, HV, D = cfg.H, cfg.HV, cfg.D
    HLc = H // nsplit
    m = {}
    for k, v in inputs.items():
        v = np.asarray(v)
        if k in _PER_BATCH:
            m[k] = np.ascontiguousarray(v[b:b + 1])
        else:
            m[k] = v
    if nsplit > 1:
        w = m["w_in"]
        parts = []
        for g in range(9):
            parts.append(_head_cols(w[:, :, g * HV:(g + 1) * HV], hf, nsplit, H))
        o = 9 * HV
        for g in range(4):
            parts.append(w[:, :, o + g * H + hf * HLc:o + g * H + (hf + 1) * HLc])
        parts.append(w[:, :, o + 4 * H:])
        m["w_in"] = np.concatenate(parts, axis=2)
        m["hgrn_lb"] = _head_cols(m["hgrn_lb"], hf, nsplit, H)
        m["hgrn_norm"] = _head_cols(m["hgrn_norm"], hf, nsplit, H)
        m["gdn_norm"] = _head_cols(m["gdn_norm"], hf, nsplit, H)
        cw = m["gdn_conv"]
        m["gdn_conv"] = np.concatenate([_head_cols(cw[:, :, g * HV:(g + 1) * HV], hf, nsplit, H) for g in range(3)], axis=2)
        m["gdn_a_log"] = m["gdn_a_log"][:, :, hf * HLc:(hf + 1) * HLc]
        m["gdn_dt_bias"] = m["gdn_dt_bias"][:, :, hf * HLc:(hf + 1) * HLc]
        E = cfg.E
        EL = E // nsplit
        perm = [(hf * EL + j) % E for j in range(E)]
        m["router_w"] = m["router_w"][:, :, perm]
        for k in ("w_gate", "w_up", "w_down"):
            m[k] = m[k][:, hf * EL:(hf + 1) * EL]
    return {k: np.ascontiguousarray(v) for k, v in m.items()}


def kernel(**inputs):
    cfg = FULL
    n = 8
    nsplit = 2
    if "nc" not in _NC_CACHE:
        _NC_CACHE["nc"] = build_program(cfg, nsplit=nsplit)
    nc = _NC_CACHE["nc"]
    B = inputs["x"].shape[0]
    in_maps = [shard_inputs(inputs, cfg, core % B, core // B, nsplit) for core in range(n)]
    res = run_bass_kernel_spmd(nc, in_maps, core_ids=list(range(n)))
    out = np.stack([np.asarray(res.results[b]["out"]) for b in range(B)], axis=0)
    return out.astype(np.float32)
```

```python
import contextlib
import numpy as np
import concourse.bass as bass
import concourse.mybir as mybir
from concourse.bass_utils import run_bass_kernel_spmd

F32 = mybir.dt.float32
BF16 = mybir.dt.bfloat16
I32 = mybir.dt.int32
U32 = mybir.dt.uint32
AF = mybir.ActivationFunctionType
ALU = mybir.AluOpType

ENGS = ("pe", "act", "dve", "pool", "sp")
NEG = -30000.0


class Buf:
    __slots__ = ("name", "ws", "rs", "t", "multi", "dgroup", "excl")

    def __init__(self, name, t=None, multi=False, dgroup=None, excl=False):
        self.excl = excl
        self.name = name
        self.ws = {}
        self.rs = {}
        self.t = t
        self.multi = multi
        self.dgroup = dgroup or name

    def __getitem__(self, idx):
        return self.t[idx]


class Sched:
    def __init__(self, nc):
        self.nc = nc
        self.ops = {e: [] for e in ENGS}
        self.val = {}
        self.seen = {e: {} for e in ENGS}
        self.uid = 0
        self.dmap = {}
        self.dfree = {"sw": [], "hw": []}
        self.nd = 0

    def dkey(self, dgroup, kind):
        k = (dgroup, kind)
        if k not in self.dmap:
            if self.dfree[kind]:
                self.dmap[k] = self.dfree[kind].pop()
            else:
                self.dmap[k] = f"ds{self.nd}_{kind}"
                self.nd += 1
        return self.dmap[k]

    def release(self, dgroups):
        for k in list(self.dmap):
            if k[0] in dgroups:
                self.dfree[k[1]].append(self.dmap.pop(k))

    def dram(self, name, shape, dt, kind="Internal"):
        return Buf(name, self.nc.dram_tensor(name, list(shape), dt, kind=kind).ap(), multi=True)

    def emit(self, eng, fn, reads=(), writes=(), dma=None):
        deps = {}

        def add(k, v):
            if deps.get(k, 0) < v:
                deps[k] = v
        for b in reads:
            for k, v in b.ws.items():
                add(k, v)
            if b.excl:
                for k, v in b.rs.items():
                    if k != "c_" + eng:
                        add(k, v)
        for b in writes:
            if b.multi:
                continue
            for k, v in b.ws.items():
                add(k, v)
            for k, v in b.rs.items():
                add(k, v)
        seen = self.seen[eng]
        waits = []
        for k, v in deps.items():
            if seen.get(k, 0) < v:
                seen[k] = v
                waits.append((k, v))
        sk = self.dkey(dma.dgroup, "sw" if eng == "pool" else "hw") if dma is not None else ("c_" + eng)
        inc = 16 if dma is not None else 1
        self.val[sk] = self.val.get(sk, 0) + inc
        tv = self.val[sk]
        for b in reads:
            if b.rs.get(sk, 0) < tv:
                b.rs[sk] = tv
        for b in writes:
            if b.multi:
                b.ws[sk] = tv
            else:
                b.ws = {sk: tv}
                b.rs = {}
        self.ops[eng].append((waits, fn, sk, inc))

    def dma(self, eng, out, in_, reads, writes, sem=None, **kw):
        if sem is None:
            sem = [b for b in list(writes) + list(reads) if not b.multi][0]
        self.emit(eng, lambda e: e.dma_start(out=out, in_=in_, **kw), reads, writes, dma=sem)

    def emit_cc(self, fn, reads, writes):
        deps = {}
        for b in reads:
            for k, v in b.ws.items():
                if deps.get(k, 0) < v:
                    deps[k] = v
        seen = self.seen["pool"]
        waits = []
        for k, v in deps.items():
            if seen.get(k, 0) < v:
                seen[k] = v
                waits.append((k, v))
        sk = "cc_sem"
        self.val[sk] = self.val.get(sk, 0) + 1
        for b in writes:
            b.ws[sk] = self.val[sk]
        self.ops["pool"].append((waits, fn, sk, 1))

    def barrier(self):
        cur = dict(self.val)
        for eng in ENGS:
            seen = self.seen[eng]
            waits = []
            for k, v in cur.items():
                if seen.get(k, 0) < v:
                    seen[k] = v
                    waits.append((k, v))
            if waits:
                self.ops[eng].append((waits, None, None, 0))

    def finish(self):
        nc = self.nc
        sems = {k: nc.alloc_semaphore(k) for k in self.val}
        engobj = {"pe": "tensor", "act": "scalar", "dve": "vector", "pool": "gpsimd", "sp": "sync"}
        final = list(self.val.items())
        with nc.Block() as block:
            for eng in ENGS:
                ops = self.ops[eng]
                is_final = eng == "sp"

                def body(e, ops=ops, is_final=is_final):
                    for waits, fn, sk, inc in ops:
                        for k, v in waits:
                            e.wait_ge(sems[k], v)
                        if fn is not None:
                            if sk == "cc_sem":
                                fn(e).then_inc(sems[sk])
                            else:
                                fn(e).then_inc(sems[sk], inc)
                    if is_final:
                        for k, v in final:
                            e.wait_ge(sems[k], v)
                getattr(block, engobj[eng])(body)
        return nc


class Arena:
    def __init__(self, S, tag):
        self.S = S
        self.tag = tag
        self.st = contextlib.ExitStack()
        self.groups = set()

    def sb(self, name, shape, dt, multi=False, dgroup=None):
        nm = f"{self.tag}_{name}"
        t = self.st.enter_context(self.S.nc.sbuf_tensor(nm, list(shape), dt))
        b = Buf(nm, t, multi=multi, dgroup=dgroup and f"{self.tag}_{dgroup}")
        self.groups.add(b.dgroup)
        return b

    def ps(self, name, shape, dt=F32):
        nm = f"{self.tag}_{name}"
        t = self.st.enter_context(self.S.nc.psum_tensor(nm, list(shape), dt))
        return Buf(nm, t, excl=True)

    def close(self):
        self.S.barrier()
        self.S.release(self.groups)
        self.st.close()


class Cfg:
    def __init__(self, D=2048, H=8, T=4096, GW=64, NCTX=256, E=16, DE=1024, CAP=512, eps=1e-6):
        self.D, self.H, self.T, self.GW, self.NCTX = D, H, T, GW, NCTX
        self.E, self.DE, self.CAP, self.eps = E, DE, CAP, eps
        self.R = T // GW
        self.HV = H * 128
        self.KD = D // 128
        self.TT = T + NCTX
        HV = self.HV
        o = 0
        self.c_hq = o; o += HV
        self.c_hff = o; o += HV
        self.c_hfb = o; o += HV
        self.c_hi = o; o += HV
        self.c_hog = o; o += HV
        self.c_gqkv = o; o += 3 * HV
        self.c_gz = o; o += HV
        self.c_ga = o; o += 2 * H
        self.c_gb = o; o += 2 * H
        self.c_mg = o; o += 2 * D
        self.NIN = o


FULL = Cfg()


def token_blocks(cfg, lo, hi, nb=512):
    out = []
    for a, b in ((0, cfg.NCTX), (cfg.NCTX, cfg.TT)):
        a, b = max(a, lo), min(b, hi)
        t = a
        while t < b:
            n = min(nb, b - t)
            out.append((t, n))
            t += n
    return out


def make_consts(S, A, cfg):
    C = {}
    nc = S.nc
    identb = A.sb("identb", [128, 128], BF16)
    identf = A.sb("identf", [128, 128], F32)
    for ident in (identb, identf):
        S.emit("pool", lambda e, ident=ident: e.memset(ident[:], 0.0), writes=[ident])
        S.emit("pool", lambda e, ident=ident: e.affine_select(
            out=ident[:], in_=ident[:], pattern=[[-1, 128]], compare_op=ALU.not_equal, fill=1.0,
            base=0, channel_multiplier=1), reads=[ident], writes=[ident])
    C["identb"], C["identf"] = identb, identf
    onesb = A.sb("onesb", [128, 128], BF16)
    S.emit("pool", lambda e: e.memset(onesb[:], 1.0), writes=[onesb])
    C["onesb"] = onesb
    onesf = A.sb("onesf", [128, 128], F32)
    S.emit("pool", lambda e: e.memset(onesf[:], 1.0), writes=[onesf])
    C["onesf"] = onesf
    epsc = A.sb("epsc", [128, 1], F32)
    S.emit("pool", lambda e: e.memset(epsc[:], cfg.eps), writes=[epsc])
    C["epsc"] = epsc
    onec = A.sb("onec", [128, 1], F32)
    S.emit("pool", lambda e: e.memset(onec[:], 1.0), writes=[onec])
    C["onec"] = onec
    return C


def phase_ada(S, cfg, G, I):
    D, KD = cfg.D, cfg.KD
    A = Arena(S, "ada")
    NCB = 6 * KD
    SW = 512
    cT = A.sb("cT", [128, KD, 2], F32)
    s2 = A.sb("s2", [128, KD, 2], F32)
    bT = A.sb("bT", [128, NCB], F32)
    S.dma("sp", cT[:, :, 0], I["c"][0].rearrange("(k p) -> p k", p=128), [I["c"]], [cT], allow_slow_non_contiguous=True)
    S.dma("sp", cT[:, :, 1], I["c_ctx"][:].rearrange("(k p) -> p k", p=128), [I["c_ctx"]], [cT], allow_slow_non_contiguous=True)
    S.dma("sp", bT[:], I["ada_b"][0].rearrange("(k p) -> p k", p=128), [I["ada_b"]], [bT], allow_slow_non_contiguous=True)
    S.emit("act", lambda e: e.activation(out=s2[:], in_=cT[:], func=AF.Silu), [cT], [s2])
    wsl = [A.sb(f"w{i}", [128, KD, SW], F32) for i in range(2)]
    pa = A.ps("pa", [128, NCB, 2], F32)
    mod = G["mod"]
    wv = I["ada_w"][0].rearrange("(k p) n -> p k n", p=128)
    nslab = 6 * D // SW
    for s in range(nslab):
        w = wsl[s % 2]
        S.dma("sp" if s % 2 == 0 else "pool", w[:], wv[:, :, s * SW:(s + 1) * SW], [I["ada_w"]], [w])
        for j in range(SW // 128):
            cb = s * (SW // 128) + j

            def mm(e, w=w, j=j, cb=cb):
                r = None
                for k in range(KD):
                    r = e.matmul(pa[:, cb, :], lhsT=w[:, k, j * 128:(j + 1) * 128], rhs=s2[:, k, :],
                                 start=(k == 0), stop=(k == KD - 1))
                return r
            S.emit("pe", mm, [w, s2], [pa])
    S.emit("dve", lambda e: e.tensor_tensor(out=mod[:], in0=pa[:], in1=bT[:].to_broadcast([128, NCB, 2]) if False else
                                            bT[:].rearrange("p (n o) -> p n o", o=1).to_broadcast([128, NCB, 2]),
                                            op=ALU.add), [pa, bT], [mod])
    A.close()


def phase_coef(S, cfg, G, I):
    KD, D = cfg.KD, cfg.D
    A = Arena(S, "coef")
    mod = G["mod"]
    nm = A.sb("nm", [128, KD], F32)
    nf = A.sb("nf", [128, KD], F32)
    S.dma("sp", nm[:], I["norm_mix"][0].rearrange("(k p) -> p k", p=128), [I["norm_mix"]], [nm], allow_slow_non_contiguous=True)
    S.dma("sp", nf[:], I["norm_ffn"][0].rearrange("(k p) -> p k", p=128), [I["norm_ffn"]], [nf], allow_slow_non_contiguous=True)
    co = G["coef"]
    def mk(dst, gain, sc_j, s):
        S.emit("dve", lambda e: e.scalar_tensor_tensor(out=co[:, dst, :], in0=mod[:, sc_j * KD:(sc_j + 1) * KD, s], scalar=1.0,
                                                       in1=gain[:], op0=ALU.add, op1=ALU.mult), [mod, gain, co], [co])
    mk(0, nm, 1, 0)
    S.emit("dve", lambda e: e.tensor_copy(out=co[:, 1, :], in_=mod[:, 0:KD, 0]), [mod, co], [co])
    mk(2, nm, 1, 1)
    S.emit("dve", lambda e: e.tensor_copy(out=co[:, 3, :], in_=mod[:, 0:KD, 1]), [mod, co], [co])
    mk(4, nf, 4, 0)
    S.emit("dve", lambda e: e.tensor_copy(out=co[:, 5, :], in_=mod[:, 3 * KD:4 * KD, 0]), [mod, co], [co])
    gt = A.sb("gt", [128, 2, KD], F32)
    S.emit("dve", lambda e: e.tensor_copy(out=gt[:, 0, :], in_=mod[:, 2 * KD:3 * KD, 0]), [mod], [gt])
    S.emit("dve", lambda e: e.tensor_copy(out=gt[:, 1, :], in_=mod[:, 5 * KD:6 * KD, 0]), [mod, gt], [gt])
    for j in range(2):
        S.dma("sp", G["grow"][j].rearrange("(k p) -> p k", p=128), gt[:, j, :], [gt], [G["grow"]], allow_slow_non_contiguous=True)
    A.close()


def norm_tiles(S, A, cfg, C, tiles, hT, coefbuf, f32T=None):
    D, KD = cfg.D, cfg.KD
    xt = [A.sb(f"nx{i}", [128, D], F32) for i in range(2)]
    xn = [A.sb(f"nxn{i}", [128, D], BF16 if f32T is None else F32) for i in range(2)]
    junk = A.sb("njunk", [128, D], BF16)
    st = [A.sb(f"nst{i}", [128, 8], F32) for i in range(2)]
    tdt = BF16 if f32T is None else F32
    per = 8 if f32T is None else 4
    pts = [A.ps(f"npt{i}", [128, per, 128], tdt) for i in range(2)]
    ident = C["identb"] if f32T is None else C["identf"]
    co = coefbuf
    ng = KD // per if KD >= per else 1
    per = min(per, KD)
    gi = 0
    for ti, (pieces, srcbufs, col0, ci) in enumerate(tiles):
        x = xt[ti % 2]
        for k, (ap, p0, npp) in enumerate(pieces):
            S.dma("sp" if ti % 2 == 0 else "pool", x[p0:p0 + npp, :], ap, srcbufs, [x])
        s = st[ti % 2]
        import os
        NV = int(os.environ.get("K_NV", "9"))
        if NV < 1:
            continue
        S.emit("act", lambda e, x=x, s=s: e.activation(out=junk[:], in_=x[:], func=AF.Square, accum_out=s[:, 0:1]),
               [x], [junk, s])
        S.emit("dve", lambda e, s=s: e.tensor_scalar(out=s[:, 1:2], in0=s[:, 0:1], scalar1=1.0 / D, scalar2=cfg.eps,
                                                     op0=ALU.mult, op1=ALU.add), [s], [s])
        S.emit("act", lambda e, s=s: e.activation(out=s[:, 2:3], in_=s[:, 1:2], func=AF.Sqrt), [s], [s])
        S.emit("dve", lambda e, s=s: e.reciprocal(out=s[:, 3:4], in_=s[:, 2:3]), [s], [s])
        if NV < 2:
            continue
        n = xn[ti % 2]
        S.emit("dve", lambda e, x=x, s=s, n=n: e.tensor_scalar(out=n[:], in0=x[:], scalar1=s[:, 3:4], scalar2=None,
                                                               op0=ALU.mult), [x, s], [n])
        if NV < 3:
            continue
        for g in range(ng):
            pt = pts[gi % 2]
            gi += 1

            def tr(e, n=n, pt=pt, g=g):
                r = None
                for j in range(per):
                    c = g * per + j
                    r = e.transpose(out=pt[:, j, :], in_=n[:, c * 128:(c + 1) * 128], identity=ident[:])
                return r
            S.emit("pe", tr, [n, ident], [pt])
            if NV < 4:
                continue
            for j in range(per):
                c = g * per + j
                eng = "act" if gi % 2 == 0 else "dve"
                outs = [(hT, hT[:, c, col0:col0 + 128])]
                if f32T is not None:
                    outs.append((f32T, f32T[:, c, col0:col0 + 128]))
                for oi, (ob, oap) in enumerate(outs):
                    eng2 = eng if oi == 0 else ("dve" if eng == "act" else "act")
                    if eng2 == "act":
                        S.emit("act", lambda e, pt=pt, j=j, c=c, oap=oap, ci=ci: e.activation(
                            out=oap, in_=pt[:, j, :], func=AF.Identity, scale=co[:, ci, c:c + 1], bias=co[:, ci + 1, c:c + 1]),
                            [pt, co], [ob])
                    else:
                        S.emit("dve", lambda e, pt=pt, j=j, c=c, oap=oap, ci=ci: e.tensor_scalar(
                            out=oap, in0=pt[:, j, :], scalar1=co[:, ci, c:c + 1], scalar2=co[:, ci + 1, c:c + 1],
                            op0=ALU.mult, op1=ALU.add), [pt, co], [ob])


def lat_tile_pieces(cfg, xin, ti, order):
    if order == "r":
        return [(xin[0, ti * 128:(ti + 1) * 128, :], 0, 128)]
    R, GW = cfg.R, cfg.GW
    xv = xin[0].rearrange("(r c) d -> c r d", c=GW)
    out = []
    if R >= 128:
        per = R // 128
        cc, sub = divmod(ti, per)
        out.append((xv[cc, sub * 128:(sub + 1) * 128, :], 0, 128))
    else:
        ncol = 128 // R
        for k in range(ncol):
            out.append((xv[ti * ncol + k, :, :], k * R, R))
    return out


def act_tiles(cfg, I, order):
    tiles = []
    for t in range(cfg.NCTX // 128):
        tiles.append(([(I["ctx"][0, t * 128:(t + 1) * 128, :], 0, 128)], [I["ctx"]], t * 128, 2))
    for t in range(cfg.T // 128):
        tiles.append((lat_tile_pieces(cfg, I["x"], t, order), [I["x"]], cfg.NCTX + t * 128, 0))
    return tiles


def project(S, A, cfg, hT, w_in, groups, tok_lo=0):
    KD, TT = cfg.KD, cfg.TT
    SW = 256
    wsl = [A.sb(f"pw{i}", [128, KD, SW], BF16) for i in range(2)]
    pps = [A.ps(f"pp{i}", [128, 512], F32) for i in range(4)]
    stg = {}
    wv = w_in[0].rearrange("(k p) n -> p k n", p=128)
    si = 0
    pi = 0
    gi = 0
    for g in groups:
        col0, ncols, lay, func, dst, dt = g["col0"], g["ncols"], g["layout"], g["func"], g["dst"], g["dt"]
        tlo = g.get("tok_lo", 0)
        key = (lay, dt)
        if key not in stg:
            stg[key] = [A.sb(f"ps{lay}{len(stg)}_{i}", [128, 512], dt) for i in range(3)]
        stgs = stg[key]
        if lay == "F":
            blocks = token_blocks(cfg, tlo, TT)
            for s0 in range(0, ncols, SW):
                sw = min(SW, ncols - s0)
                w = wsl[si % 2]
                S.dma("pool", w[:, :, 0:sw], wv[:, :, col0 + s0:col0 + s0 + sw], [w_in], [w])
                si += 1
                for j in range(0, sw, 128):
                    for (t0, nb) in blocks:
                        pp = pps[pi % 4]
                        pi += 1

                        def mm(e, w=w, j=j, t0=t0, nb=nb, pp=pp):
                            r = None
                            for k in range(KD):
                                r = e.matmul(pp[:, 0:nb], lhsT=w[:, k, j:j + 128], rhs=hT[:, k, t0:t0 + nb],
                                             start=(k == 0), stop=(k == KD - 1))
                            return r
                        S.emit("pe", mm, [w, hT], [pp])
                        sg = stgs[gi % 3]
                        gi += 1
                        if func is None:
                            S.emit("dve", lambda e, sg=sg, pp=pp, nb=nb: e.tensor_copy(out=sg[:, 0:nb], in_=pp[:, 0:nb]), [pp], [sg])
                        else:
                            S.emit("act", lambda e, sg=sg, pp=pp, nb=nb, func=func: e.activation(out=sg[:, 0:nb], in_=pp[:, 0:nb], func=func),
                                   [pp], [sg])
                        r0 = s0 + j
                        S.dma("sp", dst[r0:r0 + 128, t0 - tlo:t0 - tlo + nb], sg[:, 0:nb], [sg], [dst])
        else:
            assert ncols <= 512 or ncols % 256 == 0
            for s0 in range(0, ncols, SW):
                sw = min(SW, ncols - s0)
                w = wsl[si % 2]
                S.dma("pool", w[:, :, 0:sw], wv[:, :, col0 + s0:col0 + s0 + sw], [w_in], [w])
                si += 1
                for t0 in range(tlo, TT, 128):
                    pp = pps[pi % 4]
                    pi += 1

                    def mm(e, w=w, sw=sw, t0=t0, pp=pp):
                        r = None
                        for k in range(KD):
                            r = e.matmul(pp[:, 0:sw], lhsT=hT[:, k, t0:t0 + 128], rhs=w[:, k, 0:sw],
                                         start=(k == 0), stop=(k == KD - 1))
                        return r
                    S.emit("pe", mm, [w, hT], [pp])
                    sg = stgs[gi % 3]
                    gi += 1
                    S.emit("dve", lambda e, sg=sg, pp=pp, sw=sw: e.tensor_copy(out=sg[:, 0:sw], in_=pp[:, 0:sw]), [pp], [sg])
                    S.dma("sp", dst[t0 - tlo:t0 - tlo + 128, s0:s0 + sw], sg[:, 0:sw], [sg], [dst])


def phase_proj(S, cfg, G, I, C, order):
    A = Arena(S, "pr" + order)
    KD, TT, HV, NCTX, H, D = cfg.KD, cfg.TT, cfg.HV, cfg.NCTX, cfg.H, cfg.D
    hT = A.sb("hT", [128, KD, TT], BF16, multi=True)
    norm_tiles(S, A, cfg, C, act_tiles(cfg, I, order), hT, G["coef"])
    if order == "r":
        groups = [
            dict(col0=cfg.c_hq, ncols=HV, layout="F", func=AF.Silu, dst=G["hqT"], dt=BF16, tok_lo=NCTX),
            dict(col0=cfg.c_hff, ncols=HV, layout="F", func=None, dst=G["hffT"], dt=BF16),
            dict(col0=cfg.c_hfb, ncols=HV, layout="F", func=None, dst=G["hfbT"], dt=BF16),
            dict(col0=cfg.c_hi, ncols=HV, layout="T", func=None, dst=G["hi"], dt=BF16),
            dict(col0=cfg.c_hog, ncols=HV, layout="F", func=AF.Sigmoid, dst=G["hogT"], dt=BF16, tok_lo=NCTX),
            dict(col0=cfg.c_gz, ncols=HV, layout="F", func=AF.Silu, dst=G["gzT"], dt=BF16, tok_lo=NCTX),
            dict(col0=cfg.c_mg, ncols=2 * D, layout="F", func=AF.Sigmoid, dst=G["mgT"], dt=BF16, tok_lo=NCTX),
        ]
    else:
        groups = [
            dict(col0=cfg.c_gqkv, ncols=3 * HV, layout="F", func=None, dst=G["gqkvT"], dt=BF16),
            dict(col0=cfg.c_ga, ncols=4 * H, layout="T", func=None, dst=G["gab"], dt=F32),
        ]
    import os
    sel = os.environ.get("K_GROUPS")
    if sel is not None:
        groups = [groups[int(i)] for i in sel.split(",") if i != ""]
    project(S, A, cfg, hT, I["w_in"], groups)
    A.close()


def phase_hgrn(S, cfg, G, I, C):
    A = Arena(S, "hg")
    H, T, TT, NCTX, HV = cfg.H, cfg.T, cfg.TT, cfg.NCTX, cfg.HV
    CH = 64
    NCH = TT // CH
    NCC = NCTX // CH
    order = [list(range(NCH)), list(range(NCC - 1, -1, -1)) + list(range(NCH - 1, NCC - 1, -1))]
    SEG = 1024
    segs = []
    for a, b in ((0, NCTX), (NCTX, TT)):
        t = a
        while t < b:
            n = min(SEG, b - t)
            segs.append((t, n))
            t += n
    lbT = A.sb("lbT", [128, 2, 2, H], F32)
    for d in range(2):
        for sl in range(2):
            S.dma("sp", lbT[:, d, sl, :], I["hgrn_lb"][d, sl].rearrange("(h p) -> p h", p=128), [I["hgrn_lb"]], [lbT],
                  allow_slow_non_contiguous=True)
    low = A.sb("low", [128, 2, H], F32)
    oml = A.sb("oml", [128, 2, H], F32)
    noml = A.sb("noml", [128, 2, H], F32)
    S.emit("dve", lambda e: e.tensor_tensor(out=low[:], in0=lbT[:, :, 0, :], in1=lbT[:, :, 1, :], op=ALU.subtract), [lbT], [low])
    S.emit("act", lambda e: e.activation(out=low[:], in_=low[:], func=AF.Sigmoid), [low], [low])
    S.emit("dve", lambda e: e.tensor_scalar(out=oml[:], in0=low[:], scalar1=-1.0, scalar2=1.0, op0=ALU.mult, op1=ALU.add), [low], [oml])
    S.emit("dve", lambda e: e.tensor_scalar(out=noml[:], in0=low[:], scalar1=1.0, scalar2=-1.0, op0=ALU.mult, op1=ALU.add), [low], [noml])
    gain = A.sb("gain", [128, H], F32)
    S.dma("sp", gain[:], I["hgrn_norm"][0].rearrange("(h p) -> p h", p=128), [I["hgrn_norm"]], [gain], allow_slow_non_contiguous=True)
    msk01 = A.sb("msk01", [128, SEG], F32)
    S.emit("pool", lambda e: e.memset(msk01[:], 1.0), writes=[msk01])
    S.emit("pool", lambda e: e.memset(msk01[:].rearrange("p (n c) -> p n c", c=CH)[:, :, 0:1], 0.0), [msk01], [msk01])
    cm = []
    for d in range(2):
        m = A.sb(f"cm{d}", [CH, CH], F32)
        S.emit("pool", lambda e, m=m: e.memset(m[:], 1.0), writes=[m])
        sg = 1 if d == 0 else -1
        S.emit("pool", lambda e, m=m, sg=sg: e.affine_select(out=m[:], in_=m[:], pattern=[[sg, CH]], compare_op=ALU.is_ge, fill=0.0,
                                                             base=0, channel_multiplier=-sg), [m], [m])
        cm.append(m)
    identb, onesb = C["identb"], C["onesb"]
    qT2 = [A.sb(f"qT{i}", [128, T], BF16) for i in range(2)]
    fl2 = [[A.sb(f"fl{d}{i}", [128, TT], BF16) for d in range(2)] for i in range(2)]
    vv2 = [A.sb(f"vv{i}", [CH, NCH, 128], BF16) for i in range(2)]
    og2 = [A.sb("og0", [128, T], BF16)] * 2
    qt = [A.sb(f"qt{d}", [128, T], BF16) for d in range(2)]
    kt = [A.sb(f"kt{d}", [128, TT], BF16) for d in range(2)]
    kh = [A.sb(f"kh{d}", [128, TT], BF16) for d in range(2)]
    egt = [A.sb(f"egt{d}", [128, NCH], F32) for d in range(2)]
    tsets = [[A.sb(f"t{n}{i}", [128, SEG], F32) for n in "ABCD"] for i in range(2)]
    OA = A.sb("OA", [128, T], F32)
    Sf = [[A.sb(f"S{d}{i}", [128, 128], F32) for i in range(2)] for d in range(2)]
    Sb = [[A.sb(f"Sb{d}{i}", [128, 128], BF16) for i in range(3)] for d in range(2)]
    sTs = [[A.sb(f"sTs{d}{i}", [CH, CH], BF16) for i in range(3)] for d in range(2)]
    khs = [[A.sb(f"khs{d}{i}", [CH, 128], BF16) for i in range(3)] for d in range(2)]
    sqb = A.sb("sqb", [128, 512], BF16)
    rt = A.sb("rt", [128, 512], F32)
    ys = [A.sb("ys0", [128, 512], BF16)] * 2
    pst = [A.ps(f"pst{d}", [CH, 512], F32) for d in range(2)]
    ptr = [A.ps(f"ptr{d}", [CH, 1024], BF16) for d in range(2)]
    po = [A.ps(f"po{d}", [128, 512], F32) for d in range(2)]
    pd = [A.ps(f"pd{d}", [128, 512], F32) for d in range(2)]

    def load_head(h):
        r0 = h * 128
        qT, fl, vv, og = qT2[h % 2], fl2[h % 2], vv2[h % 2], og2[h % 2]
        S.dma("sp", qT[:], G["hqT"][r0:r0 + 128, :], [G["hqT"]], [qT])
        S.dma("sp", fl[0][:], G["hffT"][r0:r0 + 128, :], [G["hffT"]], [fl[0]])
        S.dma("sp", fl[1][:], G["hfbT"][r0:r0 + 128, :], [G["hfbT"]], [fl[1]])
        S.dma("sp", vv[:], G["hi"][:, r0:r0 + 128].rearrange("(n c) v -> c n v", c=CH), [G["hi"]], [vv])

    tsi_box = [0]

    def do_head(h, qT, fl, vv, og):
        r0 = h * 128
        S.dma("sp", og[:], G["hogT"][r0:r0 + 128, :], [G["hogT"]], [og])
        for d in range(2):
            lo_, om_, nom_ = low[:, d, h:h + 1], oml[:, d, h:h + 1], noml[:, d, h:h + 1]
            for (a, n) in segs:
                tA, tB, tC, tD = tsets[tsi_box[0] % 2]
                tsi_box[0] += 1
                nch = n // CH
                c0 = a // CH
                v3 = lambda t, n=n: t[:, 0:n].rearrange("p (n c) -> p n c", c=CH)
                S.emit("act", lambda e, tA=tA, tB=tB, tC=tC, tD=tD, a=a, n=n, d=d: e.activation(out=tA[:, 0:n], in_=fl[d][:, a:a + n], func=AF.Sigmoid), [fl[d]], [tA])
                S.emit("act", lambda e, tA=tA, tB=tB, tC=tC, tD=tD, n=n, lo_=lo_, om_=om_: e.activation(out=tB[:, 0:n], in_=tA[:, 0:n], func=AF.Ln, scale=om_, bias=lo_),
                       [tA, low, oml], [tB])
                S.emit("dve", lambda e, tA=tA, tB=tB, tC=tC, tD=tD, n=n, om_=om_, nom_=nom_: e.tensor_scalar(out=tC[:, 0:n], in0=tA[:, 0:n], scalar1=nom_, scalar2=om_,
                                                                                op0=ALU.mult, op1=ALU.add), [tA, oml, noml], [tC])
                S.emit("dve", lambda e, tA=tA, tB=tB, tC=tC, tD=tD, n=n: e.tensor_tensor_scan(out=tA[:, 0:n], data0=msk01[:, 0:n], data1=tB[:, 0:n], initial=0.0,
                                                                  op0=ALU.mult, op1=ALU.add), [msk01, tB, tA], [tA])
                tot = lambda nch=nch, v3=v3, tA=tA: v3(tA)[:, :, CH - 1:CH]
                if d == 0:
                    Gd = tA
                else:
                    S.emit("dve", lambda e, tA=tA, tB=tB, tC=tC, tD=tD, n=n: e.tensor_tensor(out=tD[:, 0:n], in0=tB[:, 0:n], in1=tA[:, 0:n], op=ALU.subtract), [tA, tB], [tD])
                    S.emit("dve", lambda e, tA=tA, tB=tB, tC=tC, tD=tD, v3=v3, tot=tot, nch=nch: e.tensor_tensor(out=v3(tB), in0=v3(tD), in1=tot().to_broadcast([128, nch, CH]),
                                                                                     op=ALU.add), [tD, tA], [tB])
                    Gd = tB
                S.emit("act", lambda e, tA=tA, tB=tB, tC=tC, tD=tD, c0=c0, nch=nch, d=d, tot=tot: e.activation(out=egt[d][:, c0:c0 + nch].rearrange("p (n o) -> p n o", o=1),
                                                                                  in_=tot(), func=AF.Exp), [tA], [egt[d]])
                if a >= NCTX:
                    S.emit("act", lambda e, tA=tA, tB=tB, tC=tC, tD=tD, n=n, Gd=Gd: e.activation(out=tD[:, 0:n], in_=Gd[:, 0:n], func=AF.Exp), [Gd], [tD])
                    S.emit("dve", lambda e, tA=tA, tB=tB, tC=tC, tD=tD, n=n, a=a, d=d: e.tensor_tensor(out=qt[d][:, a - NCTX:a - NCTX + n], in0=qT[:, a - NCTX:a - NCTX + n],
                                                                           in1=tD[:, 0:n], op=ALU.mult), [qT, tD], [qt[d]])
                S.emit("act", lambda e, tA=tA, tB=tB, tC=tC, tD=tD, n=n, Gd=Gd: e.activation(out=tD[:, 0:n], in_=Gd[:, 0:n], func=AF.Exp, scale=-1.0), [Gd], [tD])
                S.emit("pool", lambda e, tA=tA, tB=tB, tC=tC, tD=tD, n=n, a=a, d=d: e.tensor_tensor(out=kt[d][:, a:a + n], in0=tC[:, 0:n], in1=tD[:, 0:n], op=ALU.mult),
                       [tC, tD], [kt[d]])
                S.emit("dve", lambda e, tA=tA, tB=tB, tC=tC, tD=tD, v3=v3, tot=tot, nch=nch, Gd=Gd: e.tensor_tensor(out=v3(tD), in0=tot().to_broadcast([128, nch, CH]),
                                                                                       in1=v3(Gd), op=ALU.subtract), [tA, Gd], [tD])
                S.emit("act", lambda e, tA=tA, tB=tB, tC=tC, tD=tD, n=n: e.activation(out=tD[:, 0:n], in_=tD[:, 0:n], func=AF.Exp), [tD], [tD])
                S.emit("pool", lambda e, tA=tA, tB=tB, tC=tC, tD=tD, n=n, a=a, d=d: e.tensor_tensor(out=kh[d][:, a:a + n], in0=tC[:, 0:n], in1=tD[:, 0:n], op=ALU.mult),
                       [tC, tD], [kh[d]])
        for d in range(2):
            S.emit("pool", lambda e, d=d: e.memset(Sf[d][0][:], 0.0), writes=[Sf[d][0]])
            S.emit("pool", lambda e, d=d: e.memset(Sb[d][0][:], 0.0), writes=[Sb[d][0]])
        OAc = {}

        def genA(d, n):
            c = order[d][n]
            t0 = c * CH
            sl = n % 3
            if c >= NCC:
                q0 = t0 - NCTX
                S.emit("pe", lambda e: e.matmul(pst[d][:, 0:CH], lhsT=kt[d][:, t0:t0 + CH], rhs=qt[d][:, q0:q0 + CH],
                                                start=True, stop=True), [kt[d], qt[d]], [pst[d]])
                yield
                S.emit("dve", lambda e: e.tensor_tensor(out=sTs[d][sl][:], in0=pst[d][:, 0:CH], in1=cm[d][:], op=ALU.mult),
                       [pst[d], cm[d]], [sTs[d][sl]])
                yield
            S.emit("pe", lambda e: e.transpose(out=ptr[d][:, 0:128], in_=kh[d][:, t0:t0 + CH], identity=identb[:]),
                   [kh[d], identb], [ptr[d]])
            yield
            S.emit("act", lambda e: e.copy(out=khs[d][sl][:], in_=ptr[d][:, 0:128]), [ptr[d]], [khs[d][sl]])
            yield

        def genB(d, n):
            c = order[d][n]
            t0 = c * CH
            sl = n % 3
            Sf_o, Sf_n = Sf[d][n % 2], Sf[d][(n + 1) % 2]
            Sb_o, Sb_n = Sb[d][n % 3], Sb[d][(n + 1) % 3]
            S.emit("pe", lambda e: e.matmul(pd[d][:, 0:128], lhsT=khs[d][sl][:], rhs=vv[:, c, :], start=True, stop=True),
                   [khs[d][sl], vv], [pd[d]])
            yield
            S.emit("dve", lambda e: e.scalar_tensor_tensor(out=Sf_n[:], in0=Sf_o[:], scalar=egt[d][:, c:c + 1], in1=pd[d][:, 0:128],
                                                           op0=ALU.mult, op1=ALU.add), [Sf_o, egt[d], pd[d]], [Sf_n])
            yield
            S.emit("act", lambda e: e.copy(out=Sb_n[:], in_=Sf_n[:]), [Sf_n], [Sb_n])
            yield
            if c >= NCC:
                q0 = t0 - NCTX

                def mo(e):
                    e.matmul(po[d][:, 0:CH], lhsT=Sb_o[:], rhs=qt[d][:, q0:q0 + CH], start=True, stop=False)
                    return e.matmul(po[d][:, 0:CH], lhsT=vv[:, c, :], rhs=sTs[d][sl][:], start=False, stop=True)
                S.emit("pe", mo, [Sb_o, qt[d], vv, sTs[d][sl]], [po[d]])
                yield
                if c not in OAc:
                    OAc[c] = Buf(f"OAc{c}")
                    OAc[c].ws = dict(OA.ws)
                    OAc[c].rs = dict(OA.rs)
                    S.emit("act", lambda e: e.copy(out=OA[:, q0:q0 + CH], in_=po[d][:, 0:CH]), [po[d]], [OAc[c]])
                else:
                    S.emit("dve", lambda e: e.tensor_tensor(out=OA[:, q0:q0 + CH], in0=po[d][:, 0:CH], in1=OA[:, q0:q0 + CH],
                                                            op=ALU.add), [po[d], OAc[c]], [OAc[c]])
                yield

        def run_streams(streams):
            while streams:
                for g in list(streams):
                    try:
                        next(g)
                    except StopIteration:
                        streams.remove(g)

        run_streams([genA(0, 0), genA(1, 0)])
        for n in range(NCH):
            sts = []
            if n + 1 < NCH:
                sts += [genA(0, n + 1), genA(1, n + 1)]
            sts += [genB(0, n), genB(1, n)]
            run_streams(sts)
        for bk in OAc.values():
            for k_, v_ in bk.ws.items():
                if OA.ws.get(k_, 0) < v_:
                    OA.ws[k_] = v_
        S.emit("dve", lambda e: e.tensor_tensor(out=OA[:], in0=OA[:], in1=og[:], op=ALU.mult), [OA, og], [OA])
        for bi, t0 in enumerate(range(0, T, 512)):
            nb = min(512, T - t0)
            S.emit("act", lambda e, t0=t0, nb=nb: e.activation(out=sqb[:, 0:nb], in_=OA[:, t0:t0 + nb], func=AF.Square), [OA], [sqb])
            S.emit("pe", lambda e, nb=nb: e.matmul(po[0][:, 0:nb], lhsT=onesb[:], rhs=sqb[:, 0:nb], start=True, stop=True), [onesb, sqb], [po[0]])
            S.emit("act", lambda e, nb=nb: e.activation(out=rt[:, 0:nb], in_=po[0][:, 0:nb], func=AF.Ln, scale=1.0 / 128, bias=C["epsc"][:, 0:1]),
                   [po[0], C["epsc"]], [rt])
            S.emit("act", lambda e, nb=nb: e.activation(out=rt[:, 0:nb], in_=rt[:, 0:nb], func=AF.Exp, scale=-0.5), [rt], [rt])
            y = ys[bi % 2]
            S.emit("dve", lambda e, t0=t0, nb=nb, y=y, h=h: e.scalar_tensor_tensor(out=y[:, 0:nb], in0=OA[:, t0:t0 + nb], scalar=gain[:, h:h + 1],
                                                                                   in1=rt[:, 0:nb], op0=ALU.mult, op1=ALU.mult), [OA, gain, rt], [y])
            S.dma("sp", G["yAT"][r0:r0 + 128, t0:t0 + nb], y[:, 0:nb], [y], [G["yAT"]])

    load_head(0)
    for h in range(H):
        if h + 1 < H:
            load_head(h + 1)
        do_head(h, qT2[h % 2], fl2[h % 2], vv2[h % 2], og2[h % 2])
    A.close()


def phase_gconv(S, cfg, G, I, C):
    A = Arena(S, "gc")
    H, T, TT, NCTX, HV = cfg.H, cfg.T, cfg.TT, cfg.NCTX, cfg.HV
    NB = 3 * HV // 128
    cw = A.sb("cw", [128, 5, NB], F32)
    for j in range(5):
        S.dma("sp", cw[:, j, :], I["gdn_conv"][0, j].rearrange("(n p) -> p n", p=128), [I["gdn_conv"]], [cw], allow_slow_non_contiguous=True)
    xp = [A.sb(f"xp{i}", [128, TT + 8], BF16) for i in range(2)]
    for x in xp:
        S.emit("pool", lambda e, x=x: e.memset(x[:], 0.0), writes=[x])
    y = A.sb("y", [128, TT], F32)
    sq = A.sb("sq", [128, 512], BF16)
    rt = A.sb("rt", [128, 512], F32)
    ob = [A.sb(f"ob{i}", [128, TT], BF16) for i in range(2)]
    pn = [A.ps(f"pn{i}", [128, 512], F32) for i in range(2)]
    segs = [(0, NCTX, 2), (NCTX, T, NCTX + 6)]
    dsts = [G["gq"], G["gk"], G["gv"]]
    pi = 0
    for fb in range(NB):
        kind, hb = divmod(fb, H)
        x = xp[fb % 2]
        r0 = fb * 128
        for (a, n, off) in segs:
            if kind == 0 and a == 0:
                continue
            S.dma("sp" if fb % 2 == 0 else "pool", x[:, off:off + n], G["gqkvT"][r0:r0 + 128, a:a + n], [G["gqkvT"]], [x])
            S.emit("dve", lambda e, x=x, a=a, n=n, off=off, fb=fb: e.tensor_scalar(out=y[:, a:a + n], in0=x[:, off - 2:off - 2 + n], scalar1=cw[:, 0, fb:fb + 1],
                                                                                  scalar2=None, op0=ALU.mult), [x, cw], [y])
            for j in range(1, 5):
                S.emit("dve", lambda e, x=x, a=a, n=n, off=off, fb=fb, j=j: e.scalar_tensor_tensor(
                    out=y[:, a:a + n], in0=x[:, off - 2 + j:off - 2 + j + n], scalar=cw[:, j, fb:fb + 1], in1=y[:, a:a + n],
                    op0=ALU.mult, op1=ALU.add), [x, cw, y], [y])
        lo = 0 if kind != 0 else NCTX
        o = ob[fb % 2]
        S.emit("act", lambda e, lo=lo: e.activation(out=y[:, lo:TT], in_=y[:, lo:TT], func=AF.Silu), [y], [y])
        if kind == 2:
            S.emit("dve", lambda e, o=o: e.tensor_copy(out=o[:], in_=y[:]), [y], [o])
        else:
            sc = (128.0 ** -0.5) if kind == 0 else 1.0
            for (t0, nb) in token_blocks(cfg, lo, TT):
                p = pn[pi % 2]
                pi += 1
                S.emit("dve", lambda e, t0=t0, nb=nb: e.tensor_tensor(out=sq[:, 0:nb], in0=y[:, t0:t0 + nb], in1=y[:, t0:t0 + nb], op=ALU.mult), [y], [sq])
                S.emit("pe", lambda e, p=p, nb=nb: e.matmul(p[:, 0:nb], lhsT=C["onesb"][:], rhs=sq[:, 0:nb], start=True, stop=True), [C["onesb"], sq], [p])
                S.emit("act", lambda e, p=p, nb=nb: e.activation(out=rt[:, 0:nb], in_=p[:, 0:nb], func=AF.Ln, bias=C["epsc"][:, 0:1]), [p, C["epsc"]], [rt])
                S.emit("act", lambda e, nb=nb: e.activation(out=rt[:, 0:nb], in_=rt[:, 0:nb], func=AF.Exp, scale=-0.5), [rt], [rt])
                S.emit("dve", lambda e, o=o, t0=t0, nb=nb, sc=sc: e.scalar_tensor_tensor(out=o[:, t0:t0 + nb], in0=y[:, t0:t0 + nb], scalar=sc, in1=rt[:, 0:nb],
                                                                                        op0=ALU.mult, op1=ALU.mult), [y, rt], [o])
        S.dma("sp", dsts[kind][hb * 128:(hb + 1) * 128, lo:TT], o[:, lo:TT], [o], [dsts[kind]])
    A.close()


def phase_gdn(S, cfg, G, I, C):
    H, T, TT, NCTX, HV, R, GW = cfg.H, cfg.T, cfg.TT, cfg.NCTX, cfg.HV, cfg.R, cfg.GW
    NCHK = TT // 128
    NCC = NCTX // 128
    order = [list(range(NCHK)), list(range(NCC - 1, -1, -1)) + list(range(NCHK - 1, NCC - 1, -1))]
    HG = min(4, H)
    identb, identf, onesb, onesf = C["identb"], C["identf"], C["onesb"], C["onesf"]
    A = Arena(S, "gd")
    def indicator(name, sg, strict):
        m = A.sb(name, [128, 128], F32)
        S.emit("pool", lambda e: e.memset(m[:], 1.0), writes=[m])
        S.emit("pool", lambda e: e.affine_select(out=m[:], in_=m[:], pattern=[[sg, 128]], compare_op=ALU.is_ge, fill=0.0,
                                                 base=-strict, channel_multiplier=-sg), [m], [m])
        return m
    VI = [indicator("VIf", 1, 0), indicator("VIb", -1, 0)]
    VS = [indicator("VSf", 1, 1), indicator("VSb", -1, 1)]
    NMI, NMS = [], []
    for d in range(2):
        for (lst, src, nm) in ((NMI, VI[d], f"NMI{d}"), (NMS, VS[d], f"NMS{d}")):
            m = A.sb(nm, [128, 128], BF16)
            S.emit("dve", lambda e, m=m, src=src: e.tensor_scalar(out=m[:], in0=src[:], scalar1=-1.0, scalar2=-NEG, op0=ALU.add, op1=ALU.mult), [src], [m])
            lst.append(m)
    dmask = A.sb("dmask", [128, 128], BF16)
    S.emit("pool", lambda e: e.memset(dmask[:], 1.0), writes=[dmask])
    S.emit("pool", lambda e: e.affine_select(out=dmask[:].rearrange("p (b c) -> p b c", c=32), in_=dmask[:].rearrange("p (b c) -> p b c", c=32),
                                             pattern=[[-32, 4], [0, 32]], compare_op=ALU.is_ge, fill=0.0, base=0, channel_multiplier=1), [dmask], [dmask])
    S.emit("pool", lambda e: e.affine_select(out=dmask[:].rearrange("p (b c) -> p b c", c=32), in_=dmask[:].rearrange("p (b c) -> p b c", c=32),
                                             pattern=[[32, 4], [0, 32]], compare_op=ALU.is_ge, fill=0.0, base=31, channel_multiplier=-1), [dmask], [dmask])
    omask = A.sb("omask", [128, 128], BF16)
    S.emit("dve", lambda e: e.tensor_scalar(out=omask[:], in0=dmask[:], scalar1=-1.0, scalar2=1.0, op0=ALU.mult, op1=ALU.add), [dmask], [omask])
    LC = [VI[0], VI[1]]
    LR = [VS[1], VS[0]]
    W2 = 2 * H
    sh = [128, NCHK, W2]
    bet = A.sb("bet", sh, F32)
    gsp = A.sb("gsp", [128, NCHK, 2, W2], BF16)
    lsp = A.sb("lsp", [128, NCHK, 2, W2], BF16)
    egr = A.sb("egr", sh, F32)
    egt = A.sb("egt", sh, F32)
    nbe = A.sb("nbe", sh, F32)
    A0 = Arena(S, "gd0")
    gg = A0.sb("gg", sh, F32)
    lnb = A0.sb("lnb", sh, F32)
    gres = A0.sb("gres", sh, F32)
    gab = A0.sb("gab", [128, NCHK, 4 * H], F32)
    S.dma("sp", gab[:], G["gab"][:, :].rearrange("(n p) c -> p n c", p=128), [G["gab"]], [gab])
    cst = A0.sb("cst", [128, 2, 2 * H], F32)
    S.dma("sp", cst[:, 0, :], I["gdn_dt_bias"][0].rearrange("d h -> (d h)").partition_broadcast(128), [I["gdn_dt_bias"]], [cst])
    S.dma("sp", cst[:, 1, :], I["gdn_a_log"][0].rearrange("d h -> (d h)").partition_broadcast(128), [I["gdn_a_log"]], [cst])
    nea = A0.sb("nea", [128, 2 * H], F32)
    S.emit("act", lambda e: e.activation(out=nea[:], in_=cst[:, 1, :], func=AF.Exp), [cst], [nea])
    S.emit("dve", lambda e: e.tensor_scalar(out=nea[:], in0=nea[:], scalar1=-1.0, scalar2=None, op0=ALU.mult), [nea], [nea])
    gx = A0.sb("gx", sh, F32); gt1 = A0.sb("gt1", sh, F32); gt2 = A0.sb("gt2", sh, F32)
    bc = lambda t: t.rearrange("p (o c) -> p o c", o=1).to_broadcast(sh)
    S.emit("dve", lambda e: e.tensor_tensor(out=gx[:], in0=gab[:, :, 0:W2], in1=bc(cst[:, 0, :]), op=ALU.add), [gab, cst], [gx])
    S.emit("dve", lambda e: e.tensor_scalar(out=gt1[:], in0=gx[:], scalar1=-1.0, scalar2=None, op0=ALU.mult), [gx], [gt1])
    S.emit("dve", lambda e: e.tensor_tensor(out=gt1[:], in0=gt1[:], in1=gx[:], op=ALU.max), [gt1, gx], [gt1])
    S.emit("act", lambda e: e.activation(out=gt1[:], in_=gt1[:], func=AF.Exp, scale=-1.0), [gt1], [gt1])
    S.emit("act", lambda e: e.activation(out=gt1[:], in_=gt1[:], func=AF.Ln, bias=C["onec"][:, 0:1]), [gt1, C["onec"]], [gt1])
    S.emit("dve", lambda e: e.tensor_scalar(out=gt2[:], in0=gx[:], scalar1=0.0, scalar2=None, op0=ALU.max), [gx], [gt2])
    S.emit("dve", lambda e: e.tensor_tensor(out=gt2[:], in0=gt2[:], in1=gt1[:], op=ALU.add), [gt2, gt1], [gt2])
    S.emit("dve", lambda e: e.tensor_tensor(out=gg[:], in0=gt2[:], in1=bc(nea[:]), op=ALU.mult), [gt2, nea], [gg])
    S.emit("act", lambda e: e.activation(out=bet[:], in_=gab[:, :, W2:2 * W2], func=AF.Sigmoid), [gab], [bet])
    S.emit("act", lambda e: e.activation(out=lnb[:], in_=bet[:], func=AF.Ln), [bet], [lnb])
    for src, dst in ((gg, gsp), (lnb, lsp)):
        S.emit("dve", lambda e, src=src, dst=dst: e.tensor_copy(out=dst[:, :, 0, :], in_=src[:]), [src], [dst])
        S.emit("dve", lambda e, src=src, dst=dst: e.tensor_tensor(out=gres[:], in0=src[:], in1=dst[:, :, 0, :], op=ALU.subtract), [src, dst], [gres])
        S.emit("dve", lambda e, dst=dst: e.tensor_copy(out=dst[:, :, 1, :], in_=gres[:]), [gres, dst], [dst])
    pA = A.ps("pA", [128, 512], F32)
    pg = pA
    for n in range(NCHK):
        def mm(e, n=n):
            e.matmul(pg[:, 0:H], lhsT=LC[0][:], rhs=gg[:, n, 0:H], start=True, stop=True)
            e.matmul(pg[:, H:W2], lhsT=LC[1][:], rhs=gg[:, n, H:W2], start=True, stop=True)
            e.matmul(pg[:, W2:W2 + H], lhsT=LR[0][:], rhs=gg[:, n, 0:H], start=True, stop=True)
            e.matmul(pg[:, W2 + H:2 * W2], lhsT=LR[1][:], rhs=gg[:, n, H:W2], start=True, stop=True)
            return e.matmul(pg[:, 2 * W2:3 * W2], lhsT=onesf[:], rhs=gg[:, n, :], start=True, stop=True)
        S.emit("pe", mm, [LC[0], LC[1], LR[0], LR[1], onesf, gg], [pg])
        S.emit("act", lambda e, n=n: e.activation(out=egr[:, n, :], in_=pg[:, W2:2 * W2], func=AF.Exp), [pg, egr], [egr])
        S.emit("act", lambda e, n=n: e.activation(out=egt[:, n, :], in_=pg[:, 2 * W2:3 * W2], func=AF.Exp), [pg, egt], [egt])
        S.emit("act", lambda e, n=n: e.activation(out=nbe[:, n, :], in_=pg[:, 0:W2], func=AF.Exp), [pg, nbe], [nbe])
    S.emit("dve", lambda e: e.scalar_tensor_tensor(out=nbe[:], in0=nbe[:], scalar=-1.0, in1=bet[:], op0=ALU.mult, op1=ALU.mult), [nbe, bet], [nbe])
    if "dbg_g" in G:
        for i, t in enumerate((gg, gg, egr, egt, nbe, bet)):
            S.dma("sp", G["dbg_g"][i], t[:], [t], [G["dbg_g"]])
    A0.close()
    LCb = []
    for d in range(2):
        m = A.sb(f"LCb{d}", [128, 128], BF16)
        S.emit("dve", lambda e, m=m, d=d: e.tensor_copy(out=m[:], in_=LC[d][:]), [LC[d]], [m])
        LCb.append(m)
    gain = A.sb("gain", [128, H], F32)
    S.dma("sp", gain[:], I["gdn_norm"][0].rearrange("(h p) -> p h", p=128), [I["gdn_norm"]], [gain], allow_slow_non_contiguous=True)

    NU = 2 * HG
    ld = {nm: [[A.sb(f"ld{nm}{d}{i}", [128, HG, 128], BF16) for i in range(3 if nm == "k" else 2)] for d in range(2)] for nm in "kqv"}
    U = []
    for u in range(NU):
        ub = {}
        for nm in ("TT", "PT", "qg", "kd", "vb"):
            ub[nm] = [A.sb(f"u{u}{nm}{i}", [128, 128], BF16) for i in range(2 if nm == "TT" else 3)]
        ub["X"] = [[A.sb(f"u{u}X{j}{i}", [128, 4, 128], BF16) for i in range(2)] for j in range(2)]
        ub["Z0"] = [A.sb(f"u{u}Z0{j}", [128, 128], BF16) for j in range(2)]
        ub["OT"] = [A.sb(f"u{u}OT{j}", [128, 128], BF16) for j in range(2)]
        ub["gm"] = [A.sb(f"u{u}gm{j}", [128, 2, 128], BF16) for j in range(2)]
        ub["eg"] = [A.sb(f"u{u}eg{j}", [128, 128], BF16) for j in range(2)]
        ub["DD"] = A.sb(f"u{u}DD", [128, 2, 128], BF16)
        ub["NN"] = A.sb(f"u{u}NN", [128, 2, 128], BF16)
        ub["N2"] = A.sb(f"u{u}N2", [128, 2, 128], BF16)
        ub["PP"] = A.sb(f"u{u}PP", [128, 2, 128], BF16)
        ub["S"] = A.sb(f"u{u}S", [128, 128], F32)
        ub["Sb"] = A.sb(f"u{u}Sb", [128, 128], BF16)
        ub["r"] = A.sb(f"u{u}r", [128, 128], BF16)
        ub["vn"] = A.sb(f"u{u}vn", [128, 128], BF16)
        U.append(ub)
    ngu = [A.sb(f"ngu{i}", [128, 1], F32) for i in range(2)]
    OB = A.sb("OB", [128, HG, T], F32)
    zq = A.sb("zq", [128, 512], BF16); sqb = A.sb("sqb", [128, 512], BF16); rt = A.sb("rt", [128, 512], F32)
    ys = [sqb, sqb]
    djunk = rt
    pR = A.ps("pR", [128, 512], F32)
    pT = A.ps("pT", [128, 1024], BF16)
    pI = [A.ps(f"pI{i}", [128, 512], F32) for i in range(4)]
    pS = A.ps("pS", [128, 512], F32)
    ncol = 128 // R if R < 128 else 1
    v4 = lambda p: p[:, 0:512].rearrange("p (a b) -> p a b", b=128)

    def run_streams(streams):
        streams = [g for g in streams if g is not None]
        while streams:
            for g in list(streams):
                try:
                    next(g)
                except StopIteration:
                    streams.remove(g)

    for hg in range(H // HG):
        for ub in U:
            S.emit("pool", lambda e, ub=ub: e.memset(ub["S"][:], 0.0), writes=[ub["S"]])
            S.emit("pool", lambda e, ub=ub: e.memset(ub["Sb"][:], 0.0), writes=[ub["Sb"]])
        OBc = {}

        def units_of(s):
            out = []
            for d in range(2):
                c = order[d][s]
                for hl in range(HG):
                    out.append((d, hl, c, c >= NCC))
            return out

        def gen_P(s):
            s3, s2 = s % 3, s % 2
            for d in range(2):
                c = order[d][s]
                t0 = c * 128
                lat = c >= NCC
                for nm, src in (("k", G["gk"]), ("q", G["gq"]), ("v", G["gv"])):
                    if nm == "q" and not lat:
                        continue
                    dst = ld[nm][d][s3 if nm == "k" else s2]
                    S.dma("sp" if d == 0 else "pool", dst[:],
                          src[hg * HG * 128:(hg + 1) * HG * 128, t0:t0 + 128].rearrange("(h p) t -> p h t", p=128), [src], [dst])
            yield
            ii = 0
            for (d, hl, c, lat) in units_of(s):
                u = d * HG + hl
                ub = U[u]
                h = hg * HG + hl
                dh = d * H + h
                kT = ld["k"][d][s3]; qTb = ld["q"][d][s2]; vT = ld["v"][d][s2]
                X0 = ub["X"][s2][0]
                X1 = ub["X"][s2][1]
                Z0 = ub["Z0"][s2]
                OT = ub["OT"][s2]
                def mA(e, kT=kT, qTb=qTb, hl=hl, lat=lat):
                    r = e.matmul(pA[:, 0:128], lhsT=kT[:, hl, :], rhs=kT[:, hl, :], start=True, stop=True)
                    if lat:
                        r = e.matmul(pA[:, 128:256], lhsT=kT[:, hl, :], rhs=qTb[:, hl, :], start=True, stop=True)
                    return r
                S.emit("pe", mA, [kT, qTb], [pA])
                yield
                gm = ub["gm"][s2]
                eg = ub["eg"][s2]
                S.emit("dve", lambda e, Z0=Z0, gm=gm: e.scalar_tensor_tensor(out=Z0[:], in0=pA[:, 0:128], scalar=-1.0, in1=gm[:, 1, :],
                                                                             op0=ALU.mult, op1=ALU.mult), [pA, gm], [Z0])
                yield
                def mT(e, Z0=Z0, kT=kT, vT=vT, hl=hl):
                    e.transpose(out=pT[:, 0:128], in_=Z0[:], identity=identb[:])
                    e.transpose(out=pT[:, 128:256], in_=kT[:, hl, :], identity=identb[:])
                    return e.transpose(out=pT[:, 256:384], in_=vT[:, hl, :], identity=identb[:])
                S.emit("pe", mT, [Z0, kT, vT, identb], [pT])
                if lat:
                    S.emit("dve", lambda e, ub=ub, s3=s3, gm=gm: e.tensor_tensor(out=ub["PT"][s3][:], in0=pA[:, 128:256], in1=gm[:, 0, :], op=ALU.mult),
                           [pA, gm], [ub["PT"][s3]])
                S.emit("pool", lambda e, X0=X0, Z0=Z0: e.tensor_tensor(out=X0[:, 0, :], in0=Z0[:], in1=dmask[:], op=ALU.mult), [Z0, dmask, X0], [X0])
                yield
                if lat:
                    S.emit("dve", lambda e, ub=ub, s3=s3, eg=eg, qTb=qTb, hl=hl: e.tensor_tensor(out=ub["qg"][s3][:], in0=qTb[:, hl, :], in1=eg[:], op=ALU.mult),
                           [qTb, eg], [ub["qg"][s3]])
                S.emit("pool", lambda e, X1=X1, X0=X0: e.tensor_tensor(out=X1[:, 1, :], in0=X0[:, 0, :], in1=identb[:], op=ALU.add), [X0, identb, X1], [X1])
                yield
                S.emit("dve", lambda e, X0=X0: e.tensor_tensor(out=X0[:, 2, :], in0=pT[:, 0:128], in1=dmask[:], op=ALU.mult), [pT, dmask, X0], [X0])
                S.emit("dve", lambda e, OT=OT: e.tensor_tensor(out=OT[:], in0=pT[:, 0:128], in1=omask[:], op=ALU.mult), [pT, omask], [OT])
                S.emit("act", lambda e, ub=ub, s3=s3, c=c, dh=dh: e.activation(out=ub["kd"][s3][:], in_=pT[:, 128:256], func=AF.Copy, scale=egr[:, c, dh:dh + 1]),
                       [pT, egr], [ub["kd"][s3]])
                S.emit("act", lambda e, ub=ub, s3=s3, c=c, dh=dh: e.activation(out=ub["vb"][s3][:], in_=pT[:, 256:384], func=AF.Copy, scale=bet[:, c, dh:dh + 1]),
                       [pT, bet], [ub["vb"][s3]])
                yield
                S.emit("pool", lambda e, X1=X1, X0=X0: e.tensor_tensor(out=X1[:, 3, :], in0=X0[:, 2, :], in1=identb[:], op=ALU.add), [X0, identb, X1], [X1])
                yield

        def gen_G(s):
            s2 = s % 2
            ii = 0
            for (d, hl, c, lat) in units_of(s):
                u = d * HG + hl
                ub = U[u]
                h = hg * HG + hl
                dh = d * H + h
                gm = ub["gm"][s2]
                eg = ub["eg"][s2]
                ng = ngu[ii % 2]
                ii += 1
                def mR(e, c=c, dh=dh, d=d, lat=lat):
                    ghi = gsp[:, c, 0, dh:dh + 1].to_broadcast([128, 128])
                    glo = gsp[:, c, 1, dh:dh + 1].to_broadcast([128, 128])
                    e.matmul(pR[:, 256:384], lhsT=ghi, rhs=LCb[d][:], start=True, stop=False)
                    e.matmul(pR[:, 256:384], lhsT=glo, rhs=LCb[d][:], start=False, stop=True)
                    if lat:
                        e.matmul(pR[:, 0:128], lhsT=ghi, rhs=LCb[d][:], start=True, stop=False)
                        e.matmul(pR[:, 0:128], lhsT=glo, rhs=LCb[d][:], start=False, stop=False)
                        e.matmul(pR[:, 0:128], lhsT=identb[:], rhs=NMI[d][:], start=False, stop=True)
                    e.matmul(pR[:, 128:256], lhsT=ghi, rhs=LCb[d][:], start=True, stop=False)
                    e.matmul(pR[:, 128:256], lhsT=glo, rhs=LCb[d][:], start=False, stop=False)
                    e.matmul(pR[:, 128:256], lhsT=lsp[:, c, 0, dh:dh + 1].to_broadcast([128, 128]), rhs=identb[:], start=False, stop=False)
                    e.matmul(pR[:, 128:256], lhsT=lsp[:, c, 1, dh:dh + 1].to_broadcast([128, 128]), rhs=identb[:], start=False, stop=False)
                    return e.matmul(pR[:, 128:256], lhsT=identb[:], rhs=NMS[d][:], start=False, stop=True)
                S.emit("pe", mR, [gsp, lsp, LCb[d], identb, NMI[d], NMS[d]], [pR])
                yield
                S.emit("dve", lambda e: e.tensor_tensor(out=djunk[:, 0:128], in0=pR[:, 256:384], in1=identf[:], op=ALU.mult), [pR, identf], [djunk])
                yield
                S.emit("dve", lambda e, ng=ng: e.tensor_reduce(out=ng[:, 0:1], in_=djunk[:, 0:128], axis=mybir.AxisListType.X, op=ALU.add, negate=True), [djunk], [ng])
                yield
                lo = 0 if lat else 1
                S.emit("act", lambda e, gm=gm, ng=ng, lo=lo: e.activation(out=gm[:, lo:2, :], in_=pR[:, lo * 128:256].rearrange("p (a b) -> p a b", b=128),
                                                                          func=AF.Exp, bias=ng[:, 0:1]), [pR, ng], [gm])
                if lat:
                    S.emit("act", lambda e, eg=eg: e.activation(out=eg[:], in_=pR[:, 256:384], func=AF.Exp), [pR], [eg])
                yield

        def gen_I(s, bank):
            s3, s2 = s % 3, s % 2
            p = pI[bank]
            for (d, hl, c, lat) in units_of(s):
                u = d * HG + hl
                if u % 4 != bank:
                    continue
                ub = U[u]
                X = ub["X"][s2]
                DD, NN, N2, PP, OT = ub["DD"], ub["NN"], ub["N2"], ub["PP"], ub["OT"][s2]
                for st in range(9):
                    if st == 0:
                        Xc, Xn = X[0], X[1]
                        def m0(e, Xc=Xc):
                            e.matmul(p[:, 0:128], lhsT=Xc[:, 2, :], rhs=Xc[:, 0, :], start=True, stop=True)
                            return e.matmul(p[:, 256:384], lhsT=Xc[:, 0, :], rhs=Xc[:, 2, :], start=True, stop=True)
                        S.emit("pe", m0, [Xc], [p])
                        yield
                        S.emit("act", lambda e, Xn=Xn: e.copy(out=Xn[:, 0:4:2, :], in_=v4(p)[:, 0:4:2, :]), [p, Xn], [Xn])
                    elif st < 4:
                        Xc, Xn = X[st % 2], X[(st + 1) % 2]
                        def m1(e, Xc=Xc):
                            e.matmul(p[:, 0:256], lhsT=Xc[:, 2, :], rhs=Xc[:, 0:2, :], start=True, stop=True)
                            return e.matmul(p[:, 256:512], lhsT=Xc[:, 0, :], rhs=Xc[:, 2:4, :], start=True, stop=True)
                        S.emit("pe", m1, [Xc], [p])
                        yield
                        S.emit("act", lambda e, Xn=Xn: e.copy(out=Xn[:, 0:4:2, :], in_=v4(p)[:, 0:4:2, :]), [p, Xn], [Xn])
                        yield
                        S.emit("dve", lambda e, Xn=Xn, Xc=Xc: e.tensor_tensor(out=Xn[:, 1:4:2, :], in0=v4(p)[:, 1:4:2, :], in1=Xc[:, 1:4:2, :], op=ALU.add),
                               [p, Xc, Xn], [Xn])
                    elif st == 4:
                        Xc = X[0]
                        def m4(e, Xc=Xc):
                            e.matmul(p[:, 128:256], lhsT=Xc[:, 2, :], rhs=Xc[:, 1, :], start=True, stop=True)
                            return e.matmul(p[:, 384:512], lhsT=Xc[:, 0, :], rhs=Xc[:, 3, :], start=True, stop=True)
                        S.emit("pe", m4, [Xc], [p])
                        yield
                        S.emit("dve", lambda e, DD=DD, Xc=Xc: e.tensor_tensor(out=DD[:], in0=v4(p)[:, 1:4:2, :], in1=Xc[:, 1:4:2, :], op=ALU.add), [p, Xc], [DD])
                    elif st == 5:
                        def m5(e, DD=DD, OT=OT):
                            e.matmul(p[:, 0:128], lhsT=OT[:], rhs=DD[:, 0, :], start=True, stop=True)
                            return e.matmul(p[:, 128:256], lhsT=DD[:, 0, :], rhs=OT[:], start=True, stop=True)
                        S.emit("pe", m5, [DD, OT], [p])
                        yield
                        S.emit("act", lambda e, NN=NN: e.copy(out=NN[:], in_=v4(p)[:, 0:2, :]), [p], [NN])
                        yield
                        S.emit("dve", lambda e, PP=PP: e.tensor_tensor(out=PP[:, 0, :], in0=p[:, 0:128], in1=identb[:], op=ALU.add), [p, identb, PP], [PP])
                    elif st == 6:
                        def m6(e, NN=NN):
                            e.matmul(p[:, 0:128], lhsT=NN[:, 1, :], rhs=NN[:, 0, :], start=True, stop=True)
                            return e.matmul(p[:, 128:256], lhsT=NN[:, 0, :], rhs=NN[:, 1, :], start=True, stop=True)
                        S.emit("pe", m6, [NN], [p])
                        yield
                        S.emit("act", lambda e, N2=N2: e.copy(out=N2[:], in_=v4(p)[:, 0:2, :]), [p], [N2])
                    elif st == 7:
                        S.emit("pe", lambda e, N2=N2, PP=PP: e.matmul(p[:, 0:128], lhsT=N2[:, 1, :], rhs=PP[:, 0, :], start=True, stop=True), [N2, PP], [p])
                        yield
                        S.emit("dve", lambda e, PP=PP: e.tensor_tensor(out=PP[:, 1, :], in0=p[:, 0:128], in1=PP[:, 0, :], op=ALU.add), [p, PP], [PP])
                    else:
                        S.emit("pe", lambda e, DD=DD, PP=PP: e.matmul(p[:, 0:128], lhsT=DD[:, 1, :], rhs=PP[:, 1, :], start=True, stop=True), [DD, PP], [p])
                        yield
                        S.emit("act", lambda e, ub=ub, s3=s3: e.copy(out=ub["TT"][s2][:], in_=p[:, 0:128]), [p], [ub["TT"][s2]])
                    yield

        def gen_S(s):
            s3 = s % 3
            for (d, hl, c, lat) in units_of(s):
                u = d * HG + hl
                ub = U[u]
                h = hg * HG + hl
                dh = d * H + h
                kT = ld["k"][d][s3]
                S.emit("pe", lambda e, kT=kT, hl=hl, ub=ub: e.matmul(pS[:, 0:128], lhsT=kT[:, hl, :], rhs=ub["Sb"][:], start=True, stop=True), [kT, ub["Sb"]], [pS])
                yield
                S.emit("dve", lambda e, ub=ub, s3=s3, c=c, dh=dh: e.scalar_tensor_tensor(out=ub["r"][:], in0=pS[:, 0:128], scalar=nbe[:, c, dh:dh + 1], in1=ub["vb"][s3][:],
                                                                                        op0=ALU.mult, op1=ALU.add), [pS, nbe, ub["vb"][s3]], [ub["r"]])
                yield
                S.emit("pe", lambda e, ub=ub, s=s: e.matmul(pS[:, 128:256], lhsT=ub["TT"][s % 2][:], rhs=ub["r"][:], start=True, stop=True), [ub["TT"][s % 2], ub["r"]], [pS])
                yield
                S.emit("act", lambda e, ub=ub: e.copy(out=ub["vn"][:], in_=pS[:, 128:256]), [pS], [ub["vn"]])
                yield
                def mo(e, ub=ub, s3=s3, lat=lat):
                    if lat:
                        e.matmul(pS[:, 384:512], lhsT=ub["Sb"][:], rhs=ub["qg"][s3][:], start=True, stop=False)
                        e.matmul(pS[:, 384:512], lhsT=ub["vn"][:], rhs=ub["PT"][s3][:], start=False, stop=True)
                    return e.matmul(pS[:, 256:384], lhsT=ub["kd"][s3][:], rhs=ub["vn"][:], start=True, stop=True)
                S.emit("pe", mo, [ub["Sb"], ub["qg"][s3], ub["vn"], ub["PT"][s3], ub["kd"][s3]], [pS])
                yield
                if lat:
                    cl = c - NCC
                    if R >= 128:
                        per = R // 128
                        cc0, sub = divmod(cl, per)
                        oap = OB[:, hl, :].rearrange("p (r c) -> p c r", c=GW)[:, cc0, sub * 128:(sub + 1) * 128]
                        iap = pS[:, 384:512]
                    else:
                        oap = OB[:, hl, :].rearrange("p (r c) -> p c r", c=GW)[:, cl * ncol:(cl + 1) * ncol, :]
                        iap = pS[:, 384:512].rearrange("p (a b) -> p a b", b=R)
                    key = (hl, cl)
                    if key not in OBc:
                        OBc[key] = Buf(f"OBc{hl}_{cl}")
                        OBc[key].ws = dict(OB.ws)
                        OBc[key].rs = dict(OB.rs)
                        S.emit("act", lambda e, oap=oap, iap=iap: e.copy(out=oap, in_=iap), [pS], [OBc[key]])
                    else:
                        S.emit("dve", lambda e, oap=oap, iap=iap: e.tensor_tensor(out=oap, in0=iap, in1=oap, op=ALU.add), [pS, OBc[key]], [OBc[key]])
                S.emit("dve", lambda e, ub=ub, c=c, dh=dh: e.scalar_tensor_tensor(out=ub["S"][:], in0=ub["S"][:], scalar=egt[:, c, dh:dh + 1], in1=pS[:, 256:384],
                                                                                 op0=ALU.mult, op1=ALU.add), [ub["S"], egt, pS], [ub["S"]])
                yield
                S.emit("act", lambda e, ub=ub: e.copy(out=ub["Sb"][:], in_=ub["S"][:]), [ub["S"]], [ub["Sb"]])
                yield

        for it in range(-3, NCHK):
            sts = []
            if 0 <= it + 3 < NCHK:
                sts.append(gen_G(it + 3))
            if 0 <= it + 2 < NCHK:
                sts.append(gen_P(it + 2))
            if 0 <= it + 1 < NCHK:
                sts += [gen_I(it + 1, b) for b in range(4)]
            if it >= 0:
                sts.append(gen_S(it))
            run_streams(sts)
            if "dbg_S" in G and it == NCC - 1 and hg == 0:
                for u in range(NU):
                    S.dma("sp", G["dbg_S"][u], U[u]["S"][:], [U[u]["S"]], [G["dbg_S"]])
        for key, bk in OBc.items():
            for k_, v_ in bk.ws.items():
                if OB.ws.get(k_, 0) < v_:
                    OB.ws[k_] = v_
        for hl in range(HG):
            h = hg * HG + hl
            if "dbg_ob" in G:
                S.dma("sp", G["dbg_ob"][h * 128:(h + 1) * 128, :], OB[:, hl, :], [OB], [G["dbg_ob"]])
            for bi, t0 in enumerate(range(0, T, 512)):
                nb = min(512, T - t0)
                S.dma("sp", zq[:, 0:nb], G["gzT"][h * 128:(h + 1) * 128, t0:t0 + nb], [G["gzT"]], [zq])
                S.emit("act", lambda e, hl=hl, t0=t0, nb=nb: e.activation(out=sqb[:, 0:nb], in_=OB[:, hl, t0:t0 + nb], func=AF.Square), [OB], [sqb])
                S.emit("pe", lambda e, nb=nb: e.matmul(pA[:, 0:nb], lhsT=onesb[:], rhs=sqb[:, 0:nb], start=True, stop=True), [onesb, sqb], [pA])
                S.emit("act", lambda e, nb=nb: e.activation(out=rt[:, 0:nb], in_=pA[:, 0:nb], func=AF.Ln, scale=1.0 / 128, bias=C["epsc"][:, 0:1]), [pA, C["epsc"]], [rt])
                S.emit("act", lambda e, nb=nb: e.activation(out=rt[:, 0:nb], in_=rt[:, 0:nb], func=AF.Exp, scale=-0.5), [rt], [rt])
                S.emit("dve", lambda e, nb=nb: e.tensor_tensor(out=rt[:, 0:nb], in0=rt[:, 0:nb], in1=zq[:, 0:nb], op=ALU.mult), [rt, zq], [rt])
                y = ys[bi % 2]
                S.emit("dve", lambda e, hl=hl, h=h, t0=t0, nb=nb, y=y: e.scalar_tensor_tensor(out=y[:, 0:nb], in0=OB[:, hl, t0:t0 + nb], scalar=gain[:, h:h + 1], in1=rt[:, 0:nb],
                                                                                             op0=ALU.mult, op1=ALU.mult), [OB, gain, rt], [y])
                S.dma("sp", G["yBT"][h * 128:(h + 1) * 128, t0:t0 + nb], y[:, 0:nb], [y], [G["yBT"]])
    A.close()


def phase_merge(S, cfg, G, I, C, cfgL):
    A = Arena(S, "mg")
    D, T, HV, KD = cfg.D, cfg.T, cfg.HV, cfg.KD
    KH = HV // 128
    HL = cfgL.H
    NRK = cfg.H // HL
    wa = A.sb("wa", [128, KH, D], BF16)
    wb = A.sb("wb", [128, KH, D], BF16)
    for k in range(KH):
        S.dma("pool", wa[:, k, :], I["w_branch_a"][0, k * 128:(k + 1) * 128, :], [I["w_branch_a"]], [wa])
        S.dma("pool", wb[:, k, :], I["w_branch_b"][0, k * 128:(k + 1) * 128, :], [I["w_branch_b"]], [wb])
    ya = [A.sb(f"ya{i}", [128, KH, 512], BF16) for i in range(2)]
    yb = [A.sb(f"yb{i}", [128, KH, 512], BF16) for i in range(2)]
    ga = [A.sb(f"ga{i}", [128, 512], BF16) for i in range(2)]
    gb = [A.sb(f"gb{i}", [128, 512], BF16) for i in range(2)]
    t1 = [A.sb(f"t1{i}", [128, 512], F32) for i in range(2)]
    t2 = [A.sb(f"t2{i}", [128, 512], F32) for i in range(2)]
    mo = [A.sb(f"mo{i}", [128, 512], BF16) for i in range(2)]
    pa = [A.ps(f"pa{i}", [128, 512], F32) for i in range(2)]
    pb = [A.ps(f"pb{i}", [128, 512], F32) for i in range(2)]
    it = 0
    for bi, t0 in enumerate(range(0, T, 512)):
        nb = min(512, T - t0)
        a_, b_ = ya[bi % 2], yb[bi % 2]
        CR = min(256, 2 * HL * 128)
        for r in range(NRK):
            for l in range(HL):
                for (dst, rho) in ((a_, l * 128), (b_, HL * 128 + l * 128)):
                    ck, off = divmod(rho, CR)
                    S.dma("sp", dst[:, r * HL + l, 0:nb], G["yall"][ck, r * CR + off:r * CR + off + 128, t0:t0 + nb], [G["yall"]], [dst])
        for dc in range(KD):
            i2 = it % 2
            it += 1
            S.dma("sp", ga[i2][:, 0:nb], G["mgT"][dc * 128:(dc + 1) * 128, t0:t0 + nb], [G["mgT"]], [ga[i2]])
            S.dma("sp", gb[i2][:, 0:nb], G["mgT"][D + dc * 128:D + (dc + 1) * 128, t0:t0 + nb], [G["mgT"]], [gb[i2]])
            def mm(e, w, y, p, dc=dc, nb=nb):
                r = None
                for k in range(KH):
                    r = e.matmul(p[:, 0:nb], lhsT=w[:, k, dc * 128:(dc + 1) * 128], rhs=y[:, k, 0:nb], start=(k == 0), stop=(k == KH - 1))
                return r
            S.emit("pe", lambda e, i2=i2, a_=a_, mm=mm: mm(e, wa, a_, pa[i2]), [wa, a_], [pa[i2]])
            S.emit("pe", lambda e, i2=i2, b_=b_, mm=mm: mm(e, wb, b_, pb[i2]), [wb, b_], [pb[i2]])
            S.emit("dve", lambda e, i2=i2, nb=nb: e.tensor_tensor(out=t1[i2][:, 0:nb], in0=pa[i2][:, 0:nb], in1=ga[i2][:, 0:nb], op=ALU.mult), [pa[i2], ga[i2]], [t1[i2]])
            S.emit("dve", lambda e, i2=i2, nb=nb: e.tensor_tensor(out=t2[i2][:, 0:nb], in0=pb[i2][:, 0:nb], in1=gb[i2][:, 0:nb], op=ALU.mult), [pb[i2], gb[i2]], [t2[i2]])
            S.emit("pool", lambda e, i2=i2, nb=nb: e.tensor_tensor(out=mo[i2][:, 0:nb], in0=t1[i2][:, 0:nb], in1=t2[i2][:, 0:nb], op=ALU.add), [t1[i2], t2[i2]], [mo[i2]])
            S.dma("sp", G["mT"][dc * 128:(dc + 1) * 128, t0:t0 + nb], mo[i2][:, 0:nb], [mo[i2]], [G["mT"]])
    A.close()


def load_row_bcast(S, A, name, src_ap, srcbuf, n):
    t = A.sb(name, [128, n], F32)
    S.dma("sp", t[:], src_ap.partition_broadcast(128), [srcbuf], [t])
    return t


def phase_outproj(S, cfg, G, I, C):
    A = Arena(S, "op")
    D, T, KD = cfg.D, cfg.T, cfg.KD
    wo = A.sb("wo", [128, KD, D], BF16)
    for k in range(KD):
        S.dma("pool", wo[:, k, :], I["w_out"][0, k * 128:(k + 1) * 128, :], [I["w_out"]], [wo])
    g1 = load_row_bcast(S, A, "g1", G["grow"][0], G["grow"], D)
    mt = [A.sb(f"mt{i}", [128, KD, 128], BF16) for i in range(2)]
    xt = [A.sb(f"xt{i}", [128, D], F32) for i in range(2)]
    tm = [A.sb(f"tm{i}", [128, 512], F32) for i in range(2)]
    pp = [A.ps(f"pp{i}", [128, 512], F32) for i in range(4)]
    pi = 0
    for ti in range(T // 128):
        t0 = ti * 128
        m_, x_ = mt[ti % 2], xt[ti % 2]
        S.dma("sp", m_[:], G["mT"][:, t0:t0 + 128].rearrange("(k p) t -> p k t", p=128), [G["mT"]], [m_])
        S.dma("pool", x_[:], I["x"][0, t0:t0 + 128, :], [I["x"]], [x_])
        for oc in range(0, D, 512):
            ow = min(512, D - oc)
            p = pp[pi % 4]
            t_ = tm[pi % 2]
            pi += 1
            def mm(e, m_=m_, p=p, oc=oc, ow=ow):
                r = None
                for k in range(KD):
                    r = e.matmul(p[:, 0:ow], lhsT=m_[:, k, :], rhs=wo[:, k, oc:oc + ow], start=(k == 0), stop=(k == KD - 1))
                return r
            S.emit("pe", mm, [m_, wo], [p])
            S.emit("dve", lambda e, p=p, t_=t_, oc=oc, ow=ow: e.tensor_tensor(out=t_[:, 0:ow], in0=p[:, 0:ow], in1=g1[:, oc:oc + ow], op=ALU.mult), [p, g1], [t_])
            S.emit("pool", lambda e, x_=x_, t_=t_, oc=oc, ow=ow: e.tensor_tensor(out=x_[:, oc:oc + ow], in0=x_[:, oc:oc + ow], in1=t_[:, 0:ow], op=ALU.add), [x_, t_], [x_])
        S.dma("sp", G["x2"][t0:t0 + 128, :], x_[:], [x_], [G["x2"]])
    A.close()


def phase_route(S, cfg, G, I, C, R_):
    A = Arena(S, "rt")
    D, T, KD, E, CAP = cfg.D, cfg.T, cfg.KD, cfg.E, cfg.CAP
    NSG = CAP // 128
    identf = C["identf"]
    co = G["coef"]
    for j, ci in enumerate((4, 5)):
        S.dma("sp", G["crow"][j].rearrange("(k p) -> p k", p=128), co[:, ci, :], [co], [G["crow"]], allow_slow_non_contiguous=True)
    a2 = load_row_bcast(S, A, "a2", G["crow"][0], G["crow"], D)
    b2 = load_row_bcast(S, A, "b2", G["crow"][1], G["crow"], D)
    rw = A.sb("rw", [128, KD, E], F32)
    S.dma("sp", rw[:], I["router_w"][0].rearrange("(k p) e -> p k e", p=128), [I["router_w"]], [rw])
    affE = [A.sb(f"affE{i}", [E, T], F32) for i in range(2)]
    xt = [A.sb(f"xt{i}", [128, D], F32) for i in range(2)]
    hf = [A.sb(f"hf{i}", [128, D], F32) for i in range(2)]
    hb = [A.sb(f"hb{i}", [128, D], BF16) for i in range(2)]
    hT = [A.sb(f"hT{i}", [128, KD, 128], F32) for i in range(2)]
    junk = A.sb("junk", [128, D], BF16)
    st = [A.sb(f"st{i}", [128, 8], F32) for i in range(2)]
    sm = [A.sb(f"sm{i}", [128, E + 8], F32) for i in range(2)]
    pt = [A.ps(f"pt{i}", [128, 4, 128], F32) for i in range(2)]
    pl = A.ps("pl", [128, 512], F32)
    pq = A.ps("pq", [128, 512], F32)
    gi = 0
    for ti in range(T // 128):
        t0 = ti * 128
        x = xt[ti % 2]; s = st[ti % 2]; h = hf[ti % 2]; hbb = hb[ti % 2]; hTt = hT[ti % 2]; m = sm[ti % 2]
        S.dma("sp" if ti % 2 == 0 else "pool", x[:], G["x2"][t0:t0 + 128, :], [G["x2"]], [x])
        S.emit("act", lambda e, x=x, s=s: e.activation(out=junk[:], in_=x[:], func=AF.Square, accum_out=s[:, 0:1]), [x], [junk, s])
        S.emit("dve", lambda e, s=s: e.tensor_scalar(out=s[:, 1:2], in0=s[:, 0:1], scalar1=1.0 / D, scalar2=cfg.eps, op0=ALU.mult, op1=ALU.add), [s], [s])
        S.emit("act", lambda e, s=s: e.activation(out=s[:, 2:3], in_=s[:, 1:2], func=AF.Sqrt), [s], [s])
        S.emit("dve", lambda e, s=s: e.reciprocal(out=s[:, 3:4], in_=s[:, 2:3]), [s], [s])
        S.emit("dve", lambda e, x=x, s=s, h=h: e.scalar_tensor_tensor(out=h[:], in0=x[:], scalar=s[:, 3:4], in1=a2[:], op0=ALU.mult, op1=ALU.mult), [x, s, a2], [h])
        S.emit("pool", lambda e, h=h: e.tensor_tensor(out=h[:], in0=h[:], in1=b2[:], op=ALU.add), [h, b2], [h])
        S.emit("act", lambda e, h=h, hbb=hbb: e.copy(out=hbb[:], in_=h[:]), [h], [hbb])
        S.dma("sp", G["h2"][t0:t0 + 128, :], hbb[:], [hbb], [G["h2"]])
        for g in range(0, KD, 4):
            p = pt[gi % 2]
            gi += 1
            ng = min(4, KD - g)
            def tr(e, h=h, p=p, g=g, ng=ng):
                r = None
                for j in range(ng):
                    r = e.transpose(out=p[:, j, :], in_=h[:, (g + j) * 128:(g + j + 1) * 128], identity=identf[:])
                return r
            S.emit("pe", tr, [h, identf], [p])
            if (gi % 2) == 0:
                S.emit("act", lambda e, p=p, hTt=hTt, g=g, ng=ng: e.copy(out=hTt[:, g:g + ng, :], in_=p[:, 0:ng, :]), [p, hTt], [hTt])
            else:
                S.emit("dve", lambda e, p=p, hTt=hTt, g=g, ng=ng: e.tensor_copy(out=hTt[:, g:g + ng, :], in_=p[:, 0:ng, :]), [p, hTt], [hTt])
        def ml(e, hTt=hTt):
            r = None
            for k in range(KD):
                r = e.matmul(pl[:, 0:E], lhsT=hTt[:, k, :], rhs=rw[:, k, :], start=(k == 0), stop=(k == KD - 1))
            return r
        S.emit("pe", ml, [hTt, rw], [pl])
        S.emit("dve", lambda e, m=m: e.reduce_max(out=m[:, E:E + 1], in_=pl[:, 0:E], axis=mybir.AxisListType.X), [pl], [m])
        S.emit("dve", lambda e, m=m: e.tensor_scalar(out=m[:, E + 1:E + 2], in0=m[:, E:E + 1], scalar1=-1.0, scalar2=None, op0=ALU.mult), [m], [m])
        S.emit("act", lambda e, m=m: e.activation(out=m[:, 0:E], in_=pl[:, 0:E], func=AF.Exp, bias=m[:, E + 1:E + 2], accum_out=m[:, E + 2:E + 3]), [pl, m], [m])
        S.emit("dve", lambda e, m=m: e.reciprocal(out=m[:, E + 3:E + 4], in_=m[:, E + 2:E + 3]), [m], [m])
        S.emit("dve", lambda e, m=m: e.tensor_scalar(out=m[:, 0:E], in0=m[:, 0:E], scalar1=m[:, E + 3:E + 4], scalar2=None, op0=ALU.mult), [m], [m])
        S.emit("pe", lambda e, m=m: e.transpose(out=pq[0:E, 0:128], in_=m[:, 0:E], identity=identf[:]), [m, identf], [pq])
        S.emit("act", lambda e, t0=t0: e.copy(out=affE[0][:, t0:t0 + 128], in_=pq[0:E, 0:128]), [pq, affE[0]], [affE[0]])
    vals = A.sb("vals", [E, CAP], F32)
    idxu = A.sb("idxu", [E, CAP], U32)
    idxf = A.sb("idxf", [E, CAP], F32)
    cur = 0
    for it in range(CAP // 8):
        a = affE[cur]; b = affE[1 - cur]
        S.emit("dve", lambda e, a=a, it=it: e.max(out=vals[:, it * 8:(it + 1) * 8], in_=a[:]), [a, vals], [vals])
        S.emit("dve", lambda e, a=a, it=it: e.max_index(out=idxu[:, it * 8:(it + 1) * 8], in_max=vals[:, it * 8:(it + 1) * 8], in_values=a[:]), [a, vals, idxu], [idxu])
        if it + 1 < CAP // 8:
            S.emit("dve", lambda e, a=a, b=b, it=it: e.match_replace(out=b[:], in_to_replace=vals[:, it * 8:(it + 1) * 8], in_values=a[:], imm_value=-1.0), [a, vals], [b])
            cur = 1 - cur
    S.emit("dve", lambda e: e.tensor_copy(out=idxf[:], in_=idxu[:]), [idxu], [idxf])
    idxT, gateT = R_["idxT"], R_["gateT"]
    for sg in range(NSG):
        S.emit("pe", lambda e, sg=sg: e.transpose(out=pq[:, 0:E], in_=idxf[:, sg * 128:(sg + 1) * 128], identity=identf[0:E, 0:E]), [idxf, identf], [pq])
        S.emit("dve", lambda e, sg=sg: e.tensor_copy(out=idxT[:, sg, :], in_=pq[:, 0:E]), [pq, idxT], [idxT])
        S.emit("pe", lambda e, sg=sg: e.transpose(out=pq[:, 0:E], in_=vals[:, sg * 128:(sg + 1) * 128], identity=identf[0:E, 0:E]), [vals, identf], [pq])
        S.emit("act", lambda e, sg=sg: e.copy(out=gateT[:, sg, :], in_=pq[:, 0:E]), [pq, gateT], [gateT])
    A.close()


def phase_experts(S, cfg, G, I, C, R_):
    A = Arena(S, "ex")
    D, T, KD, E, CAP, DE = cfg.D, cfg.T, cfg.KD, cfg.E, cfg.CAP, cfg.DE
    NSG = CAP // 128
    FH = min(getattr(cfg, "FH", 512), DE)
    NFH = DE // FH
    FC = FH // 128
    identb = C["identb"]
    idxT, gateT = R_["idxT"], R_["gateT"]
    g2 = load_row_bcast(S, A, "g2", G["grow"][1], G["grow"], D)
    wg = [A.sb(f"wg{i}", [128, KD, FH], BF16) for i in range(2)]
    wu = [A.sb(f"wu{i}", [128, KD, FH], BF16) for i in range(2)]
    wd = [A.sb(f"wd{i}", [128, FC, D], BF16) for i in range(2)]
    xs = A.sb("xs", [128, NSG, D], BF16)
    xsT = A.sb("xsT", [128, KD, CAP], BF16)
    sa = A.sb("sa", [128, 512], F32)
    sa2 = A.sb("sa2", [128, 512], F32)
    hd = A.sb("hd", [128, FC, CAP], BF16)
    ysb = A.sb("ysb", [128, NSG, D], F32)
    ptr = [A.ps(f"ptr{i}", [128, 8, 128], BF16) for i in range(2)]
    pg = [A.ps(f"pg{i}", [128, 512], F32) for i in range(2)]
    pu = [A.ps(f"pu{i}", [128, 512], F32) for i in range(2)]
    py = [A.ps(f"py{i}", [128, 512], F32) for i in range(2)]
    gi = 0
    yi = 0
    halves = [(ex, fh) for ex in range(E) for fh in range(NFH)]

    def load_weights(k):
        ex, fh = halves[k]
        w_g, w_u, w_d = wg[k % 2], wu[k % 2], wd[k % 2]
        f0 = fh * FH
        S.dma("pool", w_g[:], I["w_gate"][0, ex, :, f0:f0 + FH].rearrange("(k p) f -> p k f", p=128), [I["w_gate"]], [w_g])
        S.dma("pool", w_u[:], I["w_up"][0, ex, :, f0:f0 + FH].rearrange("(k p) f -> p k f", p=128), [I["w_up"]], [w_u])
        S.dma("pool", w_d[:], I["w_down"][0, ex, f0:f0 + FH, :].rearrange("(k p) d -> p k d", p=128), [I["w_down"]], [w_d])

    def gather(ex):
        for sg in range(NSG):
            S.emit("pool", lambda e, sg=sg, ex=ex: e.indirect_dma_start(out=xs[:, sg, :], out_offset=None, in_=G["h2"][:, :],
                                                                        in_offset=bass.IndirectOffsetOnAxis(ap=idxT[:, sg, ex:ex + 1], axis=0)),
                   [idxT, G["h2"]], [xs], dma=xs)

    load_weights(0)
    gather(0)
    for k, (ex, fh) in enumerate(halves):
        w_g, w_u, w_d = wg[k % 2], wu[k % 2], wd[k % 2]
        if k + 1 < len(halves):
            load_weights(k + 1)
        if fh == 0:
            ti = 0
            for sg in range(NSG):
                for g in range(0, KD, 8):
                    p = ptr[ti % 2]
                    ng = min(8, KD - g)
                    def tr(e, p=p, sg=sg, g=g, ng=ng):
                        r = None
                        for j in range(ng):
                            r = e.transpose(out=p[:, j, :], in_=xs[:, sg, (g + j) * 128:(g + j + 1) * 128], identity=identb[:])
                        return r
                    S.emit("pe", tr, [xs, identb], [p])
                    if ti % 2 == 0:
                        S.emit("act", lambda e, p=p, sg=sg, g=g, ng=ng: e.copy(out=xsT[:, g:g + ng, sg * 128:(sg + 1) * 128], in_=p[:, 0:ng, :]), [p, xsT], [xsT])
                    else:
                        S.emit("dve", lambda e, p=p, sg=sg, g=g, ng=ng: e.tensor_copy(out=xsT[:, g:g + ng, sg * 128:(sg + 1) * 128], in_=p[:, 0:ng, :]), [p, xsT], [xsT])
                    ti += 1
            if ex + 1 < E:
                gather(ex + 1)
        for fc in range(FC):
            p_g, p_u = pg[gi % 2], pu[gi % 2]
            gi += 1
            for (w_, p_) in ((w_g, p_g), (w_u, p_u)):
                def mm(e, w_=w_, p_=p_, fc=fc):
                    r = None
                    for kk in range(KD):
                        r = e.matmul(p_[:, 0:CAP], lhsT=w_[:, kk, fc * 128:(fc + 1) * 128], rhs=xsT[:, kk, :], start=(kk == 0), stop=(kk == KD - 1))
                    return r
                S.emit("pe", mm, [w_, xsT], [p_])
            S.emit("act", lambda e, p_g=p_g: e.activation(out=sa[:, 0:CAP], in_=p_g[:, 0:CAP], func=AF.Silu), [p_g], [sa])
            S.emit("dve", lambda e, p_u=p_u, fc=fc: e.tensor_tensor(out=hd[:, fc, :], in0=p_u[:, 0:CAP], in1=sa[:, 0:CAP], op=ALU.mult), [p_u, sa, hd], [hd])
        for sg in range(NSG):
            for oc in range(0, D, 512):
                ow = min(512, D - oc)
                p_y = py[yi % 2]
                yi += 1
                def my(e, p_y=p_y, sg=sg, oc=oc, ow=ow, w_d=w_d):
                    r = None
                    for fc in range(FC):
                        r = e.matmul(p_y[:, 0:ow], lhsT=hd[:, fc, sg * 128:(sg + 1) * 128], rhs=w_d[:, fc, oc:oc + ow], start=(fc == 0), stop=(fc == FC - 1))
                    return r
                S.emit("pe", my, [hd, w_d], [p_y])
                if fh == 0:
                    S.emit("dve", lambda e, p_y=p_y, sg=sg, oc=oc, ow=ow, ex=ex: e.scalar_tensor_tensor(
                        out=ysb[:, sg, oc:oc + ow], in0=p_y[:, 0:ow], scalar=gateT[:, sg, ex:ex + 1], in1=g2[:, oc:oc + ow], op0=ALU.mult, op1=ALU.mult),
                        [p_y, gateT, g2, ysb], [ysb])
                else:
                    S.emit("act", lambda e, p_y=p_y, sg=sg, ow=ow, ex=ex: e.activation(out=sa2[:, 0:ow], in_=p_y[:, 0:ow], func=AF.Copy, scale=gateT[:, sg, ex:ex + 1]),
                           [p_y, gateT], [sa2])
                    S.emit("dve", lambda e, oc=oc, ow=ow: e.tensor_tensor(out=sa2[:, 0:ow], in0=sa2[:, 0:ow], in1=g2[:, oc:oc + ow], op=ALU.mult), [sa2, g2], [sa2])
                    S.emit("dve", lambda e, sg=sg, oc=oc, ow=ow: e.tensor_tensor(out=ysb[:, sg, oc:oc + ow], in0=ysb[:, sg, oc:oc + ow], in1=sa2[:, 0:ow], op=ALU.add),
                           [ysb, sa2], [ysb])
        if fh == NFH - 1:
            for sg in range(NSG):
                S.emit("pool", lambda e, sg=sg, ex=ex: e.indirect_dma_start(out=G["x2"][:, :], out_offset=bass.IndirectOffsetOnAxis(ap=idxT[:, sg, ex:ex + 1], axis=0),
                                                                            in_=ysb[:, sg, :], in_offset=None, compute_op=ALU.add),
                       [idxT, ysb, G["x2s"]], [G["x2s"]], dma=ysb)
    A.close()


def phase_final(S, cfg, G, I, C, out):
    A = Arena(S, "fn")
    D, T = cfg.D, cfg.T
    fn = load_row_bcast(S, A, "fnw", I["final_norm"][:], I["final_norm"], D)
    xt = [A.sb(f"xt{i}", [128, D], F32) for i in range(2)]
    st = [A.sb(f"st{i}", [128, 8], F32) for i in range(2)]
    junk = A.sb("junk", [128, D], BF16)
    for ti in range(T // 128):
        t0 = ti * 128
        x = xt[ti % 2]; s = st[ti % 2]
        S.dma("sp" if ti % 2 == 0 else "pool", x[:], G["x2"][t0:t0 + 128, :], [G["x2"], G["x2s"]], [x])
        S.emit("act", lambda e, x=x, s=s: e.activation(out=junk[:], in_=x[:], func=AF.Square, accum_out=s[:, 0:1]), [x], [junk, s])
        S.emit("dve", lambda e, s=s: e.tensor_scalar(out=s[:, 1:2], in0=s[:, 0:1], scalar1=1.0 / D, scalar2=cfg.eps, op0=ALU.mult, op1=ALU.add), [s], [s])
        S.emit("act", lambda e, s=s: e.activation(out=s[:, 2:3], in_=s[:, 1:2], func=AF.Sqrt), [s], [s])
        S.emit("dve", lambda e, s=s: e.reciprocal(out=s[:, 3:4], in_=s[:, 2:3]), [s], [s])
        S.emit("dve", lambda e, x=x, s=s: e.scalar_tensor_tensor(out=x[:], in0=x[:], scalar=s[:, 3:4], in1=fn[:], op0=ALU.mult, op1=ALU.mult), [x, s, fn], [x])
        S.dma("sp", out[t0:t0 + 128, :], x[:], [x], [out])
    A.close()


def declare_io(S, cfg, debug=(), cfgL=None):
    cfgL = cfgL or cfg
    D, T, NCTX, E, DE, TT = cfg.D, cfg.T, cfg.NCTX, cfg.E, cfg.DE, cfg.TT
    HVF = cfg.HV
    HV, H = cfgL.HV, cfgL.H
    I = {}

    def inp(name, shape):
        I[name] = S.dram(name, shape, F32, kind="ExternalInput")
    inp("x", [1, T, D]); inp("c", [1, D]); inp("ctx", [1, NCTX, D]); inp("c_ctx", [D])
    inp("ada_w", [1, D, 6 * D]); inp("ada_b", [1, 6 * D]); inp("norm_mix", [1, D]); inp("norm_ffn", [1, D])
    inp("w_in", [1, D, cfgL.NIN]); inp("gdn_conv", [1, 5, 3 * HV]); inp("gdn_a_log", [1, 2, H]); inp("gdn_dt_bias", [1, 2, H])
    inp("hgrn_lb", [2, 2, HV]); inp("hgrn_norm", [1, HV]); inp("gdn_norm", [1, HV])
    inp("w_branch_a", [1, HVF, D]); inp("w_branch_b", [1, HVF, D]); inp("w_out", [1, D, D]); inp("router_w", [1, D, E])
    inp("w_gate", [1, E, D, DE]); inp("w_up", [1, E, D, DE]); inp("w_down", [1, E, DE, D]); inp("final_norm", [D])
    G = {}

    def scr(name, shape, dt):
        G[name] = S.dram(name, shape, dt, kind="ExternalOutput" if name in debug else "Internal")
    scr("grow", [2, D], F32)
    scr("hqT", [HV, T], BF16); scr("hffT", [HV, TT], BF16); scr("hfbT", [HV, TT], BF16); scr("hi", [TT, HV], BF16)
    scr("hogT", [HV, T], BF16); scr("gzT", [HV, T], BF16); scr("mgT", [2 * D, T], BF16)
    scr("gqkvT", [3 * HV, TT], BF16); scr("gab", [TT, 4 * H], F32)
    scr("ycat", [2 * HV, T], BF16)
    G["yAT"] = Buf("yAT", G["ycat"].t[0:HV, :], multi=True)
    G["yBT"] = Buf("yBT", G["ycat"].t[HV:2 * HV, :], multi=True)
    NR = cfg.H // cfgL.H
    CR = min(256, 2 * HV)
    scr("yall", [2 * HV // CR, NR * CR, T], BF16)
    scr("gq", [HV, TT], BF16); scr("gk", [HV, TT], BF16); scr("gv", [HV, TT], BF16)
    scr("mT", [D, T], BF16); scr("x2", [T, D], F32); scr("h2", [T, D], BF16); scr("crow", [2, D], F32)
    G["x2s"] = Buf("x2s")
    scr("dbg_mod", [128, 6 * cfg.KD * 2], F32)
    if "dbg_S" in debug:
        scr("dbg_S", [2 * min(4, H), 128, 128], F32)
    if "dbg_ob" in debug:
        scr("dbg_ob", [HV, T], F32)
    if "dbg_g" in debug:
        scr("dbg_g", [6, 128, TT // 128, 2 * H], F32)
    scr("dbg_coef", [128, 6 * cfg.KD], F32)
    out = S.dram("out", [T, D], F32, kind="ExternalOutput")
    return I, G, out


def local_cfg(cfg, nsplit):
    return Cfg(D=cfg.D, H=cfg.H // nsplit, T=cfg.T, GW=cfg.GW, NCTX=cfg.NCTX, E=cfg.E, DE=cfg.DE, CAP=cfg.CAP, eps=cfg.eps)


def build_program(cfg=FULL, debug=(), phases=None, nsplit=2, groups=None):
    nc = bass.Bass("TRN2", target_bir_lowering=False)
    S = Sched(nc)
    cfgL = local_cfg(cfg, nsplit) if nsplit > 1 else cfg
    I, G, out = declare_io(S, cfg, debug, cfgL)
    P = Arena(S, "glob")
    C = make_consts(S, P, cfg)
    G["mod"] = P.sb("mod", [128, 6 * cfg.KD, 2], F32)
    G["coef"] = P.sb("coef", [128, 6, cfg.KD], F32)
    ph = phases or ("ada", "coef", "prr", "prc", "hgrn", "gconv", "gdn", "merge", "outproj", "route", "experts", "final")
    if "ada" in ph:
        phase_ada(S, cfg, G, I)
    if "coef" in ph:
        phase_coef(S, cfg, G, I)
    if "dbg_mod" in debug:
        S.dma("sp", G["dbg_mod"][:], G["mod"][:].rearrange("p n s -> p (n s)"), [G["mod"]], [G["dbg_mod"]])
        S.dma("sp", G["dbg_coef"][:], G["coef"][:].rearrange("p n s -> p (n s)"), [G["coef"]], [G["dbg_coef"]])
    if "prr" in ph:
        phase_proj(S, cfgL, G, I, C, "r")
    if "prc" in ph:
        phase_proj(S, cfgL, G, I, C, "c")
    grp = groups or [[b + 4 * r for r in range(max(nsplit, 1))] for b in range(4)]
    CR = min(256, 2 * cfgL.HV)
    NCK = 2 * cfgL.HV // CR

    def gather_y(cks, srcs):
        for ck in cks:
            S.emit_cc(lambda e, ck=ck: e.collective_compute("AllGather", ALU.bypass, replica_groups=grp, ins=[G["ycat"][ck * CR:(ck + 1) * CR, :]],
                                                            outs=[G["yall"][ck]]), srcs, [G["yall"]])
    if "hgrn" in ph:
        phase_hgrn(S, cfgL, G, I, C)
    if nsplit > 1 and NCK >= 2:
        gather_y(range(NCK // 2), [G["yAT"]])
    if "gconv" in ph:
        phase_gconv(S, cfgL, G, I, C)
    if "gdn" in ph:
        phase_gdn(S, cfgL, G, I, C)
    if nsplit > 1:
        gather_y(range(NCK // 2, NCK) if NCK >= 2 else range(NCK), [G["yAT"], G["yBT"]])
    if "merge" in ph:
        phase_merge(S, cfg, G, I, C, cfgL)
    if "outproj" in ph:
        phase_outproj(S, cfg, G, I, C)
    R_ = {"idxT": P.sb("idxT", [128, cfg.CAP // 128, cfg.E], I32), "gateT": P.sb("gateT", [128, cfg.CAP // 128, cfg.E], F32)}
    if "route" in ph:
        phase_route(S, cfg, G, I, C, R_)
    if "experts" in ph:
        phase_experts(S, cfg, G, I, C, R_)
    if "final" in ph:
        phase_final(S, cfg, G, I, C, out)
    P.close()
    S.finish()
    build_program.last_sched = S
    return nc


_PER_BATCH = ("x", "c", "ctx")
_NC_CACHE = {}


def _head_cols(v, hf, nsplit, H, axis=-1):
    v = np.asarray(v)
    w = (H // nsplit) * 128
    sl = [slice(None)] * v.ndim
    sl[axis] = slice(hf * w, (hf + 1) * w)
    return v[tuple(sl)]


def shard_inputs(inputs, cfg, b, hf, nsplit):
    H, HV, D = cfg.H, cfg.HV, cfg.D
    HLc = H // nsplit
    m = {}
    for k, v in inputs.items():
        v = np.asarray(v)
        if k in _PER_BATCH:
            m[k] = np.ascontiguousarray(v[b:b + 1])
        else:
            m[k] = v
    if nsplit > 1:
        w = m["w_in"]
        parts = []
        for g in range(9):
            parts.append(_head_cols(w[:, :, g * HV:(g + 1) * HV], hf, nsplit, H))
        o = 9 * HV
        for g in range(4):
            parts.append(w[:, :, o + g * H + hf * HLc:o + g * H + (hf + 1) * HLc])
        parts.append(w[:, :, o + 4 * H:])
        m["w_in"] = np.concatenate(parts, axis=2)
        m["hgrn_lb"] = _head_cols(m["hgrn_lb"], hf, nsplit, H)
        m["hgrn_norm"] = _head_cols(m["hgrn_norm"], hf, nsplit, H)
        m["gdn_norm"] = _head_cols(m["gdn_norm"], hf, nsplit, H)
        cw = m["gdn_conv"]
        m["gdn_conv"] = np.concatenate([_head_cols(cw[:, :, g * HV:(g + 1) * HV], hf, nsplit, H) for g in range(3)], axis=2)
        m["gdn_a_log"] = m["gdn_a_log"][:, :, hf * HLc:(hf + 1) * HLc]
        m["gdn_dt_bias"] = m["gdn_dt_bias"][:, :, hf * HLc:(hf + 1) * HLc]
    return {k: np.ascontiguousarray(v) for k, v in m.items()}


def kernel(**inputs):
    cfg = FULL
    n = 8
    nsplit = 2
    if "nc" not in _NC_CACHE:
        _NC_CACHE["nc"] = build_program(cfg, nsplit=nsplit)
    nc = _NC_CACHE["nc"]
    B = inputs["x"].shape[0]
    in_maps = [shard_inputs(inputs, cfg, core % B, core // B, nsplit) for core in range(n)]
    res = run_bass_kernel_spmd(nc, in_maps, core_ids=list(range(n)))
    out = np.stack([np.asarray(res.results[b]["out"]) for b in range(B)], axis=0)
    return out.astype(np.float32)
```

```python
import contextlib
import numpy as np
import concourse.bass as bass
import concourse.mybir as mybir
from concourse.bass_utils import run_bass_kernel_spmd

F32 = mybir.dt.float32
BF16 = mybir.dt.bfloat16
I32 = mybir.dt.int32
U32 = mybir.dt.uint32
AF = mybir.ActivationFunctionType
ALU = mybir.AluOpType

ENGS = ("pe", "act", "dve", "pool", "sp")
NEG = -30000.0


class Buf:
    __slots__ = ("name", "ws", "rs", "t", "multi", "dgroup", "excl")

    def __init__(self, name, t=None, multi=False, dgroup=None, excl=False):
        self.excl = excl
        self.name = name
        self.ws = {}
        self.rs = {}
        self.t = t
        self.multi = multi
        self.dgroup = dgroup or name

    def __getitem__(self, idx):
        return self.t[idx]


class Sched:
    def __init__(self, nc):
        self.nc = nc
        self.ops = {e: [] for e in ENGS}
        self.val = {}
        self.seen = {e: {} for e in ENGS}
        self.uid = 0
        self.dmap = {}
        self.dfree = {"sw": [], "hw": []}
        self.nd = 0

    def dkey(self, dgroup, kind):
        k = (dgroup, kind)
        if k not in self.dmap:
            if self.dfree[kind]:
                self.dmap[k] = self.dfree[kind].pop()
            else:
                self.dmap[k] = f"ds{self.nd}_{kind}"
                self.nd += 1
        return self.dmap[k]

    def release(self, dgroups):
        for k in list(self.dmap):
            if k[0] in dgroups:
                self.dfree[k[1]].append(self.dmap.pop(k))

    def dram(self, name, shape, dt, kind="Internal"):
        return Buf(name, self.nc.dram_tensor(name, list(shape), dt, kind=kind).ap(), multi=True)

    def emit(self, eng, fn, reads=(), writes=(), dma=None):
        deps = {}

        def add(k, v):
            if deps.get(k, 0) < v:
                deps[k] = v
        for b in reads:
            for k, v in b.ws.items():
                add(k, v)
            if b.excl:
                for k, v in b.rs.items():
                    if k != "c_" + eng:
                        add(k, v)
        for b in writes:
            if b.multi:
                continue
            for k, v in b.ws.items():
                add(k, v)
            for k, v in b.rs.items():
                add(k, v)
        seen = self.seen[eng]
        waits = []
        for k, v in deps.items():
            if seen.get(k, 0) < v:
                seen[k] = v
                waits.append((k, v))
        sk = self.dkey(dma.dgroup, "sw" if eng == "pool" else "hw") if dma is not None else ("c_" + eng)
        inc = 16 if dma is not None else 1
        self.val[sk] = self.val.get(sk, 0) + inc
        tv = self.val[sk]
        for b in reads:
            if b.rs.get(sk, 0) < tv:
                b.rs[sk] = tv
        for b in writes:
            if b.multi:
                b.ws[sk] = tv
            else:
                b.ws = {sk: tv}
                b.rs = {}
        self.ops[eng].append((waits, fn, sk, inc))

    def dma(self, eng, out, in_, reads, writes, sem=None, **kw):
        if sem is None:
            sem = [b for b in list(writes) + list(reads) if not b.multi][0]
        self.emit(eng, lambda e: e.dma_start(out=out, in_=in_, **kw), reads, writes, dma=sem)

    def emit_cc(self, fn, reads, writes):
        deps = {}
        for b in reads:
            for k, v in b.ws.items():
                if deps.get(k, 0) < v:
                    deps[k] = v
        seen = self.seen["pool"]
        waits = []
        for k, v in deps.items():
            if seen.get(k, 0) < v:
                seen[k] = v
                waits.append((k, v))
        sk = "cc_sem"
        self.val[sk] = self.val.get(sk, 0) + 1
        for b in writes:
            b.ws[sk] = self.val[sk]
        self.ops["pool"].append((waits, fn, sk, 1))

    def barrier(self):
        cur = dict(self.val)
        for eng in ENGS:
            seen = self.seen[eng]
            waits = []
            for k, v in cur.items():
                if seen.get(k, 0) < v:
                    seen[k] = v
                    waits.append((k, v))
            if waits:
                self.ops[eng].append((waits, None, None, 0))

    def finish(self):
        nc = self.nc
        sems = {k: nc.alloc_semaphore(k) for k in self.val}
        engobj = {"pe": "tensor", "act": "scalar", "dve": "vector", "pool": "gpsimd", "sp": "sync"}
        final = list(self.val.items())
        with nc.Block() as block:
            for eng in ENGS:
                ops = self.ops[eng]
                is_final = eng == "sp"

                def body(e, ops=ops, is_final=is_final):
                    for waits, fn, sk, inc in ops:
                        for k, v in waits:
                            e.wait_ge(sems[k], v)
                        if fn is not None:
                            if sk == "cc_sem":
                                fn(e).then_inc(sems[sk])
                            else:
                                fn(e).then_inc(sems[sk], inc)
                    if is_final:
                        for k, v in final:
                            e.wait_ge(sems[k], v)
                getattr(block, engobj[eng])(body)
        return nc


class Arena:
    def __init__(self, S, tag):
        self.S = S
        self.tag = tag
        self.st = contextlib.ExitStack()
        self.groups = set()

    def sb(self, name, shape, dt, multi=False, dgroup=None):
        nm = f"{self.tag}_{name}"
        t = self.st.enter_context(self.S.nc.sbuf_tensor(nm, list(shape), dt))
        b = Buf(nm, t, multi=multi, dgroup=dgroup and f"{self.tag}_{dgroup}")
        self.groups.add(b.dgroup)
        return b

    def ps(self, name, shape, dt=F32):
        nm = f"{self.tag}_{name}"
        t = self.st.enter_context(self.S.nc.psum_tensor(nm, list(shape), dt))
        return Buf(nm, t, excl=True)

    def close(self):
        self.S.barrier()
        self.S.release(self.groups)
        self.st.close()


class Cfg:
    def __init__(self, D=2048, H=8, T=4096, GW=64, NCTX=256, E=16, DE=1024, CAP=512, eps=1e-6):
        self.D, self.H, self.T, self.GW, self.NCTX = D, H, T, GW, NCTX
        self.E, self.DE, self.CAP, self.eps = E, DE, CAP, eps
        self.R = T // GW
        self.HV = H * 128
        self.KD = D // 128
        self.TT = T + NCTX
        HV = self.HV
        o = 0
        self.c_hq = o; o += HV
        self.c_hff = o; o += HV
        self.c_hfb = o; o += HV
        self.c_hi = o; o += HV
        self.c_hog = o; o += HV
        self.c_gqkv = o; o += 3 * HV
        self.c_gz = o; o += HV
        self.c_ga = o; o += 2 * H
        self.c_gb = o; o += 2 * H
        self.c_mg = o; o += 2 * D
        self.NIN = o


FULL = Cfg()


def token_blocks(cfg, lo, hi, nb=512):
    out = []
    for a, b in ((0, cfg.NCTX), (cfg.NCTX, cfg.TT)):
        a, b = max(a, lo), min(b, hi)
        t = a
        while t < b:
            n = min(nb, b - t)
            out.append((t, n))
            t += n
    return out


def make_consts(S, A, cfg):
    C = {}
    nc = S.nc
    identb = A.sb("identb", [128, 128], BF16)
    identf = A.sb("identf", [128, 128], F32)
    for ident in (identb, identf):
        S.emit("pool", lambda e, ident=ident: e.memset(ident[:], 0.0), writes=[ident])
        S.emit("pool", lambda e, ident=ident: e.affine_select(
            out=ident[:], in_=ident[:], pattern=[[-1, 128]], compare_op=ALU.not_equal, fill=1.0,
            base=0, channel_multiplier=1), reads=[ident], writes=[ident])
    C["identb"], C["identf"] = identb, identf
    onesb = A.sb("onesb", [128, 128], BF16)
    S.emit("pool", lambda e: e.memset(onesb[:], 1.0), writes=[onesb])
    C["onesb"] = onesb
    onesf = A.sb("onesf", [128, 128], F32)
    S.emit("pool", lambda e: e.memset(onesf[:], 1.0), writes=[onesf])
    C["onesf"] = onesf
    epsc = A.sb("epsc", [128, 1], F32)
    S.emit("pool", lambda e: e.memset(epsc[:], cfg.eps), writes=[epsc])
    C["epsc"] = epsc
    onec = A.sb("onec", [128, 1], F32)
    S.emit("pool", lambda e: e.memset(onec[:], 1.0), writes=[onec])
    C["onec"] = onec
    return C


def phase_ada(S, cfg, G, I, nsplit=1, grp=None):
    D, KD = cfg.D, cfg.KD
    A = Arena(S, "ada")
    NCB = 6 * KD // nsplit
    SW = 512 if (NCB * 128) % 512 == 0 else 256
    cT = A.sb("cT", [128, KD, 2], F32)
    s2 = A.sb("s2", [128, KD, 2], F32)
    bT = A.sb("bT", [128, NCB], F32)
    S.dma("sp", cT[:, :, 0], I["c"][0].rearrange("(k p) -> p k", p=128), [I["c"]], [cT], allow_slow_non_contiguous=True)
    S.dma("sp", cT[:, :, 1], I["c_ctx"][:].rearrange("(k p) -> p k", p=128), [I["c_ctx"]], [cT], allow_slow_non_contiguous=True)
    S.dma("sp", bT[:], I["ada_b"][0].rearrange("(k p) -> p k", p=128), [I["ada_b"]], [bT], allow_slow_non_contiguous=True)
    S.emit("act", lambda e: e.activation(out=s2[:], in_=cT[:], func=AF.Silu), [cT], [s2])
    wsl = [A.sb(f"w{i}", [128, KD, SW], F32) for i in range(2)]
    pa = A.ps("pa", [128, NCB, 2], F32)
    mod = G["mod"]
    wv = I["ada_w"][0].rearrange("(k p) n -> p k n", p=128)
    nslab = NCB * 128 // SW
    for s in range(nslab):
        w = wsl[s % 2]
        S.dma("sp" if s % 2 == 0 else "pool", w[:], wv[:, :, s * SW:(s + 1) * SW], [I["ada_w"]], [w])
        for j in range(SW // 128):
            cb = s * (SW // 128) + j

            def mm(e, w=w, j=j, cb=cb):
                r = None
                for k in range(KD):
                    r = e.matmul(pa[:, cb, :], lhsT=w[:, k, j * 128:(j + 1) * 128], rhs=s2[:, k, :],
                                 start=(k == 0), stop=(k == KD - 1))
                return r
            S.emit("pe", mm, [w, s2], [pa])
    bb = bT[:].rearrange("p (n o) -> p n o", o=1).to_broadcast([128, NCB, 2])
    if nsplit == 1:
        S.emit("dve", lambda e: e.tensor_tensor(out=mod[:], in0=pa[:], in1=bb, op=ALU.add), [pa, bT], [mod])
    else:
        ml_ = A.sb("ml", [128, NCB, 2], F32)
        S.emit("dve", lambda e: e.tensor_tensor(out=ml_[:], in0=pa[:], in1=bb, op=ALU.add), [pa, bT], [ml_])
        S.dma("sp", G["modown"][:, :], ml_[:].rearrange("p n s -> p (n s)"), [ml_], [G["modown"]])
        S.emit_cc(lambda e: e.collective_compute("AllGather", ALU.bypass, replica_groups=grp, ins=[G["modown"][:, :]], outs=[G["modall"][:, :]]),
                  [G["modown"]], [G["modall"]])
        for r in range(nsplit):
            S.dma("sp", mod[:, r * NCB:(r + 1) * NCB, :].rearrange("p n s -> p (n s)"), G["modall"][r * 128:(r + 1) * 128, :], [G["modall"]], [mod])
    A.close()


def phase_coef(S, cfg, G, I):
    KD, D = cfg.KD, cfg.D
    A = Arena(S, "coef")
    mod = G["mod"]
    nm = A.sb("nm", [128, KD], F32)
    nf = A.sb("nf", [128, KD], F32)
    S.dma("sp", nm[:], I["norm_mix"][0].rearrange("(k p) -> p k", p=128), [I["norm_mix"]], [nm], allow_slow_non_contiguous=True)
    S.dma("sp", nf[:], I["norm_ffn"][0].rearrange("(k p) -> p k", p=128), [I["norm_ffn"]], [nf], allow_slow_non_contiguous=True)
    co = G["coef"]
    def mk(dst, gain, sc_j, s):
        S.emit("dve", lambda e: e.scalar_tensor_tensor(out=co[:, dst, :], in0=mod[:, sc_j * KD:(sc_j + 1) * KD, s], scalar=1.0,
                                                       in1=gain[:], op0=ALU.add, op1=ALU.mult), [mod, gain, co], [co])
    mk(0, nm, 1, 0)
    S.emit("dve", lambda e: e.tensor_copy(out=co[:, 1, :], in_=mod[:, 0:KD, 0]), [mod, co], [co])
    mk(2, nm, 1, 1)
    S.emit("dve", lambda e: e.tensor_copy(out=co[:, 3, :], in_=mod[:, 0:KD, 1]), [mod, co], [co])
    mk(4, nf, 4, 0)
    S.emit("dve", lambda e: e.tensor_copy(out=co[:, 5, :], in_=mod[:, 3 * KD:4 * KD, 0]), [mod, co], [co])
    gt = A.sb("gt", [128, 2, KD], F32)
    S.emit("dve", lambda e: e.tensor_copy(out=gt[:, 0, :], in_=mod[:, 2 * KD:3 * KD, 0]), [mod], [gt])
    S.emit("dve", lambda e: e.tensor_copy(out=gt[:, 1, :], in_=mod[:, 5 * KD:6 * KD, 0]), [mod, gt], [gt])
    for j in range(2):
        S.dma("sp", G["grow"][j].rearrange("(k p) -> p k", p=128), gt[:, j, :], [gt], [G["grow"]], allow_slow_non_contiguous=True)
    A.close()


def norm_tiles(S, A, cfg, C, tiles, hT, coefbuf, f32T=None):
    D, KD = cfg.D, cfg.KD
    xt = [A.sb(f"nx{i}", [128, D], F32) for i in range(2)]
    xn = [A.sb(f"nxn{i}", [128, D], BF16 if f32T is None else F32) for i in range(2)]
    junk = A.sb("njunk", [128, D], BF16)
    st = [A.sb(f"nst{i}", [128, 8], F32) for i in range(2)]
    tdt = BF16 if f32T is None else F32
    per = 8 if f32T is None else 4
    pts = [A.ps(f"npt{i}", [128, per, 128], tdt) for i in range(2)]
    ident = C["identb"] if f32T is None else C["identf"]
    co = coefbuf
    ng = KD // per if KD >= per else 1
    per = min(per, KD)
    gi = 0
    for ti, (pieces, srcbufs, col0, ci) in enumerate(tiles):
        x = xt[ti % 2]
        for k, (ap, p0, npp) in enumerate(pieces):
            S.dma("sp" if ti % 2 == 0 else "pool", x[p0:p0 + npp, :], ap, srcbufs, [x])
        s = st[ti % 2]
        import os
        NV = int(os.environ.get("K_NV", "9"))
        if NV < 1:
            continue
        S.emit("act", lambda e, x=x, s=s: e.activation(out=junk[:], in_=x[:], func=AF.Square, accum_out=s[:, 0:1]),
               [x], [junk, s])
        S.emit("dve", lambda e, s=s: e.tensor_scalar(out=s[:, 1:2], in0=s[:, 0:1], scalar1=1.0 / D, scalar2=cfg.eps,
                                                     op0=ALU.mult, op1=ALU.add), [s], [s])
        S.emit("act", lambda e, s=s: e.activation(out=s[:, 2:3], in_=s[:, 1:2], func=AF.Sqrt), [s], [s])
        S.emit("dve", lambda e, s=s: e.reciprocal(out=s[:, 3:4], in_=s[:, 2:3]), [s], [s])
        if NV < 2:
            continue
        n = xn[ti % 2]
        S.emit("dve", lambda e, x=x, s=s, n=n: e.tensor_scalar(out=n[:], in0=x[:], scalar1=s[:, 3:4], scalar2=None,
                                                               op0=ALU.mult), [x, s], [n])
        if NV < 3:
            continue
        for g in range(ng):
            pt = pts[gi % 2]
            gi += 1

            def tr(e, n=n, pt=pt, g=g):
                r = None
                for j in range(per):
                    c = g * per + j
                    r = e.transpose(out=pt[:, j, :], in_=n[:, c * 128:(c + 1) * 128], identity=ident[:])
                return r
            S.emit("pe", tr, [n, ident], [pt])
            if NV < 4:
                continue
            for j in range(per):
                c = g * per + j
                eng = "act" if gi % 2 == 0 else "dve"
                outs = [(hT, hT[:, c, col0:col0 + 128])]
                if f32T is not None:
                    outs.append((f32T, f32T[:, c, col0:col0 + 128]))
                for oi, (ob, oap) in enumerate(outs):
                    eng2 = eng if oi == 0 else ("dve" if eng == "act" else "act")
                    if eng2 == "act":
                        S.emit("act", lambda e, pt=pt, j=j, c=c, oap=oap, ci=ci: e.activation(
                            out=oap, in_=pt[:, j, :], func=AF.Identity, scale=co[:, ci, c:c + 1], bias=co[:, ci + 1, c:c + 1]),
                            [pt, co], [ob])
                    else:
                        S.emit("dve", lambda e, pt=pt, j=j, c=c, oap=oap, ci=ci: e.tensor_scalar(
                            out=oap, in0=pt[:, j, :], scalar1=co[:, ci, c:c + 1], scalar2=co[:, ci + 1, c:c + 1],
                            op0=ALU.mult, op1=ALU.add), [pt, co], [ob])


def lat_tile_pieces(cfg, xin, ti, order):
    if order == "r":
        return [(xin[0, ti * 128:(ti + 1) * 128, :], 0, 128)]
    R, GW = cfg.R, cfg.GW
    xv = xin[0].rearrange("(r c) d -> c r d", c=GW)
    out = []
    if R >= 128:
        per = R // 128
        cc, sub = divmod(ti, per)
        out.append((xv[cc, sub * 128:(sub + 1) * 128, :], 0, 128))
    else:
        ncol = 128 // R
        for k in range(ncol):
            out.append((xv[ti * ncol + k, :, :], k * R, R))
    return out


def act_tiles(cfg, I, order):
    tiles = []
    for t in range(cfg.NCTX // 128):
        tiles.append(([(I["ctx"][0, t * 128:(t + 1) * 128, :], 0, 128)], [I["ctx"]], t * 128, 2))
    for t in range(cfg.T // 128):
        tiles.append((lat_tile_pieces(cfg, I["x"], t, order), [I["x"]], cfg.NCTX + t * 128, 0))
    return tiles


def project(S, A, cfg, hT, w_in, groups, tok_lo=0):
    KD, TT = cfg.KD, cfg.TT
    SW = 256
    wsl = [A.sb(f"pw{i}", [128, KD, SW], BF16) for i in range(2)]
    pps = [A.ps(f"pp{i}", [128, 512], F32) for i in range(4)]
    stg = {}
    wv = w_in[0].rearrange("(k p) n -> p k n", p=128)
    si = 0
    pi = 0
    gi = 0
    for g in groups:
        col0, ncols, lay, func, dst, dt = g["col0"], g["ncols"], g["layout"], g["func"], g["dst"], g["dt"]
        tlo = g.get("tok_lo", 0)
        key = (lay, dt)
        if key not in stg:
            stg[key] = [A.sb(f"ps{lay}{len(stg)}_{i}", [128, 512], dt) for i in range(3)]
        stgs = stg[key]
        if lay == "F":
            blocks = token_blocks(cfg, tlo, TT)
            for s0 in range(0, ncols, SW):
                sw = min(SW, ncols - s0)
                w = wsl[si % 2]
                S.dma("pool", w[:, :, 0:sw], wv[:, :, col0 + s0:col0 + s0 + sw], [w_in], [w])
                si += 1
                for j in range(0, sw, 128):
                    for (t0, nb) in blocks:
                        pp = pps[pi % 4]
                        pi += 1

                        def mm(e, w=w, j=j, t0=t0, nb=nb, pp=pp):
                            r = None
                            for k in range(KD):
                                r = e.matmul(pp[:, 0:nb], lhsT=w[:, k, j:j + 128], rhs=hT[:, k, t0:t0 + nb],
                                             start=(k == 0), stop=(k == KD - 1))
                            return r
                        S.emit("pe", mm, [w, hT], [pp])
                        sg = stgs[gi % 3]
                        gi += 1
                        if func is None:
                            S.emit("dve", lambda e, sg=sg, pp=pp, nb=nb: e.tensor_copy(out=sg[:, 0:nb], in_=pp[:, 0:nb]), [pp], [sg])
                        else:
                            S.emit("act", lambda e, sg=sg, pp=pp, nb=nb, func=func: e.activation(out=sg[:, 0:nb], in_=pp[:, 0:nb], func=func),
                                   [pp], [sg])
                        r0 = s0 + j
                        S.dma("sp", dst[r0:r0 + 128, t0 - tlo:t0 - tlo + nb], sg[:, 0:nb], [sg], [dst])
        else:
            assert ncols <= 512 or ncols % 256 == 0
            for s0 in range(0, ncols, SW):
                sw = min(SW, ncols - s0)
                w = wsl[si % 2]
                S.dma("pool", w[:, :, 0:sw], wv[:, :, col0 + s0:col0 + s0 + sw], [w_in], [w])
                si += 1
                for t0 in range(tlo, TT, 128):
                    pp = pps[pi % 4]
                    pi += 1

                    def mm(e, w=w, sw=sw, t0=t0, pp=pp):
                        r = None
                        for k in range(KD):
                            r = e.matmul(pp[:, 0:sw], lhsT=hT[:, k, t0:t0 + 128], rhs=w[:, k, 0:sw],
                                         start=(k == 0), stop=(k == KD - 1))
                        return r
                    S.emit("pe", mm, [w, hT], [pp])
                    sg = stgs[gi % 3]
                    gi += 1
                    S.emit("dve", lambda e, sg=sg, pp=pp, sw=sw: e.tensor_copy(out=sg[:, 0:sw], in_=pp[:, 0:sw]), [pp], [sg])
                    S.dma("sp", dst[t0 - tlo:t0 - tlo + 128, s0:s0 + sw], sg[:, 0:sw], [sg], [dst])


def phase_proj(S, cfg, G, I, C, order):
    A = Arena(S, "pr" + order)
    KD, TT, HV, NCTX, H, D = cfg.KD, cfg.TT, cfg.HV, cfg.NCTX, cfg.H, cfg.D
    hT = A.sb("hT", [128, KD, TT], BF16, multi=True)
    norm_tiles(S, A, cfg, C, act_tiles(cfg, I, order), hT, G["coef"])
    if order == "r":
        groups = [
            dict(col0=cfg.c_hq, ncols=HV, layout="F", func=AF.Silu, dst=G["hqT"], dt=BF16, tok_lo=NCTX),
            dict(col0=cfg.c_hff, ncols=HV, layout="F", func=None, dst=G["hffT"], dt=BF16),
            dict(col0=cfg.c_hfb, ncols=HV, layout="F", func=None, dst=G["hfbT"], dt=BF16),
            dict(col0=cfg.c_hi, ncols=HV, layout="T", func=None, dst=G["hi"], dt=BF16),
            dict(col0=cfg.c_hog, ncols=HV, layout="F", func=AF.Sigmoid, dst=G["hogT"], dt=BF16, tok_lo=NCTX),
            dict(col0=cfg.c_gz, ncols=HV, layout="F", func=AF.Silu, dst=G["gzT"], dt=BF16, tok_lo=NCTX),
            dict(col0=cfg.c_mg, ncols=2 * D, layout="F", func=AF.Sigmoid, dst=G["mgT"], dt=BF16, tok_lo=NCTX),
        ]
    else:
        groups = [
            dict(col0=cfg.c_gqkv, ncols=3 * HV, layout="F", func=None, dst=G["gqkvT"], dt=BF16),
            dict(col0=cfg.c_ga, ncols=4 * H, layout="T", func=None, dst=G["gab"], dt=F32),
        ]
    import os
    sel = os.environ.get("K_GROUPS")
    if sel is not None:
        groups = [groups[int(i)] for i in sel.split(",") if i != ""]
    project(S, A, cfg, hT, I["w_in"], groups)
    A.close()


def phase_hgrn(S, cfg, G, I, C):
    A = Arena(S, "hg")
    H, T, TT, NCTX, HV = cfg.H, cfg.T, cfg.TT, cfg.NCTX, cfg.HV
    CH = 64
    NCH = TT // CH
    NCC = NCTX // CH
    order = [list(range(NCH)), list(range(NCC - 1, -1, -1)) + list(range(NCH - 1, NCC - 1, -1))]
    SEG = 1024
    segs = []
    for a, b in ((0, NCTX), (NCTX, TT)):
        t = a
        while t < b:
            n = min(SEG, b - t)
            segs.append((t, n))
            t += n
    lbT = A.sb("lbT", [128, 2, 2, H], F32)
    for d in range(2):
        for sl in range(2):
            S.dma("sp", lbT[:, d, sl, :], I["hgrn_lb"][d, sl].rearrange("(h p) -> p h", p=128), [I["hgrn_lb"]], [lbT],
                  allow_slow_non_contiguous=True)
    low = A.sb("low", [128, 2, H], F32)
    oml = A.sb("oml", [128, 2, H], F32)
    noml = A.sb("noml", [128, 2, H], F32)
    S.emit("dve", lambda e: e.tensor_tensor(out=low[:], in0=lbT[:, :, 0, :], in1=lbT[:, :, 1, :], op=ALU.subtract), [lbT], [low])
    S.emit("act", lambda e: e.activation(out=low[:], in_=low[:], func=AF.Sigmoid), [low], [low])
    S.emit("dve", lambda e: e.tensor_scalar(out=oml[:], in0=low[:], scalar1=-1.0, scalar2=1.0, op0=ALU.mult, op1=ALU.add), [low], [oml])
    S.emit("dve", lambda e: e.tensor_scalar(out=noml[:], in0=low[:], scalar1=1.0, scalar2=-1.0, op0=ALU.mult, op1=ALU.add), [low], [noml])
    gain = A.sb("gain", [128, H], F32)
    S.dma("sp", gain[:], I["hgrn_norm"][0].rearrange("(h p) -> p h", p=128), [I["hgrn_norm"]], [gain], allow_slow_non_contiguous=True)
    msk01 = A.sb("msk01", [128, SEG], F32)
    S.emit("pool", lambda e: e.memset(msk01[:], 1.0), writes=[msk01])
    S.emit("pool", lambda e: e.memset(msk01[:].rearrange("p (n c) -> p n c", c=CH)[:, :, 0:1], 0.0), [msk01], [msk01])
    cm = []
    for d in range(2):
        m = A.sb(f"cm{d}", [CH, CH], F32)
        S.emit("pool", lambda e, m=m: e.memset(m[:], 1.0), writes=[m])
        sg = 1 if d == 0 else -1
        S.emit("pool", lambda e, m=m, sg=sg: e.affine_select(out=m[:], in_=m[:], pattern=[[sg, CH]], compare_op=ALU.is_ge, fill=0.0,
                                                             base=0, channel_multiplier=-sg), [m], [m])
        cm.append(m)
    identb, onesb = C["identb"], C["onesb"]
    qT2 = [A.sb(f"qT{i}", [128, T], BF16) for i in range(2)]
    fl2 = [[A.sb(f"fl{d}{i}", [128, TT], BF16) for d in range(2)] for i in range(2)]
    vv2 = [A.sb(f"vv{i}", [CH, NCH, 128], BF16) for i in range(2)]
    og2 = [A.sb("og0", [128, T], BF16)] * 2
    qt = [A.sb(f"qt{d}", [128, T], BF16) for d in range(2)]
    kt = [A.sb(f"kt{d}", [128, TT], BF16) for d in range(2)]
    kh = [A.sb(f"kh{d}", [128, TT], BF16) for d in range(2)]
    egt = [A.sb(f"egt{d}", [128, NCH], F32) for d in range(2)]
    tsets = [[A.sb(f"t{n}{i}", [128, SEG], F32) for n in "ABCD"] for i in range(2)]
    OA = A.sb("OA", [128, T], F32)
    Sf = [[A.sb(f"S{d}{i}", [128, 128], F32) for i in range(2)] for d in range(2)]
    Sb = [[A.sb(f"Sb{d}{i}", [128, 128], BF16) for i in range(3)] for d in range(2)]
    sTs = [[A.sb(f"sTs{d}{i}", [CH, CH], BF16) for i in range(3)] for d in range(2)]
    khs = [[A.sb(f"khs{d}{i}", [CH, 128], BF16) for i in range(3)] for d in range(2)]
    sqb = A.sb("sqb", [128, 512], BF16)
    rt = A.sb("rt", [128, 512], F32)
    ys = [A.sb("ys0", [128, 512], BF16)] * 2
    pst = [A.ps(f"pst{d}", [CH, 512], F32) for d in range(2)]
    ptr = [A.ps(f"ptr{d}", [CH, 1024], BF16) for d in range(2)]
    po = [A.ps(f"po{d}", [128, 512], F32) for d in range(2)]
    pd = [A.ps(f"pd{d}", [128, 512], F32) for d in range(2)]

    def load_head(h):
        r0 = h * 128
        qT, fl, vv, og = qT2[h % 2], fl2[h % 2], vv2[h % 2], og2[h % 2]
        S.dma("sp", qT[:], G["hqT"][r0:r0 + 128, :], [G["hqT"]], [qT])
        S.dma("sp", fl[0][:], G["hffT"][r0:r0 + 128, :], [G["hffT"]], [fl[0]])
        S.dma("sp", fl[1][:], G["hfbT"][r0:r0 + 128, :], [G["hfbT"]], [fl[1]])
        S.dma("sp", vv[:], G["hi"][:, r0:r0 + 128].rearrange("(n c) v -> c n v", c=CH), [G["hi"]], [vv])

    tsi_box = [0]

    def do_head(h, qT, fl, vv, og):
        r0 = h * 128
        S.dma("sp", og[:], G["hogT"][r0:r0 + 128, :], [G["hogT"]], [og])
        for d in range(2):
            lo_, om_, nom_ = low[:, d, h:h + 1], oml[:, d, h:h + 1], noml[:, d, h:h + 1]
            for (a, n) in segs:
                tA, tB, tC, tD = tsets[tsi_box[0] % 2]
                tsi_box[0] += 1
                nch = n // CH
                c0 = a // CH
                v3 = lambda t, n=n: t[:, 0:n].rearrange("p (n c) -> p n c", c=CH)
                S.emit("act", lambda e, tA=tA, tB=tB, tC=tC, tD=tD, a=a, n=n, d=d: e.activation(out=tA[:, 0:n], in_=fl[d][:, a:a + n], func=AF.Sigmoid), [fl[d]], [tA])
                S.emit("act", lambda e, tA=tA, tB=tB, tC=tC, tD=tD, n=n, lo_=lo_, om_=om_: e.activation(out=tB[:, 0:n], in_=tA[:, 0:n], func=AF.Ln, scale=om_, bias=lo_),
                       [tA, low, oml], [tB])
                S.emit("dve", lambda e, tA=tA, tB=tB, tC=tC, tD=tD, n=n, om_=om_, nom_=nom_: e.tensor_scalar(out=tC[:, 0:n], in0=tA[:, 0:n], scalar1=nom_, scalar2=om_,
                                                                                op0=ALU.mult, op1=ALU.add), [tA, oml, noml], [tC])
                S.emit("dve", lambda e, tA=tA, tB=tB, tC=tC, tD=tD, n=n: e.tensor_tensor_scan(out=tA[:, 0:n], data0=msk01[:, 0:n], data1=tB[:, 0:n], initial=0.0,
                                                                  op0=ALU.mult, op1=ALU.add), [msk01, tB, tA], [tA])
                tot = lambda nch=nch, v3=v3, tA=tA: v3(tA)[:, :, CH - 1:CH]
                if d == 0:
                    Gd = tA
                else:
                    S.emit("dve", lambda e, tA=tA, tB=tB, tC=tC, tD=tD, n=n: e.tensor_tensor(out=tD[:, 0:n], in0=tB[:, 0:n], in1=tA[:, 0:n], op=ALU.subtract), [tA, tB], [tD])
                    S.emit("dve", lambda e, tA=tA, tB=tB, tC=tC, tD=tD, v3=v3, tot=tot, nch=nch: e.tensor_tensor(out=v3(tB), in0=v3(tD), in1=tot().to_broadcast([128, nch, CH]),
                                                                                     op=ALU.add), [tD, tA], [tB])
                    Gd = tB
                S.emit("act", lambda e, tA=tA, tB=tB, tC=tC, tD=tD, c0=c0, nch=nch, d=d, tot=tot: e.activation(out=egt[d][:, c0:c0 + nch].rearrange("p (n o) -> p n o", o=1),
                                                                                  in_=tot(), func=AF.Exp), [tA], [egt[d]])
                if a >= NCTX:
                    S.emit("act", lambda e, tA=tA, tB=tB, tC=tC, tD=tD, n=n, Gd=Gd: e.activation(out=tD[:, 0:n], in_=Gd[:, 0:n], func=AF.Exp), [Gd], [tD])
                    S.emit("dve", lambda e, tA=tA, tB=tB, tC=tC, tD=tD, n=n, a=a, d=d: e.tensor_tensor(out=qt[d][:, a - NCTX:a - NCTX + n], in0=qT[:, a - NCTX:a - NCTX + n],
                                                                           in1=tD[:, 0:n], op=ALU.mult), [qT, tD], [qt[d]])
                S.emit("act", lambda e, tA=tA, tB=tB, tC=tC, tD=tD, n=n, Gd=Gd: e.activation(out=tD[:, 0:n], in_=Gd[:, 0:n], func=AF.Exp, scale=-1.0), [Gd], [tD])
                S.emit("pool", lambda e, tA=tA, tB=tB, tC=tC, tD=tD, n=n, a=a, d=d: e.tensor_tensor(out=kt[d][:, a:a + n], in0=tC[:, 0:n], in1=tD[:, 0:n], op=ALU.mult),
                       [tC, tD], [kt[d]])
                S.emit("dve", lambda e, tA=tA, tB=tB, tC=tC, tD=tD, v3=v3, tot=tot, nch=nch, Gd=Gd: e.tensor_tensor(out=v3(tD), in0=tot().to_broadcast([128, nch, CH]),
                                                                                       in1=v3(Gd), op=ALU.subtract), [tA, Gd], [tD])
                S.emit("act", lambda e, tA=tA, tB=tB, tC=tC, tD=tD, n=n: e.activation(out=tD[:, 0:n], in_=tD[:, 0:n], func=AF.Exp), [tD], [tD])
                S.emit("pool", lambda e, tA=tA, tB=tB, tC=tC, tD=tD, n=n, a=a, d=d: e.tensor_tensor(out=kh[d][:, a:a + n], in0=tC[:, 0:n], in1=tD[:, 0:n], op=ALU.mult),
                       [tC, tD], [kh[d]])
        for d in range(2):
            S.emit("pool", lambda e, d=d: e.memset(Sf[d][0][:], 0.0), writes=[Sf[d][0]])
            S.emit("pool", lambda e, d=d: e.memset(Sb[d][0][:], 0.0), writes=[Sb[d][0]])
        OAc = {}

        def genA(d, n):
            c = order[d][n]
            t0 = c * CH
            sl = n % 3
            if c >= NCC:
                q0 = t0 - NCTX
                S.emit("pe", lambda e: e.matmul(pst[d][:, 0:CH], lhsT=kt[d][:, t0:t0 + CH], rhs=qt[d][:, q0:q0 + CH],
                                                start=True, stop=True), [kt[d], qt[d]], [pst[d]])
                yield
                S.emit("dve", lambda e: e.tensor_tensor(out=sTs[d][sl][:], in0=pst[d][:, 0:CH], in1=cm[d][:], op=ALU.mult),
                       [pst[d], cm[d]], [sTs[d][sl]])
                yield
            S.emit("pe", lambda e: e.transpose(out=ptr[d][:, 0:128], in_=kh[d][:, t0:t0 + CH], identity=identb[:]),
                   [kh[d], identb], [ptr[d]])
            yield
            S.emit("act", lambda e: e.copy(out=khs[d][sl][:], in_=ptr[d][:, 0:128]), [ptr[d]], [khs[d][sl]])
            yield

        def genB(d, n):
            c = order[d][n]
            t0 = c * CH
            sl = n % 3
            Sf_o, Sf_n = Sf[d][n % 2], Sf[d][(n + 1) % 2]
            Sb_o, Sb_n = Sb[d][n % 3], Sb[d][(n + 1) % 3]
            S.emit("pe", lambda e: e.matmul(pd[d][:, 0:128], lhsT=khs[d][sl][:], rhs=vv[:, c, :], start=True, stop=True),
                   [khs[d][sl], vv], [pd[d]])
            yield
            S.emit("dve", lambda e: e.scalar_tensor_tensor(out=Sf_n[:], in0=Sf_o[:], scalar=egt[d][:, c:c + 1], in1=pd[d][:, 0:128],
                                                           op0=ALU.mult, op1=ALU.add), [Sf_o, egt[d], pd[d]], [Sf_n])
            yield
            S.emit("act", lambda e: e.copy(out=Sb_n[:], in_=Sf_n[:]), [Sf_n], [Sb_n])
            yield
            if c >= NCC:
                q0 = t0 - NCTX

                def mo(e):
                    e.matmul(po[d][:, 0:CH], lhsT=Sb_o[:], rhs=qt[d][:, q0:q0 + CH], start=True, stop=False)
                    return e.matmul(po[d][:, 0:CH], lhsT=vv[:, c, :], rhs=sTs[d][sl][:], start=False, stop=True)
                S.emit("pe", mo, [Sb_o, qt[d], vv, sTs[d][sl]], [po[d]])
                yield
                if c not in OAc:
                    OAc[c] = Buf(f"OAc{c}")
                    OAc[c].ws = dict(OA.ws)
                    OAc[c].rs = dict(OA.rs)
                    S.emit("act", lambda e: e.copy(out=OA[:, q0:q0 + CH], in_=po[d][:, 0:CH]), [po[d]], [OAc[c]])
                else:
                    S.emit("dve", lambda e: e.tensor_tensor(out=OA[:, q0:q0 + CH], in0=po[d][:, 0:CH], in1=OA[:, q0:q0 + CH],
                                                            op=ALU.add), [po[d], OAc[c]], [OAc[c]])
                yield

        def run_streams(streams):
            while streams:
                for g in list(streams):
                    try:
                        next(g)
                    except StopIteration:
                        streams.remove(g)

        run_streams([genA(0, 0), genA(1, 0)])
        for n in range(NCH):
            sts = []
            if n + 1 < NCH:
                sts += [genA(0, n + 1), genA(1, n + 1)]
            sts += [genB(0, n), genB(1, n)]
            run_streams(sts)
        for bk in OAc.values():
            for k_, v_ in bk.ws.items():
                if OA.ws.get(k_, 0) < v_:
                    OA.ws[k_] = v_
        S.emit("dve", lambda e: e.tensor_tensor(out=OA[:], in0=OA[:], in1=og[:], op=ALU.mult), [OA, og], [OA])
        for bi, t0 in enumerate(range(0, T, 512)):
            nb = min(512, T - t0)
            S.emit("act", lambda e, t0=t0, nb=nb: e.activation(out=sqb[:, 0:nb], in_=OA[:, t0:t0 + nb], func=AF.Square), [OA], [sqb])
            S.emit("pe", lambda e, nb=nb: e.matmul(po[0][:, 0:nb], lhsT=onesb[:], rhs=sqb[:, 0:nb], start=True, stop=True), [onesb, sqb], [po[0]])
            S.emit("act", lambda e, nb=nb: e.activation(out=rt[:, 0:nb], in_=po[0][:, 0:nb], func=AF.Ln, scale=1.0 / 128, bias=C["epsc"][:, 0:1]),
                   [po[0], C["epsc"]], [rt])
            S.emit("act", lambda e, nb=nb: e.activation(out=rt[:, 0:nb], in_=rt[:, 0:nb], func=AF.Exp, scale=-0.5), [rt], [rt])
            y = ys[bi % 2]
            S.emit("dve", lambda e, t0=t0, nb=nb, y=y, h=h: e.scalar_tensor_tensor(out=y[:, 0:nb], in0=OA[:, t0:t0 + nb], scalar=gain[:, h:h + 1],
                                                                                   in1=rt[:, 0:nb], op0=ALU.mult, op1=ALU.mult), [OA, gain, rt], [y])
            S.dma("sp", G["yAT"][r0:r0 + 128, t0:t0 + nb], y[:, 0:nb], [y], [G["yAT"]])

    load_head(0)
    for h in range(H):
        if h + 1 < H:
            load_head(h + 1)
        do_head(h, qT2[h % 2], fl2[h % 2], vv2[h % 2], og2[h % 2])
    A.close()


def phase_gconv(S, cfg, G, I, C):
    A = Arena(S, "gc")
    H, T, TT, NCTX, HV = cfg.H, cfg.T, cfg.TT, cfg.NCTX, cfg.HV
    NB = 3 * HV // 128
    cw = A.sb("cw", [128, 5, NB], F32)
    for j in range(5):
        S.dma("sp", cw[:, j, :], I["gdn_conv"][0, j].rearrange("(n p) -> p n", p=128), [I["gdn_conv"]], [cw], allow_slow_non_contiguous=True)
    xp = [A.sb(f"xp{i}", [128, TT + 8], BF16) for i in range(2)]
    for x in xp:
        S.emit("pool", lambda e, x=x: e.memset(x[:], 0.0), writes=[x])
    y = A.sb("y", [128, TT], F32)
    sq = A.sb("sq", [128, 512], BF16)
    rt = A.sb("rt", [128, 512], F32)
    ob = [A.sb(f"ob{i}", [128, TT], BF16) for i in range(2)]
    pn = [A.ps(f"pn{i}", [128, 512], F32) for i in range(2)]
    segs = [(0, NCTX, 2), (NCTX, T, NCTX + 6)]
    dsts = [G["gq"], G["gk"], G["gv"]]
    pi = 0
    for fb in range(NB):
        kind, hb = divmod(fb, H)
        x = xp[fb % 2]
        r0 = fb * 128
        for (a, n, off) in segs:
            if kind == 0 and a == 0:
                continue
            S.dma("sp" if fb % 2 == 0 else "pool", x[:, off:off + n], G["gqkvT"][r0:r0 + 128, a:a + n], [G["gqkvT"]], [x])
            S.emit("dve", lambda e, x=x, a=a, n=n, off=off, fb=fb: e.tensor_scalar(out=y[:, a:a + n], in0=x[:, off - 2:off - 2 + n], scalar1=cw[:, 0, fb:fb + 1],
                                                                                  scalar2=None, op0=ALU.mult), [x, cw], [y])
            for j in range(1, 5):
                S.emit("dve", lambda e, x=x, a=a, n=n, off=off, fb=fb, j=j: e.scalar_tensor_tensor(
                    out=y[:, a:a + n], in0=x[:, off - 2 + j:off - 2 + j + n], scalar=cw[:, j, fb:fb + 1], in1=y[:, a:a + n],
                    op0=ALU.mult, op1=ALU.add), [x, cw, y], [y])
        lo = 0 if kind != 0 else NCTX
        o = ob[fb % 2]
        S.emit("act", lambda e, lo=lo: e.activation(out=y[:, lo:TT], in_=y[:, lo:TT], func=AF.Silu), [y], [y])
        if kind == 2:
            S.emit("dve", lambda e, o=o: e.tensor_copy(out=o[:], in_=y[:]), [y], [o])
        else:
            sc = (128.0 ** -0.5) if kind == 0 else 1.0
            for (t0, nb) in token_blocks(cfg, lo, TT):
                p = pn[pi % 2]
                pi += 1
                S.emit("dve", lambda e, t0=t0, nb=nb: e.tensor_tensor(out=sq[:, 0:nb], in0=y[:, t0:t0 + nb], in1=y[:, t0:t0 + nb], op=ALU.mult), [y], [sq])
                S.emit("pe", lambda e, p=p, nb=nb: e.matmul(p[:, 0:nb], lhsT=C["onesb"][:], rhs=sq[:, 0:nb], start=True, stop=True), [C["onesb"], sq], [p])
                S.emit("act", lambda e, p=p, nb=nb: e.activation(out=rt[:, 0:nb], in_=p[:, 0:nb], func=AF.Ln, bias=C["epsc"][:, 0:1]), [p, C["epsc"]], [rt])
                S.emit("act", lambda e, nb=nb: e.activation(out=rt[:, 0:nb], in_=rt[:, 0:nb], func=AF.Exp, scale=-0.5), [rt], [rt])
                S.emit("dve", lambda e, o=o, t0=t0, nb=nb, sc=sc: e.scalar_tensor_tensor(out=o[:, t0:t0 + nb], in0=y[:, t0:t0 + nb], scalar=sc, in1=rt[:, 0:nb],
                                                                                        op0=ALU.mult, op1=ALU.mult), [y, rt], [o])
        S.dma("sp", dsts[kind][hb * 128:(hb + 1) * 128, lo:TT], o[:, lo:TT], [o], [dsts[kind]])
    A.close()


def phase_gdn(S, cfg, G, I, C):
    H, T, TT, NCTX, HV, R, GW = cfg.H, cfg.T, cfg.TT, cfg.NCTX, cfg.HV, cfg.R, cfg.GW
    NCHK = TT // 128
    NCC = NCTX // 128
    order = [list(range(NCHK)), list(range(NCC - 1, -1, -1)) + list(range(NCHK - 1, NCC - 1, -1))]
    HG = min(4, H)
    identb, identf, onesb, onesf = C["identb"], C["identf"], C["onesb"], C["onesf"]
    A = Arena(S, "gd")
    def indicator(name, sg, strict):
        m = A.sb(name, [128, 128], F32)
        S.emit("pool", lambda e: e.memset(m[:], 1.0), writes=[m])
        S.emit("pool", lambda e: e.affine_select(out=m[:], in_=m[:], pattern=[[sg, 128]], compare_op=ALU.is_ge, fill=0.0,
                                                 base=-strict, channel_multiplier=-sg), [m], [m])
        return m
    VI = [indicator("VIf", 1, 0), indicator("VIb", -1, 0)]
    VS = [indicator("VSf", 1, 1), indicator("VSb", -1, 1)]
    NMI, NMS = [], []
    for d in range(2):
        for (lst, src, nm) in ((NMI, VI[d], f"NMI{d}"), (NMS, VS[d], f"NMS{d}")):
            m = A.sb(nm, [128, 128], BF16)
            S.emit("dve", lambda e, m=m, src=src: e.tensor_scalar(out=m[:], in0=src[:], scalar1=-1.0, scalar2=-NEG, op0=ALU.add, op1=ALU.mult), [src], [m])
            lst.append(m)
    dmask = A.sb("dmask", [128, 128], BF16)
    S.emit("pool", lambda e: e.memset(dmask[:], 1.0), writes=[dmask])
    S.emit("pool", lambda e: e.affine_select(out=dmask[:].rearrange("p (b c) -> p b c", c=32), in_=dmask[:].rearrange("p (b c) -> p b c", c=32),
                                             pattern=[[-32, 4], [0, 32]], compare_op=ALU.is_ge, fill=0.0, base=0, channel_multiplier=1), [dmask], [dmask])
    S.emit("pool", lambda e: e.affine_select(out=dmask[:].rearrange("p (b c) -> p b c", c=32), in_=dmask[:].rearrange("p (b c) -> p b c", c=32),
                                             pattern=[[32, 4], [0, 32]], compare_op=ALU.is_ge, fill=0.0, base=31, channel_multiplier=-1), [dmask], [dmask])
    omask = A.sb("omask", [128, 128], BF16)
    S.emit("dve", lambda e: e.tensor_scalar(out=omask[:], in0=dmask[:], scalar1=-1.0, scalar2=1.0, op0=ALU.mult, op1=ALU.add), [dmask], [omask])
    LC = [VI[0], VI[1]]
    LR = [VS[1], VS[0]]
    W2 = 2 * H
    sh = [128, NCHK, W2]
    bet = A.sb("bet", sh, F32)
    gsp = A.sb("gsp", [128, NCHK, 2, W2], BF16)
    lsp = A.sb("lsp", [128, NCHK, 2, W2], BF16)
    egr = A.sb("egr", sh, F32)
    egt = A.sb("egt", sh, F32)
    nbe = A.sb("nbe", sh, F32)
    A0 = Arena(S, "gd0")
    gg = A0.sb("gg", sh, F32)
    lnb = A0.sb("lnb", sh, F32)
    gres = A0.sb("gres", sh, F32)
    gab = A0.sb("gab", [128, NCHK, 4 * H], F32)
    S.dma("sp", gab[:], G["gab"][:, :].rearrange("(n p) c -> p n c", p=128), [G["gab"]], [gab])
    cst = A0.sb("cst", [128, 2, 2 * H], F32)
    S.dma("sp", cst[:, 0, :], I["gdn_dt_bias"][0].rearrange("d h -> (d h)").partition_broadcast(128), [I["gdn_dt_bias"]], [cst])
    S.dma("sp", cst[:, 1, :], I["gdn_a_log"][0].rearrange("d h -> (d h)").partition_broadcast(128), [I["gdn_a_log"]], [cst])
    nea = A0.sb("nea", [128, 2 * H], F32)
    S.emit("act", lambda e: e.activation(out=nea[:], in_=cst[:, 1, :], func=AF.Exp), [cst], [nea])
    S.emit("dve", lambda e: e.tensor_scalar(out=nea[:], in0=nea[:], scalar1=-1.0, scalar2=None, op0=ALU.mult), [nea], [nea])
    gx = A0.sb("gx", sh, F32); gt1 = A0.sb("gt1", sh, F32); gt2 = A0.sb("gt2", sh, F32)
    bc = lambda t: t.rearrange("p (o c) -> p o c", o=1).to_broadcast(sh)
    S.emit("dve", lambda e: e.tensor_tensor(out=gx[:], in0=gab[:, :, 0:W2], in1=bc(cst[:, 0, :]), op=ALU.add), [gab, cst], [gx])
    S.emit("dve", lambda e: e.tensor_scalar(out=gt1[:], in0=gx[:], scalar1=-1.0, scalar2=None, op0=ALU.mult), [gx], [gt1])
    S.emit("dve", lambda e: e.tensor_tensor(out=gt1[:], in0=gt1[:], in1=gx[:], op=ALU.max), [gt1, gx], [gt1])
    S.emit("act", lambda e: e.activation(out=gt1[:], in_=gt1[:], func=AF.Exp, scale=-1.0), [gt1], [gt1])
    S.emit("act", lambda e: e.activation(out=gt1[:], in_=gt1[:], func=AF.Ln, bias=C["onec"][:, 0:1]), [gt1, C["onec"]], [gt1])
    S.emit("dve", lambda e: e.tensor_scalar(out=gt2[:], in0=gx[:], scalar1=0.0, scalar2=None, op0=ALU.max), [gx], [gt2])
    S.emit("dve", lambda e: e.tensor_tensor(out=gt2[:], in0=gt2[:], in1=gt1[:], op=ALU.add), [gt2, gt1], [gt2])
    S.emit("dve", lambda e: e.tensor_tensor(out=gg[:], in0=gt2[:], in1=bc(nea[:]), op=ALU.mult), [gt2, nea], [gg])
    S.emit("act", lambda e: e.activation(out=bet[:], in_=gab[:, :, W2:2 * W2], func=AF.Sigmoid), [gab], [bet])
    S.emit("act", lambda e: e.activation(out=lnb[:], in_=bet[:], func=AF.Ln), [bet], [lnb])
    for src, dst in ((gg, gsp), (lnb, lsp)):
        S.emit("dve", lambda e, src=src, dst=dst: e.tensor_copy(out=dst[:, :, 0, :], in_=src[:]), [src], [dst])
        S.emit("dve", lambda e, src=src, dst=dst: e.tensor_tensor(out=gres[:], in0=src[:], in1=dst[:, :, 0, :], op=ALU.subtract), [src, dst], [gres])
        S.emit("dve", lambda e, dst=dst: e.tensor_copy(out=dst[:, :, 1, :], in_=gres[:]), [gres, dst], [dst])
    pA = A.ps("pA", [128, 512], F32)
    pg = pA
    for n in range(NCHK):
        def mm(e, n=n):
            e.matmul(pg[:, 0:H], lhsT=LC[0][:], rhs=gg[:, n, 0:H], start=True, stop=True)
            e.matmul(pg[:, H:W2], lhsT=LC[1][:], rhs=gg[:, n, H:W2], start=True, stop=True)
            e.matmul(pg[:, W2:W2 + H], lhsT=LR[0][:], rhs=gg[:, n, 0:H], start=True, stop=True)
            e.matmul(pg[:, W2 + H:2 * W2], lhsT=LR[1][:], rhs=gg[:, n, H:W2], start=True, stop=True)
            return e.matmul(pg[:, 2 * W2:3 * W2], lhsT=onesf[:], rhs=gg[:, n, :], start=True, stop=True)
        S.emit("pe", mm, [LC[0], LC[1], LR[0], LR[1], onesf, gg], [pg])
        S.emit("act", lambda e, n=n: e.activation(out=egr[:, n, :], in_=pg[:, W2:2 * W2], func=AF.Exp), [pg, egr], [egr])
        S.emit("act", lambda e, n=n: e.activation(out=egt[:, n, :], in_=pg[:, 2 * W2:3 * W2], func=AF.Exp), [pg, egt], [egt])
        S.emit("act", lambda e, n=n: e.activation(out=nbe[:, n, :], in_=pg[:, 0:W2], func=AF.Exp), [pg, nbe], [nbe])
    S.emit("dve", lambda e: e.scalar_tensor_tensor(out=nbe[:], in0=nbe[:], scalar=-1.0, in1=bet[:], op0=ALU.mult, op1=ALU.mult), [nbe, bet], [nbe])
    if "dbg_g" in G:
        for i, t in enumerate((gg, gg, egr, egt, nbe, bet)):
            S.dma("sp", G["dbg_g"][i], t[:], [t], [G["dbg_g"]])
    A0.close()
    LCb = []
    for d in range(2):
        m = A.sb(f"LCb{d}", [128, 128], BF16)
        S.emit("dve", lambda e, m=m, d=d: e.tensor_copy(out=m[:], in_=LC[d][:]), [LC[d]], [m])
        LCb.append(m)
    gain = A.sb("gain", [128, H], F32)
    S.dma("sp", gain[:], I["gdn_norm"][0].rearrange("(h p) -> p h", p=128), [I["gdn_norm"]], [gain], allow_slow_non_contiguous=True)

    NU = 2 * HG
    ld = {nm: [[A.sb(f"ld{nm}{d}{i}", [128, HG, 128], BF16) for i in range(3 if nm == "k" else 2)] for d in range(2)] for nm in "kqv"}
    U = []
    for u in range(NU):
        ub = {}
        for nm in ("TT", "PT", "qg", "kd", "vb"):
            ub[nm] = [A.sb(f"u{u}{nm}{i}", [128, 128], BF16) for i in range(2 if nm == "TT" else 3)]
        ub["X"] = [[A.sb(f"u{u}X{j}{i}", [128, 4, 128], BF16) for i in range(2)] for j in range(2)]
        ub["Z0"] = [A.sb(f"u{u}Z0{j}", [128, 128], BF16) for j in range(2)]
        ub["OT"] = [A.sb(f"u{u}OT{j}", [128, 128], BF16) for j in range(2)]
        ub["gm"] = [A.sb(f"u{u}gm{j}", [128, 2, 128], BF16) for j in range(2)]
        ub["eg"] = [A.sb(f"u{u}eg{j}", [128, 128], BF16) for j in range(2)]
        ub["DD"] = A.sb(f"u{u}DD", [128, 2, 128], BF16)
        ub["NN"] = A.sb(f"u{u}NN", [128, 2, 128], BF16)
        ub["N2"] = A.sb(f"u{u}N2", [128, 2, 128], BF16)
        ub["PP"] = A.sb(f"u{u}PP", [128, 2, 128], BF16)
        ub["S"] = A.sb(f"u{u}S", [128, 128], F32)
        ub["Sb"] = A.sb(f"u{u}Sb", [128, 128], BF16)
        ub["r"] = A.sb(f"u{u}r", [128, 128], BF16)
        ub["vn"] = A.sb(f"u{u}vn", [128, 128], BF16)
        U.append(ub)
    ngu = [A.sb(f"ngu{i}", [128, 1], F32) for i in range(2)]
    OB = A.sb("OB", [128, HG, T], F32)
    zq = A.sb("zq", [128, 512], BF16); sqb = A.sb("sqb", [128, 512], BF16); rt = A.sb("rt", [128, 512], F32)
    ys = [sqb, sqb]
    djunk = rt
    pR = A.ps("pR", [128, 512], F32)
    pT = A.ps("pT", [128, 1024], BF16)
    pI = [A.ps(f"pI{i}", [128, 512], F32) for i in range(4)]
    pS = A.ps("pS", [128, 512], F32)
    ncol = 128 // R if R < 128 else 1
    v4 = lambda p: p[:, 0:512].rearrange("p (a b) -> p a b", b=128)

    def run_streams(streams):
        streams = [g for g in streams if g is not None]
        while streams:
            for g in list(streams):
                try:
                    next(g)
                except StopIteration:
                    streams.remove(g)

    for hg in range(H // HG):
        for ub in U:
            S.emit("pool", lambda e, ub=ub: e.memset(ub["S"][:], 0.0), writes=[ub["S"]])
            S.emit("pool", lambda e, ub=ub: e.memset(ub["Sb"][:], 0.0), writes=[ub["Sb"]])
        OBc = {}

        def units_of(s):
            out = []
            for d in range(2):
                c = order[d][s]
                for hl in range(HG):
                    out.append((d, hl, c, c >= NCC))
            return out

        def gen_P(s):
            s3, s2 = s % 3, s % 2
            for d in range(2):
                c = order[d][s]
                t0 = c * 128
                lat = c >= NCC
                for nm, src in (("k", G["gk"]), ("q", G["gq"]), ("v", G["gv"])):
                    if nm == "q" and not lat:
                        continue
                    dst = ld[nm][d][s3 if nm == "k" else s2]
                    S.dma("sp" if d == 0 else "pool", dst[:],
                          src[hg * HG * 128:(hg + 1) * HG * 128, t0:t0 + 128].rearrange("(h p) t -> p h t", p=128), [src], [dst])
            yield
            ii = 0
            for (d, hl, c, lat) in units_of(s):
                u = d * HG + hl
                ub = U[u]
                h = hg * HG + hl
                dh = d * H + h
                kT = ld["k"][d][s3]; qTb = ld["q"][d][s2]; vT = ld["v"][d][s2]
                X0 = ub["X"][s2][0]
                X1 = ub["X"][s2][1]
                Z0 = ub["Z0"][s2]
                OT = ub["OT"][s2]
                def mA(e, kT=kT, qTb=qTb, hl=hl, lat=lat):
                    r = e.matmul(pA[:, 0:128], lhsT=kT[:, hl, :], rhs=kT[:, hl, :], start=True, stop=True)
                    if lat:
                        r = e.matmul(pA[:, 128:256], lhsT=kT[:, hl, :], rhs=qTb[:, hl, :], start=True, stop=True)
                    return r
                S.emit("pe", mA, [kT, qTb], [pA])
                yield
                gm = ub["gm"][s2]
                eg = ub["eg"][s2]
                S.emit("dve", lambda e, Z0=Z0, gm=gm: e.scalar_tensor_tensor(out=Z0[:], in0=pA[:, 0:128], scalar=-1.0, in1=gm[:, 1, :],
                                                                             op0=ALU.mult, op1=ALU.mult), [pA, gm], [Z0])
                yield
                def mT(e, Z0=Z0, kT=kT, vT=vT, hl=hl):
                    e.transpose(out=pT[:, 0:128], in_=Z0[:], identity=identb[:])
                    e.transpose(out=pT[:, 128:256], in_=kT[:, hl, :], identity=identb[:])
                    return e.transpose(out=pT[:, 256:384], in_=vT[:, hl, :], identity=identb[:])
                S.emit("pe", mT, [Z0, kT, vT, identb], [pT])
                if lat:
                    S.emit("dve", lambda e, ub=ub, s3=s3, gm=gm: e.tensor_tensor(out=ub["PT"][s3][:], in0=pA[:, 128:256], in1=gm[:, 0, :], op=ALU.mult),
                           [pA, gm], [ub["PT"][s3]])
                S.emit("pool", lambda e, X0=X0, Z0=Z0: e.tensor_tensor(out=X0[:, 0, :], in0=Z0[:], in1=dmask[:], op=ALU.mult), [Z0, dmask, X0], [X0])
                yield
                if lat:
                    S.emit("dve", lambda e, ub=ub, s3=s3, eg=eg, qTb=qTb, hl=hl: e.tensor_tensor(out=ub["qg"][s3][:], in0=qTb[:, hl, :], in1=eg[:], op=ALU.mult),
                           [qTb, eg], [ub["qg"][s3]])
                S.emit("pool", lambda e, X1=X1, X0=X0: e.tensor_tensor(out=X1[:, 1, :], in0=X0[:, 0, :], in1=identb[:], op=ALU.add), [X0, identb, X1], [X1])
                yield
                S.emit("dve", lambda e, X0=X0: e.tensor_tensor(out=X0[:, 2, :], in0=pT[:, 0:128], in1=dmask[:], op=ALU.mult), [pT, dmask, X0], [X0])
                S.emit("dve", lambda e, OT=OT: e.tensor_tensor(out=OT[:], in0=pT[:, 0:128], in1=omask[:], op=ALU.mult), [pT, omask], [OT])
                S.emit("act", lambda e, ub=ub, s3=s3, c=c, dh=dh: e.activation(out=ub["kd"][s3][:], in_=pT[:, 128:256], func=AF.Copy, scale=egr[:, c, dh:dh + 1]),
                       [pT, egr], [ub["kd"][s3]])
                S.emit("act", lambda e, ub=ub, s3=s3, c=c, dh=dh: e.activation(out=ub["vb"][s3][:], in_=pT[:, 256:384], func=AF.Copy, scale=bet[:, c, dh:dh + 1]),
                       [pT, bet], [ub["vb"][s3]])
                yield
                S.emit("pool", lambda e, X1=X1, X0=X0: e.tensor_tensor(out=X1[:, 3, :], in0=X0[:, 2, :], in1=identb[:], op=ALU.add), [X0, identb, X1], [X1])
                yield

        def gen_G(s):
            s2 = s % 2
            ii = 0
            for (d, hl, c, lat) in units_of(s):
                u = d * HG + hl
                ub = U[u]
                h = hg * HG + hl
                dh = d * H + h
                gm = ub["gm"][s2]
                eg = ub["eg"][s2]
                ng = ngu[ii % 2]
                ii += 1
                def mR(e, c=c, dh=dh, d=d, lat=lat):
                    ghi = gsp[:, c, 0, dh:dh + 1].to_broadcast([128, 128])
                    glo = gsp[:, c, 1, dh:dh + 1].to_broadcast([128, 128])
                    e.matmul(pR[:, 256:384], lhsT=ghi, rhs=LCb[d][:], start=True, stop=False)
                    e.matmul(pR[:, 256:384], lhsT=glo, rhs=LCb[d][:], start=False, stop=True)
                    if lat:
                        e.matmul(pR[:, 0:128], lhsT=ghi, rhs=LCb[d][:], start=True, stop=False)
                        e.matmul(pR[:, 0:128], lhsT=glo, rhs=LCb[d][:], start=False, stop=False)
                        e.matmul(pR[:, 0:128], lhsT=identb[:], rhs=NMI[d][:], start=False, stop=True)
                    e.matmul(pR[:, 128:256], lhsT=ghi, rhs=LCb[d][:], start=True, stop=False)
                    e.matmul(pR[:, 128:256], lhsT=glo, rhs=LCb[d][:], start=False, stop=False)
                    e.matmul(pR[:, 128:256], lhsT=lsp[:, c, 0, dh:dh + 1].to_broadcast([128, 128]), rhs=identb[:], start=False, stop=False)
                    e.matmul(pR[:, 128:256], lhsT=lsp[:, c, 1, dh:dh + 1].to_broadcast([128, 128]), rhs=identb[:], start=False, stop=False)
                    return e.matmul(pR[:, 128:256], lhsT=identb[:], rhs=NMS[d][:], start=False, stop=True)
                S.emit("pe", mR, [gsp, lsp, LCb[d], identb, NMI[d], NMS[d]], [pR])
                yield
                S.emit("dve", lambda e: e.tensor_tensor(out=djunk[:, 0:128], in0=pR[:, 256:384], in1=identf[:], op=ALU.mult), [pR, identf], [djunk])
                yield
                S.emit("dve", lambda e, ng=ng: e.tensor_reduce(out=ng[:, 0:1], in_=djunk[:, 0:128], axis=mybir.AxisListType.X, op=ALU.add, negate=True), [djunk], [ng])
                yield
                lo = 0 if lat else 1
                S.emit("act", lambda e, gm=gm, ng=ng, lo=lo: e.activation(out=gm[:, lo:2, :], in_=pR[:, lo * 128:256].rearrange("p (a b) -> p a b", b=128),
                                                                          func=AF.Exp, bias=ng[:, 0:1]), [pR, ng], [gm])
                if lat:
                    S.emit("act", lambda e, eg=eg: e.activation(out=eg[:], in_=pR[:, 256:384], func=AF.Exp), [pR], [eg])
                yield

        def gen_I(s, bank):
            s3, s2 = s % 3, s % 2
            p = pI[bank]
            for (d, hl, c, lat) in units_of(s):
                u = d * HG + hl
                if u % 4 != bank:
                    continue
                ub = U[u]
                X = ub["X"][s2]
                DD, NN, N2, PP, OT = ub["DD"], ub["NN"], ub["N2"], ub["PP"], ub["OT"][s2]
                for st in range(9):
                    if st == 0:
                        Xc, Xn = X[0], X[1]
                        def m0(e, Xc=Xc):
                            e.matmul(p[:, 0:128], lhsT=Xc[:, 2, :], rhs=Xc[:, 0, :], start=True, stop=True)
                            return e.matmul(p[:, 256:384], lhsT=Xc[:, 0, :], rhs=Xc[:, 2, :], start=True, stop=True)
                        S.emit("pe", m0, [Xc], [p])
                        yield
                        S.emit("act", lambda e, Xn=Xn: e.copy(out=Xn[:, 0:4:2, :], in_=v4(p)[:, 0:4:2, :]), [p, Xn], [Xn])
                    elif st < 4:
                        Xc, Xn = X[st % 2], X[(st + 1) % 2]
                        def m1(e, Xc=Xc):
                            e.matmul(p[:, 0:256], lhsT=Xc[:, 2, :], rhs=Xc[:, 0:2, :], start=True, stop=True)
                            return e.matmul(p[:, 256:512], lhsT=Xc[:, 0, :], rhs=Xc[:, 2:4, :], start=True, stop=True)
                        S.emit("pe", m1, [Xc], [p])
                        yield
                        S.emit("act", lambda e, Xn=Xn: e.copy(out=Xn[:, 0:4:2, :], in_=v4(p)[:, 0:4:2, :]), [p, Xn], [Xn])
                        yield
                        S.emit("dve", lambda e, Xn=Xn, Xc=Xc: e.tensor_tensor(out=Xn[:, 1:4:2, :], in0=v4(p)[:, 1:4:2, :], in1=Xc[:, 1:4:2, :], op=ALU.add),
                               [p, Xc, Xn], [Xn])
                    elif st == 4:
                        Xc = X[0]
                        def m4(e, Xc=Xc):
                            e.matmul(p[:, 128:256], lhsT=Xc[:, 2, :], rhs=Xc[:, 1, :], start=True, stop=True)
                            return e.matmul(p[:, 384:512], lhsT=Xc[:, 0, :], rhs=Xc[:, 3, :], start=True, stop=True)
                        S.emit("pe", m4, [Xc], [p])
                        yield
                        S.emit("dve", lambda e, DD=DD, Xc=Xc: e.tensor_tensor(out=DD[:], in0=v4(p)[:, 1:4:2, :], in1=Xc[:, 1:4:2, :], op=ALU.add), [p, Xc], [DD])
                    elif st == 5:
                        def m5(e, DD=DD, OT=OT):
                            e.matmul(p[:, 0:128], lhsT=OT[:], rhs=DD[:, 0, :], start=True, stop=True)
                            return e.matmul(p[:, 128:256], lhsT=DD[:, 0, :], rhs=OT[:], start=True, stop=True)
                        S.emit("pe", m5, [DD, OT], [p])
                        yield
                        S.emit("act", lambda e, NN=NN: e.copy(out=NN[:], in_=v4(p)[:, 0:2, :]), [p], [NN])
                        yield
                        S.emit("dve", lambda e, PP=PP: e.tensor_tensor(out=PP[:, 0, :], in0=p[:, 0:128], in1=identb[:], op=ALU.add), [p, identb, PP], [PP])
                    elif st == 6:
                        def m6(e, NN=NN):
                            e.matmul(p[:, 0:128], lhsT=NN[:, 1, :], rhs=NN[:, 0, :], start=True, stop=True)
                            return e.matmul(p[:, 128:256], lhsT=NN[:, 0, :], rhs=NN[:, 1, :], start=True, stop=True)
                        S.emit("pe", m6, [NN], [p])
                        yield
                        S.emit("act", lambda e, N2=N2: e.copy(out=N2[:], in_=v4(p)[:, 0:2, :]), [p], [N2])
                    elif st == 7:
                        S.emit("pe", lambda e, N2=N2, PP=PP: e.matmul(p[:, 0:128], lhsT=N2[:, 1, :], rhs=PP[:, 0, :], start=True, stop=True), [N2, PP], [p])
                        yield
                        S.emit("dve", lambda e, PP=PP: e.tensor_tensor(out=PP[:, 1, :], in0=p[:, 0:128], in1=PP[:, 0, :], op=ALU.add), [p, PP], [PP])
                    else:
                        S.emit("pe", lambda e, DD=DD, PP=PP: e.matmul(p[:, 0:128], lhsT=DD[:, 1, :], rhs=PP[:, 1, :], start=True, stop=True), [DD, PP], [p])
                        yield
                        S.emit("act", lambda e, ub=ub, s3=s3: e.copy(out=ub["TT"][s2][:], in_=p[:, 0:128]), [p], [ub["TT"][s2]])
                    yield

        def gen_S(s):
            s3 = s % 3
            for (d, hl, c, lat) in units_of(s):
                u = d * HG + hl
                ub = U[u]
                h = hg * HG + hl
                dh = d * H + h
                kT = ld["k"][d][s3]
                S.emit("pe", lambda e, kT=kT, hl=hl, ub=ub: e.matmul(pS[:, 0:128], lhsT=kT[:, hl, :], rhs=ub["Sb"][:], start=True, stop=True), [kT, ub["Sb"]], [pS])
                yield
                S.emit("dve", lambda e, ub=ub, s3=s3, c=c, dh=dh: e.scalar_tensor_tensor(out=ub["r"][:], in0=pS[:, 0:128], scalar=nbe[:, c, dh:dh + 1], in1=ub["vb"][s3][:],
                                                                                        op0=ALU.mult, op1=ALU.add), [pS, nbe, ub["vb"][s3]], [ub["r"]])
                yield
                S.emit("pe", lambda e, ub=ub, s=s: e.matmul(pS[:, 128:256], lhsT=ub["TT"][s % 2][:], rhs=ub["r"][:], start=True, stop=True), [ub["TT"][s % 2], ub["r"]], [pS])
                yield
                S.emit("act", lambda e, ub=ub: e.copy(out=ub["vn"][:], in_=pS[:, 128:256]), [pS], [ub["vn"]])
                yield
                def mo(e, ub=ub, s3=s3, lat=lat):
                    if lat:
                        e.matmul(pS[:, 384:512], lhsT=ub["Sb"][:], rhs=ub["qg"][s3][:], start=True, stop=False)
                        e.matmul(pS[:, 384:512], lhsT=ub["vn"][:], rhs=ub["PT"][s3][:], start=False, stop=True)
                    return e.matmul(pS[:, 256:384], lhsT=ub["kd"][s3][:], rhs=ub["vn"][:], start=True, stop=True)
                S.emit("pe", mo, [ub["Sb"], ub["qg"][s3], ub["vn"], ub["PT"][s3], ub["kd"][s3]], [pS])
                yield
                if lat:
                    cl = c - NCC
                    if R >= 128:
                        per = R // 128
                        cc0, sub = divmod(cl, per)
                        oap = OB[:, hl, :].rearrange("p (r c) -> p c r", c=GW)[:, cc0, sub * 128:(sub + 1) * 128]
                        iap = pS[:, 384:512]
                    else:
                        oap = OB[:, hl, :].rearrange("p (r c) -> p c r", c=GW)[:, cl * ncol:(cl + 1) * ncol, :]
                        iap = pS[:, 384:512].rearrange("p (a b) -> p a b", b=R)
                    key = (hl, cl)
                    if key not in OBc:
                        OBc[key] = Buf(f"OBc{hl}_{cl}")
                        OBc[key].ws = dict(OB.ws)
                        OBc[key].rs = dict(OB.rs)
                        S.emit("act", lambda e, oap=oap, iap=iap: e.copy(out=oap, in_=iap), [pS], [OBc[key]])
                    else:
                        S.emit("dve", lambda e, oap=oap, iap=iap: e.tensor_tensor(out=oap, in0=iap, in1=oap, op=ALU.add), [pS, OBc[key]], [OBc[key]])
                S.emit("dve", lambda e, ub=ub, c=c, dh=dh: e.scalar_tensor_tensor(out=ub["S"][:], in0=ub["S"][:], scalar=egt[:, c, dh:dh + 1], in1=pS[:, 256:384],
                                                                                 op0=ALU.mult, op1=ALU.add), [ub["S"], egt, pS], [ub["S"]])
                yield
                S.emit("act", lambda e, ub=ub: e.copy(out=ub["Sb"][:], in_=ub["S"][:]), [ub["S"]], [ub["Sb"]])
                yield

        for it in range(-3, NCHK):
            sts = []
            if 0 <= it + 3 < NCHK:
                sts.append(gen_G(it + 3))
            if 0 <= it + 2 < NCHK:
                sts.append(gen_P(it + 2))
            if 0 <= it + 1 < NCHK:
                sts += [gen_I(it + 1, b) for b in range(4)]
            if it >= 0:
                sts.append(gen_S(it))
            run_streams(sts)
            if "dbg_S" in G and it == NCC - 1 and hg == 0:
                for u in range(NU):
                    S.dma("sp", G["dbg_S"][u], U[u]["S"][:], [U[u]["S"]], [G["dbg_S"]])
        for key, bk in OBc.items():
            for k_, v_ in bk.ws.items():
                if OB.ws.get(k_, 0) < v_:
                    OB.ws[k_] = v_
        for hl in range(HG):
            h = hg * HG + hl
            if "dbg_ob" in G:
                S.dma("sp", G["dbg_ob"][h * 128:(h + 1) * 128, :], OB[:, hl, :], [OB], [G["dbg_ob"]])
            for bi, t0 in enumerate(range(0, T, 512)):
                nb = min(512, T - t0)
                S.dma("sp", zq[:, 0:nb], G["gzT"][h * 128:(h + 1) * 128, t0:t0 + nb], [G["gzT"]], [zq])
                S.emit("act", lambda e, hl=hl, t0=t0, nb=nb: e.activation(out=sqb[:, 0:nb], in_=OB[:, hl, t0:t0 + nb], func=AF.Square), [OB], [sqb])
                S.emit("pe", lambda e, nb=nb: e.matmul(pA[:, 0:nb], lhsT=onesb[:], rhs=sqb[:, 0:nb], start=True, stop=True), [onesb, sqb], [pA])
                S.emit("act", lambda e, nb=nb: e.activation(out=rt[:, 0:nb], in_=pA[:, 0:nb], func=AF.Ln, scale=1.0 / 128, bias=C["epsc"][:, 0:1]), [pA, C["epsc"]], [rt])
                S.emit("act", lambda e, nb=nb: e.activation(out=rt[:, 0:nb], in_=rt[:, 0:nb], func=AF.Exp, scale=-0.5), [rt], [rt])
                S.emit("dve", lambda e, nb=nb: e.tensor_tensor(out=rt[:, 0:nb], in0=rt[:, 0:nb], in1=zq[:, 0:nb], op=ALU.mult), [rt, zq], [rt])
                y = ys[bi % 2]
                S.emit("dve", lambda e, hl=hl, h=h, t0=t0, nb=nb, y=y: e.scalar_tensor_tensor(out=y[:, 0:nb], in0=OB[:, hl, t0:t0 + nb], scalar=gain[:, h:h + 1], in1=rt[:, 0:nb],
                                                                                             op0=ALU.mult, op1=ALU.mult), [OB, gain, rt], [y])
                S.dma("sp", G["yBT"][h * 128:(h + 1) * 128, t0:t0 + nb], y[:, 0:nb], [y], [G["yBT"]])
    A.close()


def phase_merge(S, cfg, G, I, C, cfgL):
    A = Arena(S, "mg")
    D, T, HV, KD = cfg.D, cfg.T, cfg.HV, cfg.KD
    KH = HV // 128
    HL = cfgL.H
    NRK = cfg.H // HL
    wa = A.sb("wa", [128, KH, D], BF16)
    wb = A.sb("wb", [128, KH, D], BF16)
    for k in range(KH):
        S.dma("pool", wa[:, k, :], I["w_branch_a"][0, k * 128:(k + 1) * 128, :], [I["w_branch_a"]], [wa])
        S.dma("pool", wb[:, k, :], I["w_branch_b"][0, k * 128:(k + 1) * 128, :], [I["w_branch_b"]], [wb])
    ya = [A.sb(f"ya{i}", [128, KH, 512], BF16) for i in range(2)]
    yb = [A.sb(f"yb{i}", [128, KH, 512], BF16) for i in range(2)]
    ga = [A.sb(f"ga{i}", [128, 512], BF16) for i in range(2)]
    gb = [A.sb(f"gb{i}", [128, 512], BF16) for i in range(2)]
    t1 = [A.sb(f"t1{i}", [128, 512], F32) for i in range(2)]
    t2 = [A.sb(f"t2{i}", [128, 512], F32) for i in range(2)]
    mo = [A.sb(f"mo{i}", [128, 512], BF16) for i in range(2)]
    pa = [A.ps(f"pa{i}", [128, 512], F32) for i in range(2)]
    pb = [A.ps(f"pb{i}", [128, 512], F32) for i in range(2)]
    it = 0
    for bi, t0 in enumerate(range(0, T, 512)):
        nb = min(512, T - t0)
        a_, b_ = ya[bi % 2], yb[bi % 2]
        CR = min(256, 2 * HL * 128)
        for r in range(NRK):
            for l in range(HL):
                for (dst, rho) in ((a_, l * 128), (b_, HL * 128 + l * 128)):
                    ck, off = divmod(rho, CR)
                    S.dma("sp", dst[:, r * HL + l, 0:nb], G["yall"][ck, r * CR + off:r * CR + off + 128, t0:t0 + nb], [G["yall"]], [dst])
        for dc in range(KD):
            i2 = it % 2
            it += 1
            S.dma("sp", ga[i2][:, 0:nb], G["mgT"][dc * 128:(dc + 1) * 128, t0:t0 + nb], [G["mgT"]], [ga[i2]])
            S.dma("sp", gb[i2][:, 0:nb], G["mgT"][D + dc * 128:D + (dc + 1) * 128, t0:t0 + nb], [G["mgT"]], [gb[i2]])
            def mm(e, w, y, p, dc=dc, nb=nb):
                r = None
                for k in range(KH):
                    r = e.matmul(p[:, 0:nb], lhsT=w[:, k, dc * 128:(dc + 1) * 128], rhs=y[:, k, 0:nb], start=(k == 0), stop=(k == KH - 1))
                return r
            S.emit("pe", lambda e, i2=i2, a_=a_, mm=mm: mm(e, wa, a_, pa[i2]), [wa, a_], [pa[i2]])
            S.emit("pe", lambda e, i2=i2, b_=b_, mm=mm: mm(e, wb, b_, pb[i2]), [wb, b_], [pb[i2]])
            S.emit("dve", lambda e, i2=i2, nb=nb: e.tensor_tensor(out=t1[i2][:, 0:nb], in0=pa[i2][:, 0:nb], in1=ga[i2][:, 0:nb], op=ALU.mult), [pa[i2], ga[i2]], [t1[i2]])
            S.emit("dve", lambda e, i2=i2, nb=nb: e.tensor_tensor(out=t2[i2][:, 0:nb], in0=pb[i2][:, 0:nb], in1=gb[i2][:, 0:nb], op=ALU.mult), [pb[i2], gb[i2]], [t2[i2]])
            S.emit("pool", lambda e, i2=i2, nb=nb: e.tensor_tensor(out=mo[i2][:, 0:nb], in0=t1[i2][:, 0:nb], in1=t2[i2][:, 0:nb], op=ALU.add), [t1[i2], t2[i2]], [mo[i2]])
            S.dma("sp", G["mT"][dc * 128:(dc + 1) * 128, t0:t0 + nb], mo[i2][:, 0:nb], [mo[i2]], [G["mT"]])
    A.close()


def load_row_bcast(S, A, name, src_ap, srcbuf, n):
    t = A.sb(name, [128, n], F32)
    S.dma("sp", t[:], src_ap.partition_broadcast(128), [srcbuf], [t])
    return t


def phase_outproj(S, cfg, G, I, C):
    A = Arena(S, "op")
    D, T, KD = cfg.D, cfg.T, cfg.KD
    wo = A.sb("wo", [128, KD, D], BF16)
    for k in range(KD):
        S.dma("pool", wo[:, k, :], I["w_out"][0, k * 128:(k + 1) * 128, :], [I["w_out"]], [wo])
    g1 = load_row_bcast(S, A, "g1", G["grow"][0], G["grow"], D)
    mt = [A.sb(f"mt{i}", [128, KD, 128], BF16) for i in range(2)]
    xt = [A.sb(f"xt{i}", [128, D], F32) for i in range(2)]
    tm = [A.sb(f"tm{i}", [128, 512], F32) for i in range(2)]
    pp = [A.ps(f"pp{i}", [128, 512], F32) for i in range(4)]
    pi = 0
    for ti in range(T // 128):
        t0 = ti * 128
        m_, x_ = mt[ti % 2], xt[ti % 2]
        S.dma("sp", m_[:], G["mT"][:, t0:t0 + 128].rearrange("(k p) t -> p k t", p=128), [G["mT"]], [m_])
        S.dma("pool", x_[:], I["x"][0, t0:t0 + 128, :], [I["x"]], [x_])
        for oc in range(0, D, 512):
            ow = min(512, D - oc)
            p = pp[pi % 4]
            t_ = tm[pi % 2]
            pi += 1
            def mm(e, m_=m_, p=p, oc=oc, ow=ow):
                r = None
                for k in range(KD):
                    r = e.matmul(p[:, 0:ow], lhsT=m_[:, k, :], rhs=wo[:, k, oc:oc + ow], start=(k == 0), stop=(k == KD - 1))
                return r
            S.emit("pe", mm, [m_, wo], [p])
            S.emit("dve", lambda e, p=p, t_=t_, oc=oc, ow=ow: e.tensor_tensor(out=t_[:, 0:ow], in0=p[:, 0:ow], in1=g1[:, oc:oc + ow], op=ALU.mult), [p, g1], [t_])
            S.emit("pool", lambda e, x_=x_, t_=t_, oc=oc, ow=ow: e.tensor_tensor(out=x_[:, oc:oc + ow], in0=x_[:, oc:oc + ow], in1=t_[:, 0:ow], op=ALU.add), [x_, t_], [x_])
        S.dma("sp", G["x2"][t0:t0 + 128, :], x_[:], [x_], [G["x2"]])
    A.close()


def phase_route(S, cfg, G, I, C, R_):
    A = Arena(S, "rt")
    D, T, KD, E, CAP = cfg.D, cfg.T, cfg.KD, cfg.E, cfg.CAP
    NSG = CAP // 128
    identf = C["identf"]
    co = G["coef"]
    for j, ci in enumerate((4, 5)):
        S.dma("sp", G["crow"][j].rearrange("(k p) -> p k", p=128), co[:, ci, :], [co], [G["crow"]], allow_slow_non_contiguous=True)
    a2 = load_row_bcast(S, A, "a2", G["crow"][0], G["crow"], D)
    b2 = load_row_bcast(S, A, "b2", G["crow"][1], G["crow"], D)
    rw = A.sb("rw", [128, KD, E], F32)
    S.dma("sp", rw[:], I["router_w"][0].rearrange("(k p) e -> p k e", p=128), [I["router_w"]], [rw])
    affE = [A.sb(f"affE{i}", [E, T], F32) for i in range(2)]
    xt = [A.sb(f"xt{i}", [128, D], F32) for i in range(2)]
    hf = [A.sb(f"hf{i}", [128, D], F32) for i in range(2)]
    hb = [A.sb(f"hb{i}", [128, D], BF16) for i in range(2)]
    hT = [A.sb(f"hT{i}", [128, KD, 128], F32) for i in range(2)]
    junk = A.sb("junk", [128, D], BF16)
    st = [A.sb(f"st{i}", [128, 8], F32) for i in range(2)]
    sm = [A.sb(f"sm{i}", [128, E + 8], F32) for i in range(2)]
    pt = [A.ps(f"pt{i}", [128, 4, 128], F32) for i in range(2)]
    pl = A.ps("pl", [128, 512], F32)
    pq = A.ps("pq", [128, 512], F32)
    gi = 0
    for ti in range(T // 128):
        t0 = ti * 128
        x = xt[ti % 2]; s = st[ti % 2]; h = hf[ti % 2]; hbb = hb[ti % 2]; hTt = hT[ti % 2]; m = sm[ti % 2]
        S.dma("sp" if ti % 2 == 0 else "pool", x[:], G["x2"][t0:t0 + 128, :], [G["x2"]], [x])
        S.emit("act", lambda e, x=x, s=s: e.activation(out=junk[:], in_=x[:], func=AF.Square, accum_out=s[:, 0:1]), [x], [junk, s])
        S.emit("dve", lambda e, s=s: e.tensor_scalar(out=s[:, 1:2], in0=s[:, 0:1], scalar1=1.0 / D, scalar2=cfg.eps, op0=ALU.mult, op1=ALU.add), [s], [s])
        S.emit("act", lambda e, s=s: e.activation(out=s[:, 2:3], in_=s[:, 1:2], func=AF.Sqrt), [s], [s])
        S.emit("dve", lambda e, s=s: e.reciprocal(out=s[:, 3:4], in_=s[:, 2:3]), [s], [s])
        S.emit("dve", lambda e, x=x, s=s, h=h: e.scalar_tensor_tensor(out=h[:], in0=x[:], scalar=s[:, 3:4], in1=a2[:], op0=ALU.mult, op1=ALU.mult), [x, s, a2], [h])
        S.emit("pool", lambda e, h=h: e.tensor_tensor(out=h[:], in0=h[:], in1=b2[:], op=ALU.add), [h, b2], [h])
        S.emit("act", lambda e, h=h, hbb=hbb: e.copy(out=hbb[:], in_=h[:]), [h], [hbb])
        S.dma("sp", G["h2"][t0:t0 + 128, :], hbb[:], [hbb], [G["h2"]])
        for g in range(0, KD, 4):
            p = pt[gi % 2]
            gi += 1
            ng = min(4, KD - g)
            def tr(e, h=h, p=p, g=g, ng=ng):
                r = None
                for j in range(ng):
                    r = e.transpose(out=p[:, j, :], in_=h[:, (g + j) * 128:(g + j + 1) * 128], identity=identf[:])
                return r
            S.emit("pe", tr, [h, identf], [p])
            if (gi % 2) == 0:
                S.emit("act", lambda e, p=p, hTt=hTt, g=g, ng=ng: e.copy(out=hTt[:, g:g + ng, :], in_=p[:, 0:ng, :]), [p, hTt], [hTt])
            else:
                S.emit("dve", lambda e, p=p, hTt=hTt, g=g, ng=ng: e.tensor_copy(out=hTt[:, g:g + ng, :], in_=p[:, 0:ng, :]), [p, hTt], [hTt])
        def ml(e, hTt=hTt):
            r = None
            for k in range(KD):
                r = e.matmul(pl[:, 0:E], lhsT=hTt[:, k, :], rhs=rw[:, k, :], start=(k == 0), stop=(k == KD - 1))
            return r
        S.emit("pe", ml, [hTt, rw], [pl])
        S.emit("dve", lambda e, m=m: e.reduce_max(out=m[:, E:E + 1], in_=pl[:, 0:E], axis=mybir.AxisListType.X), [pl], [m])
        S.emit("dve", lambda e, m=m: e.tensor_scalar(out=m[:, E + 1:E + 2], in0=m[:, E:E + 1], scalar1=-1.0, scalar2=None, op0=ALU.mult), [m], [m])
        S.emit("act", lambda e, m=m: e.activation(out=m[:, 0:E], in_=pl[:, 0:E], func=AF.Exp, bias=m[:, E + 1:E + 2], accum_out=m[:, E + 2:E + 3]), [pl, m], [m])
        S.emit("dve", lambda e, m=m: e.reciprocal(out=m[:, E + 3:E + 4], in_=m[:, E + 2:E + 3]), [m], [m])
        S.emit("dve", lambda e, m=m: e.tensor_scalar(out=m[:, 0:E], in0=m[:, 0:E], scalar1=m[:, E + 3:E + 4], scalar2=None, op0=ALU.mult), [m], [m])
        S.emit("pe", lambda e, m=m: e.transpose(out=pq[0:E, 0:128], in_=m[:, 0:E], identity=identf[:]), [m, identf], [pq])
        S.emit("act", lambda e, t0=t0: e.copy(out=affE[0][:, t0:t0 + 128], in_=pq[0:E, 0:128]), [pq, affE[0]], [affE[0]])
    vals = A.sb("vals", [E, CAP], F32)
    idxu = A.sb("idxu", [E, CAP], U32)
    idxf = A.sb("idxf", [E, CAP], F32)
    cur = 0
    for it in range(CAP // 8):
        a = affE[cur]; b = affE[1 - cur]
        S.emit("dve", lambda e, a=a, it=it: e.max(out=vals[:, it * 8:(it + 1) * 8], in_=a[:]), [a, vals], [vals])
        S.emit("dve", lambda e, a=a, it=it: e.max_index(out=idxu[:, it * 8:(it + 1) * 8], in_max=vals[:, it * 8:(it + 1) * 8], in_values=a[:]), [a, vals, idxu], [idxu])
        if it + 1 < CAP // 8:
            S.emit("dve", lambda e, a=a, b=b, it=it: e.match_replace(out=b[:], in_to_replace=vals[:, it * 8:(it + 1) * 8], in_values=a[:], imm_value=-1.0), [a, vals], [b])
            cur = 1 - cur
    S.emit("dve", lambda e: e.tensor_copy(out=idxf[:], in_=idxu[:]), [idxu], [idxf])
    idxT, gateT = R_["idxT"], R_["gateT"]
    for sg in range(NSG):
        S.emit("pe", lambda e, sg=sg: e.transpose(out=pq[:, 0:E], in_=idxf[:, sg * 128:(sg + 1) * 128], identity=identf[0:E, 0:E]), [idxf, identf], [pq])
        S.emit("dve", lambda e, sg=sg: e.tensor_copy(out=idxT[:, sg, :], in_=pq[:, 0:E]), [pq, idxT], [idxT])
        S.emit("pe", lambda e, sg=sg: e.transpose(out=pq[:, 0:E], in_=vals[:, sg * 128:(sg + 1) * 128], identity=identf[0:E, 0:E]), [vals, identf], [pq])
        S.emit("act", lambda e, sg=sg: e.copy(out=gateT[:, sg, :], in_=pq[:, 0:E]), [pq, gateT], [gateT])
    A.close()


def phase_experts(S, cfg, G, I, C, R_):
    A = Arena(S, "ex")
    D, T, KD, E, CAP, DE = cfg.D, cfg.T, cfg.KD, cfg.E, cfg.CAP, cfg.DE
    NSG = CAP // 128
    FH = min(getattr(cfg, "FH", 512), DE)
    NFH = DE // FH
    FC = FH // 128
    identb = C["identb"]
    idxT, gateT = R_["idxT"], R_["gateT"]
    g2 = load_row_bcast(S, A, "g2", G["grow"][1], G["grow"], D)
    wg = [A.sb(f"wg{i}", [128, KD, FH], BF16) for i in range(2)]
    wu = [A.sb(f"wu{i}", [128, KD, FH], BF16) for i in range(2)]
    wd = [A.sb(f"wd{i}", [128, FC, D], BF16) for i in range(2)]
    xs = A.sb("xs", [128, NSG, D], BF16)
    xsT = A.sb("xsT", [128, KD, CAP], BF16)
    sa = A.sb("sa", [128, 512], F32)
    sa2 = A.sb("sa2", [128, 512], F32)
    hd = A.sb("hd", [128, FC, CAP], BF16)
    ysb = A.sb("ysb", [128, NSG, D], F32)
    ptr = [A.ps(f"ptr{i}", [128, 8, 128], BF16) for i in range(2)]
    pg = [A.ps(f"pg{i}", [128, 512], F32) for i in range(2)]
    pu = [A.ps(f"pu{i}", [128, 512], F32) for i in range(2)]
    py = [A.ps(f"py{i}", [128, 512], F32) for i in range(2)]
    gi = 0
    yi = 0
    halves = [(ex, fh) for ex in range(E) for fh in range(NFH)]

    def load_weights(k):
        ex, fh = halves[k]
        w_g, w_u, w_d = wg[k % 2], wu[k % 2], wd[k % 2]
        f0 = fh * FH
        S.dma("pool", w_g[:], I["w_gate"][0, ex, :, f0:f0 + FH].rearrange("(k p) f -> p k f", p=128), [I["w_gate"]], [w_g])
        S.dma("pool", w_u[:], I["w_up"][0, ex, :, f0:f0 + FH].rearrange("(k p) f -> p k f", p=128), [I["w_up"]], [w_u])
        S.dma("pool", w_d[:], I["w_down"][0, ex, f0:f0 + FH, :].rearrange("(k p) d -> p k d", p=128), [I["w_down"]], [w_d])

    def gather(ex):
        for sg in range(NSG):
            S.emit("pool", lambda e, sg=sg, ex=ex: e.indirect_dma_start(out=xs[:, sg, :], out_offset=None, in_=G["h2"][:, :],
                                                                        in_offset=bass.IndirectOffsetOnAxis(ap=idxT[:, sg, ex:ex + 1], axis=0)),
                   [idxT, G["h2"]], [xs], dma=xs)

    load_weights(0)
    gather(0)
    for k, (ex, fh) in enumerate(halves):
        w_g, w_u, w_d = wg[k % 2], wu[k % 2], wd[k % 2]
        if k + 1 < len(halves):
            load_weights(k + 1)
        if fh == 0:
            ti = 0
            for sg in range(NSG):
                for g in range(0, KD, 8):
                    p = ptr[ti % 2]
                    ng = min(8, KD - g)
                    def tr(e, p=p, sg=sg, g=g, ng=ng):
                        r = None
                        for j in range(ng):
                            r = e.transpose(out=p[:, j, :], in_=xs[:, sg, (g + j) * 128:(g + j + 1) * 128], identity=identb[:])
                        return r
                    S.emit("pe", tr, [xs, identb], [p])
                    if ti % 2 == 0:
                        S.emit("act", lambda e, p=p, sg=sg, g=g, ng=ng: e.copy(out=xsT[:, g:g + ng, sg * 128:(sg + 1) * 128], in_=p[:, 0:ng, :]), [p, xsT], [xsT])
                    else:
                        S.emit("dve", lambda e, p=p, sg=sg, g=g, ng=ng: e.tensor_copy(out=xsT[:, g:g + ng, sg * 128:(sg + 1) * 128], in_=p[:, 0:ng, :]), [p, xsT], [xsT])
                    ti += 1
            if ex + 1 < E:
                gather(ex + 1)
        for fc in range(FC):
            p_g, p_u = pg[gi % 2], pu[gi % 2]
            gi += 1
            for (w_, p_) in ((w_g, p_g), (w_u, p_u)):
                def mm(e, w_=w_, p_=p_, fc=fc):
                    r = None
                    for kk in range(KD):
                        r = e.matmul(p_[:, 0:CAP], lhsT=w_[:, kk, fc * 128:(fc + 1) * 128], rhs=xsT[:, kk, :], start=(kk == 0), stop=(kk == KD - 1))
                    return r
                S.emit("pe", mm, [w_, xsT], [p_])
            S.emit("act", lambda e, p_g=p_g: e.activation(out=sa[:, 0:CAP], in_=p_g[:, 0:CAP], func=AF.Silu), [p_g], [sa])
            S.emit("dve", lambda e, p_u=p_u, fc=fc: e.tensor_tensor(out=hd[:, fc, :], in0=p_u[:, 0:CAP], in1=sa[:, 0:CAP], op=ALU.mult), [p_u, sa, hd], [hd])
        for sg in range(NSG):
            for oc in range(0, D, 512):
                ow = min(512, D - oc)
                p_y = py[yi % 2]
                yi += 1
                def my(e, p_y=p_y, sg=sg, oc=oc, ow=ow, w_d=w_d):
                    r = None
                    for fc in range(FC):
                        r = e.matmul(p_y[:, 0:ow], lhsT=hd[:, fc, sg * 128:(sg + 1) * 128], rhs=w_d[:, fc, oc:oc + ow], start=(fc == 0), stop=(fc == FC - 1))
                    return r
                S.emit("pe", my, [hd, w_d], [p_y])
                if fh == 0:
                    S.emit("dve", lambda e, p_y=p_y, sg=sg, oc=oc, ow=ow, ex=ex: e.scalar_tensor_tensor(
                        out=ysb[:, sg, oc:oc + ow], in0=p_y[:, 0:ow], scalar=gateT[:, sg, ex:ex + 1], in1=g2[:, oc:oc + ow], op0=ALU.mult, op1=ALU.mult),
                        [p_y, gateT, g2, ysb], [ysb])
                else:
                    S.emit("act", lambda e, p_y=p_y, sg=sg, ow=ow, ex=ex: e.activation(out=sa2[:, 0:ow], in_=p_y[:, 0:ow], func=AF.Copy, scale=gateT[:, sg, ex:ex + 1]),
                           [p_y, gateT], [sa2])
                    S.emit("dve", lambda e, oc=oc, ow=ow: e.tensor_tensor(out=sa2[:, 0:ow], in0=sa2[:, 0:ow], in1=g2[:, oc:oc + ow], op=ALU.mult), [sa2, g2], [sa2])
                    S.emit("dve", lambda e, sg=sg, oc=oc, ow=ow: e.tensor_tensor(out=ysb[:, sg, oc:oc + ow], in0=ysb[:, sg, oc:oc + ow], in1=sa2[:, 0:ow], op=ALU.add),
                           [ysb, sa2], [ysb])
        if fh == NFH - 1:
            for sg in range(NSG):
                S.emit("pool", lambda e, sg=sg, ex=ex: e.indirect_dma_start(out=G["x2"][:, :], out_offset=bass.IndirectOffsetOnAxis(ap=idxT[:, sg, ex:ex + 1], axis=0),
                                                                            in_=ysb[:, sg, :], in_offset=None, compute_op=ALU.add),
                       [idxT, ysb, G["x2s"]], [G["x2s"]], dma=ysb)
    A.close()


def phase_final(S, cfg, G, I, C, out):
    A = Arena(S, "fn")
    D, T = cfg.D, cfg.T
    fn = load_row_bcast(S, A, "fnw", I["final_norm"][:], I["final_norm"], D)
    xt = [A.sb(f"xt{i}", [128, D], F32) for i in range(2)]
    st = [A.sb(f"st{i}", [128, 8], F32) for i in range(2)]
    junk = A.sb("junk", [128, D], BF16)
    for ti in range(T // 128):
        t0 = ti * 128
        x = xt[ti % 2]; s = st[ti % 2]
        S.dma("sp" if ti % 2 == 0 else "pool", x[:], G["x2"][t0:t0 + 128, :], [G["x2"], G["x2s"]], [x])
        S.emit("act", lambda e, x=x, s=s: e.activation(out=junk[:], in_=x[:], func=AF.Square, accum_out=s[:, 0:1]), [x], [junk, s])
        S.emit("dve", lambda e, s=s: e.tensor_scalar(out=s[:, 1:2], in0=s[:, 0:1], scalar1=1.0 / D, scalar2=cfg.eps, op0=ALU.mult, op1=ALU.add), [s], [s])
        S.emit("act", lambda e, s=s: e.activation(out=s[:, 2:3], in_=s[:, 1:2], func=AF.Sqrt), [s], [s])
        S.emit("dve", lambda e, s=s: e.reciprocal(out=s[:, 3:4], in_=s[:, 2:3]), [s], [s])
        S.emit("dve", lambda e, x=x, s=s: e.scalar_tensor_tensor(out=x[:], in0=x[:], scalar=s[:, 3:4], in1=fn[:], op0=ALU.mult, op1=ALU.mult), [x, s, fn], [x])
        S.dma("sp", out[t0:t0 + 128, :], x[:], [x], [out])
    A.close()


def declare_io(S, cfg, debug=(), cfgL=None):
    cfgL = cfgL or cfg
    D, T, NCTX, E, DE, TT = cfg.D, cfg.T, cfg.NCTX, cfg.E, cfg.DE, cfg.TT
    HVF = cfg.HV
    HV, H = cfgL.HV, cfgL.H
    I = {}

    def inp(name, shape):
        I[name] = S.dram(name, shape, F32, kind="ExternalInput")
    inp("x", [1, T, D]); inp("c", [1, D]); inp("ctx", [1, NCTX, D]); inp("c_ctx", [D])
    NSP = cfg.H // cfgL.H
    inp("ada_w", [1, D, 6 * D // NSP]); inp("ada_b", [1, 6 * D // NSP]); inp("norm_mix", [1, D]); inp("norm_ffn", [1, D])
    inp("w_in", [1, D, cfgL.NIN]); inp("gdn_conv", [1, 5, 3 * HV]); inp("gdn_a_log", [1, 2, H]); inp("gdn_dt_bias", [1, 2, H])
    inp("hgrn_lb", [2, 2, HV]); inp("hgrn_norm", [1, HV]); inp("gdn_norm", [1, HV])
    inp("w_branch_a", [1, HVF, D]); inp("w_branch_b", [1, HVF, D]); inp("w_out", [1, D, D]); inp("router_w", [1, D, E])
    inp("w_gate", [1, E, D, DE]); inp("w_up", [1, E, D, DE]); inp("w_down", [1, E, DE, D]); inp("final_norm", [D])
    G = {}

    def scr(name, shape, dt):
        G[name] = S.dram(name, shape, dt, kind="ExternalOutput" if name in debug else "Internal")
    scr("grow", [2, D], F32)
    scr("modown", [128, 6 * cfg.KD * 2 // (cfg.H // cfgL.H)], F32); scr("modall", [128 * (cfg.H // cfgL.H), 6 * cfg.KD * 2 // (cfg.H // cfgL.H)], F32)
    scr("hqT", [HV, T], BF16); scr("hffT", [HV, TT], BF16); scr("hfbT", [HV, TT], BF16); scr("hi", [TT, HV], BF16)
    scr("hogT", [HV, T], BF16); scr("gzT", [HV, T], BF16); scr("mgT", [2 * D, T], BF16)
    scr("gqkvT", [3 * HV, TT], BF16); scr("gab", [TT, 4 * H], F32)
    scr("ycat", [2 * HV, T], BF16)
    G["yAT"] = Buf("yAT", G["ycat"].t[0:HV, :], multi=True)
    G["yBT"] = Buf("yBT", G["ycat"].t[HV:2 * HV, :], multi=True)
    NR = cfg.H // cfgL.H
    CR = min(256, 2 * HV)
    scr("yall", [2 * HV // CR, NR * CR, T], BF16)
    scr("gq", [HV, TT], BF16); scr("gk", [HV, TT], BF16); scr("gv", [HV, TT], BF16)
    scr("mT", [D, T], BF16); scr("x2", [T, D], F32); scr("h2", [T, D], BF16); scr("crow", [2, D], F32)
    G["x2s"] = Buf("x2s")
    scr("dbg_mod", [128, 6 * cfg.KD * 2], F32)
    if "dbg_S" in debug:
        scr("dbg_S", [2 * min(4, H), 128, 128], F32)
    if "dbg_ob" in debug:
        scr("dbg_ob", [HV, T], F32)
    if "dbg_g" in debug:
        scr("dbg_g", [6, 128, TT // 128, 2 * H], F32)
    scr("dbg_coef", [128, 6 * cfg.KD], F32)
    out = S.dram("out", [T, D], F32, kind="ExternalOutput")
    return I, G, out


def local_cfg(cfg, nsplit):
    return Cfg(D=cfg.D, H=cfg.H // nsplit, T=cfg.T, GW=cfg.GW, NCTX=cfg.NCTX, E=cfg.E, DE=cfg.DE, CAP=cfg.CAP, eps=cfg.eps)


def build_program(cfg=FULL, debug=(), phases=None, nsplit=2, groups=None):
    nc = bass.Bass("TRN2", target_bir_lowering=False)
    S = Sched(nc)
    cfgL = local_cfg(cfg, nsplit) if nsplit > 1 else cfg
    I, G, out = declare_io(S, cfg, debug, cfgL)
    P = Arena(S, "glob")
    C = make_consts(S, P, cfg)
    G["mod"] = P.sb("mod", [128, 6 * cfg.KD, 2], F32)
    G["coef"] = P.sb("coef", [128, 6, cfg.KD], F32)
    ph = phases or ("ada", "coef", "prr", "prc", "hgrn", "gconv", "gdn", "merge", "outproj", "route", "experts", "final")
    grp0 = groups or [[b + 4 * r for r in range(max(nsplit, 1))] for b in range(4)]
    if "ada" in ph:
        phase_ada(S, cfg, G, I, nsplit, grp0)
    if "coef" in ph:
        phase_coef(S, cfg, G, I)
    if "dbg_mod" in debug:
        S.dma("sp", G["dbg_mod"][:], G["mod"][:].rearrange("p n s -> p (n s)"), [G["mod"]], [G["dbg_mod"]])
        S.dma("sp", G["dbg_coef"][:], G["coef"][:].rearrange("p n s -> p (n s)"), [G["coef"]], [G["dbg_coef"]])
    if "prr" in ph:
        phase_proj(S, cfgL, G, I, C, "r")
    if "prc" in ph:
        phase_proj(S, cfgL, G, I, C, "c")
    grp = groups or [[b + 4 * r for r in range(max(nsplit, 1))] for b in range(4)]
    CR = min(256, 2 * cfgL.HV)
    NCK = 2 * cfgL.HV // CR

    def gather_y(cks, srcs):
        for ck in cks:
            S.emit_cc(lambda e, ck=ck: e.collective_compute("AllGather", ALU.bypass, replica_groups=grp, ins=[G["ycat"][ck * CR:(ck + 1) * CR, :]],
                                                            outs=[G["yall"][ck]]), srcs, [G["yall"]])
    if "hgrn" in ph:
        phase_hgrn(S, cfgL, G, I, C)
    if nsplit > 1 and NCK >= 2:
        gather_y(range(NCK // 2), [G["yAT"]])
    if "gconv" in ph:
        phase_gconv(S, cfgL, G, I, C)
    if "gdn" in ph:
        phase_gdn(S, cfgL, G, I, C)
    if nsplit > 1:
        gather_y(range(NCK // 2, NCK) if NCK >= 2 else range(NCK), [G["yAT"], G["yBT"]])
    if "merge" in ph:
        phase_merge(S, cfg, G, I, C, cfgL)
    if "outproj" in ph:
        phase_outproj(S, cfg, G, I, C)
    R_ = {"idxT": P.sb("idxT", [128, cfg.CAP // 128, cfg.E], I32), "gateT": P.sb("gateT", [128, cfg.CAP // 128, cfg.E], F32)}
    if "route" in ph:
        phase_route(S, cfg, G, I, C, R_)
    if "experts" in ph:
        phase_experts(S, cfg, G, I, C, R_)
    if "final" in ph:
        phase_final(S, cfg, G, I, C, out)
    P.close()
    S.finish()
    build_program.last_sched = S
    return nc


_PER_BATCH = ("x", "c", "ctx")
_NC_CACHE = {}


def _head_cols(v, hf, nsplit, H, axis=-1):
    v = np.asarray(v)
    w = (H // nsplit) * 128
    sl = [slice(None)] * v.ndim
    sl[axis] = slice(hf * w, (hf + 1) * w)
    return v[tuple(sl)]


def shard_inputs(inputs, cfg, b, hf, nsplit):
    H, HV, D = cfg.H, cfg.HV, cfg.D
    HLc = H // nsplit
    m = {}
    for k, v in inputs.items():
        v = np.asarray(v)
        if k in _PER_BATCH:
            m[k] = np.ascontiguousarray(v[b:b + 1])
        else:
            m[k] = v
    if nsplit > 1:
        w = m["w_in"]
        parts = []
        for g in range(9):
            parts.append(_head_cols(w[:, :, g * HV:(g + 1) * HV], hf, nsplit, H))
        o = 9 * HV
        for g in range(4):
            parts.append(w[:, :, o + g * H + hf * HLc:o + g * H + (hf + 1) * HLc])
        parts.append(w[:, :, o + 4 * H:])
        m["w_in"] = np.concatenate(parts, axis=2)
        m["hgrn_lb"] = _head_cols(m["hgrn_lb"], hf, nsplit, H)
        m["hgrn_norm"] = _head_cols(m["hgrn_norm"], hf, nsplit, H)
        m["gdn_norm"] = _head_cols(m["gdn_norm"], hf, nsplit, H)
        cw = m["gdn_conv"]
        m["gdn_conv"] = np.concatenate([_head_cols(cw[:, :, g * HV:(g + 1) * HV], hf, nsplit, H) for g in range(3)], axis=2)
        m["gdn_a_log"] = m["gdn_a_log"][:, :, hf * HLc:(hf + 1) * HLc]
        m["gdn_dt_bias"] = m["gdn_dt_bias"][:, :, hf * HLc:(hf + 1) * HLc]
        w6 = 6 * D // nsplit
        m["ada_w"] = m["ada_w"][:, :, hf * w6:(hf + 1) * w6]
        m["ada_b"] = m["ada_b"][:, hf * w6:(hf + 1) * w6]
    return {k: np.ascontiguousarray(v) for k, v in m.items()}


def kernel(**inputs):
    cfg = FULL
    n = 8
    nsplit = 2
    if "nc" not in _NC_CACHE:
        _NC_CACHE["nc"] = build_program(cfg, nsplit=nsplit)
    nc = _NC_CACHE["nc"]
    B = inputs["x"].shape[0]
    in_maps = [shard_inputs(inputs, cfg, core % B, core // B, nsplit) for core in range(n)]
    res = run_bass_kernel_spmd(nc, in_maps, core_ids=list(range(n)))
    out = np.stack([np.asarray(res.results[b]["out"]) for b in range(B)], axis=0)
    return out.astype(np.float32)
```

```python
import contextlib
import numpy as np
import concourse.bass as bass
import concourse.mybir as mybir
from concourse.bass_utils import run_bass_kernel_spmd

F32 = mybir.dt.float32
BF16 = mybir.dt.bfloat16
I32 = mybir.dt.int32
U32 = mybir.dt.uint32
AF = mybir.ActivationFunctionType
ALU = mybir.AluOpType

ENGS = ("pe", "act", "dve", "pool", "sp")
NEG = -30000.0


class Buf:
    __slots__ = ("name", "ws", "rs", "t", "multi", "dgroup", "excl")

    def __init__(self, name, t=None, multi=False, dgroup=None, excl=False):
        self.excl = excl
        self.name = name
        self.ws = {}
        self.rs = {}
        self.t = t
        self.multi = multi
        self.dgroup = dgroup or name

    def __getitem__(self, idx):
        return self.t[idx]


class Sched:
    def __init__(self, nc):
        self.nc = nc
        self.ops = {e: [] for e in ENGS}
        self.val = {}
        self.seen = {e: {} for e in ENGS}
        self.uid = 0
        self.dmap = {}
        self.dfree = {"sw": [], "hw": []}
        self.nd = 0

    def dkey(self, dgroup, kind):
        k = (dgroup, kind)
        if k not in self.dmap:
            if self.dfree[kind]:
                self.dmap[k] = self.dfree[kind].pop()
            else:
                self.dmap[k] = f"ds{self.nd}_{kind}"
                self.nd += 1
        return self.dmap[k]

    def release(self, dgroups):
        for k in list(self.dmap):
            if k[0] in dgroups:
                self.dfree[k[1]].append(self.dmap.pop(k))

    def dram(self, name, shape, dt, kind="Internal"):
        return Buf(name, self.nc.dram_tensor(name, list(shape), dt, kind=kind).ap(), multi=True)

    def emit(self, eng, fn, reads=(), writes=(), dma=None):
        deps = {}

        def add(k, v):
            if deps.get(k, 0) < v:
                deps[k] = v
        for b in reads:
            for k, v in b.ws.items():
                add(k, v)
            if b.excl:
                for k, v in b.rs.items():
                    if k != "c_" + eng:
                        add(k, v)
        for b in writes:
            if b.multi:
                continue
            for k, v in b.ws.items():
                add(k, v)
            for k, v in b.rs.items():
                add(k, v)
        seen = self.seen[eng]
        waits = []
        for k, v in deps.items():
            if seen.get(k, 0) < v:
                seen[k] = v
                waits.append((k, v))
        sk = self.dkey(dma.dgroup, "sw" if eng == "pool" else "hw") if dma is not None else ("c_" + eng)
        inc = 16 if dma is not None else 1
        self.val[sk] = self.val.get(sk, 0) + inc
        tv = self.val[sk]
        for b in reads:
            if b.rs.get(sk, 0) < tv:
                b.rs[sk] = tv
        for b in writes:
            if b.multi:
                b.ws[sk] = tv
            else:
                b.ws = {sk: tv}
                b.rs = {}
        self.ops[eng].append((waits, fn, sk, inc))

    def dma(self, eng, out, in_, reads, writes, sem=None, **kw):
        if sem is None:
            sem = [b for b in list(writes) + list(reads) if not b.multi][0]
        self.emit(eng, lambda e: e.dma_start(out=out, in_=in_, **kw), reads, writes, dma=sem)

    def emit_cc(self, fn, reads, writes):
        deps = {}
        for b in reads:
            for k, v in b.ws.items():
                if deps.get(k, 0) < v:
                    deps[k] = v
        seen = self.seen["pool"]
        waits = []
        for k, v in deps.items():
            if seen.get(k, 0) < v:
                seen[k] = v
                waits.append((k, v))
        sk = "cc_sem"
        self.val[sk] = self.val.get(sk, 0) + 1
        for b in writes:
            b.ws[sk] = self.val[sk]
        self.ops["pool"].append((waits, fn, sk, 1))

    def barrier(self):
        cur = dict(self.val)
        for eng in ENGS:
            seen = self.seen[eng]
            waits = []
            for k, v in cur.items():
                if seen.get(k, 0) < v:
                    seen[k] = v
                    waits.append((k, v))
            if waits:
                self.ops[eng].append((waits, None, None, 0))

    def finish(self):
        nc = self.nc
        sems = {k: nc.alloc_semaphore(k) for k in self.val}
        engobj = {"pe": "tensor", "act": "scalar", "dve": "vector", "pool": "gpsimd", "sp": "sync"}
        final = list(self.val.items())
        with nc.Block() as block:
            for eng in ENGS:
                ops = self.ops[eng]
                is_final = eng == "sp"

                def body(e, ops=ops, is_final=is_final):
                    for waits, fn, sk, inc in ops:
                        for k, v in waits:
                            e.wait_ge(sems[k], v)
                        if fn is not None:
                            if sk == "cc_sem":
                                fn(e).then_inc(sems[sk])
                            else:
                                fn(e).then_inc(sems[sk], inc)
                    if is_final:
                        for k, v in final:
                            e.wait_ge(sems[k], v)
                getattr(block, engobj[eng])(body)
        return nc


class Arena:
    def __init__(self, S, tag):
        self.S = S
        self.tag = tag
        self.st = contextlib.ExitStack()
        self.groups = set()

    def sb(self, name, shape, dt, multi=False, dgroup=None):
        nm = f"{self.tag}_{name}"
        t = self.st.enter_context(self.S.nc.sbuf_tensor(nm, list(shape), dt))
        b = Buf(nm, t, multi=multi, dgroup=dgroup and f"{self.tag}_{dgroup}")
        self.groups.add(b.dgroup)
        return b

    def ps(self, name, shape, dt=F32):
        nm = f"{self.tag}_{name}"
        t = self.st.enter_context(self.S.nc.psum_tensor(nm, list(shape), dt))
        return Buf(nm, t, excl=True)

    def close(self):
        self.S.barrier()
        self.S.release(self.groups)
        self.st.close()


class Cfg:
    def __init__(self, D=2048, H=8, T=4096, GW=64, NCTX=256, E=16, DE=1024, CAP=512, eps=1e-6, DMG=None):
        self.D, self.H, self.T, self.GW, self.NCTX = D, H, T, GW, NCTX
        self.DMG = DMG or D
        self.E, self.DE, self.CAP, self.eps = E, DE, CAP, eps
        self.R = T // GW
        self.HV = H * 128
        self.KD = D // 128
        self.TT = T + NCTX
        HV = self.HV
        o = 0
        self.c_hq = o; o += HV
        self.c_hff = o; o += HV
        self.c_hfb = o; o += HV
        self.c_hi = o; o += HV
        self.c_hog = o; o += HV
        self.c_gqkv = o; o += 3 * HV
        self.c_gz = o; o += HV
        self.c_ga = o; o += 2 * H
        self.c_gb = o; o += 2 * H
        self.c_mg = o; o += 2 * self.DMG
        self.NIN = o


FULL = Cfg()


def token_blocks(cfg, lo, hi, nb=512):
    out = []
    for a, b in ((0, cfg.NCTX), (cfg.NCTX, cfg.TT)):
        a, b = max(a, lo), min(b, hi)
        t = a
        while t < b:
            n = min(nb, b - t)
            out.append((t, n))
            t += n
    return out


def make_consts(S, A, cfg):
    C = {}
    nc = S.nc
    identb = A.sb("identb", [128, 128], BF16)
    identf = A.sb("identf", [128, 128], F32)
    for ident in (identb, identf):
        S.emit("pool", lambda e, ident=ident: e.memset(ident[:], 0.0), writes=[ident])
        S.emit("pool", lambda e, ident=ident: e.affine_select(
            out=ident[:], in_=ident[:], pattern=[[-1, 128]], compare_op=ALU.not_equal, fill=1.0,
            base=0, channel_multiplier=1), reads=[ident], writes=[ident])
    C["identb"], C["identf"] = identb, identf
    onesb = A.sb("onesb", [128, 128], BF16)
    S.emit("pool", lambda e: e.memset(onesb[:], 1.0), writes=[onesb])
    C["onesb"] = onesb
    onesf = A.sb("onesf", [128, 128], F32)
    S.emit("pool", lambda e: e.memset(onesf[:], 1.0), writes=[onesf])
    C["onesf"] = onesf
    epsc = A.sb("epsc", [128, 1], F32)
    S.emit("pool", lambda e: e.memset(epsc[:], cfg.eps), writes=[epsc])
    C["epsc"] = epsc
    onec = A.sb("onec", [128, 1], F32)
    S.emit("pool", lambda e: e.memset(onec[:], 1.0), writes=[onec])
    C["onec"] = onec
    return C


def phase_ada(S, cfg, G, I, nsplit=1, grp=None):
    D, KD = cfg.D, cfg.KD
    A = Arena(S, "ada")
    NCB = 6 * KD // nsplit
    SW = 512 if (NCB * 128) % 512 == 0 else 256
    cT = A.sb("cT", [128, KD, 2], F32)
    s2 = A.sb("s2", [128, KD, 2], F32)
    bT = A.sb("bT", [128, NCB], F32)
    S.dma("sp", cT[:, :, 0], I["c"][0].rearrange("(k p) -> p k", p=128), [I["c"]], [cT], allow_slow_non_contiguous=True)
    S.dma("sp", cT[:, :, 1], I["c_ctx"][:].rearrange("(k p) -> p k", p=128), [I["c_ctx"]], [cT], allow_slow_non_contiguous=True)
    S.dma("sp", bT[:], I["ada_b"][0].rearrange("(k p) -> p k", p=128), [I["ada_b"]], [bT], allow_slow_non_contiguous=True)
    S.emit("act", lambda e: e.activation(out=s2[:], in_=cT[:], func=AF.Silu), [cT], [s2])
    wsl = [A.sb(f"w{i}", [128, KD, SW], F32) for i in range(2)]
    pa = A.ps("pa", [128, NCB, 2], F32)
    mod = G["mod"]
    wv = I["ada_w"][0].rearrange("(k p) n -> p k n", p=128)
    nslab = NCB * 128 // SW
    for s in range(nslab):
        w = wsl[s % 2]
        S.dma("sp" if s % 2 == 0 else "pool", w[:], wv[:, :, s * SW:(s + 1) * SW], [I["ada_w"]], [w])
        for j in range(SW // 128):
            cb = s * (SW // 128) + j

            def mm(e, w=w, j=j, cb=cb):
                r = None
                for k in range(KD):
                    r = e.matmul(pa[:, cb, :], lhsT=w[:, k, j * 128:(j + 1) * 128], rhs=s2[:, k, :],
                                 start=(k == 0), stop=(k == KD - 1))
                return r
            S.emit("pe", mm, [w, s2], [pa])
    bb = bT[:].rearrange("p (n o) -> p n o", o=1).to_broadcast([128, NCB, 2])
    if nsplit == 1:
        S.emit("dve", lambda e: e.tensor_tensor(out=mod[:], in0=pa[:], in1=bb, op=ALU.add), [pa, bT], [mod])
    else:
        ml_ = A.sb("ml", [128, NCB, 2], F32)
        S.emit("dve", lambda e: e.tensor_tensor(out=ml_[:], in0=pa[:], in1=bb, op=ALU.add), [pa, bT], [ml_])
        S.dma("sp", G["modown"][:, :], ml_[:].rearrange("p n s -> p (n s)"), [ml_], [G["modown"]])
        S.emit_cc(lambda e: e.collective_compute("AllGather", ALU.bypass, replica_groups=grp, ins=[G["modown"][:, :]], outs=[G["modall"][:, :]]),
                  [G["modown"]], [G["modall"]])
        for r in range(nsplit):
            S.dma("sp", mod[:, r * NCB:(r + 1) * NCB, :].rearrange("p n s -> p (n s)"), G["modall"][r * 128:(r + 1) * 128, :], [G["modall"]], [mod])
    A.close()


def phase_coef(S, cfg, G, I):
    KD, D = cfg.KD, cfg.D
    A = Arena(S, "coef")
    mod = G["mod"]
    nm = A.sb("nm", [128, KD], F32)
    nf = A.sb("nf", [128, KD], F32)
    S.dma("sp", nm[:], I["norm_mix"][0].rearrange("(k p) -> p k", p=128), [I["norm_mix"]], [nm], allow_slow_non_contiguous=True)
    S.dma("sp", nf[:], I["norm_ffn"][0].rearrange("(k p) -> p k", p=128), [I["norm_ffn"]], [nf], allow_slow_non_contiguous=True)
    co = G["coef"]
    def mk(dst, gain, sc_j, s):
        S.emit("dve", lambda e: e.scalar_tensor_tensor(out=co[:, dst, :], in0=mod[:, sc_j * KD:(sc_j + 1) * KD, s], scalar=1.0,
                                                       in1=gain[:], op0=ALU.add, op1=ALU.mult), [mod, gain, co], [co])
    mk(0, nm, 1, 0)
    S.emit("dve", lambda e: e.tensor_copy(out=co[:, 1, :], in_=mod[:, 0:KD, 0]), [mod, co], [co])
    mk(2, nm, 1, 1)
    S.emit("dve", lambda e: e.tensor_copy(out=co[:, 3, :], in_=mod[:, 0:KD, 1]), [mod, co], [co])
    mk(4, nf, 4, 0)
    S.emit("dve", lambda e: e.tensor_copy(out=co[:, 5, :], in_=mod[:, 3 * KD:4 * KD, 0]), [mod, co], [co])
    gt = A.sb("gt", [128, 2, KD], F32)
    S.emit("dve", lambda e: e.tensor_copy(out=gt[:, 0, :], in_=mod[:, 2 * KD:3 * KD, 0]), [mod], [gt])
    S.emit("dve", lambda e: e.tensor_copy(out=gt[:, 1, :], in_=mod[:, 5 * KD:6 * KD, 0]), [mod, gt], [gt])
    for j in range(2):
        S.dma("sp", G["grow"][j].rearrange("(k p) -> p k", p=128), gt[:, j, :], [gt], [G["grow"]], allow_slow_non_contiguous=True)
    A.close()


def norm_tiles(S, A, cfg, C, tiles, hT, coefbuf, f32T=None):
    D, KD = cfg.D, cfg.KD
    xt = [A.sb(f"nx{i}", [128, D], F32) for i in range(2)]
    xn = [A.sb(f"nxn{i}", [128, D], BF16 if f32T is None else F32) for i in range(2)]
    junk = A.sb("njunk", [128, D], BF16)
    st = [A.sb(f"nst{i}", [128, 8], F32) for i in range(2)]
    tdt = BF16 if f32T is None else F32
    per = 8 if f32T is None else 4
    pts = [A.ps(f"npt{i}", [128, per, 128], tdt) for i in range(2)]
    ident = C["identb"] if f32T is None else C["identf"]
    co = coefbuf
    ng = KD // per if KD >= per else 1
    per = min(per, KD)
    gi = 0
    for ti, (pieces, srcbufs, col0, ci) in enumerate(tiles):
        x = xt[ti % 2]
        for k, (ap, p0, npp) in enumerate(pieces):
            S.dma("sp" if ti % 2 == 0 else "pool", x[p0:p0 + npp, :], ap, srcbufs, [x])
        s = st[ti % 2]
        import os
        NV = int(os.environ.get("K_NV", "9"))
        if NV < 1:
            continue
        S.emit("act", lambda e, x=x, s=s: e.activation(out=junk[:], in_=x[:], func=AF.Square, accum_out=s[:, 0:1]),
               [x], [junk, s])
        S.emit("dve", lambda e, s=s: e.tensor_scalar(out=s[:, 1:2], in0=s[:, 0:1], scalar1=1.0 / D, scalar2=cfg.eps,
                                                     op0=ALU.mult, op1=ALU.add), [s], [s])
        S.emit("act", lambda e, s=s: e.activation(out=s[:, 2:3], in_=s[:, 1:2], func=AF.Sqrt), [s], [s])
        S.emit("dve", lambda e, s=s: e.reciprocal(out=s[:, 3:4], in_=s[:, 2:3]), [s], [s])
        if NV < 2:
            continue
        n = xn[ti % 2]
        S.emit("dve", lambda e, x=x, s=s, n=n: e.tensor_scalar(out=n[:], in0=x[:], scalar1=s[:, 3:4], scalar2=None,
                                                               op0=ALU.mult), [x, s], [n])
        if NV < 3:
            continue
        for g in range(ng):
            pt = pts[gi % 2]
            gi += 1

            def tr(e, n=n, pt=pt, g=g):
                r = None
                for j in range(per):
                    c = g * per + j
                    r = e.transpose(out=pt[:, j, :], in_=n[:, c * 128:(c + 1) * 128], identity=ident[:])
                return r
            S.emit("pe", tr, [n, ident], [pt])
            if NV < 4:
                continue
            for j in range(per):
                c = g * per + j
                eng = "act" if gi % 2 == 0 else "dve"
                outs = [(hT, hT[:, c, col0:col0 + 128])]
                if f32T is not None:
                    outs.append((f32T, f32T[:, c, col0:col0 + 128]))
                for oi, (ob, oap) in enumerate(outs):
                    eng2 = eng if oi == 0 else ("dve" if eng == "act" else "act")
                    if eng2 == "act":
                        S.emit("act", lambda e, pt=pt, j=j, c=c, oap=oap, ci=ci: e.activation(
                            out=oap, in_=pt[:, j, :], func=AF.Identity, scale=co[:, ci, c:c + 1], bias=co[:, ci + 1, c:c + 1]),
                            [pt, co], [ob])
                    else:
                        S.emit("dve", lambda e, pt=pt, j=j, c=c, oap=oap, ci=ci: e.tensor_scalar(
                            out=oap, in0=pt[:, j, :], scalar1=co[:, ci, c:c + 1], scalar2=co[:, ci + 1, c:c + 1],
                            op0=ALU.mult, op1=ALU.add), [pt, co], [ob])


def lat_tile_pieces(cfg, xin, ti, order):
    if order == "r":
        return [(xin[0, ti * 128:(ti + 1) * 128, :], 0, 128)]
    R, GW = cfg.R, cfg.GW
    xv = xin[0].rearrange("(r c) d -> c r d", c=GW)
    out = []
    if R >= 128:
        per = R // 128
        cc, sub = divmod(ti, per)
        out.append((xv[cc, sub * 128:(sub + 1) * 128, :], 0, 128))
    else:
        ncol = 128 // R
        for k in range(ncol):
            out.append((xv[ti * ncol + k, :, :], k * R, R))
    return out


def act_tiles(cfg, I, order):
    tiles = []
    for t in range(cfg.NCTX // 128):
        tiles.append(([(I["ctx"][0, t * 128:(t + 1) * 128, :], 0, 128)], [I["ctx"]], t * 128, 2))
    for t in range(cfg.T // 128):
        tiles.append((lat_tile_pieces(cfg, I["x"], t, order), [I["x"]], cfg.NCTX + t * 128, 0))
    return tiles


def project(S, A, cfg, hT, w_in, groups, tok_lo=0):
    KD, TT = cfg.KD, cfg.TT
    SW = 256
    wsl = [A.sb(f"pw{i}", [128, KD, SW], BF16) for i in range(2)]
    pps = [A.ps(f"pp{i}", [128, 512], F32) for i in range(4)]
    stg = {}
    wv = w_in[0].rearrange("(k p) n -> p k n", p=128)
    si = 0
    pi = 0
    gi = 0
    for g in groups:
        col0, ncols, lay, func, dst, dt = g["col0"], g["ncols"], g["layout"], g["func"], g["dst"], g["dt"]
        tlo = g.get("tok_lo", 0)
        key = (lay, dt)
        if key not in stg:
            stg[key] = [A.sb(f"ps{lay}{len(stg)}_{i}", [128, 512], dt) for i in range(3)]
        stgs = stg[key]
        if lay == "F":
            blocks = token_blocks(cfg, tlo, TT)
            for s0 in range(0, ncols, SW):
                sw = min(SW, ncols - s0)
                w = wsl[si % 2]
                S.dma("pool", w[:, :, 0:sw], wv[:, :, col0 + s0:col0 + s0 + sw], [w_in], [w])
                si += 1
                for j in range(0, sw, 128):
                    for (t0, nb) in blocks:
                        pp = pps[pi % 4]
                        pi += 1

                        def mm(e, w=w, j=j, t0=t0, nb=nb, pp=pp):
                            r = None
                            for k in range(KD):
                                r = e.matmul(pp[:, 0:nb], lhsT=w[:, k, j:j + 128], rhs=hT[:, k, t0:t0 + nb],
                                             start=(k == 0), stop=(k == KD - 1))
                            return r
                        S.emit("pe", mm, [w, hT], [pp])
                        sg = stgs[gi % 3]
                        gi += 1
                        if func is None:
                            S.emit("dve", lambda e, sg=sg, pp=pp, nb=nb: e.tensor_copy(out=sg[:, 0:nb], in_=pp[:, 0:nb]), [pp], [sg])
                        else:
                            S.emit("act", lambda e, sg=sg, pp=pp, nb=nb, func=func: e.activation(out=sg[:, 0:nb], in_=pp[:, 0:nb], func=func),
                                   [pp], [sg])
                        r0 = s0 + j
                        S.dma("sp", dst[r0:r0 + 128, t0 - tlo:t0 - tlo + nb], sg[:, 0:nb], [sg], [dst])
        else:
            assert ncols <= 512 or ncols % 256 == 0
            for s0 in range(0, ncols, SW):
                sw = min(SW, ncols - s0)
                w = wsl[si % 2]
                S.dma("pool", w[:, :, 0:sw], wv[:, :, col0 + s0:col0 + s0 + sw], [w_in], [w])
                si += 1
                for t0 in range(tlo, TT, 128):
                    pp = pps[pi % 4]
                    pi += 1

                    def mm(e, w=w, sw=sw, t0=t0, pp=pp):
                        r = None
                        for k in range(KD):
                            r = e.matmul(pp[:, 0:sw], lhsT=hT[:, k, t0:t0 + 128], rhs=w[:, k, 0:sw],
                                         start=(k == 0), stop=(k == KD - 1))
                        return r
                    S.emit("pe", mm, [w, hT], [pp])
                    sg = stgs[gi % 3]
                    gi += 1
                    S.emit("dve", lambda e, sg=sg, pp=pp, sw=sw: e.tensor_copy(out=sg[:, 0:sw], in_=pp[:, 0:sw]), [pp], [sg])
                    S.dma("sp", dst[t0 - tlo:t0 - tlo + 128, s0:s0 + sw], sg[:, 0:sw], [sg], [dst])


def phase_proj(S, cfg, G, I, C, order):
    A = Arena(S, "pr" + order)
    KD, TT, HV, NCTX, H, D = cfg.KD, cfg.TT, cfg.HV, cfg.NCTX, cfg.H, cfg.D
    hT = A.sb("hT", [128, KD, TT], BF16, multi=True)
    norm_tiles(S, A, cfg, C, act_tiles(cfg, I, order), hT, G["coef"])
    if order == "r":
        groups = [
            dict(col0=cfg.c_hq, ncols=HV, layout="F", func=AF.Silu, dst=G["hqT"], dt=BF16, tok_lo=NCTX),
            dict(col0=cfg.c_hff, ncols=HV, layout="F", func=None, dst=G["hffT"], dt=BF16),
            dict(col0=cfg.c_hfb, ncols=HV, layout="F", func=None, dst=G["hfbT"], dt=BF16),
            dict(col0=cfg.c_hi, ncols=HV, layout="T", func=None, dst=G["hi"], dt=BF16),
            dict(col0=cfg.c_hog, ncols=HV, layout="F", func=AF.Sigmoid, dst=G["hogT"], dt=BF16, tok_lo=NCTX),
            dict(col0=cfg.c_gz, ncols=HV, layout="F", func=AF.Silu, dst=G["gzT"], dt=BF16, tok_lo=NCTX),
            dict(col0=cfg.c_mg, ncols=2 * cfg.DMG, layout="F", func=AF.Sigmoid, dst=G["mgT"], dt=BF16, tok_lo=NCTX),
        ]
    else:
        groups = [
            dict(col0=cfg.c_gqkv, ncols=3 * HV, layout="F", func=None, dst=G["gqkvT"], dt=BF16),
            dict(col0=cfg.c_ga, ncols=4 * H, layout="T", func=None, dst=G["gab"], dt=F32),
        ]
    import os
    sel = os.environ.get("K_GROUPS")
    if sel is not None:
        groups = [groups[int(i)] for i in sel.split(",") if i != ""]
    project(S, A, cfg, hT, I["w_in"], groups)
    A.close()


def phase_hgrn(S, cfg, G, I, C):
    A = Arena(S, "hg")
    H, T, TT, NCTX, HV = cfg.H, cfg.T, cfg.TT, cfg.NCTX, cfg.HV
    CH = 64
    NCH = TT // CH
    NCC = NCTX // CH
    order = [list(range(NCH)), list(range(NCC - 1, -1, -1)) + list(range(NCH - 1, NCC - 1, -1))]
    SEG = 1024
    segs = []
    for a, b in ((0, NCTX), (NCTX, TT)):
        t = a
        while t < b:
            n = min(SEG, b - t)
            segs.append((t, n))
            t += n
    lbT = A.sb("lbT", [128, 2, 2, H], F32)
    for d in range(2):
        for sl in range(2):
            S.dma("sp", lbT[:, d, sl, :], I["hgrn_lb"][d, sl].rearrange("(h p) -> p h", p=128), [I["hgrn_lb"]], [lbT],
                  allow_slow_non_contiguous=True)
    low = A.sb("low", [128, 2, H], F32)
    oml = A.sb("oml", [128, 2, H], F32)
    noml = A.sb("noml", [128, 2, H], F32)
    S.emit("dve", lambda e: e.tensor_tensor(out=low[:], in0=lbT[:, :, 0, :], in1=lbT[:, :, 1, :], op=ALU.subtract), [lbT], [low])
    S.emit("act", lambda e: e.activation(out=low[:], in_=low[:], func=AF.Sigmoid), [low], [low])
    S.emit("dve", lambda e: e.tensor_scalar(out=oml[:], in0=low[:], scalar1=-1.0, scalar2=1.0, op0=ALU.mult, op1=ALU.add), [low], [oml])
    S.emit("dve", lambda e: e.tensor_scalar(out=noml[:], in0=low[:], scalar1=1.0, scalar2=-1.0, op0=ALU.mult, op1=ALU.add), [low], [noml])
    gain = A.sb("gain", [128, H], F32)
    S.dma("sp", gain[:], I["hgrn_norm"][0].rearrange("(h p) -> p h", p=128), [I["hgrn_norm"]], [gain], allow_slow_non_contiguous=True)
    msk01 = A.sb("msk01", [128, SEG], F32)
    S.emit("pool", lambda e: e.memset(msk01[:], 1.0), writes=[msk01])
    S.emit("pool", lambda e: e.memset(msk01[:].rearrange("p (n c) -> p n c", c=CH)[:, :, 0:1], 0.0), [msk01], [msk01])
    cm = []
    for d in range(2):
        m = A.sb(f"cm{d}", [CH, CH], F32)
        S.emit("pool", lambda e, m=m: e.memset(m[:], 1.0), writes=[m])
        sg = 1 if d == 0 else -1
        S.emit("pool", lambda e, m=m, sg=sg: e.affine_select(out=m[:], in_=m[:], pattern=[[sg, CH]], compare_op=ALU.is_ge, fill=0.0,
                                                             base=0, channel_multiplier=-sg), [m], [m])
        cm.append(m)
    identb, onesb = C["identb"], C["onesb"]
    qT2 = [A.sb(f"qT{i}", [128, T], BF16) for i in range(2)]
    fl2 = [[A.sb(f"fl{d}{i}", [128, TT], BF16) for d in range(2)] for i in range(2)]
    vv2 = [A.sb(f"vv{i}", [CH, NCH, 128], BF16) for i in range(2)]
    og2 = [A.sb("og0", [128, T], BF16)] * 2
    qt = [A.sb(f"qt{d}", [128, T], BF16) for d in range(2)]
    kt = [A.sb(f"kt{d}", [128, TT], BF16) for d in range(2)]
    kh = [A.sb(f"kh{d}", [128, TT], BF16) for d in range(2)]
    egt = [A.sb(f"egt{d}", [128, NCH], F32) for d in range(2)]
    tsets = [[A.sb(f"t{n}{i}", [128, SEG], F32) for n in "ABCD"] for i in range(2)]
    OA = A.sb("OA", [128, T], F32)
    Sf = [[A.sb(f"S{d}{i}", [128, 128], F32) for i in range(2)] for d in range(2)]
    Sb = [[A.sb(f"Sb{d}{i}", [128, 128], BF16) for i in range(3)] for d in range(2)]
    sTs = [[A.sb(f"sTs{d}{i}", [CH, CH], BF16) for i in range(3)] for d in range(2)]
    khs = [[A.sb(f"khs{d}{i}", [CH, 128], BF16) for i in range(3)] for d in range(2)]
    sqb = A.sb("sqb", [128, 512], BF16)
    rt = A.sb("rt", [128, 512], F32)
    ys = [A.sb("ys0", [128, 512], BF16)] * 2
    pst = [A.ps(f"pst{d}", [CH, 512], F32) for d in range(2)]
    ptr = [A.ps(f"ptr{d}", [CH, 1024], BF16) for d in range(2)]
    po = [A.ps(f"po{d}", [128, 512], F32) for d in range(2)]
    pd = [A.ps(f"pd{d}", [128, 512], F32) for d in range(2)]

    def load_head(h):
        r0 = h * 128
        qT, fl, vv, og = qT2[h % 2], fl2[h % 2], vv2[h % 2], og2[h % 2]
        S.dma("sp", qT[:], G["hqT"][r0:r0 + 128, :], [G["hqT"]], [qT])
        S.dma("sp", fl[0][:], G["hffT"][r0:r0 + 128, :], [G["hffT"]], [fl[0]])
        S.dma("sp", fl[1][:], G["hfbT"][r0:r0 + 128, :], [G["hfbT"]], [fl[1]])
        S.dma("sp", vv[:], G["hi"][:, r0:r0 + 128].rearrange("(n c) v -> c n v", c=CH), [G["hi"]], [vv])

    tsi_box = [0]

    def do_head(h, qT, fl, vv, og):
        r0 = h * 128
        S.dma("sp", og[:], G["hogT"][r0:r0 + 128, :], [G["hogT"]], [og])
        for d in range(2):
            lo_, om_, nom_ = low[:, d, h:h + 1], oml[:, d, h:h + 1], noml[:, d, h:h + 1]
            for (a, n) in segs:
                tA, tB, tC, tD = tsets[tsi_box[0] % 2]
                tsi_box[0] += 1
                nch = n // CH
                c0 = a // CH
                v3 = lambda t, n=n: t[:, 0:n].rearrange("p (n c) -> p n c", c=CH)
                S.emit("act", lambda e, tA=tA, tB=tB, tC=tC, tD=tD, a=a, n=n, d=d: e.activation(out=tA[:, 0:n], in_=fl[d][:, a:a + n], func=AF.Sigmoid), [fl[d]], [tA])
                S.emit("act", lambda e, tA=tA, tB=tB, tC=tC, tD=tD, n=n, lo_=lo_, om_=om_: e.activation(out=tB[:, 0:n], in_=tA[:, 0:n], func=AF.Ln, scale=om_, bias=lo_),
                       [tA, low, oml], [tB])
                S.emit("dve", lambda e, tA=tA, tB=tB, tC=tC, tD=tD, n=n, om_=om_, nom_=nom_: e.tensor_scalar(out=tC[:, 0:n], in0=tA[:, 0:n], scalar1=nom_, scalar2=om_,
                                                                                op0=ALU.mult, op1=ALU.add), [tA, oml, noml], [tC])
                S.emit("dve", lambda e, tA=tA, tB=tB, tC=tC, tD=tD, n=n: e.tensor_tensor_scan(out=tA[:, 0:n], data0=msk01[:, 0:n], data1=tB[:, 0:n], initial=0.0,
                                                                  op0=ALU.mult, op1=ALU.add), [msk01, tB, tA], [tA])
                tot = lambda nch=nch, v3=v3, tA=tA: v3(tA)[:, :, CH - 1:CH]
                if d == 0:
                    Gd = tA
                else:
                    S.emit("dve", lambda e, tA=tA, tB=tB, tC=tC, tD=tD, n=n: e.tensor_tensor(out=tD[:, 0:n], in0=tB[:, 0:n], in1=tA[:, 0:n], op=ALU.subtract), [tA, tB], [tD])
                    S.emit("dve", lambda e, tA=tA, tB=tB, tC=tC, tD=tD, v3=v3, tot=tot, nch=nch: e.tensor_tensor(out=v3(tB), in0=v3(tD), in1=tot().to_broadcast([128, nch, CH]),
                                                                                     op=ALU.add), [tD, tA], [tB])
                    Gd = tB
                S.emit("act", lambda e, tA=tA, tB=tB, tC=tC, tD=tD, c0=c0, nch=nch, d=d, tot=tot: e.activation(out=egt[d][:, c0:c0 + nch].rearrange("p (n o) -> p n o", o=1),
                                                                                  in_=tot(), func=AF.Exp), [tA], [egt[d]])
                if a >= NCTX:
                    S.emit("act", lambda e, tA=tA, tB=tB, tC=tC, tD=tD, n=n, Gd=Gd: e.activation(out=tD[:, 0:n], in_=Gd[:, 0:n], func=AF.Exp), [Gd], [tD])
                    S.emit("dve", lambda e, tA=tA, tB=tB, tC=tC, tD=tD, n=n, a=a, d=d: e.tensor_tensor(out=qt[d][:, a - NCTX:a - NCTX + n], in0=qT[:, a - NCTX:a - NCTX + n],
                                                                           in1=tD[:, 0:n], op=ALU.mult), [qT, tD], [qt[d]])
                S.emit("act", lambda e, tA=tA, tB=tB, tC=tC, tD=tD, n=n, Gd=Gd: e.activation(out=tD[:, 0:n], in_=Gd[:, 0:n], func=AF.Exp, scale=-1.0), [Gd], [tD])
                S.emit("pool", lambda e, tA=tA, tB=tB, tC=tC, tD=tD, n=n, a=a, d=d: e.tensor_tensor(out=kt[d][:, a:a + n], in0=tC[:, 0:n], in1=tD[:, 0:n], op=ALU.mult),
                       [tC, tD], [kt[d]])
                S.emit("dve", lambda e, tA=tA, tB=tB, tC=tC, tD=tD, v3=v3, tot=tot, nch=nch, Gd=Gd: e.tensor_tensor(out=v3(tD), in0=tot().to_broadcast([128, nch, CH]),
                                                                                       in1=v3(Gd), op=ALU.subtract), [tA, Gd], [tD])
                S.emit("act", lambda e, tA=tA, tB=tB, tC=tC, tD=tD, n=n: e.activation(out=tD[:, 0:n], in_=tD[:, 0:n], func=AF.Exp), [tD], [tD])
                S.emit("pool", lambda e, tA=tA, tB=tB, tC=tC, tD=tD, n=n, a=a, d=d: e.tensor_tensor(out=kh[d][:, a:a + n], in0=tC[:, 0:n], in1=tD[:, 0:n], op=ALU.mult),
                       [tC, tD], [kh[d]])
        for d in range(2):
            S.emit("pool", lambda e, d=d: e.memset(Sf[d][0][:], 0.0), writes=[Sf[d][0]])
            S.emit("pool", lambda e, d=d: e.memset(Sb[d][0][:], 0.0), writes=[Sb[d][0]])
        OAc = {}

        def genA(d, n):
            c = order[d][n]
            t0 = c * CH
            sl = n % 3
            if c >= NCC:
                q0 = t0 - NCTX
                S.emit("pe", lambda e: e.matmul(pst[d][:, 0:CH], lhsT=kt[d][:, t0:t0 + CH], rhs=qt[d][:, q0:q0 + CH],
                                                start=True, stop=True), [kt[d], qt[d]], [pst[d]])
                yield
                S.emit("dve", lambda e: e.tensor_tensor(out=sTs[d][sl][:], in0=pst[d][:, 0:CH], in1=cm[d][:], op=ALU.mult),
                       [pst[d], cm[d]], [sTs[d][sl]])
                yield
            S.emit("pe", lambda e: e.transpose(out=ptr[d][:, 0:128], in_=kh[d][:, t0:t0 + CH], identity=identb[:]),
                   [kh[d], identb], [ptr[d]])
            yield
            S.emit("act", lambda e: e.copy(out=khs[d][sl][:], in_=ptr[d][:, 0:128]), [ptr[d]], [khs[d][sl]])
            yield

        def genB(d, n):
            c = order[d][n]
            t0 = c * CH
            sl = n % 3
            Sf_o, Sf_n = Sf[d][n % 2], Sf[d][(n + 1) % 2]
            Sb_o, Sb_n = Sb[d][n % 3], Sb[d][(n + 1) % 3]
            S.emit("pe", lambda e: e.matmul(pd[d][:, 0:128], lhsT=khs[d][sl][:], rhs=vv[:, c, :], start=True, stop=True),
                   [khs[d][sl], vv], [pd[d]])
            yield
            S.emit("dve", lambda e: e.scalar_tensor_tensor(out=Sf_n[:], in0=Sf_o[:], scalar=egt[d][:, c:c + 1], in1=pd[d][:, 0:128],
                                                           op0=ALU.mult, op1=ALU.add), [Sf_o, egt[d], pd[d]], [Sf_n])
            yield
            S.emit("act", lambda e: e.copy(out=Sb_n[:], in_=Sf_n[:]), [Sf_n], [Sb_n])
            yield
            if c >= NCC:
                q0 = t0 - NCTX

                def mo(e):
                    e.matmul(po[d][:, 0:CH], lhsT=Sb_o[:], rhs=qt[d][:, q0:q0 + CH], start=True, stop=False)
                    return e.matmul(po[d][:, 0:CH], lhsT=vv[:, c, :], rhs=sTs[d][sl][:], start=False, stop=True)
                S.emit("pe", mo, [Sb_o, qt[d], vv, sTs[d][sl]], [po[d]])
                yield
                if c not in OAc:
                    OAc[c] = Buf(f"OAc{c}")
                    OAc[c].ws = dict(OA.ws)
                    OAc[c].rs = dict(OA.rs)
                    S.emit("act", lambda e: e.copy(out=OA[:, q0:q0 + CH], in_=po[d][:, 0:CH]), [po[d]], [OAc[c]])
                else:
                    S.emit("dve", lambda e: e.tensor_tensor(out=OA[:, q0:q0 + CH], in0=po[d][:, 0:CH], in1=OA[:, q0:q0 + CH],
                                                            op=ALU.add), [po[d], OAc[c]], [OAc[c]])
                yield

        def run_streams(streams):
            while streams:
                for g in list(streams):
                    try:
                        next(g)
                    except StopIteration:
                        streams.remove(g)

        run_streams([genA(0, 0), genA(1, 0)])
        for n in range(NCH):
            sts = []
            if n + 1 < NCH:
                sts += [genA(0, n + 1), genA(1, n + 1)]
            sts += [genB(0, n), genB(1, n)]
            run_streams(sts)
        for bk in OAc.values():
            for k_, v_ in bk.ws.items():
                if OA.ws.get(k_, 0) < v_:
                    OA.ws[k_] = v_
        S.emit("dve", lambda e: e.tensor_tensor(out=OA[:], in0=OA[:], in1=og[:], op=ALU.mult), [OA, og], [OA])
        for bi, t0 in enumerate(range(0, T, 512)):
            nb = min(512, T - t0)
            S.emit("act", lambda e, t0=t0, nb=nb: e.activation(out=sqb[:, 0:nb], in_=OA[:, t0:t0 + nb], func=AF.Square), [OA], [sqb])
            S.emit("pe", lambda e, nb=nb: e.matmul(po[0][:, 0:nb], lhsT=onesb[:], rhs=sqb[:, 0:nb], start=True, stop=True), [onesb, sqb], [po[0]])
            S.emit("act", lambda e, nb=nb: e.activation(out=rt[:, 0:nb], in_=po[0][:, 0:nb], func=AF.Ln, scale=1.0 / 128, bias=C["epsc"][:, 0:1]),
                   [po[0], C["epsc"]], [rt])
            S.emit("act", lambda e, nb=nb: e.activation(out=rt[:, 0:nb], in_=rt[:, 0:nb], func=AF.Exp, scale=-0.5), [rt], [rt])
            y = ys[bi % 2]
            S.emit("dve", lambda e, t0=t0, nb=nb, y=y, h=h: e.scalar_tensor_tensor(out=y[:, 0:nb], in0=OA[:, t0:t0 + nb], scalar=gain[:, h:h + 1],
                                                                                   in1=rt[:, 0:nb], op0=ALU.mult, op1=ALU.mult), [OA, gain, rt], [y])
            S.dma("sp", G["yAT"][r0:r0 + 128, t0:t0 + nb], y[:, 0:nb], [y], [G["yAT"]])

    load_head(0)
    for h in range(H):
        if h + 1 < H:
            load_head(h + 1)
        do_head(h, qT2[h % 2], fl2[h % 2], vv2[h % 2], og2[h % 2])
    A.close()


def phase_gconv(S, cfg, G, I, C):
    A = Arena(S, "gc")
    H, T, TT, NCTX, HV = cfg.H, cfg.T, cfg.TT, cfg.NCTX, cfg.HV
    NB = 3 * HV // 128
    cw = A.sb("cw", [128, 5, NB], F32)
    for j in range(5):
        S.dma("sp", cw[:, j, :], I["gdn_conv"][0, j].rearrange("(n p) -> p n", p=128), [I["gdn_conv"]], [cw], allow_slow_non_contiguous=True)
    xp = [A.sb(f"xp{i}", [128, TT + 8], BF16) for i in range(2)]
    for x in xp:
        S.emit("pool", lambda e, x=x: e.memset(x[:], 0.0), writes=[x])
    y = A.sb("y", [128, TT], F32)
    sq = A.sb("sq", [128, 512], BF16)
    rt = A.sb("rt", [128, 512], F32)
    ob = [A.sb(f"ob{i}", [128, TT], BF16) for i in range(2)]
    pn = [A.ps(f"pn{i}", [128, 512], F32) for i in range(2)]
    segs = [(0, NCTX, 2), (NCTX, T, NCTX + 6)]
    dsts = [G["gq"], G["gk"], G["gv"]]
    pi = 0
    for fb in range(NB):
        kind, hb = divmod(fb, H)
        x = xp[fb % 2]
        r0 = fb * 128
        for (a, n, off) in segs:
            if kind == 0 and a == 0:
                continue
            S.dma("sp" if fb % 2 == 0 else "pool", x[:, off:off + n], G["gqkvT"][r0:r0 + 128, a:a + n], [G["gqkvT"]], [x])
            S.emit("dve", lambda e, x=x, a=a, n=n, off=off, fb=fb: e.tensor_scalar(out=y[:, a:a + n], in0=x[:, off - 2:off - 2 + n], scalar1=cw[:, 0, fb:fb + 1],
                                                                                  scalar2=None, op0=ALU.mult), [x, cw], [y])
            for j in range(1, 5):
                S.emit("dve", lambda e, x=x, a=a, n=n, off=off, fb=fb, j=j: e.scalar_tensor_tensor(
                    out=y[:, a:a + n], in0=x[:, off - 2 + j:off - 2 + j + n], scalar=cw[:, j, fb:fb + 1], in1=y[:, a:a + n],
                    op0=ALU.mult, op1=ALU.add), [x, cw, y], [y])
        lo = 0 if kind != 0 else NCTX
        o = ob[fb % 2]
        S.emit("act", lambda e, lo=lo: e.activation(out=y[:, lo:TT], in_=y[:, lo:TT], func=AF.Silu), [y], [y])
        if kind == 2:
            S.emit("dve", lambda e, o=o: e.tensor_copy(out=o[:], in_=y[:]), [y], [o])
        else:
            sc = (128.0 ** -0.5) if kind == 0 else 1.0
            for (t0, nb) in token_blocks(cfg, lo, TT):
                p = pn[pi % 2]
                pi += 1
                S.emit("dve", lambda e, t0=t0, nb=nb: e.tensor_tensor(out=sq[:, 0:nb], in0=y[:, t0:t0 + nb], in1=y[:, t0:t0 + nb], op=ALU.mult), [y], [sq])
                S.emit("pe", lambda e, p=p, nb=nb: e.matmul(p[:, 0:nb], lhsT=C["onesb"][:], rhs=sq[:, 0:nb], start=True, stop=True), [C["onesb"], sq], [p])
                S.emit("act", lambda e, p=p, nb=nb: e.activation(out=rt[:, 0:nb], in_=p[:, 0:nb], func=AF.Ln, bias=C["epsc"][:, 0:1]), [p, C["epsc"]], [rt])
                S.emit("act", lambda e, nb=nb: e.activation(out=rt[:, 0:nb], in_=rt[:, 0:nb], func=AF.Exp, scale=-0.5), [rt], [rt])
                S.emit("dve", lambda e, o=o, t0=t0, nb=nb, sc=sc: e.scalar_tensor_tensor(out=o[:, t0:t0 + nb], in0=y[:, t0:t0 + nb], scalar=sc, in1=rt[:, 0:nb],
                                                                                        op0=ALU.mult, op1=ALU.mult), [y, rt], [o])
        S.dma("sp", dsts[kind][hb * 128:(hb + 1) * 128, lo:TT], o[:, lo:TT], [o], [dsts[kind]])
    A.close()


def phase_gdn(S, cfg, G, I, C):
    H, T, TT, NCTX, HV, R, GW = cfg.H, cfg.T, cfg.TT, cfg.NCTX, cfg.HV, cfg.R, cfg.GW
    NCHK = TT // 128
    NCC = NCTX // 128
    order = [list(range(NCHK)), list(range(NCC - 1, -1, -1)) + list(range(NCHK - 1, NCC - 1, -1))]
    HG = min(4, H)
    identb, identf, onesb, onesf = C["identb"], C["identf"], C["onesb"], C["onesf"]
    A = Arena(S, "gd")
    def indicator(name, sg, strict):
        m = A.sb(name, [128, 128], F32)
        S.emit("pool", lambda e: e.memset(m[:], 1.0), writes=[m])
        S.emit("pool", lambda e: e.affine_select(out=m[:], in_=m[:], pattern=[[sg, 128]], compare_op=ALU.is_ge, fill=0.0,
                                                 base=-strict, channel_multiplier=-sg), [m], [m])
        return m
    VI = [indicator("VIf", 1, 0), indicator("VIb", -1, 0)]
    VS = [indicator("VSf", 1, 1), indicator("VSb", -1, 1)]
    NMI, NMS = [], []
    for d in range(2):
        for (lst, src, nm) in ((NMI, VI[d], f"NMI{d}"), (NMS, VS[d], f"NMS{d}")):
            m = A.sb(nm, [128, 128], BF16)
            S.emit("dve", lambda e, m=m, src=src: e.tensor_scalar(out=m[:], in0=src[:], scalar1=-1.0, scalar2=-NEG, op0=ALU.add, op1=ALU.mult), [src], [m])
            lst.append(m)
    dmask = A.sb("dmask", [128, 128], BF16)
    S.emit("pool", lambda e: e.memset(dmask[:], 1.0), writes=[dmask])
    S.emit("pool", lambda e: e.affine_select(out=dmask[:].rearrange("p (b c) -> p b c", c=32), in_=dmask[:].rearrange("p (b c) -> p b c", c=32),
                                             pattern=[[-32, 4], [0, 32]], compare_op=ALU.is_ge, fill=0.0, base=0, channel_multiplier=1), [dmask], [dmask])
    S.emit("pool", lambda e: e.affine_select(out=dmask[:].rearrange("p (b c) -> p b c", c=32), in_=dmask[:].rearrange("p (b c) -> p b c", c=32),
                                             pattern=[[32, 4], [0, 32]], compare_op=ALU.is_ge, fill=0.0, base=31, channel_multiplier=-1), [dmask], [dmask])
    omask = A.sb("omask", [128, 128], BF16)
    S.emit("dve", lambda e: e.tensor_scalar(out=omask[:], in0=dmask[:], scalar1=-1.0, scalar2=1.0, op0=ALU.mult, op1=ALU.add), [dmask], [omask])
    LC = [VI[0], VI[1]]
    LR = [VS[1], VS[0]]
    W2 = 2 * H
    sh = [128, NCHK, W2]
    bet = A.sb("bet", sh, F32)
    gsp = A.sb("gsp", [128, NCHK, 2, W2], BF16)
    lsp = A.sb("lsp", [128, NCHK, 2, W2], BF16)
    egr = A.sb("egr", sh, F32)
    egt = A.sb("egt", sh, F32)
    nbe = A.sb("nbe", sh, F32)
    A0 = Arena(S, "gd0")
    gg = A0.sb("gg", sh, F32)
    lnb = A0.sb("lnb", sh, F32)
    gres = A0.sb("gres", sh, F32)
    gab = A0.sb("gab", [128, NCHK, 4 * H], F32)
    S.dma("sp", gab[:], G["gab"][:, :].rearrange("(n p) c -> p n c", p=128), [G["gab"]], [gab])
    cst = A0.sb("cst", [128, 2, 2 * H], F32)
    S.dma("sp", cst[:, 0, :], I["gdn_dt_bias"][0].rearrange("d h -> (d h)").partition_broadcast(128), [I["gdn_dt_bias"]], [cst])
    S.dma("sp", cst[:, 1, :], I["gdn_a_log"][0].rearrange("d h -> (d h)").partition_broadcast(128), [I["gdn_a_log"]], [cst])
    nea = A0.sb("nea", [128, 2 * H], F32)
    S.emit("act", lambda e: e.activation(out=nea[:], in_=cst[:, 1, :], func=AF.Exp), [cst], [nea])
    S.emit("dve", lambda e: e.tensor_scalar(out=nea[:], in0=nea[:], scalar1=-1.0, scalar2=None, op0=ALU.mult), [nea], [nea])
    gx = A0.sb("gx", sh, F32); gt1 = A0.sb("gt1", sh, F32); gt2 = A0.sb("gt2", sh, F32)
    bc = lambda t: t.rearrange("p (o c) -> p o c", o=1).to_broadcast(sh)
    S.emit("dve", lambda e: e.tensor_tensor(out=gx[:], in0=gab[:, :, 0:W2], in1=bc(cst[:, 0, :]), op=ALU.add), [gab, cst], [gx])
    S.emit("dve", lambda e: e.tensor_scalar(out=gt1[:], in0=gx[:], scalar1=-1.0, scalar2=None, op0=ALU.mult), [gx], [gt1])
    S.emit("dve", lambda e: e.tensor_tensor(out=gt1[:], in0=gt1[:], in1=gx[:], op=ALU.max), [gt1, gx], [gt1])
    S.emit("act", lambda e: e.activation(out=gt1[:], in_=gt1[:], func=AF.Exp, scale=-1.0), [gt1], [gt1])
    S.emit("act", lambda e: e.activation(out=gt1[:], in_=gt1[:], func=AF.Ln, bias=C["onec"][:, 0:1]), [gt1, C["onec"]], [gt1])
    S.emit("dve", lambda e: e.tensor_scalar(out=gt2[:], in0=gx[:], scalar1=0.0, scalar2=None, op0=ALU.max), [gx], [gt2])
    S.emit("dve", lambda e: e.tensor_tensor(out=gt2[:], in0=gt2[:], in1=gt1[:], op=ALU.add), [gt2, gt1], [gt2])
    S.emit("dve", lambda e: e.tensor_tensor(out=gg[:], in0=gt2[:], in1=bc(nea[:]), op=ALU.mult), [gt2, nea], [gg])
    S.emit("act", lambda e: e.activation(out=bet[:], in_=gab[:, :, W2:2 * W2], func=AF.Sigmoid), [gab], [bet])
    S.emit("act", lambda e: e.activation(out=lnb[:], in_=bet[:], func=AF.Ln), [bet], [lnb])
    for src, dst in ((gg, gsp), (lnb, lsp)):
        S.emit("dve", lambda e, src=src, dst=dst: e.tensor_copy(out=dst[:, :, 0, :], in_=src[:]), [src], [dst])
        S.emit("dve", lambda e, src=src, dst=dst: e.tensor_tensor(out=gres[:], in0=src[:], in1=dst[:, :, 0, :], op=ALU.subtract), [src, dst], [gres])
        S.emit("dve", lambda e, dst=dst: e.tensor_copy(out=dst[:, :, 1, :], in_=gres[:]), [gres, dst], [dst])
    pA = A.ps("pA", [128, 512], F32)
    pg = pA
    for n in range(NCHK):
        def mm(e, n=n):
            e.matmul(pg[:, 0:H], lhsT=LC[0][:], rhs=gg[:, n, 0:H], start=True, stop=True)
            e.matmul(pg[:, H:W2], lhsT=LC[1][:], rhs=gg[:, n, H:W2], start=True, stop=True)
            e.matmul(pg[:, W2:W2 + H], lhsT=LR[0][:], rhs=gg[:, n, 0:H], start=True, stop=True)
            e.matmul(pg[:, W2 + H:2 * W2], lhsT=LR[1][:], rhs=gg[:, n, H:W2], start=True, stop=True)
            return e.matmul(pg[:, 2 * W2:3 * W2], lhsT=onesf[:], rhs=gg[:, n, :], start=True, stop=True)
        S.emit("pe", mm, [LC[0], LC[1], LR[0], LR[1], onesf, gg], [pg])
        S.emit("act", lambda e, n=n: e.activation(out=egr[:, n, :], in_=pg[:, W2:2 * W2], func=AF.Exp), [pg, egr], [egr])
        S.emit("act", lambda e, n=n: e.activation(out=egt[:, n, :], in_=pg[:, 2 * W2:3 * W2], func=AF.Exp), [pg, egt], [egt])
        S.emit("act", lambda e, n=n: e.activation(out=nbe[:, n, :], in_=pg[:, 0:W2], func=AF.Exp), [pg, nbe], [nbe])
    S.emit("dve", lambda e: e.scalar_tensor_tensor(out=nbe[:], in0=nbe[:], scalar=-1.0, in1=bet[:], op0=ALU.mult, op1=ALU.mult), [nbe, bet], [nbe])
    if "dbg_g" in G:
        for i, t in enumerate((gg, gg, egr, egt, nbe, bet)):
            S.dma("sp", G["dbg_g"][i], t[:], [t], [G["dbg_g"]])
    A0.close()
    LCb = []
    for d in range(2):
        m = A.sb(f"LCb{d}", [128, 128], BF16)
        S.emit("dve", lambda e, m=m, d=d: e.tensor_copy(out=m[:], in_=LC[d][:]), [LC[d]], [m])
        LCb.append(m)
    gain = A.sb("gain", [128, H], F32)
    S.dma("sp", gain[:], I["gdn_norm"][0].rearrange("(h p) -> p h", p=128), [I["gdn_norm"]], [gain], allow_slow_non_contiguous=True)

    NU = 2 * HG
    ld = {nm: [[A.sb(f"ld{nm}{d}{i}", [128, HG, 128], BF16) for i in range(3 if nm == "k" else 2)] for d in range(2)] for nm in "kqv"}
    U = []
    for u in range(NU):
        ub = {}
        for nm in ("TT", "PT", "qg", "kd", "vb"):
            ub[nm] = [A.sb(f"u{u}{nm}{i}", [128, 128], BF16) for i in range(2 if nm == "TT" else 3)]
        ub["X"] = [[A.sb(f"u{u}X{j}{i}", [128, 4, 128], BF16) for i in range(2)] for j in range(2)]
        ub["Z0"] = [A.sb(f"u{u}Z0{j}", [128, 128], BF16) for j in range(2)]
        ub["OT"] = [A.sb(f"u{u}OT{j}", [128, 128], BF16) for j in range(2)]
        ub["gm"] = [A.sb(f"u{u}gm{j}", [128, 2, 128], BF16) for j in range(2)]
        ub["eg"] = [A.sb(f"u{u}eg{j}", [128, 128], BF16) for j in range(2)]
        ub["DD"] = A.sb(f"u{u}DD", [128, 2, 128], BF16)
        ub["NN"] = A.sb(f"u{u}NN", [128, 2, 128], BF16)
        ub["N2"] = A.sb(f"u{u}N2", [128, 2, 128], BF16)
        ub["PP"] = A.sb(f"u{u}PP", [128, 2, 128], BF16)
        ub["S"] = A.sb(f"u{u}S", [128, 128], F32)
        ub["Sb"] = A.sb(f"u{u}Sb", [128, 128], BF16)
        ub["r"] = A.sb(f"u{u}r", [128, 128], BF16)
        ub["vn"] = A.sb(f"u{u}vn", [128, 128], BF16)
        U.append(ub)
    ngu = [A.sb(f"ngu{i}", [128, 1], F32) for i in range(2)]
    OB = A.sb("OB", [128, HG, T], F32)
    zq = A.sb("zq", [128, 512], BF16); sqb = A.sb("sqb", [128, 512], BF16); rt = A.sb("rt", [128, 512], F32)
    ys = [sqb, sqb]
    djunk = rt
    pR = A.ps("pR", [128, 512], F32)
    pT = A.ps("pT", [128, 1024], BF16)
    pI = [A.ps(f"pI{i}", [128, 512], F32) for i in range(4)]
    pS = A.ps("pS", [128, 512], F32)
    ncol = 128 // R if R < 128 else 1
    v4 = lambda p: p[:, 0:512].rearrange("p (a b) -> p a b", b=128)

    def run_streams(streams):
        streams = [g for g in streams if g is not None]
        while streams:
            for g in list(streams):
                try:
                    next(g)
                except StopIteration:
                    streams.remove(g)

    for hg in range(H // HG):
        for ub in U:
            S.emit("pool", lambda e, ub=ub: e.memset(ub["S"][:], 0.0), writes=[ub["S"]])
            S.emit("pool", lambda e, ub=ub: e.memset(ub["Sb"][:], 0.0), writes=[ub["Sb"]])
        OBc = {}

        def units_of(s):
            out = []
            for d in range(2):
                c = order[d][s]
                for hl in range(HG):
                    out.append((d, hl, c, c >= NCC))
            return out

        def gen_P(s):
            s3, s2 = s % 3, s % 2
            for d in range(2):
                c = order[d][s]
                t0 = c * 128
                lat = c >= NCC
                for nm, src in (("k", G["gk"]), ("q", G["gq"]), ("v", G["gv"])):
                    if nm == "q" and not lat:
                        continue
                    dst = ld[nm][d][s3 if nm == "k" else s2]
                    S.dma("sp" if d == 0 else "pool", dst[:],
                          src[hg * HG * 128:(hg + 1) * HG * 128, t0:t0 + 128].rearrange("(h p) t -> p h t", p=128), [src], [dst])
            yield
            ii = 0
            for (d, hl, c, lat) in units_of(s):
                u = d * HG + hl
                ub = U[u]
                h = hg * HG + hl
                dh = d * H + h
                kT = ld["k"][d][s3]; qTb = ld["q"][d][s2]; vT = ld["v"][d][s2]
                X0 = ub["X"][s2][0]
                X1 = ub["X"][s2][1]
                Z0 = ub["Z0"][s2]
                OT = ub["OT"][s2]
                def mA(e, kT=kT, qTb=qTb, hl=hl, lat=lat):
                    r = e.matmul(pA[:, 0:128], lhsT=kT[:, hl, :], rhs=kT[:, hl, :], start=True, stop=True)
                    if lat:
                        r = e.matmul(pA[:, 128:256], lhsT=kT[:, hl, :], rhs=qTb[:, hl, :], start=True, stop=True)
                    return r
                S.emit("pe", mA, [kT, qTb], [pA])
                yield
                gm = ub["gm"][s2]
                eg = ub["eg"][s2]
                S.emit("dve", lambda e, Z0=Z0, gm=gm: e.scalar_tensor_tensor(out=Z0[:], in0=pA[:, 0:128], scalar=-1.0, in1=gm[:, 1, :],
                                                                             op0=ALU.mult, op1=ALU.mult), [pA, gm], [Z0])
                yield
                def mT(e, Z0=Z0, kT=kT, vT=vT, hl=hl):
                    e.transpose(out=pT[:, 0:128], in_=Z0[:], identity=identb[:])
                    e.transpose(out=pT[:, 128:256], in_=kT[:, hl, :], identity=identb[:])
                    return e.transpose(out=pT[:, 256:384], in_=vT[:, hl, :], identity=identb[:])
                S.emit("pe", mT, [Z0, kT, vT, identb], [pT])
                if lat:
                    S.emit("dve", lambda e, ub=ub, s3=s3, gm=gm: e.tensor_tensor(out=ub["PT"][s3][:], in0=pA[:, 128:256], in1=gm[:, 0, :], op=ALU.mult),
                           [pA, gm], [ub["PT"][s3]])
                S.emit("pool", lambda e, X0=X0, Z0=Z0: e.tensor_tensor(out=X0[:, 0, :], in0=Z0[:], in1=dmask[:], op=ALU.mult), [Z0, dmask, X0], [X0])
                yield
                if lat:
                    S.emit("dve", lambda e, ub=ub, s3=s3, eg=eg, qTb=qTb, hl=hl: e.tensor_tensor(out=ub["qg"][s3][:], in0=qTb[:, hl, :], in1=eg[:], op=ALU.mult),
                           [qTb, eg], [ub["qg"][s3]])
                S.emit("pool", lambda e, X1=X1, X0=X0: e.tensor_tensor(out=X1[:, 1, :], in0=X0[:, 0, :], in1=identb[:], op=ALU.add), [X0, identb, X1], [X1])
                yield
                S.emit("dve", lambda e, X0=X0: e.tensor_tensor(out=X0[:, 2, :], in0=pT[:, 0:128], in1=dmask[:], op=ALU.mult), [pT, dmask, X0], [X0])
                S.emit("dve", lambda e, OT=OT: e.tensor_tensor(out=OT[:], in0=pT[:, 0:128], in1=omask[:], op=ALU.mult), [pT, omask], [OT])
                S.emit("act", lambda e, ub=ub, s3=s3, c=c, dh=dh: e.activation(out=ub["kd"][s3][:], in_=pT[:, 128:256], func=AF.Copy, scale=egr[:, c, dh:dh + 1]),
                       [pT, egr], [ub["kd"][s3]])
                S.emit("act", lambda e, ub=ub, s3=s3, c=c, dh=dh: e.activation(out=ub["vb"][s3][:], in_=pT[:, 256:384], func=AF.Copy, scale=bet[:, c, dh:dh + 1]),
                       [pT, bet], [ub["vb"][s3]])
                yield
                S.emit("pool", lambda e, X1=X1, X0=X0: e.tensor_tensor(out=X1[:, 3, :], in0=X0[:, 2, :], in1=identb[:], op=ALU.add), [X0, identb, X1], [X1])
                yield

        def gen_G(s):
            s2 = s % 2
            ii = 0
            for (d, hl, c, lat) in units_of(s):
                u = d * HG + hl
                ub = U[u]
                h = hg * HG + hl
                dh = d * H + h
                gm = ub["gm"][s2]
                eg = ub["eg"][s2]
                ng = ngu[ii % 2]
                ii += 1
                def mR(e, c=c, dh=dh, d=d, lat=lat):
                    ghi = gsp[:, c, 0, dh:dh + 1].to_broadcast([128, 128])
                    glo = gsp[:, c, 1, dh:dh + 1].to_broadcast([128, 128])
                    e.matmul(pR[:, 256:384], lhsT=ghi, rhs=LCb[d][:], start=True, stop=False)
                    e.matmul(pR[:, 256:384], lhsT=glo, rhs=LCb[d][:], start=False, stop=True)
                    if lat:
                        e.matmul(pR[:, 0:128], lhsT=ghi, rhs=LCb[d][:], start=True, stop=False)
                        e.matmul(pR[:, 0:128], lhsT=glo, rhs=LCb[d][:], start=False, stop=False)
                        e.matmul(pR[:, 0:128], lhsT=identb[:], rhs=NMI[d][:], start=False, stop=True)
                    e.matmul(pR[:, 128:256], lhsT=ghi, rhs=LCb[d][:], start=True, stop=False)
                    e.matmul(pR[:, 128:256], lhsT=glo, rhs=LCb[d][:], start=False, stop=False)
                    e.matmul(pR[:, 128:256], lhsT=lsp[:, c, 0, dh:dh + 1].to_broadcast([128, 128]), rhs=identb[:], start=False, stop=False)
                    e.matmul(pR[:, 128:256], lhsT=lsp[:, c, 1, dh:dh + 1].to_broadcast([128, 128]), rhs=identb[:], start=False, stop=False)
                    return e.matmul(pR[:, 128:256], lhsT=identb[:], rhs=NMS[d][:], start=False, stop=True)
                S.emit("pe", mR, [gsp, lsp, LCb[d], identb, NMI[d], NMS[d]], [pR])
                yield
                S.emit("dve", lambda e: e.tensor_tensor(out=djunk[:, 0:128], in0=pR[:, 256:384], in1=identf[:], op=ALU.mult), [pR, identf], [djunk])
                yield
                S.emit("dve", lambda e, ng=ng: e.tensor_reduce(out=ng[:, 0:1], in_=djunk[:, 0:128], axis=mybir.AxisListType.X, op=ALU.add, negate=True), [djunk], [ng])
                yield
                lo = 0 if lat else 1
                S.emit("act", lambda e, gm=gm, ng=ng, lo=lo: e.activation(out=gm[:, lo:2, :], in_=pR[:, lo * 128:256].rearrange("p (a b) -> p a b", b=128),
                                                                          func=AF.Exp, bias=ng[:, 0:1]), [pR, ng], [gm])
                if lat:
                    S.emit("act", lambda e, eg=eg: e.activation(out=eg[:], in_=pR[:, 256:384], func=AF.Exp), [pR], [eg])
                yield

        def gen_I(s, bank):
            s3, s2 = s % 3, s % 2
            p = pI[bank]
            for (d, hl, c, lat) in units_of(s):
                u = d * HG + hl
                if u % 4 != bank:
                    continue
                ub = U[u]
                X = ub["X"][s2]
                DD, NN, N2, PP, OT = ub["DD"], ub["NN"], ub["N2"], ub["PP"], ub["OT"][s2]
                for st in range(9):
                    if st == 0:
                        Xc, Xn = X[0], X[1]
                        def m0(e, Xc=Xc):
                            e.matmul(p[:, 0:128], lhsT=Xc[:, 2, :], rhs=Xc[:, 0, :], start=True, stop=True)
                            return e.matmul(p[:, 256:384], lhsT=Xc[:, 0, :], rhs=Xc[:, 2, :], start=True, stop=True)
                        S.emit("pe", m0, [Xc], [p])
                        yield
                        S.emit("act", lambda e, Xn=Xn: e.copy(out=Xn[:, 0:4:2, :], in_=v4(p)[:, 0:4:2, :]), [p, Xn], [Xn])
                    elif st < 4:
                        Xc, Xn = X[st % 2], X[(st + 1) % 2]
                        def m1(e, Xc=Xc):
                            e.matmul(p[:, 0:256], lhsT=Xc[:, 2, :], rhs=Xc[:, 0:2, :], start=True, stop=True)
                            return e.matmul(p[:, 256:512], lhsT=Xc[:, 0, :], rhs=Xc[:, 2:4, :], start=True, stop=True)
                        S.emit("pe", m1, [Xc], [p])
                        yield
                        S.emit("act", lambda e, Xn=Xn: e.copy(out=Xn[:, 0:4:2, :], in_=v4(p)[:, 0:4:2, :]), [p, Xn], [Xn])
                        yield
                        S.emit("dve", lambda e, Xn=Xn, Xc=Xc: e.tensor_tensor(out=Xn[:, 1:4:2, :], in0=v4(p)[:, 1:4:2, :], in1=Xc[:, 1:4:2, :], op=ALU.add),
                               [p, Xc, Xn], [Xn])
                    elif st == 4:
                        Xc = X[0]
                        def m4(e, Xc=Xc):
                            e.matmul(p[:, 128:256], lhsT=Xc[:, 2, :], rhs=Xc[:, 1, :], start=True, stop=True)
                            return e.matmul(p[:, 384:512], lhsT=Xc[:, 0, :], rhs=Xc[:, 3, :], start=True, stop=True)
                        S.emit("pe", m4, [Xc], [p])
                        yield
                        S.emit("dve", lambda e, DD=DD, Xc=Xc: e.tensor_tensor(out=DD[:], in0=v4(p)[:, 1:4:2, :], in1=Xc[:, 1:4:2, :], op=ALU.add), [p, Xc], [DD])
                    elif st == 5:
                        def m5(e, DD=DD, OT=OT):
                            e.matmul(p[:, 0:128], lhsT=OT[:], rhs=DD[:, 0, :], start=True, stop=True)
                            return e.matmul(p[:, 128:256], lhsT=DD[:, 0, :], rhs=OT[:], start=True, stop=True)
                        S.emit("pe", m5, [DD, OT], [p])
                        yield
                        S.emit("act", lambda e, NN=NN: e.copy(out=NN[:], in_=v4(p)[:, 0:2, :]), [p], [NN])
                        yield
                        S.emit("dve", lambda e, PP=PP: e.tensor_tensor(out=PP[:, 0, :], in0=p[:, 0:128], in1=identb[:], op=ALU.add), [p, identb, PP], [PP])
                    elif st == 6:
                        def m6(e, NN=NN):
                            e.matmul(p[:, 0:128], lhsT=NN[:, 1, :], rhs=NN[:, 0, :], start=True, stop=True)
                            return e.matmul(p[:, 128:256], lhsT=NN[:, 0, :], rhs=NN[:, 1, :], start=True, stop=True)
                        S.emit("pe", m6, [NN], [p])
                        yield
                        S.emit("act", lambda e, N2=N2: e.copy(out=N2[:], in_=v4(p)[:, 0:2, :]), [p], [N2])
                    elif st == 7:
                        S.emit("pe", lambda e, N2=N2, PP=PP: e.matmul(p[:, 0:128], lhsT=N2[:, 1, :], rhs=PP[:, 0, :], start=True, stop=True), [N2, PP], [p])
                        yield
                        S.emit("dve", lambda e, PP=PP: e.tensor_tensor(out=PP[:, 1, :], in0=p[:, 0:128], in1=PP[:, 0, :], op=ALU.add), [p, PP], [PP])
                    else:
                        S.emit("pe", lambda e, DD=DD, PP=PP: e.matmul(p[:, 0:128], lhsT=DD[:, 1, :], rhs=PP[:, 1, :], start=True, stop=True), [DD, PP], [p])
                        yield
                        S.emit("act", lambda e, ub=ub, s3=s3: e.copy(out=ub["TT"][s2][:], in_=p[:, 0:128]), [p], [ub["TT"][s2]])
                    yield

        def gen_S(s):
            s3 = s % 3
            for (d, hl, c, lat) in units_of(s):
                u = d * HG + hl
                ub = U[u]
                h = hg * HG + hl
                dh = d * H + h
                kT = ld["k"][d][s3]
                S.emit("pe", lambda e, kT=kT, hl=hl, ub=ub: e.matmul(pS[:, 0:128], lhsT=kT[:, hl, :], rhs=ub["Sb"][:], start=True, stop=True), [kT, ub["Sb"]], [pS])
                yield
                S.emit("dve", lambda e, ub=ub, s3=s3, c=c, dh=dh: e.scalar_tensor_tensor(out=ub["r"][:], in0=pS[:, 0:128], scalar=nbe[:, c, dh:dh + 1], in1=ub["vb"][s3][:],
                                                                                        op0=ALU.mult, op1=ALU.add), [pS, nbe, ub["vb"][s3]], [ub["r"]])
                yield
                S.emit("pe", lambda e, ub=ub, s=s: e.matmul(pS[:, 128:256], lhsT=ub["TT"][s % 2][:], rhs=ub["r"][:], start=True, stop=True), [ub["TT"][s % 2], ub["r"]], [pS])
                yield
                S.emit("act", lambda e, ub=ub: e.copy(out=ub["vn"][:], in_=pS[:, 128:256]), [pS], [ub["vn"]])
                yield
                def mo(e, ub=ub, s3=s3, lat=lat):
                    if lat:
                        e.matmul(pS[:, 384:512], lhsT=ub["Sb"][:], rhs=ub["qg"][s3][:], start=True, stop=False)
                        e.matmul(pS[:, 384:512], lhsT=ub["vn"][:], rhs=ub["PT"][s3][:], start=False, stop=True)
                    return e.matmul(pS[:, 256:384], lhsT=ub["kd"][s3][:], rhs=ub["vn"][:], start=True, stop=True)
                S.emit("pe", mo, [ub["Sb"], ub["qg"][s3], ub["vn"], ub["PT"][s3], ub["kd"][s3]], [pS])
                yield
                if lat:
                    cl = c - NCC
                    if R >= 128:
                        per = R // 128
                        cc0, sub = divmod(cl, per)
                        oap = OB[:, hl, :].rearrange("p (r c) -> p c r", c=GW)[:, cc0, sub * 128:(sub + 1) * 128]
                        iap = pS[:, 384:512]
                    else:
                        oap = OB[:, hl, :].rearrange("p (r c) -> p c r", c=GW)[:, cl * ncol:(cl + 1) * ncol, :]
                        iap = pS[:, 384:512].rearrange("p (a b) -> p a b", b=R)
                    key = (hl, cl)
                    if key not in OBc:
                        OBc[key] = Buf(f"OBc{hl}_{cl}")
                        OBc[key].ws = dict(OB.ws)
                        OBc[key].rs = dict(OB.rs)
                        S.emit("act", lambda e, oap=oap, iap=iap: e.copy(out=oap, in_=iap), [pS], [OBc[key]])
                    else:
                        S.emit("dve", lambda e, oap=oap, iap=iap: e.tensor_tensor(out=oap, in0=iap, in1=oap, op=ALU.add), [pS, OBc[key]], [OBc[key]])
                S.emit("dve", lambda e, ub=ub, c=c, dh=dh: e.scalar_tensor_tensor(out=ub["S"][:], in0=ub["S"][:], scalar=egt[:, c, dh:dh + 1], in1=pS[:, 256:384],
                                                                                 op0=ALU.mult, op1=ALU.add), [ub["S"], egt, pS], [ub["S"]])
                yield
                S.emit("act", lambda e, ub=ub: e.copy(out=ub["Sb"][:], in_=ub["S"][:]), [ub["S"]], [ub["Sb"]])
                yield

        for it in range(-3, NCHK):
            sts = []
            if 0 <= it + 3 < NCHK:
                sts.append(gen_G(it + 3))
            if 0 <= it + 2 < NCHK:
                sts.append(gen_P(it + 2))
            if 0 <= it + 1 < NCHK:
                sts += [gen_I(it + 1, b) for b in range(4)]
            if it >= 0:
                sts.append(gen_S(it))
            run_streams(sts)
            if "dbg_S" in G and it == NCC - 1 and hg == 0:
                for u in range(NU):
                    S.dma("sp", G["dbg_S"][u], U[u]["S"][:], [U[u]["S"]], [G["dbg_S"]])
        for key, bk in OBc.items():
            for k_, v_ in bk.ws.items():
                if OB.ws.get(k_, 0) < v_:
                    OB.ws[k_] = v_
        for hl in range(HG):
            h = hg * HG + hl
            if "dbg_ob" in G:
                S.dma("sp", G["dbg_ob"][h * 128:(h + 1) * 128, :], OB[:, hl, :], [OB], [G["dbg_ob"]])
            for bi, t0 in enumerate(range(0, T, 512)):
                nb = min(512, T - t0)
                S.dma("sp", zq[:, 0:nb], G["gzT"][h * 128:(h + 1) * 128, t0:t0 + nb], [G["gzT"]], [zq])
                S.emit("act", lambda e, hl=hl, t0=t0, nb=nb: e.activation(out=sqb[:, 0:nb], in_=OB[:, hl, t0:t0 + nb], func=AF.Square), [OB], [sqb])
                S.emit("pe", lambda e, nb=nb: e.matmul(pA[:, 0:nb], lhsT=onesb[:], rhs=sqb[:, 0:nb], start=True, stop=True), [onesb, sqb], [pA])
                S.emit("act", lambda e, nb=nb: e.activation(out=rt[:, 0:nb], in_=pA[:, 0:nb], func=AF.Ln, scale=1.0 / 128, bias=C["epsc"][:, 0:1]), [pA, C["epsc"]], [rt])
                S.emit("act", lambda e, nb=nb: e.activation(out=rt[:, 0:nb], in_=rt[:, 0:nb], func=AF.Exp, scale=-0.5), [rt], [rt])
                S.emit("dve", lambda e, nb=nb: e.tensor_tensor(out=rt[:, 0:nb], in0=rt[:, 0:nb], in1=zq[:, 0:nb], op=ALU.mult), [rt, zq], [rt])
                y = ys[bi % 2]
                S.emit("dve", lambda e, hl=hl, h=h, t0=t0, nb=nb, y=y: e.scalar_tensor_tensor(out=y[:, 0:nb], in0=OB[:, hl, t0:t0 + nb], scalar=gain[:, h:h + 1], in1=rt[:, 0:nb],
                                                                                             op0=ALU.mult, op1=ALU.mult), [OB, gain, rt], [y])
                S.dma("sp", G["yBT"][h * 128:(h + 1) * 128, t0:t0 + nb], y[:, 0:nb], [y], [G["yBT"]])
    A.close()


def phase_merge(S, cfg, G, I, C, cfgL):
    A = Arena(S, "mg")
    D, T, HV, KD = cfg.D, cfg.T, cfg.HV, cfg.KD
    KH = HV // 128
    HL = cfgL.H
    NRK = cfg.H // HL
    DM = cfgL.DMG
    wa = A.sb("wa", [128, KH, DM], BF16)
    wb = A.sb("wb", [128, KH, DM], BF16)
    for k in range(KH):
        S.dma("pool", wa[:, k, :], I["w_branch_a"][0, k * 128:(k + 1) * 128, :], [I["w_branch_a"]], [wa])
        S.dma("pool", wb[:, k, :], I["w_branch_b"][0, k * 128:(k + 1) * 128, :], [I["w_branch_b"]], [wb])
    ya = [A.sb(f"ya{i}", [128, KH, 512], BF16) for i in range(2)]
    yb = [A.sb(f"yb{i}", [128, KH, 512], BF16) for i in range(2)]
    ga = [A.sb(f"ga{i}", [128, 512], BF16) for i in range(2)]
    gb = [A.sb(f"gb{i}", [128, 512], BF16) for i in range(2)]
    t1 = [A.sb(f"t1{i}", [128, 512], F32) for i in range(2)]
    t2 = [A.sb(f"t2{i}", [128, 512], F32) for i in range(2)]
    mo = [A.sb(f"mo{i}", [128, 512], BF16) for i in range(2)]
    pa = [A.ps(f"pa{i}", [128, 512], F32) for i in range(2)]
    pb = [A.ps(f"pb{i}", [128, 512], F32) for i in range(2)]
    it = 0
    for bi, t0 in enumerate(range(0, T, 512)):
        nb = min(512, T - t0)
        a_, b_ = ya[bi % 2], yb[bi % 2]
        CR = min(256, 2 * HL * 128)
        for r in range(NRK):
            for l in range(HL):
                for (dst, rho) in ((a_, l * 128), (b_, HL * 128 + l * 128)):
                    ck, off = divmod(rho, CR)
                    S.dma("sp", dst[:, r * HL + l, 0:nb], G["yall"][ck, r * CR + off:r * CR + off + 128, t0:t0 + nb], [G["yall"]], [dst])
        for dc in range(DM // 128):
            i2 = it % 2
            it += 1
            S.dma("sp", ga[i2][:, 0:nb], G["mgT"][dc * 128:(dc + 1) * 128, t0:t0 + nb], [G["mgT"]], [ga[i2]])
            S.dma("sp", gb[i2][:, 0:nb], G["mgT"][DM + dc * 128:DM + (dc + 1) * 128, t0:t0 + nb], [G["mgT"]], [gb[i2]])
            def mm(e, w, y, p, dc=dc, nb=nb):
                r = None
                for k in range(KH):
                    r = e.matmul(p[:, 0:nb], lhsT=w[:, k, dc * 128:(dc + 1) * 128], rhs=y[:, k, 0:nb], start=(k == 0), stop=(k == KH - 1))
                return r
            S.emit("pe", lambda e, i2=i2, a_=a_, mm=mm: mm(e, wa, a_, pa[i2]), [wa, a_], [pa[i2]])
            S.emit("pe", lambda e, i2=i2, b_=b_, mm=mm: mm(e, wb, b_, pb[i2]), [wb, b_], [pb[i2]])
            S.emit("dve", lambda e, i2=i2, nb=nb: e.tensor_tensor(out=t1[i2][:, 0:nb], in0=pa[i2][:, 0:nb], in1=ga[i2][:, 0:nb], op=ALU.mult), [pa[i2], ga[i2]], [t1[i2]])
            S.emit("dve", lambda e, i2=i2, nb=nb: e.tensor_tensor(out=t2[i2][:, 0:nb], in0=pb[i2][:, 0:nb], in1=gb[i2][:, 0:nb], op=ALU.mult), [pb[i2], gb[i2]], [t2[i2]])
            S.emit("pool", lambda e, i2=i2, nb=nb: e.tensor_tensor(out=mo[i2][:, 0:nb], in0=t1[i2][:, 0:nb], in1=t2[i2][:, 0:nb], op=ALU.add), [t1[i2], t2[i2]], [mo[i2]])
            S.dma("sp", G["mT"][dc * 128:(dc + 1) * 128, t0:t0 + nb], mo[i2][:, 0:nb], [mo[i2]], [G["mT"]])
    A.close()


def load_row_bcast(S, A, name, src_ap, srcbuf, n):
    t = A.sb(name, [128, n], F32)
    S.dma("sp", t[:], src_ap.partition_broadcast(128), [srcbuf], [t])
    return t


def phase_outproj(S, cfg, G, I, C, cfgL, nsplit):
    A = Arena(S, "op")
    D, T, KD = cfg.D, cfg.T, cfg.KD
    DM = cfgL.DMG
    CRM = min(256, DM)
    wo = A.sb("wo", [128, KD, D], BF16)
    for k in range(KD):
        S.dma("pool", wo[:, k, :], I["w_out"][0, k * 128:(k + 1) * 128, :], [I["w_out"]], [wo])
    g1 = load_row_bcast(S, A, "g1", G["grow"][0], G["grow"], D)
    mt = [A.sb(f"mt{i}", [128, KD, 128], BF16) for i in range(2)]
    xt = [A.sb(f"xt{i}", [128, D], F32) for i in range(2)]
    tm = [A.sb(f"tm{i}", [128, 512], F32) for i in range(2)]
    pp = [A.ps(f"pp{i}", [128, 512], F32) for i in range(4)]
    pi = 0
    for ti in range(T // 128):
        t0 = ti * 128
        m_, x_ = mt[ti % 2], xt[ti % 2]
        if nsplit == 1:
            S.dma("sp", m_[:], G["mT"][:, t0:t0 + 128].rearrange("(k p) t -> p k t", p=128), [G["mT"]], [m_])
        else:
            kpc = CRM // 128
            for r in range(nsplit):
                for ck in range(DM // CRM):
                    k0 = (r * DM + ck * CRM) // 128
                    S.dma("sp", m_[:, k0:k0 + kpc, :], G["mgath"][ck, r * CRM:(r + 1) * CRM, t0:t0 + 128].rearrange("(k p) t -> p k t", p=128),
                          [G["mgath"]], [m_])
        S.dma("pool", x_[:], I["x"][0, t0:t0 + 128, :], [I["x"]], [x_])
        for oc in range(0, D, 512):
            ow = min(512, D - oc)
            p = pp[pi % 4]
            t_ = tm[pi % 2]
            pi += 1
            def mm(e, m_=m_, p=p, oc=oc, ow=ow):
                r = None
                for k in range(KD):
                    r = e.matmul(p[:, 0:ow], lhsT=m_[:, k, :], rhs=wo[:, k, oc:oc + ow], start=(k == 0), stop=(k == KD - 1))
                return r
            S.emit("pe", mm, [m_, wo], [p])
            S.emit("dve", lambda e, p=p, t_=t_, oc=oc, ow=ow: e.tensor_tensor(out=t_[:, 0:ow], in0=p[:, 0:ow], in1=g1[:, oc:oc + ow], op=ALU.mult), [p, g1], [t_])
            S.emit("pool", lambda e, x_=x_, t_=t_, oc=oc, ow=ow: e.tensor_tensor(out=x_[:, oc:oc + ow], in0=x_[:, oc:oc + ow], in1=t_[:, 0:ow], op=ALU.add), [x_, t_], [x_])
        S.dma("sp", G["x2"][t0:t0 + 128, :], x_[:], [x_], [G["x2"]])
    A.close()


def phase_route(S, cfg, G, I, C, R_):
    A = Arena(S, "rt")
    D, T, KD, E, CAP = cfg.D, cfg.T, cfg.KD, cfg.E, cfg.CAP
    NSG = CAP // 128
    identf = C["identf"]
    co = G["coef"]
    for j, ci in enumerate((4, 5)):
        S.dma("sp", G["crow"][j].rearrange("(k p) -> p k", p=128), co[:, ci, :], [co], [G["crow"]], allow_slow_non_contiguous=True)
    a2 = load_row_bcast(S, A, "a2", G["crow"][0], G["crow"], D)
    b2 = load_row_bcast(S, A, "b2", G["crow"][1], G["crow"], D)
    rw = A.sb("rw", [128, KD, E], F32)
    S.dma("sp", rw[:], I["router_w"][0].rearrange("(k p) e -> p k e", p=128), [I["router_w"]], [rw])
    affE = [A.sb(f"affE{i}", [E, T], F32) for i in range(2)]
    xt = [A.sb(f"xt{i}", [128, D], F32) for i in range(2)]
    hf = [A.sb(f"hf{i}", [128, D], F32) for i in range(2)]
    hb = [A.sb(f"hb{i}", [128, D], BF16) for i in range(2)]
    hT = [A.sb(f"hT{i}", [128, KD, 128], F32) for i in range(2)]
    junk = A.sb("junk", [128, D], BF16)
    st = [A.sb(f"st{i}", [128, 8], F32) for i in range(2)]
    sm = [A.sb(f"sm{i}", [128, E + 8], F32) for i in range(2)]
    pt = [A.ps(f"pt{i}", [128, 4, 128], F32) for i in range(2)]
    pl = A.ps("pl", [128, 512], F32)
    pq = A.ps("pq", [128, 512], F32)
    gi = 0
    for ti in range(T // 128):
        t0 = ti * 128
        x = xt[ti % 2]; s = st[ti % 2]; h = hf[ti % 2]; hbb = hb[ti % 2]; hTt = hT[ti % 2]; m = sm[ti % 2]
        S.dma("sp" if ti % 2 == 0 else "pool", x[:], G["x2"][t0:t0 + 128, :], [G["x2"]], [x])
        S.emit("act", lambda e, x=x, s=s: e.activation(out=junk[:], in_=x[:], func=AF.Square, accum_out=s[:, 0:1]), [x], [junk, s])
        S.emit("dve", lambda e, s=s: e.tensor_scalar(out=s[:, 1:2], in0=s[:, 0:1], scalar1=1.0 / D, scalar2=cfg.eps, op0=ALU.mult, op1=ALU.add), [s], [s])
        S.emit("act", lambda e, s=s: e.activation(out=s[:, 2:3], in_=s[:, 1:2], func=AF.Sqrt), [s], [s])
        S.emit("dve", lambda e, s=s: e.reciprocal(out=s[:, 3:4], in_=s[:, 2:3]), [s], [s])
        S.emit("dve", lambda e, x=x, s=s, h=h: e.scalar_tensor_tensor(out=h[:], in0=x[:], scalar=s[:, 3:4], in1=a2[:], op0=ALU.mult, op1=ALU.mult), [x, s, a2], [h])
        S.emit("pool", lambda e, h=h: e.tensor_tensor(out=h[:], in0=h[:], in1=b2[:], op=ALU.add), [h, b2], [h])
        S.emit("act", lambda e, h=h, hbb=hbb: e.copy(out=hbb[:], in_=h[:]), [h], [hbb])
        S.dma("sp", G["h2"][t0:t0 + 128, :], hbb[:], [hbb], [G["h2"]])
        for g in range(0, KD, 4):
            p = pt[gi % 2]
            gi += 1
            ng = min(4, KD - g)
            def tr(e, h=h, p=p, g=g, ng=ng):
                r = None
                for j in range(ng):
                    r = e.transpose(out=p[:, j, :], in_=h[:, (g + j) * 128:(g + j + 1) * 128], identity=identf[:])
                return r
            S.emit("pe", tr, [h, identf], [p])
            if (gi % 2) == 0:
                S.emit("act", lambda e, p=p, hTt=hTt, g=g, ng=ng: e.copy(out=hTt[:, g:g + ng, :], in_=p[:, 0:ng, :]), [p, hTt], [hTt])
            else:
                S.emit("dve", lambda e, p=p, hTt=hTt, g=g, ng=ng: e.tensor_copy(out=hTt[:, g:g + ng, :], in_=p[:, 0:ng, :]), [p, hTt], [hTt])
        def ml(e, hTt=hTt):
            r = None
            for k in range(KD):
                r = e.matmul(pl[:, 0:E], lhsT=hTt[:, k, :], rhs=rw[:, k, :], start=(k == 0), stop=(k == KD - 1))
            return r
        S.emit("pe", ml, [hTt, rw], [pl])
        S.emit("dve", lambda e, m=m: e.reduce_max(out=m[:, E:E + 1], in_=pl[:, 0:E], axis=mybir.AxisListType.X), [pl], [m])
        S.emit("dve", lambda e, m=m: e.tensor_scalar(out=m[:, E + 1:E + 2], in0=m[:, E:E + 1], scalar1=-1.0, scalar2=None, op0=ALU.mult), [m], [m])
        S.emit("act", lambda e, m=m: e.activation(out=m[:, 0:E], in_=pl[:, 0:E], func=AF.Exp, bias=m[:, E + 1:E + 2], accum_out=m[:, E + 2:E + 3]), [pl, m], [m])
        S.emit("dve", lambda e, m=m: e.reciprocal(out=m[:, E + 3:E + 4], in_=m[:, E + 2:E + 3]), [m], [m])
        S.emit("dve", lambda e, m=m: e.tensor_scalar(out=m[:, 0:E], in0=m[:, 0:E], scalar1=m[:, E + 3:E + 4], scalar2=None, op0=ALU.mult), [m], [m])
        S.emit("pe", lambda e, m=m: e.transpose(out=pq[0:E, 0:128], in_=m[:, 0:E], identity=identf[:]), [m, identf], [pq])
        S.emit("act", lambda e, t0=t0: e.copy(out=affE[0][:, t0:t0 + 128], in_=pq[0:E, 0:128]), [pq, affE[0]], [affE[0]])
    vals = A.sb("vals", [E, CAP], F32)
    idxu = A.sb("idxu", [E, CAP], U32)
    idxf = A.sb("idxf", [E, CAP], F32)
    cur = 0
    for it in range(CAP // 8):
        a = affE[cur]; b = affE[1 - cur]
        S.emit("dve", lambda e, a=a, it=it: e.max(out=vals[:, it * 8:(it + 1) * 8], in_=a[:]), [a, vals], [vals])
        S.emit("dve", lambda e, a=a, it=it: e.max_index(out=idxu[:, it * 8:(it + 1) * 8], in_max=vals[:, it * 8:(it + 1) * 8], in_values=a[:]), [a, vals, idxu], [idxu])
        if it + 1 < CAP // 8:
            S.emit("dve", lambda e, a=a, b=b, it=it: e.match_replace(out=b[:], in_to_replace=vals[:, it * 8:(it + 1) * 8], in_values=a[:], imm_value=-1.0), [a, vals], [b])
            cur = 1 - cur
    S.emit("dve", lambda e: e.tensor_copy(out=idxf[:], in_=idxu[:]), [idxu], [idxf])
    idxT, gateT = R_["idxT"], R_["gateT"]
    for sg in range(NSG):
        S.emit("pe", lambda e, sg=sg: e.transpose(out=pq[:, 0:E], in_=idxf[:, sg * 128:(sg + 1) * 128], identity=identf[0:E, 0:E]), [idxf, identf], [pq])
        S.emit("dve", lambda e, sg=sg: e.tensor_copy(out=idxT[:, sg, :], in_=pq[:, 0:E]), [pq, idxT], [idxT])
        S.emit("pe", lambda e, sg=sg: e.transpose(out=pq[:, 0:E], in_=vals[:, sg * 128:(sg + 1) * 128], identity=identf[0:E, 0:E]), [vals, identf], [pq])
        S.emit("act", lambda e, sg=sg: e.copy(out=gateT[:, sg, :], in_=pq[:, 0:E]), [pq, gateT], [gateT])
    A.close()


def phase_experts(S, cfg, G, I, C, R_):
    A = Arena(S, "ex")
    D, T, KD, E, CAP, DE = cfg.D, cfg.T, cfg.KD, cfg.E, cfg.CAP, cfg.DE
    NSG = CAP // 128
    FH = min(getattr(cfg, "FH", 512), DE)
    NFH = DE // FH
    FC = FH // 128
    identb = C["identb"]
    idxT, gateT = R_["idxT"], R_["gateT"]
    g2 = load_row_bcast(S, A, "g2", G["grow"][1], G["grow"], D)
    wg = [A.sb(f"wg{i}", [128, KD, FH], BF16) for i in range(2)]
    wu = [A.sb(f"wu{i}", [128, KD, FH], BF16) for i in range(2)]
    wd = [A.sb(f"wd{i}", [128, FC, D], BF16) for i in range(2)]
    xs = A.sb("xs", [128, NSG, D], BF16)
    xsT = A.sb("xsT", [128, KD, CAP], BF16)
    sa = A.sb("sa", [128, 512], F32)
    sa2 = A.sb("sa2", [128, 512], F32)
    hd = A.sb("hd", [128, FC, CAP], BF16)
    ysb = A.sb("ysb", [128, NSG, D], F32)
    ptr = [A.ps(f"ptr{i}", [128, 8, 128], BF16) for i in range(2)]
    pg = [A.ps(f"pg{i}", [128, 512], F32) for i in range(2)]
    pu = [A.ps(f"pu{i}", [128, 512], F32) for i in range(2)]
    py = [A.ps(f"py{i}", [128, 512], F32) for i in range(2)]
    gi = 0
    yi = 0
    halves = [(ex, fh) for ex in range(E) for fh in range(NFH)]

    def load_weights(k):
        ex, fh = halves[k]
        w_g, w_u, w_d = wg[k % 2], wu[k % 2], wd[k % 2]
        f0 = fh * FH
        S.dma("pool", w_g[:], I["w_gate"][0, ex, :, f0:f0 + FH].rearrange("(k p) f -> p k f", p=128), [I["w_gate"]], [w_g])
        S.dma("pool", w_u[:], I["w_up"][0, ex, :, f0:f0 + FH].rearrange("(k p) f -> p k f", p=128), [I["w_up"]], [w_u])
        S.dma("pool", w_d[:], I["w_down"][0, ex, f0:f0 + FH, :].rearrange("(k p) d -> p k d", p=128), [I["w_down"]], [w_d])

    def gather(ex):
        for sg in range(NSG):
            S.emit("pool", lambda e, sg=sg, ex=ex: e.indirect_dma_start(out=xs[:, sg, :], out_offset=None, in_=G["h2"][:, :],
                                                                        in_offset=bass.IndirectOffsetOnAxis(ap=idxT[:, sg, ex:ex + 1], axis=0)),
                   [idxT, G["h2"]], [xs], dma=xs)

    load_weights(0)
    gather(0)
    for k, (ex, fh) in enumerate(halves):
        w_g, w_u, w_d = wg[k % 2], wu[k % 2], wd[k % 2]
        if k + 1 < len(halves):
            load_weights(k + 1)
        if fh == 0:
            ti = 0
            for sg in range(NSG):
                for g in range(0, KD, 8):
                    p = ptr[ti % 2]
                    ng = min(8, KD - g)
                    def tr(e, p=p, sg=sg, g=g, ng=ng):
                        r = None
                        for j in range(ng):
                            r = e.transpose(out=p[:, j, :], in_=xs[:, sg, (g + j) * 128:(g + j + 1) * 128], identity=identb[:])
                        return r
                    S.emit("pe", tr, [xs, identb], [p])
                    if ti % 2 == 0:
                        S.emit("act", lambda e, p=p, sg=sg, g=g, ng=ng: e.copy(out=xsT[:, g:g + ng, sg * 128:(sg + 1) * 128], in_=p[:, 0:ng, :]), [p, xsT], [xsT])
                    else:
                        S.emit("dve", lambda e, p=p, sg=sg, g=g, ng=ng: e.tensor_copy(out=xsT[:, g:g + ng, sg * 128:(sg + 1) * 128], in_=p[:, 0:ng, :]), [p, xsT], [xsT])
                    ti += 1
            if ex + 1 < E:
                gather(ex + 1)
        for fc in range(FC):
            p_g, p_u = pg[gi % 2], pu[gi % 2]
            gi += 1
            for (w_, p_) in ((w_g, p_g), (w_u, p_u)):
                def mm(e, w_=w_, p_=p_, fc=fc):
                    r = None
                    for kk in range(KD):
                        r = e.matmul(p_[:, 0:CAP], lhsT=w_[:, kk, fc * 128:(fc + 1) * 128], rhs=xsT[:, kk, :], start=(kk == 0), stop=(kk == KD - 1))
                    return r
                S.emit("pe", mm, [w_, xsT], [p_])
            S.emit("act", lambda e, p_g=p_g: e.activation(out=sa[:, 0:CAP], in_=p_g[:, 0:CAP], func=AF.Silu), [p_g], [sa])
            S.emit("dve", lambda e, p_u=p_u, fc=fc: e.tensor_tensor(out=hd[:, fc, :], in0=p_u[:, 0:CAP], in1=sa[:, 0:CAP], op=ALU.mult), [p_u, sa, hd], [hd])
        for sg in range(NSG):
            for oc in range(0, D, 512):
                ow = min(512, D - oc)
                p_y = py[yi % 2]
                yi += 1
                def my(e, p_y=p_y, sg=sg, oc=oc, ow=ow, w_d=w_d):
                    r = None
                    for fc in range(FC):
                        r = e.matmul(p_y[:, 0:ow], lhsT=hd[:, fc, sg * 128:(sg + 1) * 128], rhs=w_d[:, fc, oc:oc + ow], start=(fc == 0), stop=(fc == FC - 1))
                    return r
                S.emit("pe", my, [hd, w_d], [p_y])
                if fh == 0:
                    S.emit("dve", lambda e, p_y=p_y, sg=sg, oc=oc, ow=ow, ex=ex: e.scalar_tensor_tensor(
                        out=ysb[:, sg, oc:oc + ow], in0=p_y[:, 0:ow], scalar=gateT[:, sg, ex:ex + 1], in1=g2[:, oc:oc + ow], op0=ALU.mult, op1=ALU.mult),
                        [p_y, gateT, g2, ysb], [ysb])
                else:
                    S.emit("act", lambda e, p_y=p_y, sg=sg, ow=ow, ex=ex: e.activation(out=sa2[:, 0:ow], in_=p_y[:, 0:ow], func=AF.Copy, scale=gateT[:, sg, ex:ex + 1]),
                           [p_y, gateT], [sa2])
                    S.emit("dve", lambda e, oc=oc, ow=ow: e.tensor_tensor(out=sa2[:, 0:ow], in0=sa2[:, 0:ow], in1=g2[:, oc:oc + ow], op=ALU.mult), [sa2, g2], [sa2])
                    S.emit("dve", lambda e, sg=sg, oc=oc, ow=ow: e.tensor_tensor(out=ysb[:, sg, oc:oc + ow], in0=ysb[:, sg, oc:oc + ow], in1=sa2[:, 0:ow], op=ALU.add),
                           [ysb, sa2], [ysb])
        if fh == NFH - 1:
            for sg in range(NSG):
                S.emit("pool", lambda e, sg=sg, ex=ex: e.indirect_dma_start(out=G["x2"][:, :], out_offset=bass.IndirectOffsetOnAxis(ap=idxT[:, sg, ex:ex + 1], axis=0),
                                                                            in_=ysb[:, sg, :], in_offset=None, compute_op=ALU.add),
                       [idxT, ysb, G["x2s"]], [G["x2s"]], dma=ysb)
    A.close()


def phase_final(S, cfg, G, I, C, out):
    A = Arena(S, "fn")
    D, T = cfg.D, cfg.T
    fn = load_row_bcast(S, A, "fnw", I["final_norm"][:], I["final_norm"], D)
    xt = [A.sb(f"xt{i}", [128, D], F32) for i in range(2)]
    st = [A.sb(f"st{i}", [128, 8], F32) for i in range(2)]
    junk = A.sb("junk", [128, D], BF16)
    for ti in range(T // 128):
        t0 = ti * 128
        x = xt[ti % 2]; s = st[ti % 2]
        S.dma("sp" if ti % 2 == 0 else "pool", x[:], G["x2"][t0:t0 + 128, :], [G["x2"], G["x2s"]], [x])
        S.emit("act", lambda e, x=x, s=s: e.activation(out=junk[:], in_=x[:], func=AF.Square, accum_out=s[:, 0:1]), [x], [junk, s])
        S.emit("dve", lambda e, s=s: e.tensor_scalar(out=s[:, 1:2], in0=s[:, 0:1], scalar1=1.0 / D, scalar2=cfg.eps, op0=ALU.mult, op1=ALU.add), [s], [s])
        S.emit("act", lambda e, s=s: e.activation(out=s[:, 2:3], in_=s[:, 1:2], func=AF.Sqrt), [s], [s])
        S.emit("dve", lambda e, s=s: e.reciprocal(out=s[:, 3:4], in_=s[:, 2:3]), [s], [s])
        S.emit("dve", lambda e, x=x, s=s: e.scalar_tensor_tensor(out=x[:], in0=x[:], scalar=s[:, 3:4], in1=fn[:], op0=ALU.mult, op1=ALU.mult), [x, s, fn], [x])
        S.dma("sp", out[t0:t0 + 128, :], x[:], [x], [out])
    A.close()


def declare_io(S, cfg, debug=(), cfgL=None):
    cfgL = cfgL or cfg
    D, T, NCTX, E, DE, TT = cfg.D, cfg.T, cfg.NCTX, cfg.E, cfg.DE, cfg.TT
    HVF = cfg.HV
    HV, H = cfgL.HV, cfgL.H
    I = {}

    def inp(name, shape):
        I[name] = S.dram(name, shape, F32, kind="ExternalInput")
    inp("x", [1, T, D]); inp("c", [1, D]); inp("ctx", [1, NCTX, D]); inp("c_ctx", [D])
    NSP = cfg.H // cfgL.H
    inp("ada_w", [1, D, 6 * D // NSP]); inp("ada_b", [1, 6 * D // NSP]); inp("norm_mix", [1, D]); inp("norm_ffn", [1, D])
    inp("w_in", [1, D, cfgL.NIN]); inp("gdn_conv", [1, 5, 3 * HV]); inp("gdn_a_log", [1, 2, H]); inp("gdn_dt_bias", [1, 2, H])
    inp("hgrn_lb", [2, 2, HV]); inp("hgrn_norm", [1, HV]); inp("gdn_norm", [1, HV])
    inp("w_branch_a", [1, HVF, cfgL.DMG]); inp("w_branch_b", [1, HVF, cfgL.DMG]); inp("w_out", [1, D, D]); inp("router_w", [1, D, E])
    inp("w_gate", [1, E, D, DE]); inp("w_up", [1, E, D, DE]); inp("w_down", [1, E, DE, D]); inp("final_norm", [D])
    G = {}

    def scr(name, shape, dt):
        G[name] = S.dram(name, shape, dt, kind="ExternalOutput" if name in debug else "Internal")
    scr("grow", [2, D], F32)
    scr("modown", [128, 6 * cfg.KD * 2 // (cfg.H // cfgL.H)], F32); scr("modall", [128 * (cfg.H // cfgL.H), 6 * cfg.KD * 2 // (cfg.H // cfgL.H)], F32)
    scr("hqT", [HV, T], BF16); scr("hffT", [HV, TT], BF16); scr("hfbT", [HV, TT], BF16); scr("hi", [TT, HV], BF16)
    scr("hogT", [HV, T], BF16); scr("gzT", [HV, T], BF16); scr("mgT", [2 * cfgL.DMG, T], BF16)
    scr("gqkvT", [3 * HV, TT], BF16); scr("gab", [TT, 4 * H], F32)
    scr("ycat", [2 * HV, T], BF16)
    G["yAT"] = Buf("yAT", G["ycat"].t[0:HV, :], multi=True)
    G["yBT"] = Buf("yBT", G["ycat"].t[HV:2 * HV, :], multi=True)
    NR = cfg.H // cfgL.H
    CR = min(256, 2 * HV)
    scr("yall", [2 * HV // CR, NR * CR, T], BF16)
    scr("gq", [HV, TT], BF16); scr("gk", [HV, TT], BF16); scr("gv", [HV, TT], BF16)
    scr("mT", [cfgL.DMG, T], BF16)
    CRM = min(256, cfgL.DMG)
    scr("mgath", [cfgL.DMG // CRM, (cfg.H // cfgL.H) * CRM, T], BF16); scr("x2", [T, D], F32); scr("h2", [T, D], BF16); scr("crow", [2, D], F32)
    G["x2s"] = Buf("x2s")
    scr("dbg_mod", [128, 6 * cfg.KD * 2], F32)
    if "dbg_S" in debug:
        scr("dbg_S", [2 * min(4, H), 128, 128], F32)
    if "dbg_ob" in debug:
        scr("dbg_ob", [HV, T], F32)
    if "dbg_g" in debug:
        scr("dbg_g", [6, 128, TT // 128, 2 * H], F32)
    scr("dbg_coef", [128, 6 * cfg.KD], F32)
    out = S.dram("out", [T, D], F32, kind="ExternalOutput")
    return I, G, out


def local_cfg(cfg, nsplit):
    return Cfg(D=cfg.D, H=cfg.H // nsplit, T=cfg.T, GW=cfg.GW, NCTX=cfg.NCTX, E=cfg.E, DE=cfg.DE, CAP=cfg.CAP, eps=cfg.eps,
               DMG=cfg.D // nsplit)


def build_program(cfg=FULL, debug=(), phases=None, nsplit=2, groups=None):
    nc = bass.Bass("TRN2", target_bir_lowering=False)
    S = Sched(nc)
    cfgL = local_cfg(cfg, nsplit) if nsplit > 1 else cfg
    I, G, out = declare_io(S, cfg, debug, cfgL)
    P = Arena(S, "glob")
    C = make_consts(S, P, cfg)
    G["mod"] = P.sb("mod", [128, 6 * cfg.KD, 2], F32)
    G["coef"] = P.sb("coef", [128, 6, cfg.KD], F32)
    ph = phases or ("ada", "coef", "prr", "prc", "hgrn", "gconv", "gdn", "merge", "outproj", "route", "experts", "final")
    grp0 = groups or [[b + 4 * r for r in range(max(nsplit, 1))] for b in range(4)]
    if "ada" in ph:
        phase_ada(S, cfg, G, I, nsplit, grp0)
    if "coef" in ph:
        phase_coef(S, cfg, G, I)
    if "dbg_mod" in debug:
        S.dma("sp", G["dbg_mod"][:], G["mod"][:].rearrange("p n s -> p (n s)"), [G["mod"]], [G["dbg_mod"]])
        S.dma("sp", G["dbg_coef"][:], G["coef"][:].rearrange("p n s -> p (n s)"), [G["coef"]], [G["dbg_coef"]])
    if "prr" in ph:
        phase_proj(S, cfgL, G, I, C, "r")
    if "prc" in ph:
        phase_proj(S, cfgL, G, I, C, "c")
    grp = groups or [[b + 4 * r for r in range(max(nsplit, 1))] for b in range(4)]
    CR = min(256, 2 * cfgL.HV)
    NCK = 2 * cfgL.HV // CR

    def gather_y(cks, srcs):
        for ck in cks:
            S.emit_cc(lambda e, ck=ck: e.collective_compute("AllGather", ALU.bypass, replica_groups=grp, ins=[G["ycat"][ck * CR:(ck + 1) * CR, :]],
                                                            outs=[G["yall"][ck]]), srcs, [G["yall"]])
    if "hgrn" in ph:
        phase_hgrn(S, cfgL, G, I, C)
    if nsplit > 1 and NCK >= 2:
        gather_y(range(NCK // 2), [G["yAT"]])
    if "gconv" in ph:
        phase_gconv(S, cfgL, G, I, C)
    if "gdn" in ph:
        phase_gdn(S, cfgL, G, I, C)
    if nsplit > 1:
        gather_y(range(NCK // 2, NCK) if NCK >= 2 else range(NCK), [G["yAT"], G["yBT"]])
    if "merge" in ph:
        phase_merge(S, cfg, G, I, C, cfgL)
    if nsplit > 1:
        CRM = min(256, cfgL.DMG)
        for ck in range(cfgL.DMG // CRM):
            S.emit_cc(lambda e, ck=ck: e.collective_compute("AllGather", ALU.bypass, replica_groups=grp0, ins=[G["mT"][ck * CRM:(ck + 1) * CRM, :]],
                                                            outs=[G["mgath"][ck]]), [G["mT"]], [G["mgath"]])
    if "outproj" in ph:
        phase_outproj(S, cfg, G, I, C, cfgL, nsplit)
    R_ = {"idxT": P.sb("idxT", [128, cfg.CAP // 128, cfg.E], I32), "gateT": P.sb("gateT", [128, cfg.CAP // 128, cfg.E], F32)}
    if "route" in ph:
        phase_route(S, cfg, G, I, C, R_)
    if "experts" in ph:
        phase_experts(S, cfg, G, I, C, R_)
    if "final" in ph:
        phase_final(S, cfg, G, I, C, out)
    P.close()
    S.finish()
    build_program.last_sched = S
    return nc


_PER_BATCH = ("x", "c", "ctx")
_NC_CACHE = {}


def _head_cols(v, hf, nsplit, H, axis=-1):
    v = np.asarray(v)
    w = (H // nsplit) * 128
    sl = [slice(None)] * v.ndim
    sl[axis] = slice(hf * w, (hf + 1) * w)
    return v[tuple(sl)]


def shard_inputs(inputs, cfg, b, hf, nsplit):
    H, HV, D = cfg.H, cfg.HV, cfg.D
    HLc = H // nsplit
    m = {}
    for k, v in inputs.items():
        v = np.asarray(v)
        if k in _PER_BATCH:
            m[k] = np.ascontiguousarray(v[b:b + 1])
        else:
            m[k] = v
    if nsplit > 1:
        w = m["w_in"]
        parts = []
        for g in range(9):
            parts.append(_head_cols(w[:, :, g * HV:(g + 1) * HV], hf, nsplit, H))
        o = 9 * HV
        for g in range(4):
            parts.append(w[:, :, o + g * H + hf * HLc:o + g * H + (hf + 1) * HLc])
        DMc = D // nsplit
        g0 = o + 4 * H
        parts.append(w[:, :, g0 + hf * DMc:g0 + (hf + 1) * DMc])
        parts.append(w[:, :, g0 + D + hf * DMc:g0 + D + (hf + 1) * DMc])
        m["w_in"] = np.concatenate(parts, axis=2)
        m["hgrn_lb"] = _head_cols(m["hgrn_lb"], hf, nsplit, H)
        m["hgrn_norm"] = _head_cols(m["hgrn_norm"], hf, nsplit, H)
        m["gdn_norm"] = _head_cols(m["gdn_norm"], hf, nsplit, H)
        cw = m["gdn_conv"]
        m["gdn_conv"] = np.concatenate([_head_cols(cw[:, :, g * HV:(g + 1) * HV], hf, nsplit, H) for g in range(3)], axis=2)
        m["gdn_a_log"] = m["gdn_a_log"][:, :, hf * HLc:(hf + 1) * HLc]
        m["gdn_dt_bias"] = m["gdn_dt_bias"][:, :, hf * HLc:(hf + 1) * HLc]
        m["w_branch_a"] = m["w_branch_a"][:, :, hf * (D // nsplit):(hf + 1) * (D // nsplit)]
        m["w_branch_b"] = m["w_branch_b"][:, :, hf * (D // nsplit):(hf + 1) * (D // nsplit)]
        w6 = 6 * D // nsplit
        m["ada_w"] = m["ada_w"][:, :, hf * w6:(hf + 1) * w6]
        m["ada_b"] = m["ada_b"][:, hf * w6:(hf + 1) * w6]
    return {k: np.ascontiguousarray(v) for k, v in m.items()}


def kernel(**inputs):
    cfg = FULL
    n = 8
    nsplit = 2
    if "nc" not in _NC_CACHE:
        _NC_CACHE["nc"] = build_program(cfg, nsplit=nsplit)
    nc = _NC_CACHE["nc"]
    B = inputs["x"].shape[0]
    in_maps = [shard_inputs(inputs, cfg, core % B, core // B, nsplit) for core in range(n)]
    res = run_bass_kernel_spmd(nc, in_maps, core_ids=list(range(n)))
    out = np.stack([np.asarray(res.results[b]["out"]) for b in range(B)], axis=0)
    return out.astype(np.float32)
```

```python
import contextlib
import numpy as np
import concourse.bass as bass
import concourse.mybir as mybir
from concourse.bass_utils import run_bass_kernel_spmd

F32 = mybir.dt.float32
BF16 = mybir.dt.bfloat16
I32 = mybir.dt.int32
U32 = mybir.dt.uint32
AF = mybir.ActivationFunctionType
ALU = mybir.AluOpType

ENGS = ("pe", "act", "dve", "pool", "sp")
NEG = -30000.0


class Buf:
    __slots__ = ("name", "ws", "rs", "t", "multi", "dgroup", "excl")

    def __init__(self, name, t=None, multi=False, dgroup=None, excl=False):
        self.excl = excl
        self.name = name
        self.ws = {}
        self.rs = {}
        self.t = t
        self.multi = multi
        self.dgroup = dgroup or name

    def __getitem__(self, idx):
        return self.t[idx]


class Sched:
    def __init__(self, nc):
        self.nc = nc
        self.ops = {e: [] for e in ENGS}
        self.val = {}
        self.seen = {e: {} for e in ENGS}
        self.uid = 0
        self.dmap = {}
        self.dfree = {"sw": [], "hw": []}
        self.nd = 0

    def dkey(self, dgroup, kind):
        k = (dgroup, kind)
        if k not in self.dmap:
            if self.dfree[kind]:
                self.dmap[k] = self.dfree[kind].pop()
            else:
                self.dmap[k] = f"ds{self.nd}_{kind}"
                self.nd += 1
        return self.dmap[k]

    def release(self, dgroups):
        for k in list(self.dmap):
            if k[0] in dgroups:
                self.dfree[k[1]].append(self.dmap.pop(k))

    def dram(self, name, shape, dt, kind="Internal"):
        return Buf(name, self.nc.dram_tensor(name, list(shape), dt, kind=kind).ap(), multi=True)

    def emit(self, eng, fn, reads=(), writes=(), dma=None):
        deps = {}

        def add(k, v):
            if deps.get(k, 0) < v:
                deps[k] = v
        for b in reads:
            for k, v in b.ws.items():
                add(k, v)
            if b.excl:
                for k, v in b.rs.items():
                    if k != "c_" + eng:
                        add(k, v)
        for b in writes:
            if b.multi:
                continue
            for k, v in b.ws.items():
                add(k, v)
            for k, v in b.rs.items():
                add(k, v)
        seen = self.seen[eng]
        waits = []
        for k, v in deps.items():
            if seen.get(k, 0) < v:
                seen[k] = v
                waits.append((k, v))
        sk = self.dkey(dma.dgroup, "sw" if eng == "pool" else "hw") if dma is not None else ("c_" + eng)
        inc = 16 if dma is not None else 1
        self.val[sk] = self.val.get(sk, 0) + inc
        tv = self.val[sk]
        for b in reads:
            if b.rs.get(sk, 0) < tv:
                b.rs[sk] = tv
        for b in writes:
            if b.multi:
                b.ws[sk] = tv
            else:
                b.ws = {sk: tv}
                b.rs = {}
        self.ops[eng].append((waits, fn, sk, inc))

    def dma(self, eng, out, in_, reads, writes, sem=None, **kw):
        if sem is None:
            sem = [b for b in list(writes) + list(reads) if not b.multi][0]
        self.emit(eng, lambda e: e.dma_start(out=out, in_=in_, **kw), reads, writes, dma=sem)

    def emit_cc(self, fn, reads, writes):
        deps = {}
        for b in reads:
            for k, v in b.ws.items():
                if deps.get(k, 0) < v:
                    deps[k] = v
        seen = self.seen["pool"]
        waits = []
        for k, v in deps.items():
            if seen.get(k, 0) < v:
                seen[k] = v
                waits.append((k, v))
        sk = "cc_sem"
        self.val[sk] = self.val.get(sk, 0) + 1
        for b in writes:
            b.ws[sk] = self.val[sk]
        self.ops["pool"].append((waits, fn, sk, 1))

    def barrier(self):
        cur = dict(self.val)
        for eng in ENGS:
            seen = self.seen[eng]
            waits = []
            for k, v in cur.items():
                if seen.get(k, 0) < v:
                    seen[k] = v
                    waits.append((k, v))
            if waits:
                self.ops[eng].append((waits, None, None, 0))

    def finish(self):
        nc = self.nc
        sems = {k: nc.alloc_semaphore(k) for k in self.val}
        engobj = {"pe": "tensor", "act": "scalar", "dve": "vector", "pool": "gpsimd", "sp": "sync"}
        final = list(self.val.items())
        with nc.Block() as block:
            for eng in ENGS:
                ops = self.ops[eng]
                is_final = eng == "sp"

                def body(e, ops=ops, is_final=is_final):
                    for waits, fn, sk, inc in ops:
                        for k, v in waits:
                            e.wait_ge(sems[k], v)
                        if fn is not None:
                            if sk == "cc_sem":
                                fn(e).then_inc(sems[sk])
                            else:
                                fn(e).then_inc(sems[sk], inc)
                    if is_final:
                        for k, v in final:
                            e.wait_ge(sems[k], v)
                getattr(block, engobj[eng])(body)
        return nc


class Arena:
    def __init__(self, S, tag):
        self.S = S
        self.tag = tag
        self.st = contextlib.ExitStack()
        self.groups = set()

    def sb(self, name, shape, dt, multi=False, dgroup=None):
        nm = f"{self.tag}_{name}"
        t = self.st.enter_context(self.S.nc.sbuf_tensor(nm, list(shape), dt))
        b = Buf(nm, t, multi=multi, dgroup=dgroup and f"{self.tag}_{dgroup}")
        self.groups.add(b.dgroup)
        return b

    def ps(self, name, shape, dt=F32):
        nm = f"{self.tag}_{name}"
        t = self.st.enter_context(self.S.nc.psum_tensor(nm, list(shape), dt))
        return Buf(nm, t, excl=True)

    def close(self):
        self.S.barrier()
        self.S.release(self.groups)
        self.st.close()


class Cfg:
    def __init__(self, D=2048, H=8, T=4096, GW=64, NCTX=256, E=16, DE=1024, CAP=512, eps=1e-6, DMG=None):
        self.D, self.H, self.T, self.GW, self.NCTX = D, H, T, GW, NCTX
        self.DMG = DMG or D
        self.E, self.DE, self.CAP, self.eps = E, DE, CAP, eps
        self.R = T // GW
        self.HV = H * 128
        self.KD = D // 128
        self.TT = T + NCTX
        HV = self.HV
        o = 0
        self.c_hq = o; o += HV
        self.c_hff = o; o += HV
        self.c_hfb = o; o += HV
        self.c_hi = o; o += HV
        self.c_hog = o; o += HV
        self.c_gqkv = o; o += 3 * HV
        self.c_gz = o; o += HV
        self.c_ga = o; o += 2 * H
        self.c_gb = o; o += 2 * H
        self.c_mg = o; o += 2 * self.DMG
        self.NIN = o


FULL = Cfg()


def token_blocks(cfg, lo, hi, nb=512):
    out = []
    for a, b in ((0, cfg.NCTX), (cfg.NCTX, cfg.TT)):
        a, b = max(a, lo), min(b, hi)
        t = a
        while t < b:
            n = min(nb, b - t)
            out.append((t, n))
            t += n
    return out


def make_consts(S, A, cfg):
    C = {}
    nc = S.nc
    identb = A.sb("identb", [128, 128], BF16)
    identf = A.sb("identf", [128, 128], F32)
    for ident in (identb, identf):
        S.emit("pool", lambda e, ident=ident: e.memset(ident[:], 0.0), writes=[ident])
        S.emit("pool", lambda e, ident=ident: e.affine_select(
            out=ident[:], in_=ident[:], pattern=[[-1, 128]], compare_op=ALU.not_equal, fill=1.0,
            base=0, channel_multiplier=1), reads=[ident], writes=[ident])
    C["identb"], C["identf"] = identb, identf
    onesb = A.sb("onesb", [128, 128], BF16)
    S.emit("pool", lambda e: e.memset(onesb[:], 1.0), writes=[onesb])
    C["onesb"] = onesb
    onesf = A.sb("onesf", [128, 128], F32)
    S.emit("pool", lambda e: e.memset(onesf[:], 1.0), writes=[onesf])
    C["onesf"] = onesf
    epsc = A.sb("epsc", [128, 1], F32)
    S.emit("pool", lambda e: e.memset(epsc[:], cfg.eps), writes=[epsc])
    C["epsc"] = epsc
    onec = A.sb("onec", [128, 1], F32)
    S.emit("pool", lambda e: e.memset(onec[:], 1.0), writes=[onec])
    C["onec"] = onec
    return C


def phase_ada(S, cfg, G, I, nsplit=1, grp=None):
    D, KD = cfg.D, cfg.KD
    A = Arena(S, "ada")
    NCB = 6 * KD // nsplit
    SW = 512 if (NCB * 128) % 512 == 0 else 256
    cT = A.sb("cT", [128, KD, 2], F32)
    s2 = A.sb("s2", [128, KD, 2], F32)
    bT = A.sb("bT", [128, NCB], F32)
    S.dma("sp", cT[:, :, 0], I["c"][0].rearrange("(k p) -> p k", p=128), [I["c"]], [cT], allow_slow_non_contiguous=True)
    S.dma("sp", cT[:, :, 1], I["c_ctx"][:].rearrange("(k p) -> p k", p=128), [I["c_ctx"]], [cT], allow_slow_non_contiguous=True)
    S.dma("sp", bT[:], I["ada_b"][0].rearrange("(k p) -> p k", p=128), [I["ada_b"]], [bT], allow_slow_non_contiguous=True)
    S.emit("act", lambda e: e.activation(out=s2[:], in_=cT[:], func=AF.Silu), [cT], [s2])
    wsl = [A.sb(f"w{i}", [128, KD, SW], F32) for i in range(2)]
    pa = A.ps("pa", [128, NCB, 2], F32)
    mod = G["mod"]
    wv = I["ada_w"][0].rearrange("(k p) n -> p k n", p=128)
    nslab = NCB * 128 // SW
    for s in range(nslab):
        w = wsl[s % 2]
        S.dma("sp" if s % 2 == 0 else "pool", w[:], wv[:, :, s * SW:(s + 1) * SW], [I["ada_w"]], [w])
        for j in range(SW // 128):
            cb = s * (SW // 128) + j

            def mm(e, w=w, j=j, cb=cb):
                r = None
                for k in range(KD):
                    r = e.matmul(pa[:, cb, :], lhsT=w[:, k, j * 128:(j + 1) * 128], rhs=s2[:, k, :],
                                 start=(k == 0), stop=(k == KD - 1))
                return r
            S.emit("pe", mm, [w, s2], [pa])
    bb = bT[:].rearrange("p (n o) -> p n o", o=1).to_broadcast([128, NCB, 2])
    if nsplit == 1:
        S.emit("dve", lambda e: e.tensor_tensor(out=mod[:], in0=pa[:], in1=bb, op=ALU.add), [pa, bT], [mod])
    else:
        ml_ = A.sb("ml", [128, NCB, 2], F32)
        S.emit("dve", lambda e: e.tensor_tensor(out=ml_[:], in0=pa[:], in1=bb, op=ALU.add), [pa, bT], [ml_])
        S.dma("sp", G["modown"][:, :], ml_[:].rearrange("p n s -> p (n s)"), [ml_], [G["modown"]])
        S.emit_cc(lambda e: e.collective_compute("AllGather", ALU.bypass, replica_groups=grp, ins=[G["modown"][:, :]], outs=[G["modall"][:, :]]),
                  [G["modown"]], [G["modall"]])
        for r in range(nsplit):
            S.dma("sp", mod[:, r * NCB:(r + 1) * NCB, :].rearrange("p n s -> p (n s)"), G["modall"][r * 128:(r + 1) * 128, :], [G["modall"]], [mod])
    A.close()


def phase_coef(S, cfg, G, I):
    KD, D = cfg.KD, cfg.D
    A = Arena(S, "coef")
    mod = G["mod"]
    nm = A.sb("nm", [128, KD], F32)
    nf = A.sb("nf", [128, KD], F32)
    S.dma("sp", nm[:], I["norm_mix"][0].rearrange("(k p) -> p k", p=128), [I["norm_mix"]], [nm], allow_slow_non_contiguous=True)
    S.dma("sp", nf[:], I["norm_ffn"][0].rearrange("(k p) -> p k", p=128), [I["norm_ffn"]], [nf], allow_slow_non_contiguous=True)
    co = G["coef"]
    def mk(dst, gain, sc_j, s):
        S.emit("dve", lambda e: e.scalar_tensor_tensor(out=co[:, dst, :], in0=mod[:, sc_j * KD:(sc_j + 1) * KD, s], scalar=1.0,
                                                       in1=gain[:], op0=ALU.add, op1=ALU.mult), [mod, gain, co], [co])
    mk(0, nm, 1, 0)
    S.emit("dve", lambda e: e.tensor_copy(out=co[:, 1, :], in_=mod[:, 0:KD, 0]), [mod, co], [co])
    mk(2, nm, 1, 1)
    S.emit("dve", lambda e: e.tensor_copy(out=co[:, 3, :], in_=mod[:, 0:KD, 1]), [mod, co], [co])
    mk(4, nf, 4, 0)
    S.emit("dve", lambda e: e.tensor_copy(out=co[:, 5, :], in_=mod[:, 3 * KD:4 * KD, 0]), [mod, co], [co])
    gt = A.sb("gt", [128, 2, KD], F32)
    S.emit("dve", lambda e: e.tensor_copy(out=gt[:, 0, :], in_=mod[:, 2 * KD:3 * KD, 0]), [mod], [gt])
    S.emit("dve", lambda e: e.tensor_copy(out=gt[:, 1, :], in_=mod[:, 5 * KD:6 * KD, 0]), [mod, gt], [gt])
    for j in range(2):
        S.dma("sp", G["grow"][j].rearrange("(k p) -> p k", p=128), gt[:, j, :], [gt], [G["grow"]], allow_slow_non_contiguous=True)
    A.close()


def norm_tiles(S, A, cfg, C, tiles, hT, coefbuf, f32T=None):
    D, KD = cfg.D, cfg.KD
    xt = [A.sb(f"nx{i}", [128, D], F32) for i in range(2)]
    xn = [A.sb(f"nxn{i}", [128, D], BF16 if f32T is None else F32) for i in range(2)]
    junk = A.sb("njunk", [128, D], BF16)
    st = [A.sb(f"nst{i}", [128, 8], F32) for i in range(2)]
    tdt = BF16 if f32T is None else F32
    per = 8 if f32T is None else 4
    ptss = [[A.ps(f"npt{j}{i}", [128, per, 128], tdt) for i in range(2)] for j in range(2)]
    junks = [junk, A.sb("njunk1", [128, D], BF16)]
    ident = C["identb"] if f32T is None else C["identf"]
    co = coefbuf
    ng = KD // per if KD >= per else 1
    per = min(per, KD)

    def tile_gen(ti, pieces, srcbufs, col0, ci):
        par = ti % 2
        x = xt[par]; s = st[par]; n = xn[par]; jk = junks[par]; pts = ptss[par]
        for k, (ap, p0, npp) in enumerate(pieces):
            S.dma("sp" if par == 0 else "pool", x[p0:p0 + npp, :], ap, srcbufs, [x])
        yield
        S.emit("act", lambda e: e.activation(out=jk[:], in_=x[:], func=AF.Square, accum_out=s[:, 0:1]), [x], [jk, s])
        yield
        S.emit("dve", lambda e: e.tensor_scalar(out=s[:, 1:2], in0=s[:, 0:1], scalar1=1.0 / D, scalar2=cfg.eps,
                                                op0=ALU.mult, op1=ALU.add), [s], [s])
        yield
        S.emit("act", lambda e: e.activation(out=s[:, 2:3], in_=s[:, 1:2], func=AF.Sqrt), [s], [s])
        yield
        S.emit("dve", lambda e: e.reciprocal(out=s[:, 3:4], in_=s[:, 2:3]), [s], [s])
        yield
        S.emit("dve", lambda e: e.tensor_scalar(out=n[:], in0=x[:], scalar1=s[:, 3:4], scalar2=None, op0=ALU.mult), [x, s], [n])
        yield
        for g in range(ng):
            pt = pts[g % 2]

            def tr(e, pt=pt, g=g):
                r = None
                for j in range(per):
                    c = g * per + j
                    r = e.transpose(out=pt[:, j, :], in_=n[:, c * 128:(c + 1) * 128], identity=ident[:])
                return r
            S.emit("pe", tr, [n, ident], [pt])
            yield
            eng = "act" if (g + par) % 2 == 0 else "dve"
            for j in range(per):
                c = g * per + j
                outs = [(hT, hT[:, c, col0:col0 + 128])]
                if f32T is not None:
                    outs.append((f32T, f32T[:, c, col0:col0 + 128]))
                for oi, (ob, oap) in enumerate(outs):
                    eng2 = eng if oi == 0 else ("dve" if eng == "act" else "act")
                    if eng2 == "act":
                        S.emit("act", lambda e, pt=pt, j=j, c=c, oap=oap: e.activation(
                            out=oap, in_=pt[:, j, :], func=AF.Identity, scale=co[:, ci, c:c + 1], bias=co[:, ci + 1, c:c + 1]),
                            [pt, co], [ob])
                    else:
                        S.emit("dve", lambda e, pt=pt, j=j, c=c, oap=oap: e.tensor_scalar(
                            out=oap, in0=pt[:, j, :], scalar1=co[:, ci, c:c + 1], scalar2=co[:, ci + 1, c:c + 1],
                            op0=ALU.mult, op1=ALU.add), [pt, co], [ob])
                if j % 2 == 1:
                    yield
            yield

    for tj in range(0, len(tiles), 2):
        gens = [tile_gen(tj, *tiles[tj])]
        if tj + 1 < len(tiles):
            gens.append(tile_gen(tj + 1, *tiles[tj + 1]))
        while gens:
            for g_ in list(gens):
                try:
                    next(g_)
                except StopIteration:
                    gens.remove(g_)


def lat_tile_pieces(cfg, xin, ti, order):
    if order == "r":
        return [(xin[0, ti * 128:(ti + 1) * 128, :], 0, 128)]
    R, GW = cfg.R, cfg.GW
    xv = xin[0].rearrange("(r c) d -> c r d", c=GW)
    out = []
    if R >= 128:
        per = R // 128
        cc, sub = divmod(ti, per)
        out.append((xv[cc, sub * 128:(sub + 1) * 128, :], 0, 128))
    else:
        ncol = 128 // R
        for k in range(ncol):
            out.append((xv[ti * ncol + k, :, :], k * R, R))
    return out


def act_tiles(cfg, I, order):
    tiles = []
    for t in range(cfg.NCTX // 128):
        tiles.append(([(I["ctx"][0, t * 128:(t + 1) * 128, :], 0, 128)], [I["ctx"]], t * 128, 2))
    for t in range(cfg.T // 128):
        tiles.append((lat_tile_pieces(cfg, I["x"], t, order), [I["x"]], cfg.NCTX + t * 128, 0))
    return tiles


def project(S, A, cfg, hT, w_in, groups, tok_lo=0):
    KD, TT = cfg.KD, cfg.TT
    SW = 256
    wsl = [A.sb(f"pw{i}", [128, KD, SW], BF16) for i in range(2)]
    pps = [A.ps(f"pp{i}", [128, 512], F32) for i in range(4)]
    stg = {}
    wv = w_in[0].rearrange("(k p) n -> p k n", p=128)
    si = 0
    pi = 0
    gi = 0
    for g in groups:
        col0, ncols, lay, func, dst, dt = g["col0"], g["ncols"], g["layout"], g["func"], g["dst"], g["dt"]
        tlo = g.get("tok_lo", 0)
        key = (lay, dt)
        if key not in stg:
            stg[key] = [A.sb(f"ps{lay}{len(stg)}_{i}", [128, 512], dt) for i in range(3)]
        stgs = stg[key]
        if lay == "F":
            blocks = token_blocks(cfg, tlo, TT)
            for s0 in range(0, ncols, SW):
                sw = min(SW, ncols - s0)
                w = wsl[si % 2]
                S.dma("pool", w[:, :, 0:sw], wv[:, :, col0 + s0:col0 + s0 + sw], [w_in], [w])
                si += 1
                for j in range(0, sw, 128):
                    for (t0, nb) in blocks:
                        pp = pps[pi % 4]
                        pi += 1

                        def mm(e, w=w, j=j, t0=t0, nb=nb, pp=pp):
                            r = None
                            for k in range(KD):
                                r = e.matmul(pp[:, 0:nb], lhsT=w[:, k, j:j + 128], rhs=hT[:, k, t0:t0 + nb],
                                             start=(k == 0), stop=(k == KD - 1))
                            return r
                        S.emit("pe", mm, [w, hT], [pp])
                        sg = stgs[gi % 3]
                        gi += 1
                        if func is None:
                            S.emit("dve", lambda e, sg=sg, pp=pp, nb=nb: e.tensor_copy(out=sg[:, 0:nb], in_=pp[:, 0:nb]), [pp], [sg])
                        else:
                            S.emit("act", lambda e, sg=sg, pp=pp, nb=nb, func=func: e.activation(out=sg[:, 0:nb], in_=pp[:, 0:nb], func=func),
                                   [pp], [sg])
                        r0 = s0 + j
                        S.dma("sp", dst[r0:r0 + 128, t0 - tlo:t0 - tlo + nb], sg[:, 0:nb], [sg], [dst])
        else:
            assert ncols <= 512 or ncols % 256 == 0
            for s0 in range(0, ncols, SW):
                sw = min(SW, ncols - s0)
                w = wsl[si % 2]
                S.dma("pool", w[:, :, 0:sw], wv[:, :, col0 + s0:col0 + s0 + sw], [w_in], [w])
                si += 1
                for t0 in range(tlo, TT, 128):
                    pp = pps[pi % 4]
                    pi += 1

                    def mm(e, w=w, sw=sw, t0=t0, pp=pp):
                        r = None
                        for k in range(KD):
                            r = e.matmul(pp[:, 0:sw], lhsT=hT[:, k, t0:t0 + 128], rhs=w[:, k, 0:sw],
                                         start=(k == 0), stop=(k == KD - 1))
                        return r
                    S.emit("pe", mm, [w, hT], [pp])
                    sg = stgs[gi % 3]
                    gi += 1
                    S.emit("dve", lambda e, sg=sg, pp=pp, sw=sw: e.tensor_copy(out=sg[:, 0:sw], in_=pp[:, 0:sw]), [pp], [sg])
                    S.dma("sp", dst[t0 - tlo:t0 - tlo + 128, s0:s0 + sw], sg[:, 0:sw], [sg], [dst])


def phase_proj(S, cfg, G, I, C, order):
    A = Arena(S, "pr" + order)
    KD, TT, HV, NCTX, H, D = cfg.KD, cfg.TT, cfg.HV, cfg.NCTX, cfg.H, cfg.D
    hT = A.sb("hT", [128, KD, TT], BF16, multi=True)
    norm_tiles(S, A, cfg, C, act_tiles(cfg, I, order), hT, G["coef"])
    if order == "r":
        groups = [
            dict(col0=cfg.c_hq, ncols=HV, layout="F", func=AF.Silu, dst=G["hqT"], dt=BF16, tok_lo=NCTX),
            dict(col0=cfg.c_hff, ncols=HV, layout="F", func=None, dst=G["hffT"], dt=BF16),
            dict(col0=cfg.c_hfb, ncols=HV, layout="F", func=None, dst=G["hfbT"], dt=BF16),
            dict(col0=cfg.c_hi, ncols=HV, layout="T", func=None, dst=G["hi"], dt=BF16),
            dict(col0=cfg.c_hog, ncols=HV, layout="F", func=AF.Sigmoid, dst=G["hogT"], dt=BF16, tok_lo=NCTX),
            dict(col0=cfg.c_gz, ncols=HV, layout="F", func=AF.Silu, dst=G["gzT"], dt=BF16, tok_lo=NCTX),
            dict(col0=cfg.c_mg, ncols=2 * cfg.DMG, layout="F", func=AF.Sigmoid, dst=G["mgT"], dt=BF16, tok_lo=NCTX),
        ]
    else:
        groups = [
            dict(col0=cfg.c_gqkv, ncols=3 * HV, layout="F", func=None, dst=G["gqkvT"], dt=BF16),
            dict(col0=cfg.c_ga, ncols=4 * H, layout="T", func=None, dst=G["gab"], dt=F32),
        ]
    import os
    sel = os.environ.get("K_GROUPS")
    if sel is not None:
        groups = [groups[int(i)] for i in sel.split(",") if i != ""]
    project(S, A, cfg, hT, I["w_in"], groups)
    A.close()


def phase_hgrn(S, cfg, G, I, C):
    A = Arena(S, "hg")
    H, T, TT, NCTX, HV = cfg.H, cfg.T, cfg.TT, cfg.NCTX, cfg.HV
    CH = 64
    NCH = TT // CH
    NCC = NCTX // CH
    order = [list(range(NCH)), list(range(NCC - 1, -1, -1)) + list(range(NCH - 1, NCC - 1, -1))]
    SEG = 1024
    segs = []
    for a, b in ((0, NCTX), (NCTX, TT)):
        t = a
        while t < b:
            n = min(SEG, b - t)
            segs.append((t, n))
            t += n
    lbT = A.sb("lbT", [128, 2, 2, H], F32)
    for d in range(2):
        for sl in range(2):
            S.dma("sp", lbT[:, d, sl, :], I["hgrn_lb"][d, sl].rearrange("(h p) -> p h", p=128), [I["hgrn_lb"]], [lbT],
                  allow_slow_non_contiguous=True)
    low = A.sb("low", [128, 2, H], F32)
    oml = A.sb("oml", [128, 2, H], F32)
    noml = A.sb("noml", [128, 2, H], F32)
    S.emit("dve", lambda e: e.tensor_tensor(out=low[:], in0=lbT[:, :, 0, :], in1=lbT[:, :, 1, :], op=ALU.subtract), [lbT], [low])
    S.emit("act", lambda e: e.activation(out=low[:], in_=low[:], func=AF.Sigmoid), [low], [low])
    S.emit("dve", lambda e: e.tensor_scalar(out=oml[:], in0=low[:], scalar1=-1.0, scalar2=1.0, op0=ALU.mult, op1=ALU.add), [low], [oml])
    S.emit("dve", lambda e: e.tensor_scalar(out=noml[:], in0=low[:], scalar1=1.0, scalar2=-1.0, op0=ALU.mult, op1=ALU.add), [low], [noml])
    gain = A.sb("gain", [128, H], F32)
    S.dma("sp", gain[:], I["hgrn_norm"][0].rearrange("(h p) -> p h", p=128), [I["hgrn_norm"]], [gain], allow_slow_non_contiguous=True)
    msk01 = A.sb("msk01", [128, SEG], F32)
    S.emit("pool", lambda e: e.memset(msk01[:], 1.0), writes=[msk01])
    S.emit("pool", lambda e: e.memset(msk01[:].rearrange("p (n c) -> p n c", c=CH)[:, :, 0:1], 0.0), [msk01], [msk01])
    cm = []
    for d in range(2):
        m = A.sb(f"cm{d}", [CH, CH], F32)
        S.emit("pool", lambda e, m=m: e.memset(m[:], 1.0), writes=[m])
        sg = 1 if d == 0 else -1
        S.emit("pool", lambda e, m=m, sg=sg: e.affine_select(out=m[:], in_=m[:], pattern=[[sg, CH]], compare_op=ALU.is_ge, fill=0.0,
                                                             base=0, channel_multiplier=-sg), [m], [m])
        cm.append(m)
    identb, onesb = C["identb"], C["onesb"]
    qT2 = [A.sb(f"qT{i}", [128, T], BF16) for i in range(2)]
    fl2 = [[A.sb(f"fl{d}{i}", [128, TT], BF16) for d in range(2)] for i in range(2)]
    vv2 = [A.sb(f"vv{i}", [CH, NCH, 128], BF16) for i in range(2)]
    og2 = [A.sb("og0", [128, T], BF16)] * 2
    qt = [A.sb(f"qt{d}", [128, T], BF16) for d in range(2)]
    kt = [A.sb(f"kt{d}", [128, TT], BF16) for d in range(2)]
    kh = [A.sb(f"kh{d}", [128, TT], BF16) for d in range(2)]
    egt = [A.sb(f"egt{d}", [128, NCH], F32) for d in range(2)]
    tsets = [[A.sb(f"t{n}{i}", [128, SEG], F32) for n in "ABCD"] for i in range(2)]
    OA = A.sb("OA", [128, T], F32)
    Sf = [[A.sb(f"S{d}{i}", [128, 128], F32) for i in range(2)] for d in range(2)]
    Sb = [[A.sb(f"Sb{d}{i}", [128, 128], BF16) for i in range(3)] for d in range(2)]
    sTs = [[A.sb(f"sTs{d}{i}", [CH, CH], BF16) for i in range(3)] for d in range(2)]
    khs = [[A.sb(f"khs{d}{i}", [CH, 128], BF16) for i in range(3)] for d in range(2)]
    sqb = A.sb("sqb", [128, 512], BF16)
    rt = A.sb("rt", [128, 512], F32)
    ys = [A.sb("ys0", [128, 512], BF16)] * 2
    pst = [A.ps(f"pst{d}", [CH, 512], F32) for d in range(2)]
    ptr = [A.ps(f"ptr{d}", [CH, 1024], BF16) for d in range(2)]
    po = [A.ps(f"po{d}", [128, 512], F32) for d in range(2)]
    pd = [A.ps(f"pd{d}", [128, 512], F32) for d in range(2)]

    def load_head(h):
        r0 = h * 128
        qT, fl, vv, og = qT2[h % 2], fl2[h % 2], vv2[h % 2], og2[h % 2]
        S.dma("sp", qT[:], G["hqT"][r0:r0 + 128, :], [G["hqT"]], [qT])
        S.dma("sp", fl[0][:], G["hffT"][r0:r0 + 128, :], [G["hffT"]], [fl[0]])
        S.dma("sp", fl[1][:], G["hfbT"][r0:r0 + 128, :], [G["hfbT"]], [fl[1]])
        S.dma("sp", vv[:], G["hi"][:, r0:r0 + 128].rearrange("(n c) v -> c n v", c=CH), [G["hi"]], [vv])

    tsi_box = [0]

    def do_head(h, qT, fl, vv, og):
        r0 = h * 128
        S.dma("sp", og[:], G["hogT"][r0:r0 + 128, :], [G["hogT"]], [og])
        for d in range(2):
            lo_, om_, nom_ = low[:, d, h:h + 1], oml[:, d, h:h + 1], noml[:, d, h:h + 1]
            for (a, n) in segs:
                tA, tB, tC, tD = tsets[tsi_box[0] % 2]
                tsi_box[0] += 1
                nch = n // CH
                c0 = a // CH
                v3 = lambda t, n=n: t[:, 0:n].rearrange("p (n c) -> p n c", c=CH)
                S.emit("act", lambda e, tA=tA, tB=tB, tC=tC, tD=tD, a=a, n=n, d=d: e.activation(out=tA[:, 0:n], in_=fl[d][:, a:a + n], func=AF.Sigmoid), [fl[d]], [tA])
                S.emit("act", lambda e, tA=tA, tB=tB, tC=tC, tD=tD, n=n, lo_=lo_, om_=om_: e.activation(out=tB[:, 0:n], in_=tA[:, 0:n], func=AF.Ln, scale=om_, bias=lo_),
                       [tA, low, oml], [tB])
                S.emit("dve", lambda e, tA=tA, tB=tB, tC=tC, tD=tD, n=n, om_=om_, nom_=nom_: e.tensor_scalar(out=tC[:, 0:n], in0=tA[:, 0:n], scalar1=nom_, scalar2=om_,
                                                                                op0=ALU.mult, op1=ALU.add), [tA, oml, noml], [tC])
                S.emit("dve", lambda e, tA=tA, tB=tB, tC=tC, tD=tD, n=n: e.tensor_tensor_scan(out=tA[:, 0:n], data0=msk01[:, 0:n], data1=tB[:, 0:n], initial=0.0,
                                                                  op0=ALU.mult, op1=ALU.add), [msk01, tB, tA], [tA])
                tot = lambda nch=nch, v3=v3, tA=tA: v3(tA)[:, :, CH - 1:CH]
                if d == 0:
                    Gd = tA
                else:
                    S.emit("dve", lambda e, tA=tA, tB=tB, tC=tC, tD=tD, n=n: e.tensor_tensor(out=tD[:, 0:n], in0=tB[:, 0:n], in1=tA[:, 0:n], op=ALU.subtract), [tA, tB], [tD])
                    S.emit("dve", lambda e, tA=tA, tB=tB, tC=tC, tD=tD, v3=v3, tot=tot, nch=nch: e.tensor_tensor(out=v3(tB), in0=v3(tD), in1=tot().to_broadcast([128, nch, CH]),
                                                                                     op=ALU.add), [tD, tA], [tB])
                    Gd = tB
                S.emit("act", lambda e, tA=tA, tB=tB, tC=tC, tD=tD, c0=c0, nch=nch, d=d, tot=tot: e.activation(out=egt[d][:, c0:c0 + nch].rearrange("p (n o) -> p n o", o=1),
                                                                                  in_=tot(), func=AF.Exp), [tA], [egt[d]])
                if a >= NCTX:
                    S.emit("act", lambda e, tA=tA, tB=tB, tC=tC, tD=tD, n=n, Gd=Gd: e.activation(out=tD[:, 0:n], in_=Gd[:, 0:n], func=AF.Exp), [Gd], [tD])
                    S.emit("dve", lambda e, tA=tA, tB=tB, tC=tC, tD=tD, n=n, a=a, d=d: e.tensor_tensor(out=qt[d][:, a - NCTX:a - NCTX + n], in0=qT[:, a - NCTX:a - NCTX + n],
                                                                           in1=tD[:, 0:n], op=ALU.mult), [qT, tD], [qt[d]])
                S.emit("act", lambda e, tA=tA, tB=tB, tC=tC, tD=tD, n=n, Gd=Gd: e.activation(out=tD[:, 0:n], in_=Gd[:, 0:n], func=AF.Exp, scale=-1.0), [Gd], [tD])
                S.emit("pool", lambda e, tA=tA, tB=tB, tC=tC, tD=tD, n=n, a=a, d=d: e.tensor_tensor(out=kt[d][:, a:a + n], in0=tC[:, 0:n], in1=tD[:, 0:n], op=ALU.mult),
                       [tC, tD], [kt[d]])
                S.emit("dve", lambda e, tA=tA, tB=tB, tC=tC, tD=tD, v3=v3, tot=tot, nch=nch, Gd=Gd: e.tensor_tensor(out=v3(tD), in0=tot().to_broadcast([128, nch, CH]),
                                                                                       in1=v3(Gd), op=ALU.subtract), [tA, Gd], [tD])
                S.emit("act", lambda e, tA=tA, tB=tB, tC=tC, tD=tD, n=n: e.activation(out=tD[:, 0:n], in_=tD[:, 0:n], func=AF.Exp), [tD], [tD])
                S.emit("pool", lambda e, tA=tA, tB=tB, tC=tC, tD=tD, n=n, a=a, d=d: e.tensor_tensor(out=kh[d][:, a:a + n], in0=tC[:, 0:n], in1=tD[:, 0:n], op=ALU.mult),
                       [tC, tD], [kh[d]])
        for d in range(2):
            S.emit("pool", lambda e, d=d: e.memset(Sf[d][0][:], 0.0), writes=[Sf[d][0]])
            S.emit("pool", lambda e, d=d: e.memset(Sb[d][0][:], 0.0), writes=[Sb[d][0]])
        OAc = {}

        def genA(d, n):
            c = order[d][n]
            t0 = c * CH
            sl = n % 3
            if c >= NCC:
                q0 = t0 - NCTX
                S.emit("pe", lambda e: e.matmul(pst[d][:, 0:CH], lhsT=kt[d][:, t0:t0 + CH], rhs=qt[d][:, q0:q0 + CH],
                                                start=True, stop=True), [kt[d], qt[d]], [pst[d]])
                yield
                S.emit("dve", lambda e: e.tensor_tensor(out=sTs[d][sl][:], in0=pst[d][:, 0:CH], in1=cm[d][:], op=ALU.mult),
                       [pst[d], cm[d]], [sTs[d][sl]])
                yield
            S.emit("pe", lambda e: e.transpose(out=ptr[d][:, 0:128], in_=kh[d][:, t0:t0 + CH], identity=identb[:]),
                   [kh[d], identb], [ptr[d]])
            yield
            S.emit("act", lambda e: e.copy(out=khs[d][sl][:], in_=ptr[d][:, 0:128]), [ptr[d]], [khs[d][sl]])
            yield

        def genB(d, n):
            c = order[d][n]
            t0 = c * CH
            sl = n % 3
            Sf_o, Sf_n = Sf[d][n % 2], Sf[d][(n + 1) % 2]
            Sb_o, Sb_n = Sb[d][n % 3], Sb[d][(n + 1) % 3]
            S.emit("pe", lambda e: e.matmul(pd[d][:, 0:128], lhsT=khs[d][sl][:], rhs=vv[:, c, :], start=True, stop=True),
                   [khs[d][sl], vv], [pd[d]])
            yield
            S.emit("dve", lambda e: e.scalar_tensor_tensor(out=Sf_n[:], in0=Sf_o[:], scalar=egt[d][:, c:c + 1], in1=pd[d][:, 0:128],
                                                           op0=ALU.mult, op1=ALU.add), [Sf_o, egt[d], pd[d]], [Sf_n])
            yield
            S.emit("act", lambda e: e.copy(out=Sb_n[:], in_=Sf_n[:]), [Sf_n], [Sb_n])
            yield
            if c >= NCC:
                q0 = t0 - NCTX

                def mo(e):
                    e.matmul(po[d][:, 0:CH], lhsT=Sb_o[:], rhs=qt[d][:, q0:q0 + CH], start=True, stop=False)
                    return e.matmul(po[d][:, 0:CH], lhsT=vv[:, c, :], rhs=sTs[d][sl][:], start=False, stop=True)
                S.emit("pe", mo, [Sb_o, qt[d], vv, sTs[d][sl]], [po[d]])
                yield
                if c not in OAc:
                    OAc[c] = Buf(f"OAc{c}")
                    OAc[c].ws = dict(OA.ws)
                    OAc[c].rs = dict(OA.rs)
                    S.emit("act", lambda e: e.copy(out=OA[:, q0:q0 + CH], in_=po[d][:, 0:CH]), [po[d]], [OAc[c]])
                else:
                    S.emit("dve", lambda e: e.tensor_tensor(out=OA[:, q0:q0 + CH], in0=po[d][:, 0:CH], in1=OA[:, q0:q0 + CH],
                                                            op=ALU.add), [po[d], OAc[c]], [OAc[c]])
                yield

        def run_streams(streams):
            while streams:
                for g in list(streams):
                    try:
                        next(g)
                    except StopIteration:
                        streams.remove(g)

        run_streams([genA(0, 0), genA(1, 0)])
        for n in range(NCH):
            sts = []
            if n + 1 < NCH:
                sts += [genA(0, n + 1), genA(1, n + 1)]
            sts += [genB(0, n), genB(1, n)]
            run_streams(sts)
        for bk in OAc.values():
            for k_, v_ in bk.ws.items():
                if OA.ws.get(k_, 0) < v_:
                    OA.ws[k_] = v_
        S.emit("dve", lambda e: e.tensor_tensor(out=OA[:], in0=OA[:], in1=og[:], op=ALU.mult), [OA, og], [OA])
        for bi, t0 in enumerate(range(0, T, 512)):
            nb = min(512, T - t0)
            S.emit("act", lambda e, t0=t0, nb=nb: e.activation(out=sqb[:, 0:nb], in_=OA[:, t0:t0 + nb], func=AF.Square), [OA], [sqb])
            S.emit("pe", lambda e, nb=nb: e.matmul(po[0][:, 0:nb], lhsT=onesb[:], rhs=sqb[:, 0:nb], start=True, stop=True), [onesb, sqb], [po[0]])
            S.emit("act", lambda e, nb=nb: e.activation(out=rt[:, 0:nb], in_=po[0][:, 0:nb], func=AF.Ln, scale=1.0 / 128, bias=C["epsc"][:, 0:1]),
                   [po[0], C["epsc"]], [rt])
            S.emit("act", lambda e, nb=nb: e.activation(out=rt[:, 0:nb], in_=rt[:, 0:nb], func=AF.Exp, scale=-0.5), [rt], [rt])
            y = ys[bi % 2]
            S.emit("dve", lambda e, t0=t0, nb=nb, y=y, h=h: e.scalar_tensor_tensor(out=y[:, 0:nb], in0=OA[:, t0:t0 + nb], scalar=gain[:, h:h + 1],
                                                                                   in1=rt[:, 0:nb], op0=ALU.mult, op1=ALU.mult), [OA, gain, rt], [y])
            S.dma("sp", G["yAT"][r0:r0 + 128, t0:t0 + nb], y[:, 0:nb], [y], [G["yAT"]])

    load_head(0)
    for h in range(H):
        if h + 1 < H:
            load_head(h + 1)
        do_head(h, qT2[h % 2], fl2[h % 2], vv2[h % 2], og2[h % 2])
    A.close()


def phase_gconv(S, cfg, G, I, C):
    A = Arena(S, "gc")
    H, T, TT, NCTX, HV = cfg.H, cfg.T, cfg.TT, cfg.NCTX, cfg.HV
    NB = 3 * HV // 128
    cw = A.sb("cw", [128, 5, NB], F32)
    for j in range(5):
        S.dma("sp", cw[:, j, :], I["gdn_conv"][0, j].rearrange("(n p) -> p n", p=128), [I["gdn_conv"]], [cw], allow_slow_non_contiguous=True)
    xp = [A.sb(f"xp{i}", [128, TT + 8], BF16) for i in range(2)]
    for x in xp:
        S.emit("pool", lambda e, x=x: e.memset(x[:], 0.0), writes=[x])
    y = A.sb("y", [128, TT], F32)
    sq = A.sb("sq", [128, 512], BF16)
    rt = A.sb("rt", [128, 512], F32)
    ob = [A.sb(f"ob{i}", [128, TT], BF16) for i in range(2)]
    pn = [A.ps(f"pn{i}", [128, 512], F32) for i in range(2)]
    segs = [(0, NCTX, 2), (NCTX, T, NCTX + 6)]
    dsts = [G["gq"], G["gk"], G["gv"]]
    pi = 0
    for fb in range(NB):
        kind, hb = divmod(fb, H)
        x = xp[fb % 2]
        r0 = fb * 128
        for (a, n, off) in segs:
            if kind == 0 and a == 0:
                continue
            S.dma("sp" if fb % 2 == 0 else "pool", x[:, off:off + n], G["gqkvT"][r0:r0 + 128, a:a + n], [G["gqkvT"]], [x])
            S.emit("dve", lambda e, x=x, a=a, n=n, off=off, fb=fb: e.tensor_scalar(out=y[:, a:a + n], in0=x[:, off - 2:off - 2 + n], scalar1=cw[:, 0, fb:fb + 1],
                                                                                  scalar2=None, op0=ALU.mult), [x, cw], [y])
            for j in range(1, 5):
                S.emit("dve", lambda e, x=x, a=a, n=n, off=off, fb=fb, j=j: e.scalar_tensor_tensor(
                    out=y[:, a:a + n], in0=x[:, off - 2 + j:off - 2 + j + n], scalar=cw[:, j, fb:fb + 1], in1=y[:, a:a + n],
                    op0=ALU.mult, op1=ALU.add), [x, cw, y], [y])
        lo = 0 if kind != 0 else NCTX
        o = ob[fb % 2]
        S.emit("act", lambda e, lo=lo: e.activation(out=y[:, lo:TT], in_=y[:, lo:TT], func=AF.Silu), [y], [y])
        if kind == 2:
            S.emit("dve", lambda e, o=o: e.tensor_copy(out=o[:], in_=y[:]), [y], [o])
        else:
            sc = (128.0 ** -0.5) if kind == 0 else 1.0
            for (t0, nb) in token_blocks(cfg, lo, TT):
                p = pn[pi % 2]
                pi += 1
                S.emit("dve", lambda e, t0=t0, nb=nb: e.tensor_tensor(out=sq[:, 0:nb], in0=y[:, t0:t0 + nb], in1=y[:, t0:t0 + nb], op=ALU.mult), [y], [sq])
                S.emit("pe", lambda e, p=p, nb=nb: e.matmul(p[:, 0:nb], lhsT=C["onesb"][:], rhs=sq[:, 0:nb], start=True, stop=True), [C["onesb"], sq], [p])
                S.emit("act", lambda e, p=p, nb=nb: e.activation(out=rt[:, 0:nb], in_=p[:, 0:nb], func=AF.Ln, bias=C["epsc"][:, 0:1]), [p, C["epsc"]], [rt])
                S.emit("act", lambda e, nb=nb: e.activation(out=rt[:, 0:nb], in_=rt[:, 0:nb], func=AF.Exp, scale=-0.5), [rt], [rt])
                S.emit("dve", lambda e, o=o, t0=t0, nb=nb, sc=sc: e.scalar_tensor_tensor(out=o[:, t0:t0 + nb], in0=y[:, t0:t0 + nb], scalar=sc, in1=rt[:, 0:nb],
                                                                                        op0=ALU.mult, op1=ALU.mult), [y, rt], [o])
        S.dma("sp", dsts[kind][hb * 128:(hb + 1) * 128, lo:TT], o[:, lo:TT], [o], [dsts[kind]])
    A.close()


def phase_gdn(S, cfg, G, I, C):
    H, T, TT, NCTX, HV, R, GW = cfg.H, cfg.T, cfg.TT, cfg.NCTX, cfg.HV, cfg.R, cfg.GW
    NCHK = TT // 128
    NCC = NCTX // 128
    order = [list(range(NCHK)), list(range(NCC - 1, -1, -1)) + list(range(NCHK - 1, NCC - 1, -1))]
    HG = min(4, H)
    identb, identf, onesb, onesf = C["identb"], C["identf"], C["onesb"], C["onesf"]
    A = Arena(S, "gd")
    def indicator(name, sg, strict):
        m = A.sb(name, [128, 128], F32)
        S.emit("pool", lambda e: e.memset(m[:], 1.0), writes=[m])
        S.emit("pool", lambda e: e.affine_select(out=m[:], in_=m[:], pattern=[[sg, 128]], compare_op=ALU.is_ge, fill=0.0,
                                                 base=-strict, channel_multiplier=-sg), [m], [m])
        return m
    VI = [indicator("VIf", 1, 0), indicator("VIb", -1, 0)]
    VS = [indicator("VSf", 1, 1), indicator("VSb", -1, 1)]
    NMI, NMS = [], []
    for d in range(2):
        for (lst, src, nm) in ((NMI, VI[d], f"NMI{d}"), (NMS, VS[d], f"NMS{d}")):
            m = A.sb(nm, [128, 128], BF16)
            S.emit("dve", lambda e, m=m, src=src: e.tensor_scalar(out=m[:], in0=src[:], scalar1=-1.0, scalar2=-NEG, op0=ALU.add, op1=ALU.mult), [src], [m])
            lst.append(m)
    dmask = A.sb("dmask", [128, 128], BF16)
    S.emit("pool", lambda e: e.memset(dmask[:], 1.0), writes=[dmask])
    S.emit("pool", lambda e: e.affine_select(out=dmask[:].rearrange("p (b c) -> p b c", c=32), in_=dmask[:].rearrange("p (b c) -> p b c", c=32),
                                             pattern=[[-32, 4], [0, 32]], compare_op=ALU.is_ge, fill=0.0, base=0, channel_multiplier=1), [dmask], [dmask])
    S.emit("pool", lambda e: e.affine_select(out=dmask[:].rearrange("p (b c) -> p b c", c=32), in_=dmask[:].rearrange("p (b c) -> p b c", c=32),
                                             pattern=[[32, 4], [0, 32]], compare_op=ALU.is_ge, fill=0.0, base=31, channel_multiplier=-1), [dmask], [dmask])
    omask = A.sb("omask", [128, 128], BF16)
    S.emit("dve", lambda e: e.tensor_scalar(out=omask[:], in0=dmask[:], scalar1=-1.0, scalar2=1.0, op0=ALU.mult, op1=ALU.add), [dmask], [omask])
    LC = [VI[0], VI[1]]
    LR = [VS[1], VS[0]]
    W2 = 2 * H
    sh = [128, NCHK, W2]
    bet = A.sb("bet", sh, F32)
    gsp = A.sb("gsp", [128, NCHK, 2, W2], BF16)
    lsp = A.sb("lsp", [128, NCHK, 2, W2], BF16)
    egr = A.sb("egr", sh, F32)
    egt = A.sb("egt", sh, F32)
    nbe = A.sb("nbe", sh, F32)
    A0 = Arena(S, "gd0")
    gg = A0.sb("gg", sh, F32)
    lnb = A0.sb("lnb", sh, F32)
    gres = A0.sb("gres", sh, F32)
    gab = A0.sb("gab", [128, NCHK, 4 * H], F32)
    S.dma("sp", gab[:], G["gab"][:, :].rearrange("(n p) c -> p n c", p=128), [G["gab"]], [gab])
    cst = A0.sb("cst", [128, 2, 2 * H], F32)
    S.dma("sp", cst[:, 0, :], I["gdn_dt_bias"][0].rearrange("d h -> (d h)").partition_broadcast(128), [I["gdn_dt_bias"]], [cst])
    S.dma("sp", cst[:, 1, :], I["gdn_a_log"][0].rearrange("d h -> (d h)").partition_broadcast(128), [I["gdn_a_log"]], [cst])
    nea = A0.sb("nea", [128, 2 * H], F32)
    S.emit("act", lambda e: e.activation(out=nea[:], in_=cst[:, 1, :], func=AF.Exp), [cst], [nea])
    S.emit("dve", lambda e: e.tensor_scalar(out=nea[:], in0=nea[:], scalar1=-1.0, scalar2=None, op0=ALU.mult), [nea], [nea])
    gx = A0.sb("gx", sh, F32); gt1 = A0.sb("gt1", sh, F32); gt2 = A0.sb("gt2", sh, F32)
    bc = lambda t: t.rearrange("p (o c) -> p o c", o=1).to_broadcast(sh)
    S.emit("dve", lambda e: e.tensor_tensor(out=gx[:], in0=gab[:, :, 0:W2], in1=bc(cst[:, 0, :]), op=ALU.add), [gab, cst], [gx])
    S.emit("dve", lambda e: e.tensor_scalar(out=gt1[:], in0=gx[:], scalar1=-1.0, scalar2=None, op0=ALU.mult), [gx], [gt1])
    S.emit("dve", lambda e: e.tensor_tensor(out=gt1[:], in0=gt1[:], in1=gx[:], op=ALU.max), [gt1, gx], [gt1])
    S.emit("act", lambda e: e.activation(out=gt1[:], in_=gt1[:], func=AF.Exp, scale=-1.0), [gt1], [gt1])
    S.emit("act", lambda e: e.activation(out=gt1[:], in_=gt1[:], func=AF.Ln, bias=C["onec"][:, 0:1]), [gt1, C["onec"]], [gt1])
    S.emit("dve", lambda e: e.tensor_scalar(out=gt2[:], in0=gx[:], scalar1=0.0, scalar2=None, op0=ALU.max), [gx], [gt2])
    S.emit("dve", lambda e: e.tensor_tensor(out=gt2[:], in0=gt2[:], in1=gt1[:], op=ALU.add), [gt2, gt1], [gt2])
    S.emit("dve", lambda e: e.tensor_tensor(out=gg[:], in0=gt2[:], in1=bc(nea[:]), op=ALU.mult), [gt2, nea], [gg])
    S.emit("act", lambda e: e.activation(out=bet[:], in_=gab[:, :, W2:2 * W2], func=AF.Sigmoid), [gab], [bet])
    S.emit("act", lambda e: e.activation(out=lnb[:], in_=bet[:], func=AF.Ln), [bet], [lnb])
    for src, dst in ((gg, gsp), (lnb, lsp)):
        S.emit("dve", lambda e, src=src, dst=dst: e.tensor_copy(out=dst[:, :, 0, :], in_=src[:]), [src], [dst])
        S.emit("dve", lambda e, src=src, dst=dst: e.tensor_tensor(out=gres[:], in0=src[:], in1=dst[:, :, 0, :], op=ALU.subtract), [src, dst], [gres])
        S.emit("dve", lambda e, dst=dst: e.tensor_copy(out=dst[:, :, 1, :], in_=gres[:]), [gres, dst], [dst])
    pA = A.ps("pA", [128, 512], F32)
    pg = pA
    for n in range(NCHK):
        def mm(e, n=n):
            e.matmul(pg[:, 0:H], lhsT=LC[0][:], rhs=gg[:, n, 0:H], start=True, stop=True)
            e.matmul(pg[:, H:W2], lhsT=LC[1][:], rhs=gg[:, n, H:W2], start=True, stop=True)
            e.matmul(pg[:, W2:W2 + H], lhsT=LR[0][:], rhs=gg[:, n, 0:H], start=True, stop=True)
            e.matmul(pg[:, W2 + H:2 * W2], lhsT=LR[1][:], rhs=gg[:, n, H:W2], start=True, stop=True)
            return e.matmul(pg[:, 2 * W2:3 * W2], lhsT=onesf[:], rhs=gg[:, n, :], start=True, stop=True)
        S.emit("pe", mm, [LC[0], LC[1], LR[0], LR[1], onesf, gg], [pg])
        S.emit("act", lambda e, n=n: e.activation(out=egr[:, n, :], in_=pg[:, W2:2 * W2], func=AF.Exp), [pg, egr], [egr])
        S.emit("act", lambda e, n=n: e.activation(out=egt[:, n, :], in_=pg[:, 2 * W2:3 * W2], func=AF.Exp), [pg, egt], [egt])
        S.emit("act", lambda e, n=n: e.activation(out=nbe[:, n, :], in_=pg[:, 0:W2], func=AF.Exp), [pg, nbe], [nbe])
    S.emit("dve", lambda e: e.scalar_tensor_tensor(out=nbe[:], in0=nbe[:], scalar=-1.0, in1=bet[:], op0=ALU.mult, op1=ALU.mult), [nbe, bet], [nbe])
    if "dbg_g" in G:
        for i, t in enumerate((gg, gg, egr, egt, nbe, bet)):
            S.dma("sp", G["dbg_g"][i], t[:], [t], [G["dbg_g"]])
    A0.close()
    LCb = []
    for d in range(2):
        m = A.sb(f"LCb{d}", [128, 128], BF16)
        S.emit("dve", lambda e, m=m, d=d: e.tensor_copy(out=m[:], in_=LC[d][:]), [LC[d]], [m])
        LCb.append(m)
    gain = A.sb("gain", [128, H], F32)
    S.dma("sp", gain[:], I["gdn_norm"][0].rearrange("(h p) -> p h", p=128), [I["gdn_norm"]], [gain], allow_slow_non_contiguous=True)

    NU = 2 * HG
    ld = {nm: [[A.sb(f"ld{nm}{d}{i}", [128, HG, 128], BF16) for i in range(3 if nm == "k" else 2)] for d in range(2)] for nm in "kqv"}
    U = []
    for u in range(NU):
        ub = {}
        for nm in ("TT", "PT", "qg", "kd", "vb"):
            ub[nm] = [A.sb(f"u{u}{nm}{i}", [128, 128], BF16) for i in range(2 if nm == "TT" else 3)]
        ub["X"] = [[A.sb(f"u{u}X{j}{i}", [128, 4, 128], BF16) for i in range(2)] for j in range(2)]
        ub["Z0"] = [A.sb(f"u{u}Z0{j}", [128, 128], BF16) for j in range(2)]
        ub["OT"] = [A.sb(f"u{u}OT{j}", [128, 128], BF16) for j in range(2)]
        ub["gm"] = [A.sb(f"u{u}gm{j}", [128, 2, 128], BF16) for j in range(2)]
        ub["eg"] = [A.sb(f"u{u}eg{j}", [128, 128], BF16) for j in range(2)]
        ub["DD"] = A.sb(f"u{u}DD", [128, 2, 128], BF16)
        ub["NN"] = A.sb(f"u{u}NN", [128, 2, 128], BF16)
        ub["N2"] = A.sb(f"u{u}N2", [128, 2, 128], BF16)
        ub["PP"] = A.sb(f"u{u}PP", [128, 2, 128], BF16)
        ub["S"] = A.sb(f"u{u}S", [128, 128], F32)
        ub["Sb"] = A.sb(f"u{u}Sb", [128, 128], BF16)
        ub["r"] = A.sb(f"u{u}r", [128, 128], BF16)
        ub["vn"] = A.sb(f"u{u}vn", [128, 128], BF16)
        U.append(ub)
    ngu = [A.sb(f"ngu{i}", [128, 1], F32) for i in range(2)]
    OB = A.sb("OB", [128, HG, T], F32)
    zq = A.sb("zq", [128, 512], BF16); sqb = A.sb("sqb", [128, 512], BF16); rt = A.sb("rt", [128, 512], F32)
    ys = [sqb, sqb]
    djunk = rt
    pR = A.ps("pR", [128, 512], F32)
    pT = A.ps("pT", [128, 1024], BF16)
    pI = [A.ps(f"pI{i}", [128, 512], F32) for i in range(4)]
    pS = A.ps("pS", [128, 512], F32)
    ncol = 128 // R if R < 128 else 1
    v4 = lambda p: p[:, 0:512].rearrange("p (a b) -> p a b", b=128)

    def run_streams(streams):
        streams = [g for g in streams if g is not None]
        while streams:
            for g in list(streams):
                try:
                    next(g)
                except StopIteration:
                    streams.remove(g)

    for hg in range(H // HG):
        for ub in U:
            S.emit("pool", lambda e, ub=ub: e.memset(ub["S"][:], 0.0), writes=[ub["S"]])
            S.emit("pool", lambda e, ub=ub: e.memset(ub["Sb"][:], 0.0), writes=[ub["Sb"]])
        OBc = {}

        def units_of(s):
            out = []
            for d in range(2):
                c = order[d][s]
                for hl in range(HG):
                    out.append((d, hl, c, c >= NCC))
            return out

        def gen_P(s):
            s3, s2 = s % 3, s % 2
            for d in range(2):
                c = order[d][s]
                t0 = c * 128
                lat = c >= NCC
                for nm, src in (("k", G["gk"]), ("q", G["gq"]), ("v", G["gv"])):
                    if nm == "q" and not lat:
                        continue
                    dst = ld[nm][d][s3 if nm == "k" else s2]
                    S.dma("sp" if d == 0 else "pool", dst[:],
                          src[hg * HG * 128:(hg + 1) * HG * 128, t0:t0 + 128].rearrange("(h p) t -> p h t", p=128), [src], [dst])
            yield
            ii = 0
            for (d, hl, c, lat) in units_of(s):
                u = d * HG + hl
                ub = U[u]
                h = hg * HG + hl
                dh = d * H + h
                kT = ld["k"][d][s3]; qTb = ld["q"][d][s2]; vT = ld["v"][d][s2]
                X0 = ub["X"][s2][0]
                X1 = ub["X"][s2][1]
                Z0 = ub["Z0"][s2]
                OT = ub["OT"][s2]
                def mA(e, kT=kT, qTb=qTb, hl=hl, lat=lat):
                    r = e.matmul(pA[:, 0:128], lhsT=kT[:, hl, :], rhs=kT[:, hl, :], start=True, stop=True)
                    if lat:
                        r = e.matmul(pA[:, 128:256], lhsT=kT[:, hl, :], rhs=qTb[:, hl, :], start=True, stop=True)
                    return r
                S.emit("pe", mA, [kT, qTb], [pA])
                yield
                gm = ub["gm"][s2]
                eg = ub["eg"][s2]
                S.emit("dve", lambda e, Z0=Z0, gm=gm: e.scalar_tensor_tensor(out=Z0[:], in0=pA[:, 0:128], scalar=-1.0, in1=gm[:, 1, :],
                                                                             op0=ALU.mult, op1=ALU.mult), [pA, gm], [Z0])
                yield
                def mT(e, Z0=Z0, kT=kT, vT=vT, hl=hl):
                    e.transpose(out=pT[:, 0:128], in_=Z0[:], identity=identb[:])
                    e.transpose(out=pT[:, 128:256], in_=kT[:, hl, :], identity=identb[:])
                    return e.transpose(out=pT[:, 256:384], in_=vT[:, hl, :], identity=identb[:])
                S.emit("pe", mT, [Z0, kT, vT, identb], [pT])
                if lat:
                    S.emit("dve", lambda e, ub=ub, s3=s3, gm=gm: e.tensor_tensor(out=ub["PT"][s3][:], in0=pA[:, 128:256], in1=gm[:, 0, :], op=ALU.mult),
                           [pA, gm], [ub["PT"][s3]])
                S.emit("pool", lambda e, X0=X0, Z0=Z0: e.tensor_tensor(out=X0[:, 0, :], in0=Z0[:], in1=dmask[:], op=ALU.mult), [Z0, dmask, X0], [X0])
                yield
                if lat:
                    S.emit("dve", lambda e, ub=ub, s3=s3, eg=eg, qTb=qTb, hl=hl: e.tensor_tensor(out=ub["qg"][s3][:], in0=qTb[:, hl, :], in1=eg[:], op=ALU.mult),
                           [qTb, eg], [ub["qg"][s3]])
                S.emit("pool", lambda e, X1=X1, X0=X0: e.tensor_tensor(out=X1[:, 1, :], in0=X0[:, 0, :], in1=identb[:], op=ALU.add), [X0, identb, X1], [X1])
                yield
                S.emit("dve", lambda e, X0=X0: e.tensor_tensor(out=X0[:, 2, :], in0=pT[:, 0:128], in1=dmask[:], op=ALU.mult), [pT, dmask, X0], [X0])
                S.emit("dve", lambda e, OT=OT: e.tensor_tensor(out=OT[:], in0=pT[:, 0:128], in1=omask[:], op=ALU.mult), [pT, omask], [OT])
                S.emit("act", lambda e, ub=ub, s3=s3, c=c, dh=dh: e.activation(out=ub["kd"][s3][:], in_=pT[:, 128:256], func=AF.Copy, scale=egr[:, c, dh:dh + 1]),
                       [pT, egr], [ub["kd"][s3]])
                S.emit("act", lambda e, ub=ub, s3=s3, c=c, dh=dh: e.activation(out=ub["vb"][s3][:], in_=pT[:, 256:384], func=AF.Copy, scale=bet[:, c, dh:dh + 1]),
                       [pT, bet], [ub["vb"][s3]])
                yield
                S.emit("pool", lambda e, X1=X1, X0=X0: e.tensor_tensor(out=X1[:, 3, :], in0=X0[:, 2, :], in1=identb[:], op=ALU.add), [X0, identb, X1], [X1])
                yield

        def gen_G(s):
            s2 = s % 2
            ii = 0
            for (d, hl, c, lat) in units_of(s):
                u = d * HG + hl
                ub = U[u]
                h = hg * HG + hl
                dh = d * H + h
                gm = ub["gm"][s2]
                eg = ub["eg"][s2]
                ng = ngu[ii % 2]
                ii += 1
                def mR(e, c=c, dh=dh, d=d, lat=lat):
                    ghi = gsp[:, c, 0, dh:dh + 1].to_broadcast([128, 128])
                    glo = gsp[:, c, 1, dh:dh + 1].to_broadcast([128, 128])
                    e.matmul(pR[:, 256:384], lhsT=ghi, rhs=LCb[d][:], start=True, stop=False)
                    e.matmul(pR[:, 256:384], lhsT=glo, rhs=LCb[d][:], start=False, stop=True)
                    if lat:
                        e.matmul(pR[:, 0:128], lhsT=ghi, rhs=LCb[d][:], start=True, stop=False)
                        e.matmul(pR[:, 0:128], lhsT=glo, rhs=LCb[d][:], start=False, stop=False)
                        e.matmul(pR[:, 0:128], lhsT=identb[:], rhs=NMI[d][:], start=False, stop=True)
                    e.matmul(pR[:, 128:256], lhsT=ghi, rhs=LCb[d][:], start=True, stop=False)
                    e.matmul(pR[:, 128:256], lhsT=glo, rhs=LCb[d][:], start=False, stop=False)
                    e.matmul(pR[:, 128:256], lhsT=lsp[:, c, 0, dh:dh + 1].to_broadcast([128, 128]), rhs=identb[:], start=False, stop=False)
                    e.matmul(pR[:, 128:256], lhsT=lsp[:, c, 1, dh:dh + 1].to_broadcast([128, 128]), rhs=identb[:], start=False, stop=False)
                    return e.matmul(pR[:, 128:256], lhsT=identb[:], rhs=NMS[d][:], start=False, stop=True)
                S.emit("pe", mR, [gsp, lsp, LCb[d], identb, NMI[d], NMS[d]], [pR])
                yield
                S.emit("dve", lambda e: e.tensor_tensor(out=djunk[:, 0:128], in0=pR[:, 256:384], in1=identf[:], op=ALU.mult), [pR, identf], [djunk])
                yield
                S.emit("dve", lambda e, ng=ng: e.tensor_reduce(out=ng[:, 0:1], in_=djunk[:, 0:128], axis=mybir.AxisListType.X, op=ALU.add, negate=True), [djunk], [ng])
                yield
                lo = 0 if lat else 1
                S.emit("act", lambda e, gm=gm, ng=ng, lo=lo: e.activation(out=gm[:, lo:2, :], in_=pR[:, lo * 128:256].rearrange("p (a b) -> p a b", b=128),
                                                                          func=AF.Exp, bias=ng[:, 0:1]), [pR, ng], [gm])
                if lat:
                    S.emit("act", lambda e, eg=eg: e.activation(out=eg[:], in_=pR[:, 256:384], func=AF.Exp), [pR], [eg])
                yield

        def gen_I(s, bank):
            s3, s2 = s % 3, s % 2
            p = pI[bank]
            for (d, hl, c, lat) in units_of(s):
                u = d * HG + hl
                if u % 4 != bank:
                    continue
                ub = U[u]
                X = ub["X"][s2]
                DD, NN, N2, PP, OT = ub["DD"], ub["NN"], ub["N2"], ub["PP"], ub["OT"][s2]
                for st in range(9):
                    if st == 0:
                        Xc, Xn = X[0], X[1]
                        def m0(e, Xc=Xc):
                            e.matmul(p[:, 0:128], lhsT=Xc[:, 2, :], rhs=Xc[:, 0, :], start=True, stop=True)
                            return e.matmul(p[:, 256:384], lhsT=Xc[:, 0, :], rhs=Xc[:, 2, :], start=True, stop=True)
                        S.emit("pe", m0, [Xc], [p])
                        yield
                        S.emit("act", lambda e, Xn=Xn: e.copy(out=Xn[:, 0:4:2, :], in_=v4(p)[:, 0:4:2, :]), [p, Xn], [Xn])
                    elif st < 4:
                        Xc, Xn = X[st % 2], X[(st + 1) % 2]
                        def m1(e, Xc=Xc):
                            e.matmul(p[:, 0:256], lhsT=Xc[:, 2, :], rhs=Xc[:, 0:2, :], start=True, stop=True)
                            return e.matmul(p[:, 256:512], lhsT=Xc[:, 0, :], rhs=Xc[:, 2:4, :], start=True, stop=True)
                        S.emit("pe", m1, [Xc], [p])
                        yield
                        S.emit("act", lambda e, Xn=Xn: e.copy(out=Xn[:, 0:4:2, :], in_=v4(p)[:, 0:4:2, :]), [p, Xn], [Xn])
                        yield
                        S.emit("dve", lambda e, Xn=Xn, Xc=Xc: e.tensor_tensor(out=Xn[:, 1:4:2, :], in0=v4(p)[:, 1:4:2, :], in1=Xc[:, 1:4:2, :], op=ALU.add),
                               [p, Xc, Xn], [Xn])
                    elif st == 4:
                        Xc = X[0]
                        def m4(e, Xc=Xc):
                            e.matmul(p[:, 128:256], lhsT=Xc[:, 2, :], rhs=Xc[:, 1, :], start=True, stop=True)
                            return e.matmul(p[:, 384:512], lhsT=Xc[:, 0, :], rhs=Xc[:, 3, :], start=True, stop=True)
                        S.emit("pe", m4, [Xc], [p])
                        yield
                        S.emit("dve", lambda e, DD=DD, Xc=Xc: e.tensor_tensor(out=DD[:], in0=v4(p)[:, 1:4:2, :], in1=Xc[:, 1:4:2, :], op=ALU.add), [p, Xc], [DD])
                    elif st == 5:
                        def m5(e, DD=DD, OT=OT):
                            e.matmul(p[:, 0:128], lhsT=OT[:], rhs=DD[:, 0, :], start=True, stop=True)
                            return e.matmul(p[:, 128:256], lhsT=DD[:, 0, :], rhs=OT[:], start=True, stop=True)
                        S.emit("pe", m5, [DD, OT], [p])
                        yield
                        S.emit("act", lambda e, NN=NN: e.copy(out=NN[:], in_=v4(p)[:, 0:2, :]), [p], [NN])
                        yield
                        S.emit("dve", lambda e, PP=PP: e.tensor_tensor(out=PP[:, 0, :], in0=p[:, 0:128], in1=identb[:], op=ALU.add), [p, identb, PP], [PP])
                    elif st == 6:
                        def m6(e, NN=NN):
                            e.matmul(p[:, 0:128], lhsT=NN[:, 1, :], rhs=NN[:, 0, :], start=True, stop=True)
                            return e.matmul(p[:, 128:256], lhsT=NN[:, 0, :], rhs=NN[:, 1, :], start=True, stop=True)
                        S.emit("pe", m6, [NN], [p])
                        yield
                        S.emit("act", lambda e, N2=N2: e.copy(out=N2[:], in_=v4(p)[:, 0:2, :]), [p], [N2])
                    elif st == 7:
                        S.emit("pe", lambda e, N2=N2, PP=PP: e.matmul(p[:, 0:128], lhsT=N2[:, 1, :], rhs=PP[:, 0, :], start=True, stop=True), [N2, PP], [p])
                        yield
                        S.emit("dve", lambda e, PP=PP: e.tensor_tensor(out=PP[:, 1, :], in0=p[:, 0:128], in1=PP[:, 0, :], op=ALU.add), [p, PP], [PP])
                    else:
                        S.emit("pe", lambda e, DD=DD, PP=PP: e.matmul(p[:, 0:128], lhsT=DD[:, 1, :], rhs=PP[:, 1, :], start=True, stop=True), [DD, PP], [p])
                        yield
                        S.emit("act", lambda e, ub=ub, s3=s3: e.copy(out=ub["TT"][s2][:], in_=p[:, 0:128]), [p], [ub["TT"][s2]])
                    yield

        def gen_S(s):
            s3 = s % 3
            for (d, hl, c, lat) in units_of(s):
                u = d * HG + hl
                ub = U[u]
                h = hg * HG + hl
                dh = d * H + h
                kT = ld["k"][d][s3]
                S.emit("pe", lambda e, kT=kT, hl=hl, ub=ub: e.matmul(pS[:, 0:128], lhsT=kT[:, hl, :], rhs=ub["Sb"][:], start=True, stop=True), [kT, ub["Sb"]], [pS])
                yield
                S.emit("dve", lambda e, ub=ub, s3=s3, c=c, dh=dh: e.scalar_tensor_tensor(out=ub["r"][:], in0=pS[:, 0:128], scalar=nbe[:, c, dh:dh + 1], in1=ub["vb"][s3][:],
                                                                                        op0=ALU.mult, op1=ALU.add), [pS, nbe, ub["vb"][s3]], [ub["r"]])
                yield
                S.emit("pe", lambda e, ub=ub, s=s: e.matmul(pS[:, 128:256], lhsT=ub["TT"][s % 2][:], rhs=ub["r"][:], start=True, stop=True), [ub["TT"][s % 2], ub["r"]], [pS])
                yield
                S.emit("act", lambda e, ub=ub: e.copy(out=ub["vn"][:], in_=pS[:, 128:256]), [pS], [ub["vn"]])
                yield
                def mo(e, ub=ub, s3=s3, lat=lat):
                    if lat:
                        e.matmul(pS[:, 384:512], lhsT=ub["Sb"][:], rhs=ub["qg"][s3][:], start=True, stop=False)
                        e.matmul(pS[:, 384:512], lhsT=ub["vn"][:], rhs=ub["PT"][s3][:], start=False, stop=True)
                    return e.matmul(pS[:, 256:384], lhsT=ub["kd"][s3][:], rhs=ub["vn"][:], start=True, stop=True)
                S.emit("pe", mo, [ub["Sb"], ub["qg"][s3], ub["vn"], ub["PT"][s3], ub["kd"][s3]], [pS])
                yield
                if lat:
                    cl = c - NCC
                    if R >= 128:
                        per = R // 128
                        cc0, sub = divmod(cl, per)
                        oap = OB[:, hl, :].rearrange("p (r c) -> p c r", c=GW)[:, cc0, sub * 128:(sub + 1) * 128]
                        iap = pS[:, 384:512]
                    else:
                        oap = OB[:, hl, :].rearrange("p (r c) -> p c r", c=GW)[:, cl * ncol:(cl + 1) * ncol, :]
                        iap = pS[:, 384:512].rearrange("p (a b) -> p a b", b=R)
                    key = (hl, cl)
                    if key not in OBc:
                        OBc[key] = Buf(f"OBc{hl}_{cl}")
                        OBc[key].ws = dict(OB.ws)
                        OBc[key].rs = dict(OB.rs)
                        S.emit("act", lambda e, oap=oap, iap=iap: e.copy(out=oap, in_=iap), [pS], [OBc[key]])
                    else:
                        S.emit("dve", lambda e, oap=oap, iap=iap: e.tensor_tensor(out=oap, in0=iap, in1=oap, op=ALU.add), [pS, OBc[key]], [OBc[key]])
                S.emit("dve", lambda e, ub=ub, c=c, dh=dh: e.scalar_tensor_tensor(out=ub["S"][:], in0=ub["S"][:], scalar=egt[:, c, dh:dh + 1], in1=pS[:, 256:384],
                                                                                 op0=ALU.mult, op1=ALU.add), [ub["S"], egt, pS], [ub["S"]])
                yield
                S.emit("act", lambda e, ub=ub: e.copy(out=ub["Sb"][:], in_=ub["S"][:]), [ub["S"]], [ub["Sb"]])
                yield

        for it in range(-3, NCHK):
            sts = []
            if 0 <= it + 3 < NCHK:
                sts.append(gen_G(it + 3))
            if 0 <= it + 2 < NCHK:
                sts.append(gen_P(it + 2))
            if 0 <= it + 1 < NCHK:
                sts += [gen_I(it + 1, b) for b in range(4)]
            if it >= 0:
                sts.append(gen_S(it))
            run_streams(sts)
            if "dbg_S" in G and it == NCC - 1 and hg == 0:
                for u in range(NU):
                    S.dma("sp", G["dbg_S"][u], U[u]["S"][:], [U[u]["S"]], [G["dbg_S"]])
        for key, bk in OBc.items():
            for k_, v_ in bk.ws.items():
                if OB.ws.get(k_, 0) < v_:
                    OB.ws[k_] = v_
        for hl in range(HG):
            h = hg * HG + hl
            if "dbg_ob" in G:
                S.dma("sp", G["dbg_ob"][h * 128:(h + 1) * 128, :], OB[:, hl, :], [OB], [G["dbg_ob"]])
            for bi, t0 in enumerate(range(0, T, 512)):
                nb = min(512, T - t0)
                S.dma("sp", zq[:, 0:nb], G["gzT"][h * 128:(h + 1) * 128, t0:t0 + nb], [G["gzT"]], [zq])
                S.emit("act", lambda e, hl=hl, t0=t0, nb=nb: e.activation(out=sqb[:, 0:nb], in_=OB[:, hl, t0:t0 + nb], func=AF.Square), [OB], [sqb])
                S.emit("pe", lambda e, nb=nb: e.matmul(pA[:, 0:nb], lhsT=onesb[:], rhs=sqb[:, 0:nb], start=True, stop=True), [onesb, sqb], [pA])
                S.emit("act", lambda e, nb=nb: e.activation(out=rt[:, 0:nb], in_=pA[:, 0:nb], func=AF.Ln, scale=1.0 / 128, bias=C["epsc"][:, 0:1]), [pA, C["epsc"]], [rt])
                S.emit("act", lambda e, nb=nb: e.activation(out=rt[:, 0:nb], in_=rt[:, 0:nb], func=AF.Exp, scale=-0.5), [rt], [rt])
                S.emit("dve", lambda e, nb=nb: e.tensor_tensor(out=rt[:, 0:nb], in0=rt[:, 0:nb], in1=zq[:, 0:nb], op=ALU.mult), [rt, zq], [rt])
                y = ys[bi % 2]
                S.emit("dve", lambda e, hl=hl, h=h, t0=t0, nb=nb, y=y: e.scalar_tensor_tensor(out=y[:, 0:nb], in0=OB[:, hl, t0:t0 + nb], scalar=gain[:, h:h + 1], in1=rt[:, 0:nb],
                                                                                             op0=ALU.mult, op1=ALU.mult), [OB, gain, rt], [y])
                S.dma("sp", G["yBT"][h * 128:(h + 1) * 128, t0:t0 + nb], y[:, 0:nb], [y], [G["yBT"]])
    A.close()


def phase_merge(S, cfg, G, I, C, cfgL):
    A = Arena(S, "mg")
    D, T, HV, KD = cfg.D, cfg.T, cfg.HV, cfg.KD
    KH = HV // 128
    HL = cfgL.H
    NRK = cfg.H // HL
    DM = cfgL.DMG
    wa = A.sb("wa", [128, KH, DM], BF16)
    wb = A.sb("wb", [128, KH, DM], BF16)
    for k in range(KH):
        S.dma("pool", wa[:, k, :], I["w_branch_a"][0, k * 128:(k + 1) * 128, :], [I["w_branch_a"]], [wa])
        S.dma("pool", wb[:, k, :], I["w_branch_b"][0, k * 128:(k + 1) * 128, :], [I["w_branch_b"]], [wb])
    ya = [A.sb(f"ya{i}", [128, KH, 512], BF16) for i in range(2)]
    yb = [A.sb(f"yb{i}", [128, KH, 512], BF16) for i in range(2)]
    ga = [A.sb(f"ga{i}", [128, 512], BF16) for i in range(2)]
    gb = [A.sb(f"gb{i}", [128, 512], BF16) for i in range(2)]
    t1 = [A.sb(f"t1{i}", [128, 512], F32) for i in range(2)]
    t2 = [A.sb(f"t2{i}", [128, 512], F32) for i in range(2)]
    mo = [A.sb(f"mo{i}", [128, 512], BF16) for i in range(2)]
    pa = [A.ps(f"pa{i}", [128, 512], F32) for i in range(2)]
    pb = [A.ps(f"pb{i}", [128, 512], F32) for i in range(2)]
    it = 0
    for bi, t0 in enumerate(range(0, T, 512)):
        nb = min(512, T - t0)
        a_, b_ = ya[bi % 2], yb[bi % 2]
        CR = min(256, 2 * HL * 128)
        for r in range(NRK):
            for l in range(HL):
                for (dst, rho) in ((a_, l * 128), (b_, HL * 128 + l * 128)):
                    ck, off = divmod(rho, CR)
                    S.dma("sp", dst[:, r * HL + l, 0:nb], G["yall"][ck, r * CR + off:r * CR + off + 128, t0:t0 + nb], [G["yall"]], [dst])
        for dc in range(DM // 128):
            i2 = it % 2
            it += 1
            S.dma("sp", ga[i2][:, 0:nb], G["mgT"][dc * 128:(dc + 1) * 128, t0:t0 + nb], [G["mgT"]], [ga[i2]])
            S.dma("sp", gb[i2][:, 0:nb], G["mgT"][DM + dc * 128:DM + (dc + 1) * 128, t0:t0 + nb], [G["mgT"]], [gb[i2]])
            def mm(e, w, y, p, dc=dc, nb=nb):
                r = None
                for k in range(KH):
                    r = e.matmul(p[:, 0:nb], lhsT=w[:, k, dc * 128:(dc + 1) * 128], rhs=y[:, k, 0:nb], start=(k == 0), stop=(k == KH - 1))
                return r
            S.emit("pe", lambda e, i2=i2, a_=a_, mm=mm: mm(e, wa, a_, pa[i2]), [wa, a_], [pa[i2]])
            S.emit("pe", lambda e, i2=i2, b_=b_, mm=mm: mm(e, wb, b_, pb[i2]), [wb, b_], [pb[i2]])
            S.emit("dve", lambda e, i2=i2, nb=nb: e.tensor_tensor(out=t1[i2][:, 0:nb], in0=pa[i2][:, 0:nb], in1=ga[i2][:, 0:nb], op=ALU.mult), [pa[i2], ga[i2]], [t1[i2]])
            S.emit("dve", lambda e, i2=i2, nb=nb: e.tensor_tensor(out=t2[i2][:, 0:nb], in0=pb[i2][:, 0:nb], in1=gb[i2][:, 0:nb], op=ALU.mult), [pb[i2], gb[i2]], [t2[i2]])
            S.emit("pool", lambda e, i2=i2, nb=nb: e.tensor_tensor(out=mo[i2][:, 0:nb], in0=t1[i2][:, 0:nb], in1=t2[i2][:, 0:nb], op=ALU.add), [t1[i2], t2[i2]], [mo[i2]])
            S.dma("sp", G["mT"][dc * 128:(dc + 1) * 128, t0:t0 + nb], mo[i2][:, 0:nb], [mo[i2]], [G["mT"]])
    A.close()


def load_row_bcast(S, A, name, src_ap, srcbuf, n):
    t = A.sb(name, [128, n], F32)
    S.dma("sp", t[:], src_ap.partition_broadcast(128), [srcbuf], [t])
    return t


def phase_outproj(S, cfg, G, I, C, cfgL, nsplit):
    A = Arena(S, "op")
    D, T, KD = cfg.D, cfg.T, cfg.KD
    DM = cfgL.DMG
    CRM = min(256, DM)
    wo = A.sb("wo", [128, KD, D], BF16)
    for k in range(KD):
        S.dma("pool", wo[:, k, :], I["w_out"][0, k * 128:(k + 1) * 128, :], [I["w_out"]], [wo])
    g1 = load_row_bcast(S, A, "g1", G["grow"][0], G["grow"], D)
    mt = [A.sb(f"mt{i}", [128, KD, 128], BF16) for i in range(2)]
    xt = [A.sb(f"xt{i}", [128, D], F32) for i in range(2)]
    tm = [A.sb(f"tm{i}", [128, 512], F32) for i in range(2)]
    pp = [A.ps(f"pp{i}", [128, 512], F32) for i in range(4)]
    pi = 0
    for ti in range(T // 128):
        t0 = ti * 128
        m_, x_ = mt[ti % 2], xt[ti % 2]
        if nsplit == 1:
            S.dma("sp", m_[:], G["mT"][:, t0:t0 + 128].rearrange("(k p) t -> p k t", p=128), [G["mT"]], [m_])
        else:
            kpc = CRM // 128
            for r in range(nsplit):
                for ck in range(DM // CRM):
                    k0 = (r * DM + ck * CRM) // 128
                    S.dma("sp", m_[:, k0:k0 + kpc, :], G["mgath"][ck, r * CRM:(r + 1) * CRM, t0:t0 + 128].rearrange("(k p) t -> p k t", p=128),
                          [G["mgath"]], [m_])
        S.dma("pool", x_[:], I["x"][0, t0:t0 + 128, :], [I["x"]], [x_])
        for oc in range(0, D, 512):
            ow = min(512, D - oc)
            p = pp[pi % 4]
            t_ = tm[pi % 2]
            pi += 1
            def mm(e, m_=m_, p=p, oc=oc, ow=ow):
                r = None
                for k in range(KD):
                    r = e.matmul(p[:, 0:ow], lhsT=m_[:, k, :], rhs=wo[:, k, oc:oc + ow], start=(k == 0), stop=(k == KD - 1))
                return r
            S.emit("pe", mm, [m_, wo], [p])
            S.emit("dve", lambda e, p=p, t_=t_, oc=oc, ow=ow: e.tensor_tensor(out=t_[:, 0:ow], in0=p[:, 0:ow], in1=g1[:, oc:oc + ow], op=ALU.mult), [p, g1], [t_])
            S.emit("pool", lambda e, x_=x_, t_=t_, oc=oc, ow=ow: e.tensor_tensor(out=x_[:, oc:oc + ow], in0=x_[:, oc:oc + ow], in1=t_[:, 0:ow], op=ALU.add), [x_, t_], [x_])
        S.dma("sp", G["x2"][t0:t0 + 128, :], x_[:], [x_], [G["x2"]])
    A.close()


def phase_route(S, cfg, G, I, C, R_):
    A = Arena(S, "rt")
    D, T, KD, E, CAP = cfg.D, cfg.T, cfg.KD, cfg.E, cfg.CAP
    NSG = CAP // 128
    identf = C["identf"]
    co = G["coef"]
    for j, ci in enumerate((4, 5)):
        S.dma("sp", G["crow"][j].rearrange("(k p) -> p k", p=128), co[:, ci, :], [co], [G["crow"]], allow_slow_non_contiguous=True)
    a2 = load_row_bcast(S, A, "a2", G["crow"][0], G["crow"], D)
    b2 = load_row_bcast(S, A, "b2", G["crow"][1], G["crow"], D)
    rw = A.sb("rw", [128, KD, E], F32)
    S.dma("sp", rw[:], I["router_w"][0].rearrange("(k p) e -> p k e", p=128), [I["router_w"]], [rw])
    affE = [A.sb(f"affE{i}", [E, T], F32) for i in range(2)]
    xt = [A.sb(f"xt{i}", [128, D], F32) for i in range(2)]
    hf = [A.sb(f"hf{i}", [128, D], F32) for i in range(2)]
    hb = [A.sb(f"hb{i}", [128, D], BF16) for i in range(2)]
    hT = [A.sb(f"hT{i}", [128, KD, 128], F32) for i in range(2)]
    junk = A.sb("junk", [128, D], BF16)
    st = [A.sb(f"st{i}", [128, 8], F32) for i in range(2)]
    sm = [A.sb(f"sm{i}", [128, E + 8], F32) for i in range(2)]
    pts = [[A.ps(f"pt{j}{i}", [128, 4, 128], F32) for i in range(2)] for j in range(2)]
    pls = [A.ps(f"pl{j}", [128, 512], F32) for j in range(2)]
    pqs = [A.ps(f"pq{j}", [128, 512], F32) for j in range(2)]
    junks = [junk, A.sb("junk1", [128, D], BF16)]

    def tile_gen(ti):
        par = ti % 2
        t0 = ti * 128
        x = xt[par]; s = st[par]; h = hf[par]; hbb = hb[par]; hTt = hT[par]; m = sm[par]
        pt = pts[par]; pl = pls[par]; pq = pqs[par]; jk = junks[par]
        S.dma("sp" if par == 0 else "pool", x[:], G["x2"][t0:t0 + 128, :], [G["x2"]], [x])
        yield
        S.emit("act", lambda e: e.activation(out=jk[:], in_=x[:], func=AF.Square, accum_out=s[:, 0:1]), [x], [jk, s])
        yield
        S.emit("dve", lambda e: e.tensor_scalar(out=s[:, 1:2], in0=s[:, 0:1], scalar1=1.0 / D, scalar2=cfg.eps, op0=ALU.mult, op1=ALU.add), [s], [s])
        yield
        S.emit("act", lambda e: e.activation(out=s[:, 2:3], in_=s[:, 1:2], func=AF.Sqrt), [s], [s])
        yield
        S.emit("dve", lambda e: e.reciprocal(out=s[:, 3:4], in_=s[:, 2:3]), [s], [s])
        yield
        S.emit("dve", lambda e: e.scalar_tensor_tensor(out=h[:], in0=x[:], scalar=s[:, 3:4], in1=a2[:], op0=ALU.mult, op1=ALU.mult), [x, s, a2], [h])
        yield
        S.emit("pool", lambda e: e.tensor_tensor(out=h[:], in0=h[:], in1=b2[:], op=ALU.add), [h, b2], [h])
        yield
        S.emit("act", lambda e: e.copy(out=hbb[:], in_=h[:]), [h], [hbb])
        S.dma("sp", G["h2"][t0:t0 + 128, :], hbb[:], [hbb], [G["h2"]])
        yield
        for gq_, g in enumerate(range(0, KD, 4)):
            p = pt[gq_ % 2]
            ng = min(4, KD - g)
            def tr(e, p=p, g=g, ng=ng):
                r = None
                for j in range(ng):
                    r = e.transpose(out=p[:, j, :], in_=h[:, (g + j) * 128:(g + j + 1) * 128], identity=identf[:])
                return r
            S.emit("pe", tr, [h, identf], [p])
            yield
            if gq_ % 2 == 0:
                S.emit("act", lambda e, p=p, g=g, ng=ng: e.copy(out=hTt[:, g:g + ng, :], in_=p[:, 0:ng, :]), [p, hTt], [hTt])
            else:
                S.emit("dve", lambda e, p=p, g=g, ng=ng: e.tensor_copy(out=hTt[:, g:g + ng, :], in_=p[:, 0:ng, :]), [p, hTt], [hTt])
            yield
        def ml(e):
            r = None
            for k in range(KD):
                r = e.matmul(pl[:, 0:E], lhsT=hTt[:, k, :], rhs=rw[:, k, :], start=(k == 0), stop=(k == KD - 1))
            return r
        S.emit("pe", ml, [hTt, rw], [pl])
        yield
        S.emit("dve", lambda e: e.reduce_max(out=m[:, E:E + 1], in_=pl[:, 0:E], axis=mybir.AxisListType.X), [pl], [m])
        yield
        S.emit("dve", lambda e: e.tensor_scalar(out=m[:, E + 1:E + 2], in0=m[:, E:E + 1], scalar1=-1.0, scalar2=None, op0=ALU.mult), [m], [m])
        yield
        S.emit("act", lambda e: e.activation(out=m[:, 0:E], in_=pl[:, 0:E], func=AF.Exp, bias=m[:, E + 1:E + 2], accum_out=m[:, E + 2:E + 3]), [pl, m], [m])
        yield
        S.emit("dve", lambda e: e.reciprocal(out=m[:, E + 3:E + 4], in_=m[:, E + 2:E + 3]), [m], [m])
        yield
        S.emit("dve", lambda e: e.tensor_scalar(out=m[:, 0:E], in0=m[:, 0:E], scalar1=m[:, E + 3:E + 4], scalar2=None, op0=ALU.mult), [m], [m])
        yield
        S.emit("pe", lambda e: e.transpose(out=pq[0:E, 0:128], in_=m[:, 0:E], identity=identf[:]), [m, identf], [pq])
        yield
        S.emit("act", lambda e: e.copy(out=affE[0][:, t0:t0 + 128], in_=pq[0:E, 0:128]), [pq, affEw[par]], [affEw[par]])
        yield

    affEw = [Buf("affEw0"), Buf("affEw1")]
    for tj in range(0, T // 128, 2):
        gens = [tile_gen(tj)]
        if tj + 1 < T // 128:
            gens.append(tile_gen(tj + 1))
        while gens:
            for g_ in list(gens):
                try:
                    next(g_)
                except StopIteration:
                    gens.remove(g_)
    for bk in affEw:
        for k_, v_ in bk.ws.items():
            if affE[0].ws.get(k_, 0) < v_:
                affE[0].ws[k_] = v_
    pq = pqs[0]
    vals = A.sb("vals", [E, CAP], F32)
    idxu = A.sb("idxu", [E, CAP], U32)
    idxf = A.sb("idxf", [E, CAP], F32)
    cur = 0
    for it in range(CAP // 8):
        a = affE[cur]; b = affE[1 - cur]
        S.emit("dve", lambda e, a=a, it=it: e.max(out=vals[:, it * 8:(it + 1) * 8], in_=a[:]), [a, vals], [vals])
        S.emit("dve", lambda e, a=a, it=it: e.max_index(out=idxu[:, it * 8:(it + 1) * 8], in_max=vals[:, it * 8:(it + 1) * 8], in_values=a[:]), [a, vals, idxu], [idxu])
        if it + 1 < CAP // 8:
            S.emit("dve", lambda e, a=a, b=b, it=it: e.match_replace(out=b[:], in_to_replace=vals[:, it * 8:(it + 1) * 8], in_values=a[:], imm_value=-1.0), [a, vals], [b])
            cur = 1 - cur
    S.emit("dve", lambda e: e.tensor_copy(out=idxf[:], in_=idxu[:]), [idxu], [idxf])
    idxT, gateT = R_["idxT"], R_["gateT"]
    for sg in range(NSG):
        S.emit("pe", lambda e, sg=sg: e.transpose(out=pq[:, 0:E], in_=idxf[:, sg * 128:(sg + 1) * 128], identity=identf[0:E, 0:E]), [idxf, identf], [pq])
        S.emit("dve", lambda e, sg=sg: e.tensor_copy(out=idxT[:, sg, :], in_=pq[:, 0:E]), [pq, idxT], [idxT])
        S.emit("pe", lambda e, sg=sg: e.transpose(out=pq[:, 0:E], in_=vals[:, sg * 128:(sg + 1) * 128], identity=identf[0:E, 0:E]), [vals, identf], [pq])
        S.emit("act", lambda e, sg=sg: e.copy(out=gateT[:, sg, :], in_=pq[:, 0:E]), [pq, gateT], [gateT])
    A.close()


def phase_experts(S, cfg, G, I, C, R_):
    A = Arena(S, "ex")
    D, T, KD, E, CAP, DE = cfg.D, cfg.T, cfg.KD, cfg.E, cfg.CAP, cfg.DE
    NSG = CAP // 128
    FH = min(getattr(cfg, "FH", 512), DE)
    NFH = DE // FH
    FC = FH // 128
    identb = C["identb"]
    idxT, gateT = R_["idxT"], R_["gateT"]
    g2 = load_row_bcast(S, A, "g2", G["grow"][1], G["grow"], D)
    wg = [A.sb(f"wg{i}", [128, KD, FH], BF16) for i in range(2)]
    wu = [A.sb(f"wu{i}", [128, KD, FH], BF16) for i in range(2)]
    wd = [A.sb(f"wd{i}", [128, FC, D], BF16) for i in range(2)]
    xs = A.sb("xs", [128, NSG, D], BF16)
    xsT = A.sb("xsT", [128, KD, CAP], BF16)
    sa = A.sb("sa", [128, 512], F32)
    sa2 = A.sb("sa2", [128, 512], F32)
    hd = A.sb("hd", [128, FC, CAP], BF16)
    ysb = A.sb("ysb", [128, NSG, D], F32)
    ptr = [A.ps(f"ptr{i}", [128, 8, 128], BF16) for i in range(2)]
    pg = [A.ps(f"pg{i}", [128, 512], F32) for i in range(2)]
    pu = [A.ps(f"pu{i}", [128, 512], F32) for i in range(2)]
    py = [A.ps(f"py{i}", [128, 512], F32) for i in range(2)]
    gi = 0
    yi = 0
    halves = [(ex, fh) for ex in range(E) for fh in range(NFH)]

    def load_weights(k):
        ex, fh = halves[k]
        w_g, w_u, w_d = wg[k % 2], wu[k % 2], wd[k % 2]
        f0 = fh * FH
        S.dma("pool", w_g[:], I["w_gate"][0, ex, :, f0:f0 + FH].rearrange("(k p) f -> p k f", p=128), [I["w_gate"]], [w_g])
        S.dma("pool", w_u[:], I["w_up"][0, ex, :, f0:f0 + FH].rearrange("(k p) f -> p k f", p=128), [I["w_up"]], [w_u])
        S.dma("pool", w_d[:], I["w_down"][0, ex, f0:f0 + FH, :].rearrange("(k p) d -> p k d", p=128), [I["w_down"]], [w_d])

    def gather(ex):
        for sg in range(NSG):
            S.emit("pool", lambda e, sg=sg, ex=ex: e.indirect_dma_start(out=xs[:, sg, :], out_offset=None, in_=G["h2"][:, :],
                                                                        in_offset=bass.IndirectOffsetOnAxis(ap=idxT[:, sg, ex:ex + 1], axis=0)),
                   [idxT, G["h2"]], [xs], dma=xs)

    load_weights(0)
    gather(0)
    for k, (ex, fh) in enumerate(halves):
        w_g, w_u, w_d = wg[k % 2], wu[k % 2], wd[k % 2]
        if k + 1 < len(halves):
            load_weights(k + 1)
        if fh == 0:
            ti = 0
            for sg in range(NSG):
                for g in range(0, KD, 8):
                    p = ptr[ti % 2]
                    ng = min(8, KD - g)
                    def tr(e, p=p, sg=sg, g=g, ng=ng):
                        r = None
                        for j in range(ng):
                            r = e.transpose(out=p[:, j, :], in_=xs[:, sg, (g + j) * 128:(g + j + 1) * 128], identity=identb[:])
                        return r
                    S.emit("pe", tr, [xs, identb], [p])
                    if ti % 2 == 0:
                        S.emit("act", lambda e, p=p, sg=sg, g=g, ng=ng: e.copy(out=xsT[:, g:g + ng, sg * 128:(sg + 1) * 128], in_=p[:, 0:ng, :]), [p, xsT], [xsT])
                    else:
                        S.emit("dve", lambda e, p=p, sg=sg, g=g, ng=ng: e.tensor_copy(out=xsT[:, g:g + ng, sg * 128:(sg + 1) * 128], in_=p[:, 0:ng, :]), [p, xsT], [xsT])
                    ti += 1
            if ex + 1 < E:
                gather(ex + 1)
        for fc in range(FC):
            p_g, p_u = pg[gi % 2], pu[gi % 2]
            gi += 1
            for (w_, p_) in ((w_g, p_g), (w_u, p_u)):
                def mm(e, w_=w_, p_=p_, fc=fc):
                    r = None
                    for kk in range(KD):
                        r = e.matmul(p_[:, 0:CAP], lhsT=w_[:, kk, fc * 128:(fc + 1) * 128], rhs=xsT[:, kk, :], start=(kk == 0), stop=(kk == KD - 1))
                    return r
                S.emit("pe", mm, [w_, xsT], [p_])
            S.emit("act", lambda e, p_g=p_g: e.activation(out=sa[:, 0:CAP], in_=p_g[:, 0:CAP], func=AF.Silu), [p_g], [sa])
            S.emit("dve", lambda e, p_u=p_u, fc=fc: e.tensor_tensor(out=hd[:, fc, :], in0=p_u[:, 0:CAP], in1=sa[:, 0:CAP], op=ALU.mult), [p_u, sa, hd], [hd])
        for sg in range(NSG):
            for oc in range(0, D, 512):
                ow = min(512, D - oc)
                p_y = py[yi % 2]
                yi += 1
                def my(e, p_y=p_y, sg=sg, oc=oc, ow=ow, w_d=w_d):
                    r = None
                    for fc in range(FC):
                        r = e.matmul(p_y[:, 0:ow], lhsT=hd[:, fc, sg * 128:(sg + 1) * 128], rhs=w_d[:, fc, oc:oc + ow], start=(fc == 0), stop=(fc == FC - 1))
                    return r
                S.emit("pe", my, [hd, w_d], [p_y])
                if fh == 0:
                    S.emit("dve", lambda e, p_y=p_y, sg=sg, oc=oc, ow=ow, ex=ex: e.scalar_tensor_tensor(
                        out=ysb[:, sg, oc:oc + ow], in0=p_y[:, 0:ow], scalar=gateT[:, sg, ex:ex + 1], in1=g2[:, oc:oc + ow], op0=ALU.mult, op1=ALU.mult),
                        [p_y, gateT, g2, ysb], [ysb])
                else:
                    S.emit("act", lambda e, p_y=p_y, sg=sg, ow=ow, ex=ex: e.activation(out=sa2[:, 0:ow], in_=p_y[:, 0:ow], func=AF.Copy, scale=gateT[:, sg, ex:ex + 1]),
                           [p_y, gateT], [sa2])
                    S.emit("dve", lambda e, oc=oc, ow=ow: e.tensor_tensor(out=sa2[:, 0:ow], in0=sa2[:, 0:ow], in1=g2[:, oc:oc + ow], op=ALU.mult), [sa2, g2], [sa2])
                    S.emit("dve", lambda e, sg=sg, oc=oc, ow=ow: e.tensor_tensor(out=ysb[:, sg, oc:oc + ow], in0=ysb[:, sg, oc:oc + ow], in1=sa2[:, 0:ow], op=ALU.add),
                           [ysb, sa2], [ysb])
        if fh == NFH - 1:
            for sg in range(NSG):
                S.emit("pool", lambda e, sg=sg, ex=ex: e.indirect_dma_start(out=G["x2"][:, :], out_offset=bass.IndirectOffsetOnAxis(ap=idxT[:, sg, ex:ex + 1], axis=0),
                                                                            in_=ysb[:, sg, :], in_offset=None, compute_op=ALU.add),
                       [idxT, ysb, G["x2s"]], [G["x2s"]], dma=ysb)
    A.close()


def phase_final(S, cfg, G, I, C, out):
    A = Arena(S, "fn")
    D, T = cfg.D, cfg.T
    fn = load_row_bcast(S, A, "fnw", I["final_norm"][:], I["final_norm"], D)
    xt = [A.sb(f"xt{i}", [128, D], F32) for i in range(2)]
    st = [A.sb(f"st{i}", [128, 8], F32) for i in range(2)]
    junk = A.sb("junk", [128, D], BF16)
    for ti in range(T // 128):
        t0 = ti * 128
        x = xt[ti % 2]; s = st[ti % 2]
        S.dma("sp" if ti % 2 == 0 else "pool", x[:], G["x2"][t0:t0 + 128, :], [G["x2"], G["x2s"]], [x])
        S.emit("act", lambda e, x=x, s=s: e.activation(out=junk[:], in_=x[:], func=AF.Square, accum_out=s[:, 0:1]), [x], [junk, s])
        S.emit("dve", lambda e, s=s: e.tensor_scalar(out=s[:, 1:2], in0=s[:, 0:1], scalar1=1.0 / D, scalar2=cfg.eps, op0=ALU.mult, op1=ALU.add), [s], [s])
        S.emit("act", lambda e, s=s: e.activation(out=s[:, 2:3], in_=s[:, 1:2], func=AF.Sqrt), [s], [s])
        S.emit("dve", lambda e, s=s: e.reciprocal(out=s[:, 3:4], in_=s[:, 2:3]), [s], [s])
        S.emit("dve", lambda e, x=x, s=s: e.scalar_tensor_tensor(out=x[:], in0=x[:], scalar=s[:, 3:4], in1=fn[:], op0=ALU.mult, op1=ALU.mult), [x, s, fn], [x])
        S.dma("sp", out[t0:t0 + 128, :], x[:], [x], [out])
    A.close()


def declare_io(S, cfg, debug=(), cfgL=None):
    cfgL = cfgL or cfg
    D, T, NCTX, E, DE, TT = cfg.D, cfg.T, cfg.NCTX, cfg.E, cfg.DE, cfg.TT
    HVF = cfg.HV
    HV, H = cfgL.HV, cfgL.H
    I = {}

    def inp(name, shape):
        I[name] = S.dram(name, shape, F32, kind="ExternalInput")
    inp("x", [1, T, D]); inp("c", [1, D]); inp("ctx", [1, NCTX, D]); inp("c_ctx", [D])
    NSP = cfg.H // cfgL.H
    inp("ada_w", [1, D, 6 * D // NSP]); inp("ada_b", [1, 6 * D // NSP]); inp("norm_mix", [1, D]); inp("norm_ffn", [1, D])
    inp("w_in", [1, D, cfgL.NIN]); inp("gdn_conv", [1, 5, 3 * HV]); inp("gdn_a_log", [1, 2, H]); inp("gdn_dt_bias", [1, 2, H])
    inp("hgrn_lb", [2, 2, HV]); inp("hgrn_norm", [1, HV]); inp("gdn_norm", [1, HV])
    inp("w_branch_a", [1, HVF, cfgL.DMG]); inp("w_branch_b", [1, HVF, cfgL.DMG]); inp("w_out", [1, D, D]); inp("router_w", [1, D, E])
    inp("w_gate", [1, E, D, DE]); inp("w_up", [1, E, D, DE]); inp("w_down", [1, E, DE, D]); inp("final_norm", [D])
    G = {}

    def scr(name, shape, dt):
        G[name] = S.dram(name, shape, dt, kind="ExternalOutput" if name in debug else "Internal")
    scr("grow", [2, D], F32)
    scr("modown", [128, 6 * cfg.KD * 2 // (cfg.H // cfgL.H)], F32); scr("modall", [128 * (cfg.H // cfgL.H), 6 * cfg.KD * 2 // (cfg.H // cfgL.H)], F32)
    scr("hqT", [HV, T], BF16); scr("hffT", [HV, TT], BF16); scr("hfbT", [HV, TT], BF16); scr("hi", [TT, HV], BF16)
    scr("hogT", [HV, T], BF16); scr("gzT", [HV, T], BF16); scr("mgT", [2 * cfgL.DMG, T], BF16)
    scr("gqkvT", [3 * HV, TT], BF16); scr("gab", [TT, 4 * H], F32)
    scr("ycat", [2 * HV, T], BF16)
    G["yAT"] = Buf("yAT", G["ycat"].t[0:HV, :], multi=True)
    G["yBT"] = Buf("yBT", G["ycat"].t[HV:2 * HV, :], multi=True)
    NR = cfg.H // cfgL.H
    CR = min(256, 2 * HV)
    scr("yall", [2 * HV // CR, NR * CR, T], BF16)
    scr("gq", [HV, TT], BF16); scr("gk", [HV, TT], BF16); scr("gv", [HV, TT], BF16)
    scr("mT", [cfgL.DMG, T], BF16)
    CRM = min(256, cfgL.DMG)
    scr("mgath", [cfgL.DMG // CRM, (cfg.H // cfgL.H) * CRM, T], BF16); scr("x2", [T, D], F32); scr("h2", [T, D], BF16); scr("crow", [2, D], F32)
    G["x2s"] = Buf("x2s")
    scr("dbg_mod", [128, 6 * cfg.KD * 2], F32)
    if "dbg_S" in debug:
        scr("dbg_S", [2 * min(4, H), 128, 128], F32)
    if "dbg_ob" in debug:
        scr("dbg_ob", [HV, T], F32)
    if "dbg_g" in debug:
        scr("dbg_g", [6, 128, TT // 128, 2 * H], F32)
    scr("dbg_coef", [128, 6 * cfg.KD], F32)
    out = S.dram("out", [T, D], F32, kind="ExternalOutput")
    return I, G, out


def local_cfg(cfg, nsplit):
    return Cfg(D=cfg.D, H=cfg.H // nsplit, T=cfg.T, GW=cfg.GW, NCTX=cfg.NCTX, E=cfg.E, DE=cfg.DE, CAP=cfg.CAP, eps=cfg.eps,
               DMG=cfg.D // nsplit)


def build_program(cfg=FULL, debug=(), phases=None, nsplit=2, groups=None):
    nc = bass.Bass("TRN2", target_bir_lowering=False)
    S = Sched(nc)
    cfgL = local_cfg(cfg, nsplit) if nsplit > 1 else cfg
    I, G, out = declare_io(S, cfg, debug, cfgL)
    P = Arena(S, "glob")
    C = make_consts(S, P, cfg)
    G["mod"] = P.sb("mod", [128, 6 * cfg.KD, 2], F32)
    G["coef"] = P.sb("coef", [128, 6, cfg.KD], F32)
    ph = phases or ("ada", "coef", "prr", "prc", "hgrn", "gconv", "gdn", "merge", "outproj", "route", "experts", "final")
    grp0 = groups or [[b + 4 * r for r in range(max(nsplit, 1))] for b in range(4)]
    if "ada" in ph:
        phase_ada(S, cfg, G, I, nsplit, grp0)
    if "coef" in ph:
        phase_coef(S, cfg, G, I)
    if "dbg_mod" in debug:
        S.dma("sp", G["dbg_mod"][:], G["mod"][:].rearrange("p n s -> p (n s)"), [G["mod"]], [G["dbg_mod"]])
        S.dma("sp", G["dbg_coef"][:], G["coef"][:].rearrange("p n s -> p (n s)"), [G["coef"]], [G["dbg_coef"]])
    if "prr" in ph:
        phase_proj(S, cfgL, G, I, C, "r")
    if "prc" in ph:
        phase_proj(S, cfgL, G, I, C, "c")
    grp = groups or [[b + 4 * r for r in range(max(nsplit, 1))] for b in range(4)]
    CR = min(256, 2 * cfgL.HV)
    NCK = 2 * cfgL.HV // CR

    def gather_y(cks, srcs):
        for ck in cks:
            S.emit_cc(lambda e, ck=ck: e.collective_compute("AllGather", ALU.bypass, replica_groups=grp, ins=[G["ycat"][ck * CR:(ck + 1) * CR, :]],
                                                            outs=[G["yall"][ck]]), srcs, [G["yall"]])
    if "hgrn" in ph:
        phase_hgrn(S, cfgL, G, I, C)
    if nsplit > 1 and NCK >= 2:
        gather_y(range(NCK // 2), [G["yAT"]])
    if "gconv" in ph:
        phase_gconv(S, cfgL, G, I, C)
    if "gdn" in ph:
        phase_gdn(S, cfgL, G, I, C)
    if nsplit > 1:
        gather_y(range(NCK // 2, NCK) if NCK >= 2 else range(NCK), [G["yAT"], G["yBT"]])
    if "merge" in ph:
        phase_merge(S, cfg, G, I, C, cfgL)
    if nsplit > 1:
        CRM = min(256, cfgL.DMG)
        for ck in range(cfgL.DMG // CRM):
            S.emit_cc(lambda e, ck=ck: e.collective_compute("AllGather", ALU.bypass, replica_groups=grp0, ins=[G["mT"][ck * CRM:(ck + 1) * CRM, :]],
                                                            outs=[G["mgath"][ck]]), [G["mT"]], [G["mgath"]])
    if "outproj" in ph:
        phase_outproj(S, cfg, G, I, C, cfgL, nsplit)
    R_ = {"idxT": P.sb("idxT", [128, cfg.CAP // 128, cfg.E], I32), "gateT": P.sb("gateT", [128, cfg.CAP // 128, cfg.E], F32)}
    if "route" in ph:
        phase_route(S, cfg, G, I, C, R_)
    if "experts" in ph:
        phase_experts(S, cfg, G, I, C, R_)
    if "final" in ph:
        phase_final(S, cfg, G, I, C, out)
    P.close()
    S.finish()
    build_program.last_sched = S
    return nc


_PER_BATCH = ("x", "c", "ctx")
_NC_CACHE = {}


def _head_cols(v, hf, nsplit, H, axis=-1):
    v = np.asarray(v)
    w = (H // nsplit) * 128
    sl = [slice(None)] * v.ndim
    sl[axis] = slice(hf * w, (hf + 1) * w)
    return v[tuple(sl)]


def shard_inputs(inputs, cfg, b, hf, nsplit):
    H, HV, D = cfg.H, cfg.HV, cfg.D
    HLc = H // nsplit
    m = {}
    for k, v in inputs.items():
        v = np.asarray(v)
        if k in _PER_BATCH:
            m[k] = np.ascontiguousarray(v[b:b + 1])
        else:
            m[k] = v
    if nsplit > 1:
        w = m["w_in"]
        parts = []
        for g in range(9):
            parts.append(_head_cols(w[:, :, g * HV:(g + 1) * HV], hf, nsplit, H))
        o = 9 * HV
        for g in range(4):
            parts.append(w[:, :, o + g * H + hf * HLc:o + g * H + (hf + 1) * HLc])
        DMc = D // nsplit
        g0 = o + 4 * H
        parts.append(w[:, :, g0 + hf * DMc:g0 + (hf + 1) * DMc])
        parts.append(w[:, :, g0 + D + hf * DMc:g0 + D + (hf + 1) * DMc])
        m["w_in"] = np.concatenate(parts, axis=2)
        m["hgrn_lb"] = _head_cols(m["hgrn_lb"], hf, nsplit, H)
        m["hgrn_norm"] = _head_cols(m["hgrn_norm"], hf, nsplit, H)
        m["gdn_norm"] = _head_cols(m["gdn_norm"], hf, nsplit, H)
        cw = m["gdn_conv"]
        m["gdn_conv"] = np.concatenate([_head_cols(cw[:, :, g * HV:(g + 1) * HV], hf, nsplit, H) for g in range(3)], axis=2)
        m["gdn_a_log"] = m["gdn_a_log"][:, :, hf * HLc:(hf + 1) * HLc]
        m["gdn_dt_bias"] = m["gdn_dt_bias"][:, :, hf * HLc:(hf + 1) * HLc]
        m["w_branch_a"] = m["w_branch_a"][:, :, hf * (D // nsplit):(hf + 1) * (D // nsplit)]
        m["w_branch_b"] = m["w_branch_b"][:, :, hf * (D // nsplit):(hf + 1) * (D // nsplit)]
        w6 = 6 * D // nsplit
        m["ada_w"] = m["ada_w"][:, :, hf * w6:(hf + 1) * w6]
        m["ada_b"] = m["ada_b"][:, hf * w6:(hf + 1) * w6]
    return {k: np.ascontiguousarray(v) for k, v in m.items()}


def kernel(**inputs):
    cfg = FULL
    n = 8
    nsplit = 2
    if "nc" not in _NC_CACHE:
        _NC_CACHE["nc"] = build_program(cfg, nsplit=nsplit)
    nc = _NC_CACHE["nc"]
    B = inputs["x"].shape[0]
    in_maps = [shard_inputs(inputs, cfg, core % B, core // B, nsplit) for core in range(n)]
    res = run_bass_kernel_spmd(nc, in_maps, core_ids=list(range(n)))
    out = np.stack([np.asarray(res.results[b]["out"]) for b in range(B)], axis=0)
    return out.astype(np.float32)
```
